# Optimizing a Trainium2 kernel written in Bass

```python
import jax, jax.numpy as jnp
from jax import lax
import numpy as np

D_MODEL = 1024
BATCH = 4
SEQ = 4096
DEPTH = 1
DEC_BATCH = 128
DEC_SEQ = 4
PAST_LEN = 2048
PAGE_SIZE = 128

D_CONV = 512
CONV_WIDTH = 3
N_HEADS = 8
HEAD_DIM = 64
D_ATTN = N_HEADS * HEAD_DIM
Q_BLOCK = 128
N_GROUPS = 4
EXPERTS_PER_GROUP = 4
N_EXPERTS = N_GROUPS * EXPERTS_PER_GROUP
TOP_K = 2
D_EXPERT = 512
LN_EPS = 1e-5
DEEPNORM_ALPHA = (2 * DEPTH) ** 0.25
DEEPNORM_BETA = (8 * DEPTH) ** -0.25
N_IN = 3 * D_CONV + 3 * D_ATTN + N_HEADS + 2 * D_MODEL
SPLIT_POINTS = (D_CONV, 2 * D_CONV, 3 * D_CONV,
                3 * D_CONV + D_ATTN, 3 * D_CONV + 2 * D_ATTN, 3 * D_CONV + 3 * D_ATTN,
                3 * D_CONV + 3 * D_ATTN + N_HEADS, 3 * D_CONV + 3 * D_ATTN + N_HEADS + D_MODEL)

kernel_name = 'hybrid_conv_fox_hmoe_decode_step'


def layer_norm(x, g, b):
    xf = x.astype(jnp.float32)
    mu = jnp.mean(xf, axis=-1, keepdims=True)
    var = jnp.mean(jnp.square(xf - mu), axis=-1, keepdims=True)
    y = (xf - mu) * lax.rsqrt(var + LN_EPS) * g.astype(jnp.float32) + b.astype(jnp.float32)
    return y.astype(x.dtype)


def short_conv(u_ext, conv_w):
    L = u_ext.shape[1] - (CONV_WIDTH - 1)
    out = conv_w[0] * u_ext[:, 0:L]
    for j in range(1, CONV_WIDTH):
        out = out + conv_w[j] * u_ext[:, j:j + L]
    return out


def fox_attention(q, k, v, logf):
    N, Sq = q.shape[0], q.shape[1]
    Sk = k.shape[1]
    offset = Sk - Sq
    c = jnp.cumsum(logf.astype(jnp.float32), axis=1)
    ck = jnp.transpose(c, (0, 2, 1))
    cq = c[:, offset:]
    kf = k.astype(jnp.float32)
    vf = v.astype(jnp.float32)
    kpos = jnp.arange(Sk)
    qb = min(Q_BLOCK, Sq)
    nb = Sq // qb
    q_blocks = jnp.swapaxes(q.reshape(N, nb, qb, N_HEADS, HEAD_DIM), 0, 1)
    c_blocks = jnp.swapaxes(cq.reshape(N, nb, qb, N_HEADS), 0, 1)
    pos_blocks = (offset + jnp.arange(Sq)).reshape(nb, qb)
    scale = HEAD_DIM ** -0.5

    def block(args):
        qblk, cblk, qpos = args
        s = jnp.einsum('nqhd,nkhd->nhqk', qblk.astype(jnp.float32), kf) * scale
        s = s + jnp.transpose(cblk, (0, 2, 1))[..., None] - ck[:, :, None, :]
        s = jnp.where(kpos[None, :] <= qpos[:, None], s, -1e30)
        p = jax.nn.softmax(s, axis=-1)
        return jnp.einsum('nhqk,nkhd->nqhd', p, vf)

    o = lax.map(block, (q_blocks, c_blocks, pos_blocks))
    return jnp.swapaxes(o, 0, 1).reshape(N, Sq, D_ATTN).astype(q.dtype)


def hier_moe(x, w_group, w_router, w1, w3, w2):
    N, S, D = x.shape
    t = x.reshape(N * S, D)
    gl = (t @ w_group).astype(jnp.float32)
    gp = jax.nn.softmax(gl, axis=-1)
    p_g, g_idx = lax.top_k(gp, 1)
    el = (t @ w_router).astype(jnp.float32).reshape(-1, N_GROUPS, EXPERTS_PER_GROUP)
    el_sel = jnp.take_along_axis(el, g_idx[:, :, None], axis=1)[:, 0]
    ep = jax.nn.softmax(el_sel, axis=-1)
    top_w, top_i = lax.top_k(ep, TOP_K)
    top_w = top_w / jnp.sum(top_w, axis=-1, keepdims=True)
    wts = p_g * top_w
    ids = g_idx * EXPERTS_PER_GROUP + top_i
    gate = jnp.sum(jax.nn.one_hot(ids, N_EXPERTS, dtype=jnp.float32) * wts[..., None], axis=1)
    gate = gate.astype(t.dtype)
    y = jnp.zeros_like(t)
    for e in range(N_EXPERTS):
        h = jax.nn.silu(t @ w1[e]) * (t @ w3[e])
        y = y + gate[:, e:e + 1] * (h @ w2[e])
    return y.reshape(N, S, D)


def layer(x, conv_buf, k_past, v_past, logf_past,
          w_in, b_f, conv_w, w_conv_out, w_attn_out, w_o, ln1_g, ln1_b,
          w_group, w_router, w1, w3, w2, ln2_g, ln2_b):
    N, S, _ = x.shape
    h = x @ w_in
    xc, bg, cg, q, k, v, fg, ga, gb = jnp.split(h, SPLIT_POINTS, axis=-1)
    u = cg * xc
    u_ext = jnp.concatenate([conv_buf.astype(u.dtype), u], axis=1)
    y_a = (bg * short_conv(u_ext, conv_w)) @ w_conv_out
    conv_new = u_ext[:, -(CONV_WIDTH - 1):]
    q = q.reshape(N, S, N_HEADS, HEAD_DIM)
    k = k.reshape(N, S, N_HEADS, HEAD_DIM)
    v = v.reshape(N, S, N_HEADS, HEAD_DIM)
    logf = jax.nn.log_sigmoid(fg.astype(jnp.float32) + b_f.astype(jnp.float32))
    k_all = jnp.concatenate([k_past.astype(k.dtype), k], axis=1)
    v_all = jnp.concatenate([v_past.astype(v.dtype), v], axis=1)
    logf_all = jnp.concatenate([logf_past.astype(jnp.float32), logf], axis=1)
    y_b = fox_attention(q, k_all, v_all, logf_all) @ w_attn_out
    mix = (jax.nn.sigmoid(ga) * y_a + jax.nn.sigmoid(gb) * y_b) @ w_o
    x = layer_norm(DEEPNORM_ALPHA * x + mix, ln1_g, ln1_b)
    x = layer_norm(DEEPNORM_ALPHA * x + hier_moe(x, w_group, w_router, w1, w3, w2), ln2_g, ln2_b)
    return x, k, v, logf, conv_new


def setup_inputs(seed: int = 0) -> dict:
    key = jax.random.key(seed)
    ks = jax.random.split(key, 24)
    n_pages = PAST_LEN // PAGE_SIZE
    n_used = DEC_BATCH * n_pages
    n_pool = n_used + (n_used + 3) // 4
    f32 = jnp.float32
    nrm = lambda k, shape, s: jax.random.normal(k, shape, f32) * s
    page_table = jax.random.permutation(ks[0], n_pool)[:n_used].reshape(DEC_BATCH, n_pages).astype(jnp.int32)
    return {
        'x_prompt': nrm(ks[1], (BATCH, SEQ, D_MODEL), 1.0),
        'x_sample': nrm(ks[2], (DEC_BATCH, DEC_SEQ, D_MODEL), 1.0),
        'cache_k': nrm(ks[3], (DEPTH, n_pool, PAGE_SIZE, N_HEADS, HEAD_DIM), 1.0),
        'cache_v': nrm(ks[4], (DEPTH, n_pool, PAGE_SIZE, N_HEADS, HEAD_DIM), 1.0),
        'cache_logf': jax.nn.log_sigmoid(3.0 + nrm(ks[5], (DEPTH, n_pool, PAGE_SIZE, N_HEADS), 1.0)),
        'state_conv': nrm(ks[6], (DEPTH, DEC_BATCH, CONV_WIDTH - 1, D_CONV), 0.5),
        'page_table': page_table,
        'w_in': nrm(ks[7], (DEPTH, D_MODEL, N_IN), D_MODEL ** -0.5),
        'b_f': 3.0 + nrm(ks[8], (DEPTH, N_HEADS), 0.5),
        'conv_w': nrm(ks[9], (DEPTH, CONV_WIDTH, D_CONV), 0.5),
        'w_conv_out': nrm(ks[10], (DEPTH, D_CONV, D_MODEL), D_CONV ** -0.5),
        'w_attn_out': nrm(ks[11], (DEPTH, D_ATTN, D_MODEL), D_ATTN ** -0.5),
        'w_o': nrm(ks[12], (DEPTH, D_MODEL, D_MODEL), DEEPNORM_BETA * D_MODEL ** -0.5),
        'ln1_g': 1.0 + nrm(ks[13], (DEPTH, D_MODEL), 0.05),
        'ln1_b': nrm(ks[14], (DEPTH, D_MODEL), 0.02),
        'w_group': nrm(ks[15], (DEPTH, D_MODEL, N_GROUPS), D_MODEL ** -0.5),
        'w_router': nrm(ks[16], (DEPTH, D_MODEL, N_EXPERTS), D_MODEL ** -0.5),
        'w1': nrm(ks[17], (DEPTH, N_EXPERTS, D_MODEL, D_EXPERT), D_MODEL ** -0.5),
        'w3': nrm(ks[18], (DEPTH, N_EXPERTS, D_MODEL, D_EXPERT), D_MODEL ** -0.5),
        'w2': nrm(ks[19], (DEPTH, N_EXPERTS, D_EXPERT, D_MODEL), DEEPNORM_BETA * D_EXPERT ** -0.5),
        'ln2_g': 1.0 + nrm(ks[20], (DEPTH, D_MODEL), 0.05),
        'ln2_b': nrm(ks[21], (DEPTH, D_MODEL), 0.02),
    }


def reference(x_prompt, x_sample, cache_k, cache_v, cache_logf, state_conv, page_table,
              w_in, b_f, conv_w, w_conv_out, w_attn_out, w_o, ln1_g, ln1_b,
              w_group, w_router, w1, w3, w2, ln2_g, ln2_b):
    n_pages = page_table.shape[1]
    past_len = n_pages * PAGE_SIZE
    xp, xs = x_prompt, x_sample
    kp_l, vp_l, fp_l, cp_l, ks_l, vs_l, fs_l, cs_l = [], [], [], [], [], [], [], []
    for l in range(DEPTH):
        params = (w_in[l], b_f[l], conv_w[l], w_conv_out[l], w_attn_out[l], w_o[l], ln1_g[l], ln1_b[l],
                  w_group[l], w_router[l], w1[l], w3[l], w2[l], ln2_g[l], ln2_b[l])
        nb = xp.shape[0]
        xp, kp, vp, fp, cp = layer(
            xp, jnp.zeros((nb, CONV_WIDTH - 1, D_CONV), xp.dtype),
            jnp.zeros((nb, 0, N_HEADS, HEAD_DIM), xp.dtype), jnp.zeros((nb, 0, N_HEADS, HEAD_DIM), xp.dtype),
            jnp.zeros((nb, 0, N_HEADS), jnp.float32), *params)
        db = xs.shape[0]
        k_past = cache_k[l][page_table].reshape(db, past_len, N_HEADS, HEAD_DIM)
        v_past = cache_v[l][page_table].reshape(db, past_len, N_HEADS, HEAD_DIM)
        f_past = cache_logf[l][page_table].reshape(db, past_len, N_HEADS)
        xs, ksn, vsn, fsn, csn = layer(xs, state_conv[l], k_past, v_past, f_past, *params)
        kp_l.append(kp); vp_l.append(vp); fp_l.append(fp); cp_l.append(cp)
        ks_l.append(ksn); vs_l.append(vsn); fs_l.append(fsn); cs_l.append(csn)
    return (xp, xs,
            jnp.stack(kp_l), jnp.stack(vp_l), jnp.stack(fp_l), jnp.stack(cp_l),
            jnp.stack(ks_l), jnp.stack(vs_l), jnp.stack(fs_l), jnp.stack(cs_l))
```

```python
import os
import numpy as np
from contextlib import ExitStack
import concourse.bass as bass
import concourse.mybir as mybir
from concourse.bass_utils import run_bass_kernel_spmd

F32 = mybir.dt.float32
BF16 = mybir.dt.bfloat16
I32 = mybir.dt.int32
AF = mybir.ActivationFunctionType
ALU = mybir.AluOpType
AX = mybir.AxisListType

D = 1024
NIN = 5128
OWN = [[0, 3, 4, 7], [1, 2, 5, 6]]
ALPHA = 2.0 ** 0.25
EPS = 1e-5
NEG = -30000.0
C_XC, C_BG, C_CG, C_Q, C_K, C_V, C_F, C_GA, C_GB = 0, 512, 1024, 1536, 2048, 2560, 3072, 3080, 4104
NTOK = 2112


class StopBuild(Exception):
    pass


class Prog:
    def __init__(self, nc, es):
        self.nc = nc
        self.es = es
        self.eng = {"pe": nc.tensor, "act": nc.scalar, "dve": nc.vector, "pool": nc.gpsimd, "sp": nc.sync}
        self.sem = {k: es.enter_context(nc.semaphore("s_" + k)) for k in self.eng}
        self.cnt = {k: 0 for k in self.eng}
        self.waited = {k: {} for k in self.eng}
        self.last_w = {}
        self.readers = {}
        self.dsem = {}
        self.dcnt = {}
        self.dead = False

    def _deps(self, r, w):
        deps = []
        for k in r:
            if k in self.last_w:
                deps.append(self.last_w[k])
        for k in w:
            if k in self.last_w:
                deps.append(self.last_w[k])
            deps.extend(self.readers.get(k, ()))
        return deps

    def _wait(self, eng, deps):
        best = {}
        for (s, v) in deps:
            if eng == "pe" and s == "pe":
                continue
            if v > best.get(s, 0):
                best[s] = v
        for s, v in best.items():
            if self.waited[eng].get(s, 0) < v:
                semh = self.sem[s] if s in self.sem else self.dsem[s]
                self.eng[eng].wait_ge(semh, v)
                self.waited[eng][s] = v

    def _record(self, tok, r, w):
        for k in w:
            self.last_w[k] = tok
            self.readers[k] = []
        for k in r:
            if k not in w:
                self.readers.setdefault(k, []).append(tok)

    def op(self, eng, fn, r=(), w=()):
        if self.dead:
            return
        self._wait(eng, self._deps(r, w))
        ins = fn(self.eng[eng])
        self.cnt[eng] += 1
        ins.then_inc(self.sem[eng], 1)
        self._record((eng, self.cnt[eng]), r, w)

    def _dsem(self, sem):
        if sem not in self.dsem:
            self.dsem[sem] = self.es.enter_context(self.nc.semaphore("d_" + sem))
            self.dcnt[sem] = 0

    def dma(self, q, out, in_, r=(), w=(), sem=None, **kw):
        if self.dead:
            return
        self._dsem(sem)
        self._wait(q, self._deps(r, w))
        ins = self.eng[q].dma_start(out=out, in_=in_, **kw)
        self.dcnt[sem] += 16
        ins.then_inc(self.dsem[sem], 16)
        self._record((sem, self.dcnt[sem]), r, w)

    def idma(self, out, in_, idx, r=(), w=(), sem=None):
        if self.dead:
            return
        self._dsem(sem)
        self._wait("pool", self._deps(r, w))
        ins = self.nc.gpsimd.indirect_dma_start(
            out=out, out_offset=None, in_=in_, in_offset=bass.IndirectOffsetOnAxis(ap=idx, axis=0))
        self.dcnt[sem] += 16
        ins.then_inc(self.dsem[sem], 16)
        self._record((sem, self.dcnt[sem]), r, w)

    def _all(self):
        toks = [(k, self.cnt[k]) for k in self.eng if self.cnt[k] > 0]
        toks += [(k, self.dcnt[k]) for k in self.dsem if self.dcnt[k] > 0]
        return toks

    def barrier(self):
        if self.dead:
            return
        toks = self._all()
        for e in self.eng:
            self._wait(e, toks)
        self.last_w = {}
        self.readers = {}

    def finish(self):
        self._wait("sp", self._all())


def build_nc(npool=2560, stage=99, dbg=False):
    nc = bass.Bass("TRN2", target_bir_lowering=False)

    def din(name, shape, dt=F32):
        return nc.dram_tensor(name, list(shape), dt, kind="ExternalInput").ap()

    def dout(name, shape, dt=F32):
        return nc.dram_tensor(name, list(shape), dt, kind="ExternalOutput").ap()

    x_own = din("x_own", [2048, D])
    x_oth = din("x_oth", [2048, D])
    x_halo = din("x_halo", [8, D])
    x_s = din("x_s", [64, D])
    cache_k = din("cache_k", [npool * 128, 512])
    cache_v = din("cache_v", [npool * 128, 512])
    cache_f = din("cache_f", [npool * 128, 8])
    state_conv = din("state_conv", [32, 512])
    page_table = din("page_table", [1, 256], I32)
    rolec = din("rolec", [1, 80])
    w_in = din("w_in", [D, NIN])
    b_f = din("b_f", [1, 8])
    conv_w = din("conv_w", [3, 512])
    w_conv_out = din("w_conv_out", [512, D])
    w_attn_out = din("w_attn_out", [512, D])
    w_o = din("w_o", [D, D])
    ln1_g = din("ln1_g", [1, D])
    ln1_b = din("ln1_b", [1, D])
    w_gr = din("w_gr", [D, 20])
    w1 = din("w1", [16, D, 512])
    w3 = din("w3", [16, D, 512])
    w2 = din("w2", [16, 512, D])
    ln2_g = din("ln2_g", [1, D])
    ln2_b = din("ln2_b", [1, D])

    y_own = dout("y_own", [2048, D])
    y_s = dout("y_s", [64, D])
    k_own = dout("k_own", [2048, 512])
    v_own = dout("v_own", [2048, 512])
    f_own = dout("f_own", [2048, 8])
    conv_p = dout("conv_p", [2, 512])
    k_s = dout("k_s", [64, 512])
    v_s = dout("v_s", [64, 512])
    f_s = dout("f_s", [64, 8])
    conv_s = dout("conv_s", [32, 512])
    y0_d = nc.dram_tensor("y0_d", [NTOK, D], F32, kind="Internal").ap()
    x1T_d = nc.dram_tensor("x1T_d", [128, 8, NTOK], BF16, kind="Internal").ap()
    dbg_out = {}

    with ExitStack() as es:
        P = Prog(nc, es)

        try:
            def sb(name, shape, dt=F32, stack=es):
                return stack.enter_context(nc.sbuf_tensor(name, list(shape), dt))

            def chk(x):
                if stage < x:
                    P.dead = True

            def dump(name, ap, shape, dt=F32, r=()):
                if dbg:
                    d = dout("dbg_" + name, shape, dt)
                    dbg_out[name] = d
                    P.dma("sp", d, ap, r=list(r), sem="dbg_" + name)

            psf = [es.enter_context(nc.psum_tensor("ps%d" % i, [128, 512], F32)) for i in range(8)]
            ps_rr = [0]
            ps_n = [8]

            def ps():
                i = ps_rr[0] % ps_n[0]
                ps_rr[0] += 1
                return psf[i], "ps%d" % i

            ev_rr = [0]

            def evac(out, in_, r, w):
                ev_rr[0] += 1
                if ev_rr[0] % 2:
                    P.op("act", lambda e: e.copy(out=out, in_=in_), r=r, w=w)
                else:
                    P.op("dve", lambda e: e.tensor_copy(out=out, in_=in_), r=r, w=w)

            def mmg(out, pairs, r, w):
                n = len(pairs)

                def g(e):
                    ins = None
                    for i, (a, b) in enumerate(pairs):
                        ins = e.matmul(out, lhsT=a, rhs=b, start=(i == 0), stop=(i == n - 1))
                    return ins
                P.op("pe", g, r=r, w=w)

            ident = sb("ident", [128, 128])
            ident_b = sb("ident_b", [128, 128], BF16)
            U_f = sb("U_f", [128, 128])
            U_b = sb("U_b", [128, 128], BF16)
            SU4 = sb("SU4", [4, 4])
            ones_f = sb("ones_f", [128, 128])
            ones_b = sb("ones_b", [128, 1], BF16)
            sel127 = sb("sel127", [128, 128])
            hmask = sb("hmask", [32, 8])
            io_i = sb("io_i", [128, 1], I32)
            io_f = sb("io_f", [128, 1])
            bfb = sb("bfb", [128, 8])
            rc = sb("rc", [128, 80])
            LF = sb("LF", [128, 33, 8])
            gate = sb("gate", [128, 17, 16])
            pool_ops = [
                (lambda e: e.memset(ident[:], 1.0), [], ["ident"]),
                (lambda e: e.affine_select(out=ident[:], in_=ident[:], pattern=[[-1, 128]], compare_op=ALU.is_equal,
                                           fill=0.0, base=0, channel_multiplier=1), ["ident"], ["ident"]),
                (lambda e: e.tensor_copy(out=ident_b[:], in_=ident[:]), ["ident"], ["ident_b"]),
                (lambda e: e.memset(U_f[:], 1.0), [], ["U_f"]),
                (lambda e: e.affine_select(out=U_f[:], in_=U_f[:], pattern=[[1, 128]], compare_op=ALU.is_ge,
                                           fill=0.0, base=0, channel_multiplier=-1), ["U_f"], ["U_f"]),
                (lambda e: e.tensor_copy(out=U_b[:], in_=U_f[:]), ["U_f"], ["U_b"]),
                (lambda e: e.memset(SU4[:], 1.0), [], ["SU4"]),
                (lambda e: e.affine_select(out=SU4[:], in_=SU4[:], pattern=[[-1, 4]], compare_op=ALU.is_gt,
                                           fill=0.0, base=0, channel_multiplier=1), ["SU4"], ["SU4"]),
                (lambda e: e.memset(ones_f[:], 1.0), [], ["ones_f"]),
                (lambda e: e.memset(ones_b[:], 1.0), [], ["ones_b"]),
                (lambda e: e.memset(sel127[:], 1.0), [], ["sel127"]),
                (lambda e: e.affine_select(out=sel127[:], in_=sel127[:], pattern=[[0, 128]], compare_op=ALU.is_equal,
                                           fill=0.0, base=-127, channel_multiplier=1), ["sel127"], ["sel127"]),
                (lambda e: e.memset(hmask[:], 1.0), [], ["hmask"]),
                (lambda e: e.affine_select(out=hmask[:], in_=hmask[:], pattern=[[-4, 8]], compare_op=ALU.is_ge,
                                           fill=0.0, base=0, channel_multiplier=1), ["hmask"], ["hmask"]),
                (lambda e: e.affine_select(out=hmask[:], in_=hmask[:], pattern=[[4, 8]], compare_op=ALU.is_ge,
                                           fill=0.0, base=3, channel_multiplier=-1), ["hmask"], ["hmask"]),
                (lambda e: e.iota(out=io_i[:], pattern=[[0, 1]], base=0, channel_multiplier=1), [], ["io_i"]),
                (lambda e: e.tensor_copy(out=io_f[:], in_=io_i[:]), ["io_i"], ["io_f"]),
                (lambda e: e.memset(LF[:], 0.0), [], ["LFall"]),
                (lambda e: e.memset(gate[:], 0.0), [], ["gate"]),
            ]
            for fn, r_, w_ in pool_ops:
                P.op("pool", fn, r=r_, w=w_)
            P.dma("sp", bfb[:], b_f.partition_broadcast(128), w=["bfb"], sem="c0")
            P.dma("sp", rc[:], rolec.partition_broadcast(128), w=["rc"], sem="c1")
            P.barrier()
            chk(0.1)

            s_oT = ExitStack()
            es.enter_context(s_oT)
            oT = sb("oT", [128, 4, NTOK], BF16, s_oT)
            s_A = ExitStack()
            es.enter_context(s_A)
            if os.environ.get("KFIRST"):
                kT = sb("kT", [128, 4, int(os.environ.get("KTN", "4224"))], BF16, s_A)
                qT = sb("qT", [128, 4, NTOK], BF16, s_A)
            else:
                qT = sb("qT", [128, 4, NTOK], BF16, s_A)
                kT = sb("kT", [128, 4, int(os.environ.get("KTN", "4224"))], BF16, s_A)
            Vt = sb("Vt", [128, 33, 8, 66], BF16, s_A)
            Cc = sb("Cc", [128, 32, 8], F32, s_A)
            BIAS = sb("BIAS", [128, 4, 32, 8], F32, s_A)
            P.op("pool", lambda e: e.memset(Vt[:, :, :, 64:65], 1.0), w=["Vones"])

            wst = []
            wst_rr = [0]

            def wchunks(dst, dkey, src, K, N, col0=0, dcol0=0, CH=1024):
                th = []
                if N <= CH:
                    g = max(1, CH // N)
                    for k0 in range(0, K, g):
                        kk = min(g, K - k0)

                        def f(k0=k0, kk=kk):
                            i = wst_rr[0] % len(wst)
                            wst_rr[0] += 1
                            st, sk = wst[i], "wst%d" % i
                            sv = st[:, 0:kk * N].rearrange("p (k n) -> p k n", n=N)
                            P.dma("sp", sv, src[k0 * 128:(k0 + kk) * 128, col0:col0 + N].rearrange("(k p) n -> p k n", p=128), w=[sk], sem=sk)
                            P.op("act", lambda e: e.copy(out=dst[:, k0:k0 + kk, dcol0:dcol0 + N], in_=sv), r=[sk], w=[dkey])
                        th.append(f)
                else:
                    for k in range(K):
                        for c0 in range(0, N, CH):
                            n = min(CH, N - c0)

                            def f(k=k, c0=c0, n=n):
                                i = wst_rr[0] % len(wst)
                                wst_rr[0] += 1
                                st, sk = wst[i], "wst%d" % i
                                P.dma("sp", st[:, 0:n], src[k * 128:(k + 1) * 128, col0 + c0:col0 + c0 + n], w=[sk], sem=sk)
                                P.op("act", lambda e: e.copy(out=dst[:, k, dcol0 + c0:dcol0 + c0 + n], in_=st[:, 0:n]), r=[sk], w=[dkey])
                            th.append(f)
                return th

            def wload(*a, **kw):
                for f in wchunks(*a, **kw):
                    f()

            def load_xT(src, W, xi, xt, xik, xtk):
                nb = (W + 127) // 128
                pw = min(W, 128)
                if W == 512:
                    P.dma("sp", xi[:], src.rearrange("(b p) d -> p b d", p=128), w=[xik], sem=xik)
                else:
                    P.dma("sp", xi[0:pw, 0, :], src, w=[xik], sem=xik)
                for b in range(nb):
                    for half in range(2):
                        pt, ptk = ps()

                        def tr(e, pt=pt, b=b, half=half):
                            ins = None
                            for kk in range(4):
                                k = half * 4 + kk
                                ins = e.transpose(pt[:, kk * 128:kk * 128 + pw], xi[0:pw, b, k * 128:(k + 1) * 128], ident[0:pw, 0:pw])
                            return ins
                        P.op("pe", tr, r=[xik, "ident"], w=[ptk])
                        evac(xt[:, half * 4:half * 4 + 4, b * 128:b * 128 + pw],
                             pt[:].rearrange("p (k t) -> p k t", k=4)[:, :, 0:pw], r=[ptk], w=[xtk + "_%d_%d" % (b, half)])
                return [xtk + "_%d_%d" % (b, h) for b in range(nb) for h in range(2)]

            with ExitStack() as s1:
                wq = sb("wq", [128, 8, 512], BF16, s1)
                wk = sb("wk", [128, 8, 512], BF16, s1)
                wv = sb("wv", [128, 8, 512], BF16, s1)
                wf = sb("wf", [128, 8, 8], BF16, s1)
                wst[:] = [sb("wstA%d" % i, [128, 1024], F32, s1) for i in range(2)]
                for (t, c0, n, nm) in ((wq, C_Q, 512, "wq"), (wk, C_K, 512, "wk"), (wv, C_V, 512, "wv"), (wf, C_F, 8, "wf")):
                    wload(t, nm, w_in, 8, n, col0=c0, CH=1024)
                xin = [sb("xin%d" % i, [128, 4, 1024], F32, s1) for i in range(2)]
                xT = [sb("xT%d" % i, [128, 8, 512], BF16, s1) for i in range(2)]
                stg = [sb("stg%d" % i, [128, 512], F32, s1) for i in range(4)]
                ftmp = [sb("ftmp%d" % i, [128, 8], F32, s1) for i in range(2)]
                stg_rr = [0]
                tiles = []
                for s_ in range(4):
                    tiles.append((x_own[s_ * 512:(s_ + 1) * 512, :], 512, True, s_ * 512, s_ * 512, s_ * 4, s_ * 512, k_own, v_own))
                tiles.append((x_s, 64, True, 2048, 4096, 32, 0, k_s, v_s))
                for s_ in range(4):
                    tiles.append((x_oth[s_ * 512:(s_ + 1) * 512, :], 512, False, None, 2048 + s_ * 512, 16 + s_ * 4, None, None, None))

                for ti, (src, W, own, q0, k0, b0, r0, ko, vo) in enumerate(tiles):
                    if ti == 0:
                        chk(0.2)
                    if ti == 1:
                        chk(0.3)
                    if ti == 5:
                        chk(0.4)
                    xi, xt = xin[ti % 2], xT[ti % 2]
                    xik, xtk = "xin%d" % (ti % 2), "xT%d" % (ti % 2)
                    nb = (W + 127) // 128
                    pw = min(W, 128)
                    xtall = load_xT(src, W, xi, xt, xik, xtk)
                    if ti == 0:
                        chk(0.21)

                    def proj_fm(wt, wnm, dst, d0):
                        for hp in range(4):
                            pt, ptk = ps()
                            mmg(pt[:, 0:W], [(wt[:, k, hp * 128:(hp + 1) * 128], xt[:, k, 0:W]) for k in range(8)], r=xtall + [wnm], w=[ptk])
                            evac(dst[:, hp, d0:d0 + W], pt[:, 0:W], r=[ptk], w=["%s_%d_%d" % (dst.name, hp, d0)])


                    if own and not os.environ.get("SKIPQ"):
                        proj_fm(wq, "wq", qT, q0)
                    if ti == 0:
                        chk(0.22)
                    proj_fm(wq if os.environ.get("USEWQ") else wk, "wq" if os.environ.get("USEWQ") else "wk", kT, k0)
                    if ti == 0:
                        chk(0.23)
                    for b in range(nb):
                        xtb = [xtk + "_%d_%d" % (b, h) for h in range(2)]
                        tsl = slice(b * 128, b * 128 + pw)
                        pt, ptk = ps()
                        mmg(pt[0:pw, :], [(xt[:, k, tsl], wv[:, k, :]) for k in range(8)], r=xtb + ["wv"], w=[ptk])
                        P.op("act", lambda e, pt=pt, b=b: e.copy(out=Vt[0:pw, b0 + b, :, 0:64], in_=pt[0:pw, :].rearrange("p (h d) -> p h d", h=8)),
                             r=[ptk], w=["Vt%d" % (b0 + b)])
                        if ti == 0 and b == 0:
                            chk(0.2311)
                        if own:
                            si = stg_rr[0] % 4
                            stg_rr[0] += 1
                            P.op("dve", lambda e, pt=pt, si=si: e.tensor_copy(out=stg[si][0:pw, :], in_=pt[0:pw, :]), r=[ptk, "Vt%d" % (b0 + b)], w=["stg%d" % si])
                            P.dma("sp", vo[r0 + b * 128:r0 + b * 128 + pw, :], stg[si][0:pw, :], r=["stg%d" % si], sem="stg%d" % si)
                            if ti == 0 and b == 0:
                                chk(0.2312)
                            pt, ptk = ps()
                            mmg(pt[0:pw, :], [(xt[:, k, tsl], wk[:, k, :]) for k in range(8)], r=xtb + ["wk"], w=[ptk])
                            si = stg_rr[0] % 4
                            stg_rr[0] += 1
                            evac(stg[si][0:pw, :], pt[0:pw, :], r=[ptk], w=["stg%d" % si])
                            P.dma("sp", ko[r0 + b * 128:r0 + b * 128 + pw, :], stg[si][0:pw, :], r=["stg%d" % si], sem="stg%d" % si)
                        if ti == 0 and b == 0:
                            chk(0.232)
                        pt, ptk = ps()
                        mmg(pt[0:pw, 0:8], [(xt[:, k, tsl], wf[:, k, :]) for k in range(8)], r=xtb + ["wf"], w=[ptk])
                        ft = ftmp[b % 2]
                        fk = "ftmp%d" % (b % 2)
                        P.op("dve", lambda e, pt=pt, ft=ft: e.tensor_tensor(out=ft[0:pw, :], in0=pt[0:pw, 0:8], in1=bfb[0:pw, :], op=ALU.add),
                             r=[ptk, "bfb"], w=[fk])
                        if ti == 0 and b == 0:
                            chk(0.233)
                        P.op("act", lambda e, ft=ft: e.activation(out=ft[0:pw, :], in_=ft[0:pw, :], func=AF.Exp, scale=-1.0), r=[fk], w=[fk])
                        if ti == 0 and b == 0:
                            chk(0.234)
                        P.op("act", lambda e, ft=ft: e.activation(out=ft[0:pw, :], in_=ft[0:pw, :], func=AF.Ln, bias=1.0, scale=1.0), r=[fk], w=[fk])
                        P.op("dve", lambda e, ft=ft, bb=b0 + b: e.tensor_scalar(out=LF[0:pw, bb, :], in0=ft[0:pw, :], scalar1=-1.0, scalar2=None, op0=ALU.mult),
                             r=[fk, "LFall"], w=["LF%d" % (b0 + b)])
                lfall = ["LF%d" % i for i in range(33)]
                P.dma("sp", f_own.rearrange("(b p) h -> p b h", p=128), LF[:, 0:16, :], r=lfall, sem="fo")
                P.dma("sp", f_s, LF[0:64, 32, :], r=lfall, sem="fs")
                P.barrier()
                chk(0.5)

            with ExitStack() as s2:
                T1 = sb("T1", [128, 8, 8], F32, s2)
                Lprev = sb("Lprev", [128, 8, 8], F32, s2)
                Ltmp = sb("Ltmp", [128, 8, 8], F32, s2)
                Lfull = sb("Lfull", [128, 32, 8], F32, s2)
                Cb = sb("Cb", [128, 4, 8], F32, s2)
                LF4 = LF[:, 0:32, :].rearrange("p (t j) h -> p t j h", j=4)
                Lf4 = Lfull[:].rearrange("p (t j) h -> p t j h", j=4)
                P.op("dve", lambda e: e.tensor_tensor(out=T1[:], in0=LF4[:, :, 0, :], in1=LF4[:, :, 1, :], op=ALU.add), r=[], w=["T1"])
                P.op("dve", lambda e: e.tensor_tensor(out=T1[:], in0=T1[:], in1=LF4[:, :, 2, :], op=ALU.add), r=["T1"], w=["T1"])
                P.op("dve", lambda e: e.tensor_tensor(out=T1[:], in0=T1[:], in1=LF4[:, :, 3, :], op=ALU.add), r=["T1"], w=["T1"])
                rcE = rc[:, 0:64].rearrange("q (a b) -> q a b", b=8)
                for pp in range(8):
                    in0 = T1[:, pp, :].unsqueeze(1).to_broadcast([128, 8, 8])
                    in1 = rcE[:, pp, :].unsqueeze(2).to_broadcast([128, 8, 8])
                    if pp == 0:
                        P.op("dve", lambda e, in0=in0, in1=in1: e.tensor_tensor(out=Lprev[:], in0=in0, in1=in1, op=ALU.mult), r=["T1"], w=["Lprev"])
                    else:
                        P.op("dve", lambda e, in0=in0, in1=in1: e.tensor_tensor(out=Ltmp[:], in0=in0, in1=in1, op=ALU.mult), r=["T1"], w=["Ltmp"])
                        P.op("dve", lambda e: e.tensor_tensor(out=Lprev[:], in0=Lprev[:], in1=Ltmp[:], op=ALU.add), r=["Ltmp", "Lprev"], w=["Lprev"])
                P.op("dve", lambda e: e.tensor_copy(out=Lf4[:, :, 0, :], in_=Lprev[:]), r=["Lprev"], w=["Lfull"])
                for j in range(1, 4):
                    P.op("dve", lambda e, j=j: e.tensor_tensor(out=Lf4[:, :, j, :], in0=Lf4[:, :, j - 1, :], in1=LF4[:, :, j - 1, :], op=ALU.add),
                         r=["Lfull"], w=["Lfull"])
                pc, pck = ps()
                mmg(pc[:, 0:256], [(U_f[:], LF[:, 0:32, :].rearrange("p b h -> p (b h)")), (ones_f[:], Lfull[:].rearrange("p b h -> p (b h)"))],
                    r=["Lfull"], w=[pck])
                P.op("dve", lambda e: e.tensor_copy(out=Cc[:].rearrange("p b h -> p (b h)"), in_=pc[:, 0:256]), r=[pck], w=["Cc"])
                Cc4 = Cc[:].rearrange("p (t j) h -> p t j h", j=4)
                pcb, pcbk = ps()
                mmg(pcb[:, 0:32].rearrange("p (a b) -> p a b", b=8), [(sel127[:], Cc4[:, 0:4, 1, :])], r=["Cc"], w=[pcbk])
                P.op("dve", lambda e: e.tensor_copy(out=Cb[:].rearrange("p a b -> p (a b)"), in_=pcb[:, 0:32]), r=[pcbk], w=["Cb"])
                for sg in range(4):
                    P.op("dve", lambda e, sg=sg: e.scalar_tensor_tensor(out=BIAS[:, sg], in0=Cc[:], scalar=-1.0,
                                                                         in1=Cb[:, sg, :].unsqueeze(1).to_broadcast([128, 32, 8]),
                                                                         op0=ALU.mult, op1=ALU.add), r=["Cc", "Cb"], w=["BIAS"])
                    P.op("dve", lambda e, sg=sg: e.tensor_scalar(out=BIAS[:, sg, 16 + 4 * sg:20 + 4 * sg, :], in0=BIAS[:, sg, 16 + 4 * sg:20 + 4 * sg, :],
                                                                  scalar1=rc[:, 64 + sg:65 + sg], scalar2=None, op0=ALU.add), r=["BIAS"], w=["BIAS"])
                dump("Cc", Cc[:], [128, 32, 8], r=["Cc"])
                P.barrier()
                chk(1.0)

            if stage >= 2:
                with ExitStack() as s2:
                    ptb = sb("ptb", [128, 256], I32, s2)
                    ptf = sb("ptf", [128, 256], F32, s2)
                    idx = sb("idx", [128, 256], I32, s2)
                    lfp = sb("lfp", [128, 16, 16, 8], F32, s2)
                    Linc = sb("Linc", [128, 16, 16, 8], F32, s2)
                    Cbs = sb("Cbs", [128, 16, 8], F32, s2)
                    lfn4 = sb("lfn4", [4, 16, 8], F32, s2)
                    bnew = sb("bnew", [4, 16, 8], F32, s2)
                    Vn = sb("Vn", [4, 16, 512], BF16, s2)
                    Qbd = sb("Qbd", [128, 4, 16, 8], BF16, s2)
                    Kp = [sb("Kp%d" % i, [128, 16, 512], BF16, s2) for i in range(1)]
                    Vp = [sb("Vp%d" % i, [128, 16, 512], BF16, s2) for i in range(2)]
                    KTp = [sb("KTp%d" % i, [128, 4, 128], BF16, s2) for i in range(3)]
                    stmp = sb("stmp", [128, 512], F32, s2)
                    Pp = sb("Pp", [128, 16, 32], BF16, s2)
                    tn = sb("tn", [4, 32], F32, s2)
                    Pn = sb("Pn", [4, 32], BF16, s2)
                    t3 = sb("t3", [32, 512], F32, s2)
                    o32 = sb("o32", [32, 64], F32, s2)
                    rd = sb("rd", [32, 1], F32, s2)
                    ps_n[0] = 5
                    P.dma("sp", ptb[:], page_table.partition_broadcast(128), w=["ptb"], sem="ptb")
                    P.op("dve", lambda e: e.tensor_copy(out=ptf[:], in_=ptb[:]), r=["ptb"], w=["ptf"])
                    P.op("dve", lambda e: e.tensor_scalar(out=ptf[:], in0=ptf[:], scalar1=128.0, scalar2=io_f[:, 0:1], op0=ALU.mult, op1=ALU.add),
                         r=["ptf", "io_f"], w=["ptf"])
                    P.op("dve", lambda e: e.tensor_copy(out=idx[:], in_=ptf[:]), r=["ptf"], w=["idx"])
                    P.dma("sp", lfn4[:], f_s.rearrange("(b j) h -> j b h", j=4), w=["lfn4"], sem="lfn4")
                    P.dma("pool", Vn[:], v_s.rearrange("(b j) f -> j b f", j=4), w=["Vn"], sem="Vn")
                    for b in range(16):
                        for pg in range(16):
                            P.idma(lfp[:, b, pg, :], cache_f, idx[:, b * 16 + pg:b * 16 + pg + 1], r=["idx"], w=["lfp_%d_%d" % (b, pg)], sem="lfp")
                    lfpall = ["lfp_%d_%d" % (b, pg) for b in range(16) for pg in range(16)]
                    P.op("dve", lambda e: e.tensor_copy(out=Linc[:, :, 0, :], in_=lfp[:, :, 0, :]), r=lfpall, w=["Linc", "lfp"])
                    for pg in range(1, 16):
                        P.op("dve", lambda e, pg=pg: e.tensor_tensor(out=Linc[:, :, pg, :], in0=Linc[:, :, pg - 1, :], in1=lfp[:, :, pg, :], op=ALU.add),
                             r=["Linc", "lfp"], w=["Linc"])
                    pcs, pcsk = ps()
                    P.op("pe", lambda e: (e.matmul(pcs[:, 0:128].rearrange("p (b h) -> p b h", h=8), lhsT=ones_f[:], rhs=Linc[:, :, 15, :], start=True, stop=False),
                                          e.matmul(pcs[:, 0:128].rearrange("p (b h) -> p b h", h=8), lhsT=ones_f[0:4, :], rhs=lfn4[:], start=False, stop=True))[1],
                         r=["Linc", "lfn4"], w=[pcsk])
                    P.op("dve", lambda e: e.tensor_copy(out=Cbs[:].rearrange("p b h -> p (b h)"), in_=pcs[:, 0:128]), r=[pcsk], w=["Cbs"])
                    P.op("dve", lambda e: e.tensor_tensor(out=Linc[:], in0=Linc[:], in1=lfp[:], op=ALU.subtract), r=["Linc", "lfp", "Cbs"], w=["Linc"])
                    cpast = lfp
                    lfp2 = lfp[:].rearrange("p b g h -> p (b g h)")
                    Lin2 = Linc[:].rearrange("p b g h -> p (b g h)")
                    cp2 = cpast[:].rearrange("p b g h -> p (b g h)")
                    for q4 in range(4):
                        pq, pqk = ps()
                        cs = slice(q4 * 512, (q4 + 1) * 512)
                        mmg(pq[:, :], [(U_f[:], lfp2[:, cs]), (ones_f[:], Lin2[:, cs])], r=["Linc", "lfp"], w=[pqk])
                        for bb in range(4):
                            b_ = 4 * q4 + bb
                            P.op("dve", lambda e, pq=pq, bb=bb, b_=b_: e.scalar_tensor_tensor(
                                out=cpast[:, b_], in0=pq[:, bb * 128:(bb + 1) * 128].rearrange("p (g h) -> p g h", g=16), scalar=-1.0,
                                in1=Cbs[:, b_, :].unsqueeze(1).to_broadcast([128, 16, 8]), op0=ALU.mult, op1=ALU.add),
                                r=[pqk, "Cbs"], w=["cpast", "lfp"])
                    pbn, pbnk = ps()
                    mmg(pbn[0:4, 0:128].rearrange("p (b h) -> p b h", h=8), [(SU4[:], lfn4[:])], r=["lfn4", "SU4"], w=[pbnk])
                    P.op("dve", lambda e: e.tensor_copy(out=bnew[:].rearrange("p b h -> p (b h)"), in_=pbn[0:4, 0:128]), r=[pbnk], w=["bnew"])
                    P.op("pool", lambda e: e.memset(Qbd[:], 0.0), w=["Qbd"])
                    qs = qT[:, :, 2048:2112].rearrange("p c (b j) -> p c b j", j=4)
                    P.op("pool", lambda e: e.tensor_copy(out=Qbd[0:64, :, :, 0:4], in_=qs[0:64]), r=["Qbd"], w=["Qbd"])
                    P.op("pool", lambda e: e.tensor_copy(out=Qbd[64:128, :, :, 4:8], in_=qs[64:128]), r=["Qbd"], w=["Qbd"])
                    kt_rr = 0
                    for b in range(16):
                        kp, vp = Kp[0], Vp[b % 2]
                        kpk, vpk = "Kp0", "Vp%d" % (b % 2)
                        for pg in range(16):
                            P.idma(kp[:, pg, :], cache_k, idx[:, b * 16 + pg:b * 16 + pg + 1], r=["idx"], w=[kpk + "_%d" % pg], sem=kpk)
                        for pg in range(16):
                            P.idma(vp[:, pg, :], cache_v, idx[:, b * 16 + pg:b * 16 + pg + 1], r=["idx"], w=[vpk + "_%d" % pg], sem=vpk)
                        ss, ssk = psf[5], "ps5"
                        sn, snk = psf[6], "ps6"
                        acc, acck = psf[7], "ps7"
                        for pg in range(16):
                            ptb_, ptbk = ps()
                            ptv = ptb_[:].bitcast(BF16)

                            def trk(e, ptv=ptv, kp=kp, pg=pg):
                                ins = None
                                for hp in range(4):
                                    ins = e.transpose(ptv[:, hp * 128:(hp + 1) * 128], kp[:, pg, hp * 128:(hp + 1) * 128], ident_b[:])
                                return ins
                            P.op("pe", trk, r=[kpk + "_%d" % g_ for g_ in range(16)] + ["ident_b"], w=[ptbk])
                            ktp = KTp[kt_rr % 3]
                            ktk = "KTp%d" % (kt_rr % 3)
                            kt_rr += 1
                            evac(ktp[:].rearrange("p c k -> p (c k)"), ptv[:, 0:512], r=[ptbk], w=[ktk])

                            def qk(e, ktp=ktp, pg=pg, b=b):
                                ins = None
                                for hp in range(4):
                                    ins = e.matmul(ss[:, pg * 32 + hp * 8:pg * 32 + hp * 8 + 8], lhsT=ktp[:, hp, :], rhs=Qbd[:, hp, b, :], start=True, stop=True)
                                return ins
                            P.op("pe", qk, r=[ktk, "Qbd"], w=[ssk])

                        def qkn(e, b=b):
                            ins = None
                            for hp in range(4):
                                ins = e.matmul(sn[0:4, hp * 8:hp * 8 + 8], lhsT=kT[:, hp, 4096 + 4 * b:4096 + 4 * b + 4], rhs=Qbd[:, hp, b, :], start=True, stop=True)
                            return ins
                        P.op("pe", qkn, r=["Qbd"], w=[snk])
                        P.op("dve", lambda e, b=b: e.scalar_tensor_tensor(
                            out=stmp[:].rearrange("p (g h q) -> p g h q", g=16, h=8), in0=ss[:, :].rearrange("p (g h q) -> p g h q", g=16, h=8), scalar=0.125,
                            in1=cpast[:, b].unsqueeze(3).to_broadcast([128, 16, 8, 4]), op0=ALU.mult, op1=ALU.add), r=[ssk, "cpast"], w=["stmp"])
                        P.op("act", lambda e: e.activation(out=Pp[:].rearrange("p g c -> p (g c)"), in_=stmp[:], func=AF.Exp), r=["stmp"], w=["Pp"])
                        P.op("dve", lambda e, b=b: e.scalar_tensor_tensor(
                            out=tn[:].rearrange("p (h q) -> p h q", h=8), in0=sn[0:4, 0:32].rearrange("p (h q) -> p h q", h=8), scalar=0.125,
                            in1=bnew[:, b, :].unsqueeze(2).to_broadcast([4, 8, 4]), op0=ALU.mult, op1=ALU.add), r=[snk, "bnew"], w=["tn"])
                        P.op("act", lambda e: e.activation(out=tn[:], in_=tn[:], func=AF.Exp), r=["tn"], w=["tn"])
                        P.op("dve", lambda e: e.tensor_tensor(out=Pn[:].rearrange("p (h q) -> p h q", h=8), in0=tn[:].rearrange("p (h q) -> p h q", h=8),
                                                              in1=U_f[0:4, 0:4].unsqueeze(1).to_broadcast([4, 8, 4]), op=ALU.mult), r=["tn", "U_f"], w=["Pn"])

                        def pv(e, vp=vp, b=b):
                            for pg in range(16):
                                e.matmul(acc[0:32, :], lhsT=Pp[:, pg, :], rhs=vp[:, pg, :], start=(pg == 0), stop=False)
                            return e.matmul(acc[0:32, :], lhsT=Pn[:, :], rhs=Vn[:, b, :], start=False, stop=True)
                        P.op("pe", pv, r=["Pp", "Pn", "Vn"] + [vpk + "_%d" % g_ for g_ in range(16)], w=[acck])

                        def dn(e):
                            for pg in range(16):
                                e.matmul(sn[0:32, 64:65], lhsT=Pp[:, pg, :], rhs=ones_b[:, 0:1], start=(pg == 0), stop=False)
                            return e.matmul(sn[0:32, 64:65], lhsT=Pn[:, :], rhs=ones_b[0:4, 0:1], start=False, stop=True)
                        P.op("pe", dn, r=["Pp", "Pn", "tn"], w=[snk + "d"])
                        P.op("dve", lambda e: e.tensor_tensor(out=t3[:].rearrange("p (h d) -> p h d", h=8), in0=acc[0:32, :].rearrange("p (h d) -> p h d", h=8),
                                                              in1=hmask[:].unsqueeze(2).to_broadcast([32, 8, 64]), op=ALU.mult), r=[acck, "hmask"], w=["t3"])
                        P.op("dve", lambda e: e.tensor_reduce(out=o32[:], in_=t3[:].rearrange("p (h d) -> p d h", h=8), axis=AX.X, op=ALU.add), r=["t3"], w=["o32"])
                        P.op("dve", lambda e: e.reciprocal(out=rd[:], in_=sn[0:32, 64:65]), r=[snk + "d"], w=["rd"])
                        P.op("dve", lambda e: e.tensor_scalar(out=o32[:], in0=o32[:], scalar1=rd[:, 0:1], scalar2=None, op0=ALU.mult), r=["o32", "rd"], w=["o32"])
                        po, pok = ps()
                        P.op("pe", lambda e, po=po: e.transpose(po[0:64, 0:32], o32[:, :], ident[0:32, 0:32]), r=["o32", "ident"], w=[pok])
                        pov = po[0:64, 0:32].rearrange("p (c hh q) -> p c hh q", c=4, hh=2)
                        P.op("dve", lambda e, pov=pov, b=b: e.tensor_copy(out=oT[0:64, :, 2048 + 4 * b:2052 + 4 * b], in_=pov[:, :, 0, :]), r=[pok, snk, snk + "d"], w=["oTs%d" % b])
                        P.op("dve", lambda e, pov=pov, b=b: e.tensor_copy(out=oT[64:128, :, 2048 + 4 * b:2052 + 4 * b], in_=pov[:, :, 1, :]), r=[pok], w=["oTs%d" % b])
                    ps_n[0] = 8
                    P.barrier()

            if stage >= 3:
                with ExitStack() as s3:
                    Pt = [sb("Pt%d" % i, [128, 512], BF16, s3) for i in range(4)]
                    rec = sb("rec", [128, 512], F32, s3)
                    bcs = sb("bcs", [64, 512], F32, s3)
                    ps_n[0] = 5
                    pt_rr = 0
                    n_acc = 0
                    for sg in range(4):
                        for h in range(8):
                            hp, hh = h // 2, h % 2
                            pr = slice(hh * 64, hh * 64 + 64)
                            acc, acck = psf[6 + n_acc % 2], "ps%d" % (6 + n_acc % 2)
                            n_acc += 1
                            blocks = list(range(0, 4 * sg)) + list(range(16, 16 + 4 * sg + 4)) + list(range(4 * sg, 4 * sg + 4))
                            for bi, kb in enumerate(blocks):
                                diag = 4 * sg <= kb < 4 * sg + 4
                                q0 = 128 * (kb - 4 * sg) if diag else 0
                                st, stk = ps()
                                P.op("pe", lambda e, st=st, kb=kb, q0=q0: e.matmul(st[:, q0:512], lhsT=kT[pr, hp, kb * 128:(kb + 1) * 128],
                                                                                     rhs=qT[pr, hp, sg * 512 + q0:(sg + 1) * 512], start=True, stop=True),
                                     r=[], w=[stk])
                                pti = Pt[pt_rr % 4]
                                ptk = "Pt%d" % (pt_rr % 4)
                                pt_rr += 1
                                P.op("act", lambda e, st=st, pti=pti, kb=kb, q0=q0: e.activation(out=pti[:, q0:512], in_=st[:, q0:512], func=AF.Exp,
                                                                                                  bias=BIAS[:, sg, kb, h:h + 1], scale=0.125),
                                     r=[stk], w=[ptk])
                                if diag:
                                    P.op("pool", lambda e, pti=pti, q0=q0: e.tensor_tensor(out=pti[:, q0:q0 + 128], in0=pti[:, q0:q0 + 128], in1=U_b[:], op=ALU.mult),
                                         r=[ptk], w=[ptk])
                                P.op("pe", lambda e, pti=pti, kb=kb, q0=q0, bi=bi: e.matmul(acc[0:65, q0:512], lhsT=Vt[:, kb, h, 0:65], rhs=pti[:, q0:512],
                                                                                           start=(bi == 0), stop=(bi == len(blocks) - 1)),
                                     r=[ptk], w=[acck])
                            P.op("dve", lambda e: e.reciprocal(out=rec[64:65, :], in_=acc[64:65, :]), r=[acck], w=["rec"])
                            P.op("pe", lambda e: e.matmul(psf[5][0:64, :], lhsT=ones_f[64:65, 0:64], rhs=rec[64:65, :], start=True, stop=True), r=["rec"], w=["ps5"])
                            P.op("dve", lambda e: e.tensor_copy(out=bcs[:], in_=psf[5][0:64, :]), r=["ps5"], w=["bcs"])
                            P.op("dve", lambda e: e.tensor_tensor(out=oT[pr, hp, sg * 512:(sg + 1) * 512], in0=acc[0:64, :], in1=bcs[:], op=ALU.mult),
                                 r=[acck, "bcs"], w=["oT_%d_%d" % (sg, h)])
                    ps_n[0] = 8
                    dump("oT", oT[:], [128, 4, NTOK], BF16)
                    P.barrier()
            s_A.close()

            if stage >= 4:
                with ExitStack() as s4:
                    wcv = sb("wcv", [128, 8, 1536], BF16, s4)
                    wg = sb("wg", [128, 8, 2048], BF16, s4)
                    wco = sb("wco", [128, 4, 1024], BF16, s4)
                    wao = sb("wao", [128, 4, 1024], BF16, s4)
                    wo = sb("wo", [128, 8, 1024], BF16, s4)
                    wgr = sb("wgr", [128, 8, 20], F32, s4)
                    cw = sb("cw", [128, 4, 3], F32, s4)
                    g1b = sb("g1b", [128, 1024], F32, s4)
                    b1b = sb("b1b", [128, 1024], F32, s4)
                    wst[:] = [sb("wstB%d" % i, [128, 1024], F32, s4) for i in range(2)]
                    wload(wcv, "wcv", w_in, 8, 1536, col0=0, CH=1024)
                    wload(wg, "wg", w_in, 8, 1024, col0=C_GA, dcol0=0, CH=1024)
                    wload(wg, "wg", w_in, 8, 1024, col0=C_GB, dcol0=1024, CH=1024)
                    wload(wco, "wco", w_conv_out, 4, 1024, CH=1024)
                    wload(wao, "wao", w_attn_out, 4, 1024, CH=1024)
                    wload(wo, "wo", w_o, 8, 1024, CH=1024)
                    P.dma("sp", wgr[:], w_gr.rearrange("(k p) n -> p k n", p=128), w=["wgr"], sem="wgr")
                    with nc.allow_non_contiguous_dma(reason="tiny conv weight transpose"):
                        for c in range(4):
                            P.dma("sp", cw[:, c, :], conv_w[:, c * 128:(c + 1) * 128].rearrange("j p -> p j"), w=["cw%d" % c], sem="cw")
                    P.dma("sp", g1b[:], ln1_g.partition_broadcast(128), w=["g1b"], sem="g1b")
                    P.dma("sp", b1b[:], ln1_b.partition_broadcast(128), w=["b1b"], sem="b1b")

                    xin = sb("xin", [128, 4, 1024], F32, s4)
                    xT = sb("xT", [128, 8, 512], BF16, s4)
                    xh = sb("xh", [8, 1024], F32, s4)
                    xhT = sb("xhT", [128, 8, 8], BF16, s4)
                    uh = sb("uh", [128, 4, 8], F32, s4)
                    sct = sb("sct", [32, 512], F32, s4)
                    stT = sb("stT", [128, 4, 32], F32, s4)
                    xcs = sb("xcs", [128, 512], F32, s4)
                    ue = [sb("ue%d" % i, [128, 514], F32, s4) for i in range(2)]
                    tcv = sb("tcv", [128, 512], F32, s4)
                    gcv = sb("gcv", [128, 4, 512], BF16, s4)
                    ucp = sb("ucp", [128, 4, 2], F32, s4)
                    ucs = sb("ucs", [128, 4, 32], F32, s4)
                    cps = sb("cps", [2, 512], F32, s4)
                    css = sb("css", [32, 512], F32, s4)
                    sga = sb("sga", [128, 512], F32, s4)
                    sgb = sb("sgb", [128, 512], F32, s4)
                    za = sga
                    zb = sgb
                    zT = sb("zT", [128, 8, 512], BF16, s4)
                    xr = [sb("xr%d" % i, [128, 1024], F32, s4) for i in range(2)]
                    st6 = sb("st6", [128, 2, 6], F32, s4)
                    mv = sb("mv", [128, 2], F32, s4)
                    rstd = sb("rstd", [128, 1], F32, s4)
                    y0t = [sb("y0t%d" % i, [128, 1024], F32, s4) for i in range(1)]
                    x1Tb = [sb("x1Tb%d" % i, [128, 8, 128], BF16, s4) for i in range(2)]
                    x1Tf = sb("x1Tf", [128, 8, 128], F32, s4)
                    lg = sb("lg", [128, 20], F32, s4)
                    g8 = sb("g8", [128, 8], F32, s4)
                    m8 = sb("m8", [128, 8], F32, s4)
                    e8 = sb("e8", [128, 8], F32, s4)
                    me8 = sb("me8", [128, 8], F32, s4)
                    ohg = sb("ohg", [128, 4], F32, s4)
                    nm = sb("nm", [128, 2], F32, s4)
                    eg = sb("eg", [128, 4], F32, s4)
                    sgs = sb("sgs", [128, 2], F32, s4)
                    sel = sb("sel", [128, 16], F32, s4)
                    e1 = sb("e1", [128, 4], F32, s4)
                    s2m = sb("s2m", [128, 4], F32, s4)
                    wfac = sb("wfac", [128, 1], F32, s4)
                    P.op("pool", lambda e: e.memset(g8[:], -1e30), w=["g8"])
                    P.op("pool", lambda e: e.memset(e8[:], -1e30), w=["e8"])

                    P.dma("sp", xh[:], x_halo, w=["xh"], sem="xh")
                    ph, phk = ps()
                    P.op("pe", lambda e: [e.transpose(ph[:, k * 8:(k + 1) * 8], xh[0:8, k * 128:(k + 1) * 128], ident[0:8, 0:8]) for k in range(8)][-1],
                         r=["xh", "ident"], w=[phk])
                    evac(xhT[:].rearrange("p k t -> p (k t)"), ph[:, 0:64], r=[phk], w=["xhT"])
                    for c in range(4):
                        p1, p1k = ps()
                        mmg(p1[:, 0:8], [(wcv[:, k, C_XC + c * 128:C_XC + (c + 1) * 128], xhT[:, k, :]) for k in range(8)], r=["xhT", "wcv"], w=[p1k])
                        p2, p2k = ps()
                        mmg(p2[:, 0:8], [(wcv[:, k, C_CG + c * 128:C_CG + (c + 1) * 128], xhT[:, k, :]) for k in range(8)], r=["xhT", "wcv"], w=[p2k])
                        P.op("act", lambda e, p1=p1: e.copy(out=xcs[:, 0:8], in_=p1[:, 0:8]), r=[p1k], w=["xcs"])
                        P.op("dve", lambda e, p2=p2, c=c: e.tensor_tensor(out=uh[:, c, :], in0=xcs[:, 0:8], in1=p2[:, 0:8], op=ALU.mult), r=[p2k, "xcs"], w=["uh"])
                    P.dma("sp", sct[:], state_conv, w=["sct"], sem="sct")
                    pst, pstk = ps()
                    P.op("pe", lambda e: [e.transpose(pst[:, c * 32:(c + 1) * 32], sct[0:32, c * 128:(c + 1) * 128], ident[0:32, 0:32]) for c in range(4)][-1],
                         r=["sct", "ident"], w=[pstk])
                    evac(stT[:].rearrange("p c t -> p (c t)"), pst[:, 0:128], r=[pstk], w=["stT"])

                    tiles = [(x_own[s_ * 512:(s_ + 1) * 512, :], 512, s_ * 512, s_ * 4, s_) for s_ in range(4)] + [(x_s, 64, 2048, 16, 4)]
                    blk_n = 0
                    for (src, W, t0, gb0, sl) in tiles:
                        nb = (W + 127) // 128
                        pw = min(W, 128)
                        samp = (sl == 4)
                        xtall = load_xT(src, W, xin, xT, "xin", "xT")
                        for c in range(4):
                            u_ = ue[c % 2]
                            uk = "ue%d" % (c % 2)
                            p1, p1k = ps()
                            mmg(p1[:, 0:W], [(wcv[:, k, C_XC + c * 128:C_XC + (c + 1) * 128], xT[:, k, 0:W]) for k in range(8)], r=xtall + ["wcv"], w=[p1k])
                            p2, p2k = ps()
                            mmg(p2[:, 0:W], [(wcv[:, k, C_CG + c * 128:C_CG + (c + 1) * 128], xT[:, k, 0:W]) for k in range(8)], r=xtall + ["wcv"], w=[p2k])
                            p3, p3k = ps()
                            mmg(p3[:, 0:W], [(wcv[:, k, C_BG + c * 128:C_BG + (c + 1) * 128], xT[:, k, 0:W]) for k in range(8)], r=xtall + ["wcv"], w=[p3k])
                            P.op("act", lambda e, p1=p1: e.copy(out=xcs[:, 0:W], in_=p1[:, 0:W]), r=[p1k], w=["xcs"])
                            if not samp:
                                P.op("pool", lambda e, u_=u_, c=c: e.tensor_copy(out=u_[:, 0:2], in_=uh[:, c, 2 * sl:2 * sl + 2]), r=["uh"], w=[uk])
                                P.op("dve", lambda e, u_=u_, p2=p2: e.tensor_tensor(out=u_[:, 2:W + 2], in0=xcs[:, 0:W], in1=p2[:, 0:W], op=ALU.mult),
                                     r=[p2k, "xcs", uk], w=[uk])
                                uv = [u_[:, j:j + W] for j in range(3)]
                                tv = tcv[:, 0:W]
                            else:
                                u3 = u_[:, 0:96].rearrange("p (b i) -> p b i", i=6)
                                P.op("pool", lambda e, u3=u3, c=c: e.tensor_copy(out=u3[:, :, 0:2], in_=stT[:, c, :].rearrange("p (b i) -> p b i", i=2)),
                                     r=["stT"], w=[uk])
                                P.op("dve", lambda e, u3=u3, p2=p2: e.tensor_tensor(out=u3[:, :, 2:6], in0=xcs[:, 0:64].rearrange("p (b j) -> p b j", j=4),
                                                                                     in1=p2[:, 0:64].rearrange("p (b j) -> p b j", j=4), op=ALU.mult),
                                     r=[p2k, "xcs", uk], w=[uk])
                                uv = [u3[:, :, j:j + 4] for j in range(3)]
                                tv = tcv[:, 0:64].rearrange("p (b j) -> p b j", j=4)
                            P.op("pool", lambda e, uv=uv, tv=tv, c=c: e.tensor_scalar(out=tv, in0=uv[0], scalar1=cw[:, c, 0:1], scalar2=None, op0=ALU.mult),
                                 r=[uk] + ["cw%d" % i for i in range(4)], w=["tcv"])
                            for j in (1, 2):
                                P.op("dve", lambda e, uv=uv, tv=tv, c=c, j=j: e.scalar_tensor_tensor(out=tv, in0=uv[j], scalar=cw[:, c, j:j + 1], in1=tv,
                                                                                                     op0=ALU.mult, op1=ALU.add), r=[uk, "tcv"], w=["tcv"])
                            P.op("dve", lambda e, p3=p3, c=c: e.tensor_tensor(out=gcv[:, c, 0:W], in0=tcv[:, 0:W], in1=p3[:, 0:W], op=ALU.mult),
                                 r=[p3k, "tcv"], w=["gcv%d" % c])
                            if sl == 3:
                                P.op("pool", lambda e, u_=u_, c=c: e.tensor_copy(out=ucp[:, c, :], in_=u_[:, W:W + 2]), r=[uk], w=["ucp"])
                            if samp:
                                P.op("pool", lambda e, u3=u3, c=c: e.tensor_copy(out=ucs[:, c, :].rearrange("p (b i) -> p b i", i=2), in_=u3[:, :, 4:6]),
                                     r=[uk], w=["ucs"])
                        if sl == 3:
                            pcp, pcpk = ps()
                            P.op("pe", lambda e, pcp=pcp: [e.transpose(pcp[0:2, c * 128:(c + 1) * 128], ucp[:, c, :], ident[:]) for c in range(4)][-1],
                                 r=["ucp", "ident"], w=[pcpk])
                            evac(cps[:], pcp[0:2, :], r=[pcpk], w=["cps"])
                            P.dma("sp", conv_p, cps[:], r=["cps"], sem="cps")
                        if samp:
                            pcp, pcpk = ps()
                            P.op("pe", lambda e, pcp=pcp: [e.transpose(pcp[0:32, c * 128:(c + 1) * 128], ucs[:, c, :], ident[:]) for c in range(4)][-1],
                                 r=["ucs", "ident"], w=[pcpk])
                            evac(css[:], pcp[0:32, :], r=[pcpk], w=["css"])
                            P.dma("sp", conv_s, css[:], r=["css"], sem="css")
                        gall = ["gcv%d" % c for c in range(4)]
                        for m in range(8):
                            ms = slice(m * 128, (m + 1) * 128)
                            pya, pyak = ps()
                            mmg(pya[:, 0:W], [(wco[:, c, ms], gcv[:, c, 0:W]) for c in range(4)], r=gall + ["wco"], w=[pyak])
                            pga, pgak = ps()
                            mmg(pga[:, 0:W], [(wg[:, k, ms], xT[:, k, 0:W]) for k in range(8)], r=xtall + ["wg"], w=[pgak])
                            pyb, pybk = ps()
                            mmg(pyb[:, 0:W], [(wao[:, c, ms], oT[:, c, t0:t0 + W]) for c in range(4)], r=["wao"], w=[pybk])
                            pgb, pgbk = ps()
                            mmg(pgb[:, 0:W], [(wg[:, k, 1024 + m * 128:1024 + (m + 1) * 128], xT[:, k, 0:W]) for k in range(8)], r=xtall + ["wg"], w=[pgbk])
                            P.op("act", lambda e, pga=pga: e.activation(out=sga[:, 0:W], in_=pga[:, 0:W], func=AF.Sigmoid), r=[pgak], w=["sga"])
                            P.op("act", lambda e, pgb=pgb: e.activation(out=sgb[:, 0:W], in_=pgb[:, 0:W], func=AF.Sigmoid), r=[pgbk], w=["sgb"])
                            P.op("dve", lambda e, pya=pya: e.tensor_tensor(out=za[:, 0:W], in0=sga[:, 0:W], in1=pya[:, 0:W], op=ALU.mult), r=[pyak, "sga"], w=["sga"])
                            P.op("dve", lambda e, pyb=pyb: e.tensor_tensor(out=zb[:, 0:W], in0=sgb[:, 0:W], in1=pyb[:, 0:W], op=ALU.mult), r=[pybk, "sgb"], w=["sgb"])
                            P.op("pool", lambda e, m=m: e.tensor_tensor(out=zT[:, m, 0:W], in0=za[:, 0:W], in1=zb[:, 0:W], op=ALU.add), r=["sga", "sgb"], w=["zT%d" % m])
                        zall = ["zT%d" % m for m in range(8)]
                        for b in range(nb):
                            gb = gb0 + b
                            xr_ = xr[blk_n % 2]
                            xrk = "xr%d" % (blk_n % 2)
                            y0_ = y0t[0]
                            y0k = "y0t0"
                            xb_ = x1Tb[blk_n % 2]
                            xbk = "x1Tb%d" % (blk_n % 2)
                            blk_n += 1
                            tsl = slice(b * 128, b * 128 + pw)
                            for half in range(2):
                                hs = slice(half * 512, (half + 1) * 512)
                                pm, pmk = ps()
                                mmg(pm[0:pw, :], [(zT[:, m, tsl], wo[:, m, hs]) for m in range(8)], r=zall + ["wo"], w=[pmk])
                                P.op("dve", lambda e, pm=pm, hs=hs, xr_=xr_, b=b: e.scalar_tensor_tensor(out=xr_[0:pw, hs], in0=xin[0:pw, b, hs], scalar=ALPHA, in1=pm[0:pw, :],
                                                                                                        op0=ALU.mult, op1=ALU.add), r=[pmk, "xin"], w=[xrk])
                            xrh = [xrk]
                            for half in range(2):
                                P.op("dve", lambda e, half=half, xr_=xr_: e.bn_stats(out=st6[0:pw, half, :], in_=xr_[0:pw, half * 512:(half + 1) * 512]), r=xrh, w=["st6_%d" % half])
                            P.op("dve", lambda e: e.bn_aggr(out=mv[0:pw, :], in_=st6[0:pw].rearrange("p a s -> p (a s)")), r=["st6_0", "st6_1"], w=["mv"])
                            P.op("dve", lambda e: e.tensor_scalar(out=rstd[0:pw, :], in0=mv[0:pw, 1:2], scalar1=EPS, scalar2=None, op0=ALU.add), r=["mv"], w=["rstd"])
                            P.op("act", lambda e: e.sqrt(out=rstd[0:pw, :], in_=rstd[0:pw, :]), r=["rstd"], w=["rstd"])
                            P.op("dve", lambda e: e.reciprocal(out=rstd[0:pw, :], in_=rstd[0:pw, :]), r=["rstd"], w=["rstd"])
                            P.op("dve", lambda e, xr_=xr_: e.tensor_scalar(out=xr_[0:pw, :], in0=xr_[0:pw, :], scalar1=mv[0:pw, 0:1], scalar2=rstd[0:pw, 0:1],
                                                                           op0=ALU.subtract, op1=ALU.mult), r=xrh + ["mv", "rstd"], w=[xrk])
                            P.op("pool", lambda e, xr_=xr_: e.tensor_tensor(out=xr_[0:pw, :], in0=xr_[0:pw, :], in1=g1b[0:pw, :], op=ALU.mult), r=[xrk, "g1b"], w=[xrk])
                            P.op("pool", lambda e, xr_=xr_: e.tensor_tensor(out=xr_[0:pw, :], in0=xr_[0:pw, :], in1=b1b[0:pw, :], op=ALU.add), r=[xrk, "b1b"], w=[xrk])
                            P.op("act", lambda e, xr_=xr_, y0_=y0_: e.mul(out=y0_[0:pw, :], in_=xr_[0:pw, :], mul=ALPHA), r=[xrk], w=[y0k])
                            P.dma("sp", y0_d[t0 + b * 128:t0 + b * 128 + pw, :], y0_[0:pw, :], r=[y0k], sem=y0k)
                            for half in range(2):
                                ptt, pttk = ps()
                                P.op("pe", lambda e, ptt=ptt, half=half, xr_=xr_: [e.transpose(ptt[:, kk * 128:kk * 128 + pw], xr_[0:pw, (half * 4 + kk) * 128:(half * 4 + kk + 1) * 128],
                                                                                               ident[0:pw, 0:pw]) for kk in range(4)][-1], r=[xrk, "ident"], w=[pttk])
                                pv4 = ptt[:].rearrange("p (k t) -> p k t", k=4)[:, :, 0:pw]
                                P.op("act", lambda e, pv4=pv4, half=half, xb_=xb_: e.copy(out=xb_[:, half * 4:half * 4 + 4, 0:pw], in_=pv4), r=[pttk], w=[xbk + "_%d" % half])
                                P.op("dve", lambda e, pv4=pv4, half=half: e.tensor_copy(out=x1Tf[:, half * 4:half * 4 + 4, 0:pw], in_=pv4), r=[pttk, xbk + "_%d" % half], w=["x1Tf_%d" % half])
                            P.dma("sp", x1T_d[:, :, t0 + b * 128:t0 + b * 128 + pw], xb_[:, :, 0:pw], r=[xbk + "_0", xbk + "_1"], sem=xbk)
                            prt, prtk = ps()
                            mmg(prt[0:pw, 0:20], [(x1Tf[:, k, 0:pw], wgr[:, k, :]) for k in range(8)], r=["x1Tf_0", "x1Tf_1", "wgr"], w=[prtk])
                            R = slice(0, pw)
                            P.op("dve", lambda e, prt=prt: e.tensor_copy(out=lg[R, :], in_=prt[R, 0:20]), r=[prtk], w=["lg"])
                            P.op("dve", lambda e: e.tensor_copy(out=g8[R, 0:4], in_=lg[R, 0:4]), r=["lg"], w=["g8"])
                            P.op("dve", lambda e: e.max(out=m8[R, :], in_=g8[R, :]), r=["g8"], w=["m8"])
                            P.op("dve", lambda e: e.tensor_scalar(out=ohg[R, :], in0=lg[R, 0:4], scalar1=m8[R, 0:1], scalar2=None, op0=ALU.is_equal), r=["lg", "m8"], w=["ohg"])
                            P.op("dve", lambda e: e.tensor_scalar(out=nm[R, 0:1], in0=m8[R, 0:1], scalar1=-1.0, scalar2=None, op0=ALU.mult), r=["m8"], w=["nm0"])
                            P.op("act", lambda e: e.activation(out=eg[R, :], in_=lg[R, 0:4], func=AF.Exp, bias=nm[R, 0:1], scale=1.0), r=["lg", "nm0"], w=["eg"])
                            P.op("dve", lambda e: e.reduce_sum(out=sgs[R, 0:1], in_=eg[R, :], axis=AX.X), r=["eg"], w=["sgs0"])
                            lg3 = lg[R, 4:20].rearrange("p (g x) -> p g x", x=4)
                            P.op("dve", lambda e, lg3=lg3: e.tensor_tensor(out=sel[R, :].rearrange("p (g x) -> p g x", x=4), in0=lg3,
                                                                           in1=ohg[R, :].unsqueeze(2).to_broadcast([pw, 4, 4]), op=ALU.mult), r=["lg", "ohg"], w=["sel"])
                            P.op("dve", lambda e: e.tensor_reduce(out=e8[R, 0:4], in_=sel[R, :].rearrange("p (g x) -> p x g", x=4), axis=AX.X, op=ALU.add), r=["sel"], w=["e8"])
                            P.op("dve", lambda e: e.max(out=me8[R, :], in_=e8[R, :]), r=["e8"], w=["me8"])
                            P.op("dve", lambda e: e.tensor_scalar(out=nm[R, 1:2], in0=me8[R, 0:1], scalar1=-1.0, scalar2=None, op0=ALU.mult), r=["me8"], w=["nm1"])
                            P.op("act", lambda e: e.activation(out=e1[R, :], in_=e8[R, 0:4], func=AF.Exp, bias=nm[R, 1:2], scale=1.0), r=["e8", "nm1"], w=["e1"])
                            P.op("dve", lambda e: e.tensor_scalar(out=s2m[R, :], in0=e8[R, 0:4], scalar1=me8[R, 1:2], scalar2=None, op0=ALU.is_ge), r=["e8", "me8"], w=["s2m"])
                            P.op("dve", lambda e: e.tensor_tensor(out=e1[R, :], in0=e1[R, :], in1=s2m[R, :], op=ALU.mult), r=["e1", "s2m"], w=["e1"])
                            P.op("dve", lambda e: e.reduce_sum(out=sgs[R, 1:2], in_=e1[R, :], axis=AX.X), r=["e1"], w=["sgs1"])
                            P.op("dve", lambda e: e.tensor_tensor(out=wfac[R, :], in0=sgs[R, 0:1], in1=sgs[R, 1:2], op=ALU.mult), r=["sgs0", "sgs1"], w=["wfac"])
                            P.op("dve", lambda e: e.reciprocal(out=wfac[R, :], in_=wfac[R, :]), r=["wfac"], w=["wfac"])
                            P.op("dve", lambda e: e.tensor_scalar(out=e1[R, :], in0=e1[R, :], scalar1=wfac[R, 0:1], scalar2=None, op0=ALU.mult), r=["e1", "wfac"], w=["e1"])
                            P.op("dve", lambda e, gb=gb: e.tensor_tensor(out=gate[R, gb, :].rearrange("p (g x) -> p g x", x=4),
                                                                         in0=ohg[R, :].unsqueeze(2).to_broadcast([pw, 4, 4]),
                                                                         in1=e1[R, :].unsqueeze(1).to_broadcast([pw, 4, 4]), op=ALU.mult), r=["ohg", "e1", "gate"], w=["gate%d" % gb])
                    dump("gate", gate[:], [128, 17, 16], r=["gate%d" % i for i in range(17)])
                    P.barrier()
            s_oT.close()

            if stage >= 5:
                with ExitStack() as s5:
                    x1T = sb("x1T", [128, 8, NTOK], BF16, s5)
                    yacc = sb("yacc", [128, 17, 1024], F32, s5)
                    g2b = sb("g2b", [128, 1024], F32, s5)
                    b2b = sb("b2b", [128, 1024], F32, s5)
                    w1e = [sb("w1e%d" % i, [128, 8, 512], BF16, s5) for i in range(2)]
                    w3e = [sb("w3e%d" % i, [128, 8, 512], BF16, s5) for i in range(2)]
                    w2e = [sb("w2e%d" % i, [128, 4, 1024], BF16, s5) for i in range(2)]
                    s1t = [sb("s1t%d" % i, [128, 512], F32, s5) for i in range(2)]
                    hT = [sb("hT%d" % i, [128, 4, 512], BF16, s5) for i in range(2)]
                    st6 = sb("st6b", [128, 2, 6], F32, s5)
                    mv = sb("mvb", [128, 2], F32, s5)
                    rstd = sb("rstdb", [128, 1], F32, s5)
                    P.dma("sp", x1T[:], x1T_d, w=["x1T"], sem="x1T")
                    P.dma("sp", yacc[:, 0:16, :], y0_d[0:2048, :].rearrange("(b p) d -> p b d", p=128), w=["yacc"], sem="yacc")
                    P.dma("sp", yacc[0:64, 16, :], y0_d[2048:2112, :], w=["yacc"], sem="yacc")
                    P.dma("sp", g2b[:], ln2_g.partition_broadcast(128), w=["g2b"], sem="g2b")
                    P.dma("sp", b2b[:], ln2_b.partition_broadcast(128), w=["b2b"], sem="b2b")

                    wst[:] = [sb("wstC%d" % i, [128, 2048], F32, s5) for i in range(2)]

                    def load_e(e_):
                        i = e_ % 2
                        return (wchunks(w1e[i], "w1e%d" % i, w1[e_], 8, 512, CH=2048) + wchunks(w3e[i], "w3e%d" % i, w3[e_], 8, 512, CH=2048)
                                + wchunks(w2e[i], "w2e%d" % i, w2[e_], 4, 1024, CH=2048))
                    for f_ in load_e(0):
                        f_()
                    ttiles = [(0, 512), (512, 512), (1024, 512), (1536, 512), (2048, 64)]
                    it = 0
                    for e_ in range(16):
                        pend = load_e(e_ + 1) if e_ + 1 < 16 else []
                        i = e_ % 2
                        a1, a3, a2 = w1e[i], w3e[i], w2e[i]
                        k1, k3, k2 = "w1e%d" % i, "w3e%d" % i, "w2e%d" % i
                        for (t0, W) in ttiles:
                            nb = (W + 127) // 128
                            pw = min(W, 128)
                            h_ = hT[it % 2]
                            hk = "hT%d" % (it % 2)
                            it += 1
                            for f in range(4):
                                fs = slice(f * 128, (f + 1) * 128)
                                p1, p1k = ps()
                                mmg(p1[:, 0:W], [(a1[:, k, fs], x1T[:, k, t0:t0 + W]) for k in range(8)], r=["x1T", k1], w=[p1k])
                                p3, p3k = ps()
                                mmg(p3[:, 0:W], [(a3[:, k, fs], x1T[:, k, t0:t0 + W]) for k in range(8)], r=["x1T", k3], w=[p3k])
                                s_ = s1t[f % 2]
                                sk = "s1t%d" % (f % 2)
                                P.op("act", lambda e, p1=p1, s_=s_: e.activation(out=s_[:, 0:W], in_=p1[:, 0:W], func=AF.Silu), r=[p1k], w=[sk])
                                P.op("dve", lambda e, p3=p3, s_=s_, f=f, h_=h_: e.tensor_tensor(out=h_[:, f, 0:W], in0=s_[:, 0:W], in1=p3[:, 0:W], op=ALU.mult),
                                     r=[p3k, sk], w=[hk + "_%d" % f])
                            for f_ in pend[:2 if t0 == 0 else 1]:
                                f_()
                            pend = pend[2 if t0 == 0 else 1:]
                            hall = [hk + "_%d" % f for f in range(4)]
                            for b in range(nb):
                                gb = t0 // 128 + b
                                for half in range(2):
                                    hs = slice(half * 512, (half + 1) * 512)
                                    py, pyk = ps()
                                    mmg(py[0:pw, :], [(h_[:, f, b * 128:b * 128 + pw], a2[:, f, hs]) for f in range(4)], r=hall + [k2], w=[pyk])
                                    P.op("dve", lambda e, py=py, gb=gb, hs=hs: e.scalar_tensor_tensor(out=yacc[0:pw, gb, hs], in0=py[0:pw, :], scalar=gate[0:pw, gb, e_:e_ + 1],
                                                                                                     in1=yacc[0:pw, gb, hs], op0=ALU.mult, op1=ALU.add),
                                         r=[pyk, "yacc"], w=["yacc%d_%d" % (gb, half)])
                    for gb in range(17):
                        pw = 128 if gb < 16 else 64
                        yk = ["yacc%d_0" % gb, "yacc%d_1" % gb]
                        ya = yacc[0:pw, gb, :]
                        for half in range(2):
                            P.op("dve", lambda e, half=half, gb=gb, pw=pw: e.bn_stats(out=st6[0:pw, half, :], in_=yacc[0:pw, gb, half * 512:(half + 1) * 512]), r=yk, w=["st6_%d" % half])
                        P.op("dve", lambda e, pw=pw: e.bn_aggr(out=mv[0:pw, :], in_=st6[0:pw].rearrange("p a s -> p (a s)")), r=["st6_0", "st6_1"], w=["mv"])
                        P.op("dve", lambda e, pw=pw: e.tensor_scalar(out=rstd[0:pw, :], in0=mv[0:pw, 1:2], scalar1=EPS, scalar2=None, op0=ALU.add), r=["mv"], w=["rstd"])
                        P.op("act", lambda e, pw=pw: e.sqrt(out=rstd[0:pw, :], in_=rstd[0:pw, :]), r=["rstd"], w=["rstd"])
                        P.op("dve", lambda e, pw=pw: e.reciprocal(out=rstd[0:pw, :], in_=rstd[0:pw, :]), r=["rstd"], w=["rstd"])
                        P.op("dve", lambda e, ya=ya, pw=pw: e.tensor_scalar(out=ya, in0=ya, scalar1=mv[0:pw, 0:1], scalar2=rstd[0:pw, 0:1],
                                                                           op0=ALU.subtract, op1=ALU.mult), r=yk + ["mv", "rstd"], w=["yo%d" % gb])
                        P.op("pool", lambda e, ya=ya, pw=pw: e.tensor_tensor(out=ya, in0=ya, in1=g2b[0:pw, :], op=ALU.mult), r=["yo%d" % gb, "g2b"], w=["yo%d" % gb])
                        P.op("pool", lambda e, ya=ya, pw=pw: e.tensor_tensor(out=ya, in0=ya, in1=b2b[0:pw, :], op=ALU.add), r=["yo%d" % gb, "b2b"], w=["yo%d" % gb])
                        if gb < 16:
                            P.dma("sp", y_own[gb * 128:(gb + 1) * 128, :], ya, r=["yo%d" % gb], sem="yo%d" % (gb % 4))
                        else:
                            P.dma("sp", y_s, ya, r=["yo%d" % gb], sem="yo%d" % (gb % 4))
                    P.barrier()
        except StopBuild:
            pass
        P.finish()
    nc._dbg_out = list(dbg_out.keys())
    return nc


def make_in_maps(inputs, cores):
    f32 = np.float32
    xp = np.asarray(inputs["x_prompt"], f32)
    xs = np.asarray(inputs["x_sample"], f32)
    ck = np.asarray(inputs["cache_k"], f32)
    npool = ck.shape[1]
    ck = ck.reshape(npool * 128, 512)
    cv = np.asarray(inputs["cache_v"], f32).reshape(npool * 128, 512)
    cf = np.asarray(inputs["cache_logf"], f32).reshape(npool * 128, 8)
    sc = np.asarray(inputs["state_conv"], f32)[0]
    pt = np.asarray(inputs["page_table"], np.int32)
    g = lambda k: np.asarray(inputs[k], f32)
    common = {
        "cache_k": ck, "cache_v": cv, "cache_f": cf,
        "w_in": g("w_in")[0], "b_f": g("b_f").reshape(1, 8),
        "conv_w": g("conv_w")[0], "w_conv_out": g("w_conv_out")[0],
        "w_attn_out": g("w_attn_out")[0], "w_o": g("w_o")[0],
        "ln1_g": g("ln1_g").reshape(1, D), "ln1_b": g("ln1_b").reshape(1, D),
        "w_gr": np.ascontiguousarray(np.concatenate([g("w_group")[0], g("w_router")[0]], axis=1)),
        "w1": g("w1")[0], "w3": g("w3")[0], "w2": g("w2")[0],
        "ln2_g": g("ln2_g").reshape(1, D), "ln2_b": g("ln2_b").reshape(1, D),
    }
    in_maps = []
    for c in cores:
        s, r = c // 2, c % 2
        own, oth = OWN[r], OWN[1 - r]
        xt = xp[s].reshape(8, 512, D)
        halo = np.zeros((8, D), f32)
        for sl, T in enumerate(own):
            if T > 0:
                halo[2 * sl:2 * sl + 2] = xp[s, 512 * T - 2:512 * T]
        rcv = np.zeros((1, 80), f32)
        order = own + oth
        for p_ in range(8):
            for p2 in range(8):
                rcv[0, p_ * 8 + p2] = 1.0 if order[p_] < order[p2] else 0.0
        for sl in range(4):
            rcv[0, 64 + sl] = 0.0 if oth[sl] < own[sl] else NEG
        m = dict(common)
        m.update({
            "x_own": np.ascontiguousarray(xt[own].reshape(2048, D)),
            "x_oth": np.ascontiguousarray(xt[oth].reshape(2048, D)),
            "x_halo": halo,
            "x_s": np.ascontiguousarray(xs[16 * c:16 * c + 16].reshape(64, D)),
            "state_conv": np.ascontiguousarray(sc[16 * c:16 * c + 16].reshape(32, 512)),
            "page_table": np.ascontiguousarray(pt[16 * c:16 * c + 16].reshape(1, 256)),
            "rolec": rcv,
        })
        in_maps.append(m)
    return in_maps, npool


def assemble(res, cores, nseq=4, nsamp=128):
    f32 = np.float32
    y_p = np.zeros((nseq, 4096, D), f32)
    k_p = np.zeros((1, nseq, 4096, 8, 64), f32)
    v_p = np.zeros((1, nseq, 4096, 8, 64), f32)
    f_p = np.zeros((1, nseq, 4096, 8), f32)
    c_p = np.zeros((1, nseq, 2, 512), f32)
    y_sm = np.zeros((nsamp, 4, D), f32)
    k_sm = np.zeros((1, nsamp, 4, 8, 64), f32)
    v_sm = np.zeros((1, nsamp, 4, 8, 64), f32)
    f_sm = np.zeros((1, nsamp, 4, 8), f32)
    c_sm = np.zeros((1, nsamp, 2, 512), f32)
    for o, c in zip(res, cores):
        s, r = c // 2, c % 2
        for sl, T in enumerate(OWN[r]):
            rows = slice(512 * T, 512 * T + 512)
            y_p[s, rows] = o["y_own"][sl * 512:(sl + 1) * 512]
            k_p[0, s, rows] = o["k_own"][sl * 512:(sl + 1) * 512].reshape(512, 8, 64)
            v_p[0, s, rows] = o["v_own"][sl * 512:(sl + 1) * 512].reshape(512, 8, 64)
            f_p[0, s, rows] = o["f_own"][sl * 512:(sl + 1) * 512]
        if r == 0:
            c_p[0, s] = o["conv_p"]
        sl_ = slice(16 * c, 16 * c + 16)
        y_sm[sl_] = o["y_s"].reshape(16, 4, D)
        k_sm[0, sl_] = o["k_s"].reshape(16, 4, 8, 64)
        v_sm[0, sl_] = o["v_s"].reshape(16, 4, 8, 64)
        f_sm[0, sl_] = o["f_s"].reshape(16, 4, 8)
        c_sm[0, sl_] = o["conv_s"].reshape(16, 2, 512)
    return (y_p, y_sm, k_p, v_p, f_p, c_p, k_sm, v_sm, f_sm, c_sm)


_NC = {}


def kernel(**inputs):
    cores = list(range(8))
    in_maps, npool = make_in_maps(inputs, cores)
    if npool not in _NC:
        _NC[npool] = build_nc(npool)
    res = run_bass_kernel_spmd(_NC[npool], in_maps, core_ids=cores).results
    return assemble(res, cores)
```

```python
import os
import numpy as np
from contextlib import ExitStack
import concourse.bass as bass
import concourse.mybir as mybir
from concourse.bass_utils import run_bass_kernel_spmd

F32 = mybir.dt.float32
BF16 = mybir.dt.bfloat16
I32 = mybir.dt.int32
AF = mybir.ActivationFunctionType
ALU = mybir.AluOpType
AX = mybir.AxisListType

D = 1024
NIN = 5128
OWN = [[0, 3, 4, 7], [1, 2, 5, 6]]
ALPHA = 2.0 ** 0.25
EPS = 1e-5
NEG = -30000.0
C_XC, C_BG, C_CG, C_Q, C_K, C_V, C_F, C_GA, C_GB = 0, 512, 1024, 1536, 2048, 2560, 3072, 3080, 4104
NTOK = 2112


class StopBuild(Exception):
    pass


class Prog:
    def __init__(self, nc, es):
        self.nc = nc
        self.es = es
        self.eng = {"pe": nc.tensor, "act": nc.scalar, "dve": nc.vector, "pool": nc.gpsimd, "sp": nc.sync}
        self.sem = {k: es.enter_context(nc.semaphore("s_" + k)) for k in self.eng}
        self.cnt = {k: 0 for k in self.eng}
        self.waited = {k: {} for k in self.eng}
        self.last_w = {}
        self.readers = {}
        self.dsem = {}
        self.dcnt = {}
        self.dead = False

    def _deps(self, r, w):
        deps = []
        for k in r:
            if k in self.last_w:
                deps.append(self.last_w[k])
        for k in w:
            if k in self.last_w:
                deps.append(self.last_w[k])
            deps.extend(self.readers.get(k, ()))
        return deps

    def _wait(self, eng, deps):
        best = {}
        for (s, v) in deps:
            if eng == "pe" and s == "pe":
                continue
            if v > best.get(s, 0):
                best[s] = v
        for s, v in best.items():
            if self.waited[eng].get(s, 0) < v:
                semh = self.sem[s] if s in self.sem else self.dsem[s]
                self.eng[eng].wait_ge(semh, v)
                self.waited[eng][s] = v

    def _record(self, tok, r, w):
        for k in w:
            self.last_w[k] = tok
            self.readers[k] = []
        for k in r:
            if k not in w:
                self.readers.setdefault(k, []).append(tok)

    def op(self, eng, fn, r=(), w=()):
        if self.dead:
            return
        self._wait(eng, self._deps(r, w))
        ins = fn(self.eng[eng])
        self.cnt[eng] += 1
        ins.then_inc(self.sem[eng], 1)
        self._record((eng, self.cnt[eng]), r, w)

    def _dsem(self, sem):
        if sem not in self.dsem:
            self.dsem[sem] = self.es.enter_context(self.nc.semaphore("d_" + sem))
            self.dcnt[sem] = 0

    def dma(self, q, out, in_, r=(), w=(), sem=None, **kw):
        if self.dead:
            return
        self._dsem(sem)
        self._wait(q, self._deps(r, w))
        ins = self.eng[q].dma_start(out=out, in_=in_, **kw)
        self.dcnt[sem] += 16
        ins.then_inc(self.dsem[sem], 16)
        self._record((sem, self.dcnt[sem]), r, w)

    def idma(self, out, in_, idx, r=(), w=(), sem=None):
        if self.dead:
            return
        self._dsem(sem)
        self._wait("pool", self._deps(r, w))
        ins = self.nc.gpsimd.indirect_dma_start(
            out=out, out_offset=None, in_=in_, in_offset=bass.IndirectOffsetOnAxis(ap=idx, axis=0))
        self.dcnt[sem] += 16
        ins.then_inc(self.dsem[sem], 16)
        self._record((sem, self.dcnt[sem]), r, w)

    def _all(self):
        toks = [(k, self.cnt[k]) for k in self.eng if self.cnt[k] > 0]
        toks += [(k, self.dcnt[k]) for k in self.dsem if self.dcnt[k] > 0]
        return toks

    def barrier(self):
        if self.dead:
            return
        toks = self._all()
        for e in self.eng:
            self._wait(e, toks)
        self.last_w = {}
        self.readers = {}

    def finish(self):
        self._wait("sp", self._all())


def build_nc(npool=2560, stage=99, dbg=False):
    nc = bass.Bass("TRN2", target_bir_lowering=False)

    def din(name, shape, dt=F32):
        return nc.dram_tensor(name, list(shape), dt, kind="ExternalInput").ap()

    def dout(name, shape, dt=F32):
        return nc.dram_tensor(name, list(shape), dt, kind="ExternalOutput").ap()

    x_own = din("x_own", [2048, D])
    x_oth = din("x_oth", [2048, D])
    x_halo = din("x_halo", [8, D])
    x_s = din("x_s", [64, D])
    cache_k = din("cache_k", [npool * 128, 512])
    cache_v = din("cache_v", [npool * 128, 512])
    cache_f = din("cache_f", [npool * 128, 8])
    state_conv = din("state_conv", [32, 512])
    page_table = din("page_table", [1, 256], I32)
    rolec = din("rolec", [1, 80])
    w_in = din("w_in", [D, NIN])
    b_f = din("b_f", [1, 8])
    conv_w = din("conv_w", [3, 512])
    w_conv_out = din("w_conv_out", [512, D])
    w_attn_out = din("w_attn_out", [512, D])
    w_o = din("w_o", [D, D])
    ln1_g = din("ln1_g", [1, D])
    ln1_b = din("ln1_b", [1, D])
    w_gr = din("w_gr", [D, 20])
    w1 = din("w1", [16, D, 512])
    w3 = din("w3", [16, D, 512])
    w2 = din("w2", [16, 512, D])
    ln2_g = din("ln2_g", [1, D])
    ln2_b = din("ln2_b", [1, D])

    y_own = dout("y_own", [2048, D])
    y_s = dout("y_s", [64, D])
    k_own = dout("k_own", [2048, 512])
    v_own = dout("v_own", [2048, 512])
    f_own = dout("f_own", [2048, 8])
    conv_p = dout("conv_p", [2, 512])
    k_s = dout("k_s", [64, 512])
    v_s = dout("v_s", [64, 512])
    f_s = dout("f_s", [64, 8])
    conv_s = dout("conv_s", [32, 512])
    y0_d = nc.dram_tensor("y0_d", [NTOK, D], F32, kind="Internal").ap()
    x1T_d = nc.dram_tensor("x1T_d", [128, 8, NTOK], BF16, kind="Internal").ap()
    dbg_out = {}

    with ExitStack() as es:
        P = Prog(nc, es)

        try:
            def sb(name, shape, dt=F32, stack=es):
                return stack.enter_context(nc.sbuf_tensor(name, list(shape), dt))

            def chk(x):
                if stage < x:
                    P.dead = True

            def dump(name, ap, shape, dt=F32, r=()):
                if dbg:
                    d = dout("dbg_" + name, shape, dt)
                    dbg_out[name] = d
                    P.dma("sp", d, ap, r=list(r), sem="dbg_" + name)

            psf = [es.enter_context(nc.psum_tensor("ps%d" % i, [128, 512], F32)) for i in range(8)]
            ps_rr = [0]
            ps_n = [8]

            def ps():
                i = ps_rr[0] % ps_n[0]
                ps_rr[0] += 1
                return psf[i], "ps%d" % i

            ev_rr = [0]

            def evac(out, in_, r, w):
                ev_rr[0] += 1
                if ev_rr[0] % 2:
                    P.op("act", lambda e: e.copy(out=out, in_=in_), r=r, w=w)
                else:
                    P.op("dve", lambda e: e.tensor_copy(out=out, in_=in_), r=r, w=w)

            def mmg(out, pairs, r, w):
                n = len(pairs)

                def g(e):
                    ins = None
                    for i, (a, b) in enumerate(pairs):
                        ins = e.matmul(out, lhsT=a, rhs=b, start=(i == 0), stop=(i == n - 1))
                    return ins
                P.op("pe", g, r=r, w=w)

            ident = sb("ident", [128, 128])
            ident_b = sb("ident_b", [128, 128], BF16)
            U_f = sb("U_f", [128, 128])
            U_b = sb("U_b", [128, 128], BF16)
            SU4 = sb("SU4", [4, 4])
            ones_f = sb("ones_f", [128, 128])
            ones_b = sb("ones_b", [128, 1], BF16)
            sel127 = sb("sel127", [128, 128])
            hmask = sb("hmask", [32, 8])
            io_i = sb("io_i", [128, 1], I32)
            io_f = sb("io_f", [128, 1])
            bfb = sb("bfb", [128, 8])
            rc = sb("rc", [128, 80])
            LF = sb("LF", [128, 33, 8])
            gate = sb("gate", [128, 17, 16])
            pool_ops = [
                (lambda e: e.memset(ident[:], 1.0), [], ["ident"]),
                (lambda e: e.affine_select(out=ident[:], in_=ident[:], pattern=[[-1, 128]], compare_op=ALU.is_equal,
                                           fill=0.0, base=0, channel_multiplier=1), ["ident"], ["ident"]),
                (lambda e: e.tensor_copy(out=ident_b[:], in_=ident[:]), ["ident"], ["ident_b"]),
                (lambda e: e.memset(U_f[:], 1.0), [], ["U_f"]),
                (lambda e: e.affine_select(out=U_f[:], in_=U_f[:], pattern=[[1, 128]], compare_op=ALU.is_ge,
                                           fill=0.0, base=0, channel_multiplier=-1), ["U_f"], ["U_f"]),
                (lambda e: e.tensor_copy(out=U_b[:], in_=U_f[:]), ["U_f"], ["U_b"]),
                (lambda e: e.memset(SU4[:], 1.0), [], ["SU4"]),
                (lambda e: e.affine_select(out=SU4[:], in_=SU4[:], pattern=[[-1, 4]], compare_op=ALU.is_gt,
                                           fill=0.0, base=0, channel_multiplier=1), ["SU4"], ["SU4"]),
                (lambda e: e.memset(ones_f[:], 1.0), [], ["ones_f"]),
                (lambda e: e.memset(ones_b[:], 1.0), [], ["ones_b"]),
                (lambda e: e.memset(sel127[:], 1.0), [], ["sel127"]),
                (lambda e: e.affine_select(out=sel127[:], in_=sel127[:], pattern=[[0, 128]], compare_op=ALU.is_equal,
                                           fill=0.0, base=-127, channel_multiplier=1), ["sel127"], ["sel127"]),
                (lambda e: e.memset(hmask[:], 1.0), [], ["hmask"]),
                (lambda e: e.affine_select(out=hmask[:], in_=hmask[:], pattern=[[-4, 8]], compare_op=ALU.is_ge,
                                           fill=0.0, base=0, channel_multiplier=1), ["hmask"], ["hmask"]),
                (lambda e: e.affine_select(out=hmask[:], in_=hmask[:], pattern=[[4, 8]], compare_op=ALU.is_ge,
                                           fill=0.0, base=3, channel_multiplier=-1), ["hmask"], ["hmask"]),
                (lambda e: e.iota(out=io_i[:], pattern=[[0, 1]], base=0, channel_multiplier=1), [], ["io_i"]),
                (lambda e: e.tensor_copy(out=io_f[:], in_=io_i[:]), ["io_i"], ["io_f"]),
                (lambda e: e.memset(LF[:], 0.0), [], ["LFall"]),
                (lambda e: e.memset(gate[:], 0.0), [], ["gate"]),
            ]
            for fn, r_, w_ in pool_ops:
                P.op("pool", fn, r=r_, w=w_)
            P.dma("sp", bfb[:], b_f.partition_broadcast(128), w=["bfb"], sem="c0")
            P.dma("sp", rc[:], rolec.partition_broadcast(128), w=["rc"], sem="c1")
            P.barrier()
            chk(0.1)

            s_oT = ExitStack()
            es.enter_context(s_oT)
            oT = sb("oT", [128, 4, NTOK], BF16, s_oT)
            s_A = ExitStack()
            es.enter_context(s_A)
            if os.environ.get("KFIRST"):
                kT = sb("kT", [128, 4, int(os.environ.get("KTN", "4224"))], BF16, s_A)
                qT = sb("qT", [128, 4, NTOK], BF16, s_A)
            else:
                qT = sb("qT", [128, 4, NTOK], BF16, s_A)
                kT = sb("kT", [128, 4, int(os.environ.get("KTN", "4224"))], BF16, s_A)
            Vt = sb("Vt", [128, 33, 8, 66], BF16, s_A)
            Cc = sb("Cc", [128, 32, 8], F32, s_A)
            BIAS = sb("BIAS", [128, 4, 32, 8], F32, s_A)
            P.op("pool", lambda e: e.memset(Vt[:, :, :, 64:65], 1.0), w=["Vones"])

            wst = []
            wst_rr = [0]

            def wchunks(dst, dkey, src, K, N, col0=0, dcol0=0, CH=1024):
                th = []
                if N <= CH:
                    g = max(1, CH // N)
                    for k0 in range(0, K, g):
                        kk = min(g, K - k0)

                        def f(k0=k0, kk=kk):
                            i = wst_rr[0] % len(wst)
                            wst_rr[0] += 1
                            st, sk = wst[i], "wst%d" % i
                            sv = st[:, 0:kk * N].rearrange("p (k n) -> p k n", n=N)
                            P.dma("sp", sv, src[k0 * 128:(k0 + kk) * 128, col0:col0 + N].rearrange("(k p) n -> p k n", p=128), w=[sk], sem=sk)
                            P.op("act", lambda e: e.copy(out=dst[:, k0:k0 + kk, dcol0:dcol0 + N], in_=sv), r=[sk], w=[dkey])
                        th.append(f)
                else:
                    for k in range(K):
                        for c0 in range(0, N, CH):
                            n = min(CH, N - c0)

                            def f(k=k, c0=c0, n=n):
                                i = wst_rr[0] % len(wst)
                                wst_rr[0] += 1
                                st, sk = wst[i], "wst%d" % i
                                P.dma("sp", st[:, 0:n], src[k * 128:(k + 1) * 128, col0 + c0:col0 + c0 + n], w=[sk], sem=sk)
                                P.op("act", lambda e: e.copy(out=dst[:, k, dcol0 + c0:dcol0 + c0 + n], in_=st[:, 0:n]), r=[sk], w=[dkey])
                            th.append(f)
                return th

            def wload(*a, **kw):
                for f in wchunks(*a, **kw):
                    f()

            def load_xT(src, W, xi, xt, xik, xtk):
                nb = (W + 127) // 128
                pw = min(W, 128)
                if W == 512:
                    P.dma("sp", xi[:], src.rearrange("(b p) d -> p b d", p=128), w=[xik], sem=xik)
                else:
                    P.dma("sp", xi[0:pw, 0, :], src, w=[xik], sem=xik)
                for b in range(nb):
                    for half in range(2):
                        pt, ptk = ps()

                        def tr(e, pt=pt, b=b, half=half):
                            ins = None
                            for kk in range(4):
                                k = half * 4 + kk
                                ins = e.transpose(pt[:, kk * 128:kk * 128 + pw], xi[0:pw, b, k * 128:(k + 1) * 128], ident[0:pw, 0:pw])
                            return ins
                        P.op("pe", tr, r=[xik, "ident"], w=[ptk])
                        evac(xt[:, half * 4:half * 4 + 4, b * 128:b * 128 + pw],
                             pt[:].rearrange("p (k t) -> p k t", k=4)[:, :, 0:pw], r=[ptk], w=[xtk + "_%d_%d" % (b, half)])
                return [xtk + "_%d_%d" % (b, h) for b in range(nb) for h in range(2)]

            with ExitStack() as s1:
                wq = sb("wq", [128, 8, 512], BF16, s1)
                wk = sb("wk", [128, 8, 512], BF16, s1)
                wv = sb("wv", [128, 8, 512], BF16, s1)
                wf = sb("wf", [128, 8, 8], BF16, s1)
                wst[:] = [sb("wstA%d" % i, [128, 1024], F32, s1) for i in range(2)]
                for (t, c0, n, nm) in ((wq, C_Q, 512, "wq"), (wk, C_K, 512, "wk"), (wv, C_V, 512, "wv"), (wf, C_F, 8, "wf")):
                    wload(t, nm, w_in, 8, n, col0=c0, CH=1024)
                xin = [sb("xin%d" % i, [128, 4, 1024], F32, s1) for i in range(2)]
                xT = [sb("xT%d" % i, [128, 8, 512], BF16, s1) for i in range(2)]
                stg = [sb("stg%d" % i, [128, 512], F32, s1) for i in range(4)]
                ftmp = [sb("ftmp%d" % i, [128, 8], F32, s1) for i in range(2)]
                stg_rr = [0]
                tiles = []
                for s_ in range(4):
                    tiles.append((x_own[s_ * 512:(s_ + 1) * 512, :], 512, True, s_ * 512, s_ * 512, s_ * 4, s_ * 512, k_own, v_own))
                tiles.append((x_s, 64, True, 2048, 4096, 32, 0, k_s, v_s))
                for s_ in range(4):
                    tiles.append((x_oth[s_ * 512:(s_ + 1) * 512, :], 512, False, None, 2048 + s_ * 512, 16 + s_ * 4, None, None, None))

                for ti, (src, W, own, q0, k0, b0, r0, ko, vo) in enumerate(tiles):
                    if ti == 0:
                        chk(0.2)
                    if ti == 1:
                        chk(0.3)
                    if ti == 5:
                        chk(0.4)
                    xi, xt = xin[ti % 2], xT[ti % 2]
                    xik, xtk = "xin%d" % (ti % 2), "xT%d" % (ti % 2)
                    nb = (W + 127) // 128
                    pw = min(W, 128)
                    xtall = load_xT(src, W, xi, xt, xik, xtk)
                    if ti == 0:
                        chk(0.21)

                    def proj_fm(wt, wnm, dst, d0):
                        for hp in range(4):
                            pt, ptk = ps()
                            mmg(pt[:, 0:W], [(wt[:, k, hp * 128:(hp + 1) * 128], xt[:, k, 0:W]) for k in range(8)], r=xtall + [wnm], w=[ptk])
                            evac(dst[:, hp, d0:d0 + W], pt[:, 0:W], r=[ptk], w=["%s_%d_%d" % (dst.name, hp, d0)])


                    if own and not os.environ.get("SKIPQ"):
                        proj_fm(wq, "wq", qT, q0)
                    if ti == 0:
                        chk(0.22)
                    proj_fm(wq if os.environ.get("USEWQ") else wk, "wq" if os.environ.get("USEWQ") else "wk", kT, k0)
                    if ti == 0:
                        chk(0.23)
                    for b in range(nb):
                        xtb = [xtk + "_%d_%d" % (b, h) for h in range(2)]
                        tsl = slice(b * 128, b * 128 + pw)
                        pt, ptk = ps()
                        mmg(pt[0:pw, :], [(xt[:, k, tsl], wv[:, k, :]) for k in range(8)], r=xtb + ["wv"], w=[ptk])
                        P.op("act", lambda e, pt=pt, b=b: e.copy(out=Vt[0:pw, b0 + b, :, 0:64], in_=pt[0:pw, :].rearrange("p (h d) -> p h d", h=8)),
                             r=[ptk], w=["Vt%d" % (b0 + b)])
                        if ti == 0 and b == 0:
                            chk(0.2311)
                        if own:
                            si = stg_rr[0] % 4
                            stg_rr[0] += 1
                            P.op("dve", lambda e, pt=pt, si=si: e.tensor_copy(out=stg[si][0:pw, :], in_=pt[0:pw, :]), r=[ptk, "Vt%d" % (b0 + b)], w=["stg%d" % si])
                            P.dma("sp", vo[r0 + b * 128:r0 + b * 128 + pw, :], stg[si][0:pw, :], r=["stg%d" % si], sem="stg%d" % si)
                            if ti == 0 and b == 0:
                                chk(0.2312)
                            pt, ptk = ps()
                            mmg(pt[0:pw, :], [(xt[:, k, tsl], wk[:, k, :]) for k in range(8)], r=xtb + ["wk"], w=[ptk])
                            si = stg_rr[0] % 4
                            stg_rr[0] += 1
                            evac(stg[si][0:pw, :], pt[0:pw, :], r=[ptk], w=["stg%d" % si])
                            P.dma("sp", ko[r0 + b * 128:r0 + b * 128 + pw, :], stg[si][0:pw, :], r=["stg%d" % si], sem="stg%d" % si)
                        if ti == 0 and b == 0:
                            chk(0.232)
                        pt, ptk = ps()
                        mmg(pt[0:pw, 0:8], [(xt[:, k, tsl], wf[:, k, :]) for k in range(8)], r=xtb + ["wf"], w=[ptk])
                        ft = ftmp[b % 2]
                        fk = "ftmp%d" % (b % 2)
                        P.op("dve", lambda e, pt=pt, ft=ft: e.tensor_tensor(out=ft[0:pw, :], in0=pt[0:pw, 0:8], in1=bfb[0:pw, :], op=ALU.add),
                             r=[ptk, "bfb"], w=[fk])
                        if ti == 0 and b == 0:
                            chk(0.233)
                        P.op("act", lambda e, ft=ft: e.activation(out=ft[0:pw, :], in_=ft[0:pw, :], func=AF.Exp, scale=-1.0), r=[fk], w=[fk])
                        if ti == 0 and b == 0:
                            chk(0.234)
                        P.op("act", lambda e, ft=ft: e.activation(out=ft[0:pw, :], in_=ft[0:pw, :], func=AF.Ln, bias=1.0, scale=1.0), r=[fk], w=[fk])
                        P.op("dve", lambda e, ft=ft, bb=b0 + b: e.tensor_scalar(out=LF[0:pw, bb, :], in0=ft[0:pw, :], scalar1=-1.0, scalar2=None, op0=ALU.mult),
                             r=[fk, "LFall"], w=["LF%d" % (b0 + b)])
                lfall = ["LF%d" % i for i in range(33)]
                P.dma("sp", f_own.rearrange("(b p) h -> p b h", p=128), LF[:, 0:16, :], r=lfall, sem="fo")
                P.dma("sp", f_s, LF[0:64, 32, :], r=lfall, sem="fs")
                P.barrier()
                chk(0.5)

            with ExitStack() as s2:
                T1 = sb("T1", [128, 8, 8], F32, s2)
                Lprev = sb("Lprev", [128, 8, 8], F32, s2)
                Ltmp = sb("Ltmp", [128, 8, 8], F32, s2)
                Lfull = sb("Lfull", [128, 32, 8], F32, s2)
                Cb = sb("Cb", [128, 4, 8], F32, s2)
                LF4 = LF[:, 0:32, :].rearrange("p (t j) h -> p t j h", j=4)
                Lf4 = Lfull[:].rearrange("p (t j) h -> p t j h", j=4)
                P.op("dve", lambda e: e.tensor_tensor(out=T1[:], in0=LF4[:, :, 0, :], in1=LF4[:, :, 1, :], op=ALU.add), r=[], w=["T1"])
                P.op("dve", lambda e: e.tensor_tensor(out=T1[:], in0=T1[:], in1=LF4[:, :, 2, :], op=ALU.add), r=["T1"], w=["T1"])
                P.op("dve", lambda e: e.tensor_tensor(out=T1[:], in0=T1[:], in1=LF4[:, :, 3, :], op=ALU.add), r=["T1"], w=["T1"])
                rcE = rc[:, 0:64].rearrange("q (a b) -> q a b", b=8)
                for pp in range(8):
                    in0 = T1[:, pp, :].unsqueeze(1).to_broadcast([128, 8, 8])
                    in1 = rcE[:, pp, :].unsqueeze(2).to_broadcast([128, 8, 8])
                    if pp == 0:
                        P.op("dve", lambda e, in0=in0, in1=in1: e.tensor_tensor(out=Lprev[:], in0=in0, in1=in1, op=ALU.mult), r=["T1"], w=["Lprev"])
                    else:
                        P.op("dve", lambda e, in0=in0, in1=in1: e.tensor_tensor(out=Ltmp[:], in0=in0, in1=in1, op=ALU.mult), r=["T1"], w=["Ltmp"])
                        P.op("dve", lambda e: e.tensor_tensor(out=Lprev[:], in0=Lprev[:], in1=Ltmp[:], op=ALU.add), r=["Ltmp", "Lprev"], w=["Lprev"])
                P.op("dve", lambda e: e.tensor_copy(out=Lf4[:, :, 0, :], in_=Lprev[:]), r=["Lprev"], w=["Lfull"])
                for j in range(1, 4):
                    P.op("dve", lambda e, j=j: e.tensor_tensor(out=Lf4[:, :, j, :], in0=Lf4[:, :, j - 1, :], in1=LF4[:, :, j - 1, :], op=ALU.add),
                         r=["Lfull"], w=["Lfull"])
                pc, pck = ps()
                mmg(pc[:, 0:256], [(U_f[:], LF[:, 0:32, :].rearrange("p b h -> p (b h)")), (ones_f[:], Lfull[:].rearrange("p b h -> p (b h)"))],
                    r=["Lfull"], w=[pck])
                P.op("dve", lambda e: e.tensor_copy(out=Cc[:].rearrange("p b h -> p (b h)"), in_=pc[:, 0:256]), r=[pck], w=["Cc"])
                Cc4 = Cc[:].rearrange("p (t j) h -> p t j h", j=4)
                pcb, pcbk = ps()
                mmg(pcb[:, 0:32].rearrange("p (a b) -> p a b", b=8), [(sel127[:], Cc4[:, 0:4, 1, :])], r=["Cc"], w=[pcbk])
                P.op("dve", lambda e: e.tensor_copy(out=Cb[:].rearrange("p a b -> p (a b)"), in_=pcb[:, 0:32]), r=[pcbk], w=["Cb"])
                for sg in range(4):
                    P.op("dve", lambda e, sg=sg: e.scalar_tensor_tensor(out=BIAS[:, sg], in0=Cc[:], scalar=-1.0,
                                                                         in1=Cb[:, sg, :].unsqueeze(1).to_broadcast([128, 32, 8]),
                                                                         op0=ALU.mult, op1=ALU.add), r=["Cc", "Cb"], w=["BIAS"])
                    P.op("dve", lambda e, sg=sg: e.tensor_scalar(out=BIAS[:, sg, 16 + 4 * sg:20 + 4 * sg, :], in0=BIAS[:, sg, 16 + 4 * sg:20 + 4 * sg, :],
                                                                  scalar1=rc[:, 64 + sg:65 + sg], scalar2=None, op0=ALU.add), r=["BIAS"], w=["BIAS"])
                dump("Cc", Cc[:], [128, 32, 8], r=["Cc"])
                P.barrier()
                chk(1.0)

            if stage >= 2:
                with ExitStack() as s2:
                    ptb = sb("ptb", [128, 256], I32, s2)
                    ptf = sb("ptf", [128, 256], F32, s2)
                    idx = sb("idx", [128, 256], I32, s2)
                    lfp = sb("lfp", [128, 16, 16, 8], F32, s2)
                    Linc = sb("Linc", [128, 16, 16, 8], F32, s2)
                    Cbs = sb("Cbs", [128, 16, 8], F32, s2)
                    lfn4 = sb("lfn4", [4, 16, 8], F32, s2)
                    bnew = sb("bnew", [4, 16, 8], F32, s2)
                    Vn = sb("Vn", [4, 16, 512], BF16, s2)
                    Qbd = sb("Qbd", [128, 4, 16, 8], BF16, s2)
                    Kp = [sb("Kp%d" % i, [128, 16, 512], BF16, s2) for i in range(1)]
                    Vp = [sb("Vp%d" % i, [128, 16, 512], BF16, s2) for i in range(2)]
                    KTp = [sb("KTp%d" % i, [128, 4, 128], BF16, s2) for i in range(3)]
                    stmp = sb("stmp", [128, 512], F32, s2)
                    Pp = sb("Pp", [128, 16, 32], BF16, s2)
                    tn = sb("tn", [4, 32], F32, s2)
                    Pn = sb("Pn", [4, 32], BF16, s2)
                    t3 = sb("t3", [32, 512], F32, s2)
                    o32 = sb("o32", [32, 64], F32, s2)
                    rd = sb("rd", [32, 1], F32, s2)
                    ps_n[0] = 5
                    P.dma("sp", ptb[:], page_table.partition_broadcast(128), w=["ptb"], sem="ptb")
                    P.op("dve", lambda e: e.tensor_copy(out=ptf[:], in_=ptb[:]), r=["ptb"], w=["ptf"])
                    P.op("dve", lambda e: e.tensor_scalar(out=ptf[:], in0=ptf[:], scalar1=128.0, scalar2=io_f[:, 0:1], op0=ALU.mult, op1=ALU.add),
                         r=["ptf", "io_f"], w=["ptf"])
                    P.op("dve", lambda e: e.tensor_copy(out=idx[:], in_=ptf[:]), r=["ptf"], w=["idx"])
                    P.dma("sp", lfn4[:], f_s.rearrange("(b j) h -> j b h", j=4), w=["lfn4"], sem="lfn4")
                    P.dma("pool", Vn[:], v_s.rearrange("(b j) f -> j b f", j=4), w=["Vn"], sem="Vn")
                    for b in range(16):
                        for pg in range(16):
                            P.idma(lfp[:, b, pg, :], cache_f, idx[:, b * 16 + pg:b * 16 + pg + 1], r=["idx"], w=["lfp_%d_%d" % (b, pg)], sem="lfp")
                    lfpall = ["lfp_%d_%d" % (b, pg) for b in range(16) for pg in range(16)]
                    P.op("dve", lambda e: e.tensor_copy(out=Linc[:, :, 0, :], in_=lfp[:, :, 0, :]), r=lfpall, w=["Linc", "lfp"])
                    for pg in range(1, 16):
                        P.op("dve", lambda e, pg=pg: e.tensor_tensor(out=Linc[:, :, pg, :], in0=Linc[:, :, pg - 1, :], in1=lfp[:, :, pg, :], op=ALU.add),
                             r=["Linc", "lfp"], w=["Linc"])
                    pcs, pcsk = ps()
                    P.op("pe", lambda e: (e.matmul(pcs[:, 0:128].rearrange("p (b h) -> p b h", h=8), lhsT=ones_f[:], rhs=Linc[:, :, 15, :], start=True, stop=False),
                                          e.matmul(pcs[:, 0:128].rearrange("p (b h) -> p b h", h=8), lhsT=ones_f[0:4, :], rhs=lfn4[:], start=False, stop=True))[1],
                         r=["Linc", "lfn4"], w=[pcsk])
                    P.op("dve", lambda e: e.tensor_copy(out=Cbs[:].rearrange("p b h -> p (b h)"), in_=pcs[:, 0:128]), r=[pcsk], w=["Cbs"])
                    P.op("dve", lambda e: e.tensor_tensor(out=Linc[:], in0=Linc[:], in1=lfp[:], op=ALU.subtract), r=["Linc", "lfp", "Cbs"], w=["Linc"])
                    cpast = lfp
                    lfp2 = lfp[:].rearrange("p b g h -> p (b g h)")
                    Lin2 = Linc[:].rearrange("p b g h -> p (b g h)")
                    cp2 = cpast[:].rearrange("p b g h -> p (b g h)")
                    for q4 in range(4):
                        pq, pqk = ps()
                        cs = slice(q4 * 512, (q4 + 1) * 512)
                        mmg(pq[:, :], [(U_f[:], lfp2[:, cs]), (ones_f[:], Lin2[:, cs])], r=["Linc", "lfp"], w=[pqk])
                        for bb in range(4):
                            b_ = 4 * q4 + bb
                            P.op("dve", lambda e, pq=pq, bb=bb, b_=b_: e.scalar_tensor_tensor(
                                out=cpast[:, b_], in0=pq[:, bb * 128:(bb + 1) * 128].rearrange("p (g h) -> p g h", g=16), scalar=-1.0,
                                in1=Cbs[:, b_, :].unsqueeze(1).to_broadcast([128, 16, 8]), op0=ALU.mult, op1=ALU.add),
                                r=[pqk, "Cbs"], w=["cpast", "lfp"])
                    pbn, pbnk = ps()
                    mmg(pbn[0:4, 0:128].rearrange("p (b h) -> p b h", h=8), [(SU4[:], lfn4[:])], r=["lfn4", "SU4"], w=[pbnk])
                    P.op("dve", lambda e: e.tensor_copy(out=bnew[:].rearrange("p b h -> p (b h)"), in_=pbn[0:4, 0:128]), r=[pbnk], w=["bnew"])
                    P.op("pool", lambda e: e.memset(Qbd[:], 0.0), w=["Qbd"])
                    qs = qT[:, :, 2048:2112].rearrange("p c (b j) -> p c b j", j=4)
                    P.op("pool", lambda e: e.tensor_copy(out=Qbd[0:64, :, :, 0:4], in_=qs[0:64]), r=["Qbd"], w=["Qbd"])
                    P.op("pool", lambda e: e.tensor_copy(out=Qbd[64:128, :, :, 4:8], in_=qs[64:128]), r=["Qbd"], w=["Qbd"])
                    kt_rr = 0
                    for b in range(16):
                        kp, vp = Kp[0], Vp[b % 2]
                        kpk, vpk = "Kp0", "Vp%d" % (b % 2)
                        for pg in range(16):
                            P.idma(kp[:, pg, :], cache_k, idx[:, b * 16 + pg:b * 16 + pg + 1], r=["idx"], w=[kpk + "_%d" % pg], sem=kpk)
                        for pg in range(16):
                            P.idma(vp[:, pg, :], cache_v, idx[:, b * 16 + pg:b * 16 + pg + 1], r=["idx"], w=[vpk + "_%d" % pg], sem=vpk)
                        ss, ssk = psf[5], "ps5"
                        sn, snk = psf[6], "ps6"
                        acc, acck = psf[7], "ps7"
                        for pg in range(16):
                            ptb_, ptbk = ps()
                            ptv = ptb_[:].bitcast(BF16)

                            def trk(e, ptv=ptv, kp=kp, pg=pg):
                                ins = None
                                for hp in range(4):
                                    ins = e.transpose(ptv[:, hp * 128:(hp + 1) * 128], kp[:, pg, hp * 128:(hp + 1) * 128], ident_b[:])
                                return ins
                            P.op("pe", trk, r=[kpk + "_%d" % g_ for g_ in range(16)] + ["ident_b"], w=[ptbk])
                            ktp = KTp[kt_rr % 3]
                            ktk = "KTp%d" % (kt_rr % 3)
                            kt_rr += 1
                            evac(ktp[:].rearrange("p c k -> p (c k)"), ptv[:, 0:512], r=[ptbk], w=[ktk])

                            def qk(e, ktp=ktp, pg=pg, b=b):
                                ins = None
                                for hp in range(4):
                                    ins = e.matmul(ss[:, pg * 32 + hp * 8:pg * 32 + hp * 8 + 8], lhsT=ktp[:, hp, :], rhs=Qbd[:, hp, b, :], start=True, stop=True)
                                return ins
                            P.op("pe", qk, r=[ktk, "Qbd"], w=[ssk])

                        def qkn(e, b=b):
                            ins = None
                            for hp in range(4):
                                ins = e.matmul(sn[0:4, hp * 8:hp * 8 + 8], lhsT=kT[:, hp, 4096 + 4 * b:4096 + 4 * b + 4], rhs=Qbd[:, hp, b, :], start=True, stop=True)
                            return ins
                        P.op("pe", qkn, r=["Qbd"], w=[snk])
                        P.op("dve", lambda e, b=b: e.scalar_tensor_tensor(
                            out=stmp[:].rearrange("p (g h q) -> p g h q", g=16, h=8), in0=ss[:, :].rearrange("p (g h q) -> p g h q", g=16, h=8), scalar=0.125,
                            in1=cpast[:, b].unsqueeze(3).to_broadcast([128, 16, 8, 4]), op0=ALU.mult, op1=ALU.add), r=[ssk, "cpast"], w=["stmp"])
                        P.op("act", lambda e: e.activation(out=Pp[:].rearrange("p g c -> p (g c)"), in_=stmp[:], func=AF.Exp), r=["stmp"], w=["Pp"])
                        P.op("dve", lambda e, b=b: e.scalar_tensor_tensor(
                            out=tn[:].rearrange("p (h q) -> p h q", h=8), in0=sn[0:4, 0:32].rearrange("p (h q) -> p h q", h=8), scalar=0.125,
                            in1=bnew[:, b, :].unsqueeze(2).to_broadcast([4, 8, 4]), op0=ALU.mult, op1=ALU.add), r=[snk, "bnew"], w=["tn"])
                        P.op("act", lambda e: e.activation(out=tn[:], in_=tn[:], func=AF.Exp), r=["tn"], w=["tn"])
                        P.op("dve", lambda e: e.tensor_tensor(out=Pn[:].rearrange("p (h q) -> p h q", h=8), in0=tn[:].rearrange("p (h q) -> p h q", h=8),
                                                              in1=U_f[0:4, 0:4].unsqueeze(1).to_broadcast([4, 8, 4]), op=ALU.mult), r=["tn", "U_f"], w=["Pn"])

                        def pv(e, vp=vp, b=b):
                            for pg in range(16):
                                e.matmul(acc[0:32, :], lhsT=Pp[:, pg, :], rhs=vp[:, pg, :], start=(pg == 0), stop=False)
                            return e.matmul(acc[0:32, :], lhsT=Pn[:, :], rhs=Vn[:, b, :], start=False, stop=True)
                        P.op("pe", pv, r=["Pp", "Pn", "Vn"] + [vpk + "_%d" % g_ for g_ in range(16)], w=[acck])

                        def dn(e):
                            for pg in range(16):
                                e.matmul(sn[0:32, 64:65], lhsT=Pp[:, pg, :], rhs=ones_b[:, 0:1], start=(pg == 0), stop=False)
                            return e.matmul(sn[0:32, 64:65], lhsT=Pn[:, :], rhs=ones_b[0:4, 0:1], start=False, stop=True)
                        P.op("pe", dn, r=["Pp", "Pn", "tn"], w=[snk + "d"])
                        P.op("dve", lambda e: e.tensor_tensor(out=t3[:].rearrange("p (h d) -> p h d", h=8), in0=acc[0:32, :].rearrange("p (h d) -> p h d", h=8),
                                                              in1=hmask[:].unsqueeze(2).to_broadcast([32, 8, 64]), op=ALU.mult), r=[acck, "hmask"], w=["t3"])
                        P.op("dve", lambda e: e.tensor_reduce(out=o32[:], in_=t3[:].rearrange("p (h d) -> p d h", h=8), axis=AX.X, op=ALU.add), r=["t3"], w=["o32"])
                        P.op("dve", lambda e: e.reciprocal(out=rd[:], in_=sn[0:32, 64:65]), r=[snk + "d"], w=["rd"])
                        P.op("dve", lambda e: e.tensor_scalar(out=o32[:], in0=o32[:], scalar1=rd[:, 0:1], scalar2=None, op0=ALU.mult), r=["o32", "rd"], w=["o32"])
                        po, pok = ps()
                        P.op("pe", lambda e, po=po: e.transpose(po[0:64, 0:32], o32[:, :], ident[0:32, 0:32]), r=["o32", "ident"], w=[pok])
                        pov = po[0:64, 0:32].rearrange("p (c hh q) -> p c hh q", c=4, hh=2)
                        P.op("dve", lambda e, pov=pov, b=b: e.tensor_copy(out=oT[0:64, :, 2048 + 4 * b:2052 + 4 * b], in_=pov[:, :, 0, :]), r=[pok, snk, snk + "d"], w=["oTs%d" % b])
                        P.op("dve", lambda e, pov=pov, b=b: e.tensor_copy(out=oT[64:128, :, 2048 + 4 * b:2052 + 4 * b], in_=pov[:, :, 1, :]), r=[pok], w=["oTs%d" % b])
                    ps_n[0] = 8
                    P.barrier()

            if stage >= 3:
                with ExitStack() as s3:
                    Pt = [sb("Pt%d" % i, [128, 512], BF16, s3) for i in range(4)]
                    rec = sb("rec", [128, 512], F32, s3)
                    bcs = sb("bcs", [64, 512], F32, s3)
                    ps_n[0] = 5
                    pt_rr = 0
                    n_acc = 0
                    LA = 2
                    pending_norm = [None]
                    for sg in range(4):
                        for h in range(8):
                            hp, hh = h // 2, h % 2
                            pr = slice(hh * 64, hh * 64 + 64)
                            acc, acck = psf[6 + n_acc % 2], "ps%d" % (6 + n_acc % 2)
                            n_acc += 1
                            blocks = list(range(0, 4 * sg)) + list(range(16, 16 + 4 * sg + 4)) + list(range(4 * sg, 4 * sg + 4))
                            nbk = len(blocks)
                            info = {}
                            for i in range(nbk + LA):
                                if i < nbk:
                                    kb = blocks[i]
                                    diag = 4 * sg <= kb < 4 * sg + 4
                                    q0 = 128 * (kb - 4 * sg) if diag else 0
                                    st, stk = ps()
                                    P.op("pe", lambda e, st=st, kb=kb, q0=q0: e.matmul(st[:, q0:512], lhsT=kT[pr, hp, kb * 128:(kb + 1) * 128],
                                                                                         rhs=qT[pr, hp, sg * 512 + q0:(sg + 1) * 512], start=True, stop=True),
                                         r=[], w=[stk])
                                    pti = Pt[pt_rr % 4]
                                    ptk = "Pt%d" % (pt_rr % 4)
                                    pt_rr += 1
                                    P.op("act", lambda e, st=st, pti=pti, kb=kb, q0=q0: e.activation(out=pti[:, q0:512], in_=st[:, q0:512], func=AF.Exp,
                                                                                                      bias=BIAS[:, sg, kb, h:h + 1], scale=0.125),
                                         r=[stk], w=[ptk])
                                    if diag:
                                        P.op("pool", lambda e, pti=pti, q0=q0: e.tensor_tensor(out=pti[:, q0:q0 + 128], in0=pti[:, q0:q0 + 128], in1=U_b[:], op=ALU.mult),
                                             r=[ptk], w=[ptk])
                                    info[i] = (kb, q0, pti, ptk)
                                if i == LA and pending_norm[0] is not None:
                                    pending_norm[0]()
                                    pending_norm[0] = None
                                j = i - LA
                                if j >= 0:
                                    kb, q0, pti, ptk = info[j]
                                    P.op("pe", lambda e, pti=pti, kb=kb, q0=q0, j=j: e.matmul(acc[0:65, q0:512], lhsT=Vt[:, kb, h, 0:65], rhs=pti[:, q0:512],
                                                                                             start=(j == 0), stop=(j == nbk - 1)),
                                         r=[ptk], w=[acck])

                            def norm(acc=acc, acck=acck, pr=pr, hp=hp, sg=sg, h=h):
                                P.op("dve", lambda e: e.reciprocal(out=rec[64:65, :], in_=acc[64:65, :]), r=[acck], w=["rec"])
                                P.op("pe", lambda e: e.matmul(psf[5][0:64, :], lhsT=ones_f[64:65, 0:64], rhs=rec[64:65, :], start=True, stop=True), r=["rec"], w=["ps5"])
                                P.op("act", lambda e: e.copy(out=bcs[:], in_=psf[5][0:64, :]), r=["ps5"], w=["bcs"])
                                P.op("dve", lambda e: e.tensor_tensor(out=oT[pr, hp, sg * 512:(sg + 1) * 512], in0=acc[0:64, :], in1=bcs[:], op=ALU.mult),
                                     r=[acck, "bcs"], w=["oT_%d_%d" % (sg, h)])
                            pending_norm[0] = norm
                    pending_norm[0]()
                    ps_n[0] = 8
                    dump("oT", oT[:], [128, 4, NTOK], BF16)
                    P.barrier()
            s_A.close()

            if stage >= 4:
                with ExitStack() as s4:
                    wcv = sb("wcv", [128, 8, 1536], BF16, s4)
                    wg = sb("wg", [128, 8, 2048], BF16, s4)
                    wco = sb("wco", [128, 4, 1024], BF16, s4)
                    wao = sb("wao", [128, 4, 1024], BF16, s4)
                    wo = sb("wo", [128, 8, 1024], BF16, s4)
                    wgr = sb("wgr", [128, 8, 20], F32, s4)
                    cw = sb("cw", [128, 4, 3], F32, s4)
                    g1b = sb("g1b", [128, 1024], F32, s4)
                    b1b = sb("b1b", [128, 1024], F32, s4)
                    wst[:] = [sb("wstB%d" % i, [128, 1024], F32, s4) for i in range(2)]
                    wload(wcv, "wcv", w_in, 8, 1536, col0=0, CH=1024)
                    wload(wg, "wg", w_in, 8, 1024, col0=C_GA, dcol0=0, CH=1024)
                    wload(wg, "wg", w_in, 8, 1024, col0=C_GB, dcol0=1024, CH=1024)
                    wload(wco, "wco", w_conv_out, 4, 1024, CH=1024)
                    wload(wao, "wao", w_attn_out, 4, 1024, CH=1024)
                    wload(wo, "wo", w_o, 8, 1024, CH=1024)
                    P.dma("sp", wgr[:], w_gr.rearrange("(k p) n -> p k n", p=128), w=["wgr"], sem="wgr")
                    with nc.allow_non_contiguous_dma(reason="tiny conv weight transpose"):
                        for c in range(4):
                            P.dma("sp", cw[:, c, :], conv_w[:, c * 128:(c + 1) * 128].rearrange("j p -> p j"), w=["cw%d" % c], sem="cw")
                    P.dma("sp", g1b[:], ln1_g.partition_broadcast(128), w=["g1b"], sem="g1b")
                    P.dma("sp", b1b[:], ln1_b.partition_broadcast(128), w=["b1b"], sem="b1b")

                    xin = sb("xin", [128, 4, 1024], F32, s4)
                    xT = sb("xT", [128, 8, 512], BF16, s4)
                    xh = sb("xh", [8, 1024], F32, s4)
                    xhT = sb("xhT", [128, 8, 8], BF16, s4)
                    uh = sb("uh", [128, 4, 8], F32, s4)
                    sct = sb("sct", [32, 512], F32, s4)
                    stT = sb("stT", [128, 4, 32], F32, s4)
                    xcs = sb("xcs", [128, 512], F32, s4)
                    ue = [sb("ue%d" % i, [128, 514], F32, s4) for i in range(2)]
                    tcv = sb("tcv", [128, 512], F32, s4)
                    gcv = sb("gcv", [128, 4, 512], BF16, s4)
                    ucp = sb("ucp", [128, 4, 2], F32, s4)
                    ucs = sb("ucs", [128, 4, 32], F32, s4)
                    cps = sb("cps", [2, 512], F32, s4)
                    css = sb("css", [32, 512], F32, s4)
                    sga = sb("sga", [128, 512], F32, s4)
                    sgb = sb("sgb", [128, 512], F32, s4)
                    za = sga
                    zb = sgb
                    zT = sb("zT", [128, 8, 512], BF16, s4)
                    xr = [sb("xr%d" % i, [128, 1024], F32, s4) for i in range(2)]
                    st6 = sb("st6", [128, 2, 6], F32, s4)
                    mv = sb("mv", [128, 2], F32, s4)
                    rstd = sb("rstd", [128, 1], F32, s4)
                    y0t = [sb("y0t%d" % i, [128, 1024], F32, s4) for i in range(1)]
                    x1Tb = [sb("x1Tb%d" % i, [128, 8, 128], BF16, s4) for i in range(2)]
                    x1Tf = sb("x1Tf", [128, 8, 128], F32, s4)
                    lg = sb("lg", [128, 20], F32, s4)
                    g8 = sb("g8", [128, 8], F32, s4)
                    m8 = sb("m8", [128, 8], F32, s4)
                    e8 = sb("e8", [128, 8], F32, s4)
                    me8 = sb("me8", [128, 8], F32, s4)
                    ohg = sb("ohg", [128, 4], F32, s4)
                    nm = sb("nm", [128, 2], F32, s4)
                    eg = sb("eg", [128, 4], F32, s4)
                    sgs = sb("sgs", [128, 2], F32, s4)
                    sel = sb("sel", [128, 16], F32, s4)
                    e1 = sb("e1", [128, 4], F32, s4)
                    s2m = sb("s2m", [128, 4], F32, s4)
                    wfac = sb("wfac", [128, 1], F32, s4)
                    P.op("pool", lambda e: e.memset(g8[:], -1e30), w=["g8"])
                    P.op("pool", lambda e: e.memset(e8[:], -1e30), w=["e8"])

                    P.dma("sp", xh[:], x_halo, w=["xh"], sem="xh")
                    ph, phk = ps()
                    P.op("pe", lambda e: [e.transpose(ph[:, k * 8:(k + 1) * 8], xh[0:8, k * 128:(k + 1) * 128], ident[0:8, 0:8]) for k in range(8)][-1],
                         r=["xh", "ident"], w=[phk])
                    evac(xhT[:].rearrange("p k t -> p (k t)"), ph[:, 0:64], r=[phk], w=["xhT"])
                    for c in range(4):
                        p1, p1k = ps()
                        mmg(p1[:, 0:8], [(wcv[:, k, C_XC + c * 128:C_XC + (c + 1) * 128], xhT[:, k, :]) for k in range(8)], r=["xhT", "wcv"], w=[p1k])
                        p2, p2k = ps()
                        mmg(p2[:, 0:8], [(wcv[:, k, C_CG + c * 128:C_CG + (c + 1) * 128], xhT[:, k, :]) for k in range(8)], r=["xhT", "wcv"], w=[p2k])
                        P.op("act", lambda e, p1=p1: e.copy(out=xcs[:, 0:8], in_=p1[:, 0:8]), r=[p1k], w=["xcs"])
                        P.op("dve", lambda e, p2=p2, c=c: e.tensor_tensor(out=uh[:, c, :], in0=xcs[:, 0:8], in1=p2[:, 0:8], op=ALU.mult), r=[p2k, "xcs"], w=["uh"])
                    P.dma("sp", sct[:], state_conv, w=["sct"], sem="sct")
                    pst, pstk = ps()
                    P.op("pe", lambda e: [e.transpose(pst[:, c * 32:(c + 1) * 32], sct[0:32, c * 128:(c + 1) * 128], ident[0:32, 0:32]) for c in range(4)][-1],
                         r=["sct", "ident"], w=[pstk])
                    evac(stT[:].rearrange("p c t -> p (c t)"), pst[:, 0:128], r=[pstk], w=["stT"])

                    tiles = [(x_own[s_ * 512:(s_ + 1) * 512, :], 512, s_ * 512, s_ * 4, s_) for s_ in range(4)] + [(x_s, 64, 2048, 16, 4)]
                    blk_n = 0
                    for (src, W, t0, gb0, sl) in tiles:
                        nb = (W + 127) // 128
                        pw = min(W, 128)
                        samp = (sl == 4)
                        xtall = load_xT(src, W, xin, xT, "xin", "xT")
                        for c in range(4):
                            u_ = ue[c % 2]
                            uk = "ue%d" % (c % 2)
                            p1, p1k = ps()
                            mmg(p1[:, 0:W], [(wcv[:, k, C_XC + c * 128:C_XC + (c + 1) * 128], xT[:, k, 0:W]) for k in range(8)], r=xtall + ["wcv"], w=[p1k])
                            p2, p2k = ps()
                            mmg(p2[:, 0:W], [(wcv[:, k, C_CG + c * 128:C_CG + (c + 1) * 128], xT[:, k, 0:W]) for k in range(8)], r=xtall + ["wcv"], w=[p2k])
                            p3, p3k = ps()
                            mmg(p3[:, 0:W], [(wcv[:, k, C_BG + c * 128:C_BG + (c + 1) * 128], xT[:, k, 0:W]) for k in range(8)], r=xtall + ["wcv"], w=[p3k])
                            P.op("act", lambda e, p1=p1: e.copy(out=xcs[:, 0:W], in_=p1[:, 0:W]), r=[p1k], w=["xcs"])
                            if not samp:
                                P.op("pool", lambda e, u_=u_, c=c: e.tensor_copy(out=u_[:, 0:2], in_=uh[:, c, 2 * sl:2 * sl + 2]), r=["uh"], w=[uk])
                                P.op("dve", lambda e, u_=u_, p2=p2: e.tensor_tensor(out=u_[:, 2:W + 2], in0=xcs[:, 0:W], in1=p2[:, 0:W], op=ALU.mult),
                                     r=[p2k, "xcs", uk], w=[uk])
                                uv = [u_[:, j:j + W] for j in range(3)]
                                tv = tcv[:, 0:W]
                            else:
                                u3 = u_[:, 0:96].rearrange("p (b i) -> p b i", i=6)
                                P.op("pool", lambda e, u3=u3, c=c: e.tensor_copy(out=u3[:, :, 0:2], in_=stT[:, c, :].rearrange("p (b i) -> p b i", i=2)),
                                     r=["stT"], w=[uk])
                                P.op("dve", lambda e, u3=u3, p2=p2: e.tensor_tensor(out=u3[:, :, 2:6], in0=xcs[:, 0:64].rearrange("p (b j) -> p b j", j=4),
                                                                                     in1=p2[:, 0:64].rearrange("p (b j) -> p b j", j=4), op=ALU.mult),
                                     r=[p2k, "xcs", uk], w=[uk])
                                uv = [u3[:, :, j:j + 4] for j in range(3)]
                                tv = tcv[:, 0:64].rearrange("p (b j) -> p b j", j=4)
                            P.op("pool", lambda e, uv=uv, tv=tv, c=c: e.tensor_scalar(out=tv, in0=uv[0], scalar1=cw[:, c, 0:1], scalar2=None, op0=ALU.mult),
                                 r=[uk] + ["cw%d" % i for i in range(4)], w=["tcv"])
                            for j in (1, 2):
                                P.op("dve", lambda e, uv=uv, tv=tv, c=c, j=j: e.scalar_tensor_tensor(out=tv, in0=uv[j], scalar=cw[:, c, j:j + 1], in1=tv,
                                                                                                     op0=ALU.mult, op1=ALU.add), r=[uk, "tcv"], w=["tcv"])
                            P.op("dve", lambda e, p3=p3, c=c: e.tensor_tensor(out=gcv[:, c, 0:W], in0=tcv[:, 0:W], in1=p3[:, 0:W], op=ALU.mult),
                                 r=[p3k, "tcv"], w=["gcv%d" % c])
                            if sl == 3:
                                P.op("pool", lambda e, u_=u_, c=c: e.tensor_copy(out=ucp[:, c, :], in_=u_[:, W:W + 2]), r=[uk], w=["ucp"])
                            if samp:
                                P.op("pool", lambda e, u3=u3, c=c: e.tensor_copy(out=ucs[:, c, :].rearrange("p (b i) -> p b i", i=2), in_=u3[:, :, 4:6]),
                                     r=[uk], w=["ucs"])
                        if sl == 3:
                            pcp, pcpk = ps()
                            P.op("pe", lambda e, pcp=pcp: [e.transpose(pcp[0:2, c * 128:(c + 1) * 128], ucp[:, c, :], ident[:]) for c in range(4)][-1],
                                 r=["ucp", "ident"], w=[pcpk])
                            evac(cps[:], pcp[0:2, :], r=[pcpk], w=["cps"])
                            P.dma("sp", conv_p, cps[:], r=["cps"], sem="cps")
                        if samp:
                            pcp, pcpk = ps()
                            P.op("pe", lambda e, pcp=pcp: [e.transpose(pcp[0:32, c * 128:(c + 1) * 128], ucs[:, c, :], ident[:]) for c in range(4)][-1],
                                 r=["ucs", "ident"], w=[pcpk])
                            evac(css[:], pcp[0:32, :], r=[pcpk], w=["css"])
                            P.dma("sp", conv_s, css[:], r=["css"], sem="css")
                        gall = ["gcv%d" % c for c in range(4)]
                        for m in range(8):
                            ms = slice(m * 128, (m + 1) * 128)
                            pya, pyak = ps()
                            mmg(pya[:, 0:W], [(wco[:, c, ms], gcv[:, c, 0:W]) for c in range(4)], r=gall + ["wco"], w=[pyak])
                            pga, pgak = ps()
                            mmg(pga[:, 0:W], [(wg[:, k, ms], xT[:, k, 0:W]) for k in range(8)], r=xtall + ["wg"], w=[pgak])
                            pyb, pybk = ps()
                            mmg(pyb[:, 0:W], [(wao[:, c, ms], oT[:, c, t0:t0 + W]) for c in range(4)], r=["wao"], w=[pybk])
                            pgb, pgbk = ps()
                            mmg(pgb[:, 0:W], [(wg[:, k, 1024 + m * 128:1024 + (m + 1) * 128], xT[:, k, 0:W]) for k in range(8)], r=xtall + ["wg"], w=[pgbk])
                            P.op("act", lambda e, pga=pga: e.activation(out=sga[:, 0:W], in_=pga[:, 0:W], func=AF.Sigmoid), r=[pgak], w=["sga"])
                            P.op("act", lambda e, pgb=pgb: e.activation(out=sgb[:, 0:W], in_=pgb[:, 0:W], func=AF.Sigmoid), r=[pgbk], w=["sgb"])
                            P.op("dve", lambda e, pya=pya: e.tensor_tensor(out=za[:, 0:W], in0=sga[:, 0:W], in1=pya[:, 0:W], op=ALU.mult), r=[pyak, "sga"], w=["sga"])
                            P.op("dve", lambda e, pyb=pyb: e.tensor_tensor(out=zb[:, 0:W], in0=sgb[:, 0:W], in1=pyb[:, 0:W], op=ALU.mult), r=[pybk, "sgb"], w=["sgb"])
                            P.op("pool", lambda e, m=m: e.tensor_tensor(out=zT[:, m, 0:W], in0=za[:, 0:W], in1=zb[:, 0:W], op=ALU.add), r=["sga", "sgb"], w=["zT%d" % m])
                        zall = ["zT%d" % m for m in range(8)]
                        for b in range(nb):
                            gb = gb0 + b
                            xr_ = xr[blk_n % 2]
                            xrk = "xr%d" % (blk_n % 2)
                            y0_ = y0t[0]
                            y0k = "y0t0"
                            xb_ = x1Tb[blk_n % 2]
                            xbk = "x1Tb%d" % (blk_n % 2)
                            blk_n += 1
                            tsl = slice(b * 128, b * 128 + pw)
                            for half in range(2):
                                hs = slice(half * 512, (half + 1) * 512)
                                pm, pmk = ps()
                                mmg(pm[0:pw, :], [(zT[:, m, tsl], wo[:, m, hs]) for m in range(8)], r=zall + ["wo"], w=[pmk])
                                P.op("dve", lambda e, pm=pm, hs=hs, xr_=xr_, b=b: e.scalar_tensor_tensor(out=xr_[0:pw, hs], in0=xin[0:pw, b, hs], scalar=ALPHA, in1=pm[0:pw, :],
                                                                                                        op0=ALU.mult, op1=ALU.add), r=[pmk, "xin"], w=[xrk])
                            xrh = [xrk]
                            for half in range(2):
                                P.op("dve", lambda e, half=half, xr_=xr_: e.bn_stats(out=st6[0:pw, half, :], in_=xr_[0:pw, half * 512:(half + 1) * 512]), r=xrh, w=["st6_%d" % half])
                            P.op("dve", lambda e: e.bn_aggr(out=mv[0:pw, :], in_=st6[0:pw].rearrange("p a s -> p (a s)")), r=["st6_0", "st6_1"], w=["mv"])
                            P.op("dve", lambda e: e.tensor_scalar(out=rstd[0:pw, :], in0=mv[0:pw, 1:2], scalar1=EPS, scalar2=None, op0=ALU.add), r=["mv"], w=["rstd"])
                            P.op("act", lambda e: e.sqrt(out=rstd[0:pw, :], in_=rstd[0:pw, :]), r=["rstd"], w=["rstd"])
                            P.op("dve", lambda e: e.reciprocal(out=rstd[0:pw, :], in_=rstd[0:pw, :]), r=["rstd"], w=["rstd"])
                            P.op("dve", lambda e, xr_=xr_: e.tensor_scalar(out=xr_[0:pw, :], in0=xr_[0:pw, :], scalar1=mv[0:pw, 0:1], scalar2=rstd[0:pw, 0:1],
                                                                           op0=ALU.subtract, op1=ALU.mult), r=xrh + ["mv", "rstd"], w=[xrk])
                            P.op("pool", lambda e, xr_=xr_: e.tensor_tensor(out=xr_[0:pw, :], in0=xr_[0:pw, :], in1=g1b[0:pw, :], op=ALU.mult), r=[xrk, "g1b"], w=[xrk])
                            P.op("pool", lambda e, xr_=xr_: e.tensor_tensor(out=xr_[0:pw, :], in0=xr_[0:pw, :], in1=b1b[0:pw, :], op=ALU.add), r=[xrk, "b1b"], w=[xrk])
                            P.op("act", lambda e, xr_=xr_, y0_=y0_: e.mul(out=y0_[0:pw, :], in_=xr_[0:pw, :], mul=ALPHA), r=[xrk], w=[y0k])
                            P.dma("sp", y0_d[t0 + b * 128:t0 + b * 128 + pw, :], y0_[0:pw, :], r=[y0k], sem=y0k)
                            for half in range(2):
                                ptt, pttk = ps()
                                P.op("pe", lambda e, ptt=ptt, half=half, xr_=xr_: [e.transpose(ptt[:, kk * 128:kk * 128 + pw], xr_[0:pw, (half * 4 + kk) * 128:(half * 4 + kk + 1) * 128],
                                                                                               ident[0:pw, 0:pw]) for kk in range(4)][-1], r=[xrk, "ident"], w=[pttk])
                                pv4 = ptt[:].rearrange("p (k t) -> p k t", k=4)[:, :, 0:pw]
                                P.op("act", lambda e, pv4=pv4, half=half, xb_=xb_: e.copy(out=xb_[:, half * 4:half * 4 + 4, 0:pw], in_=pv4), r=[pttk], w=[xbk + "_%d" % half])
                                P.op("dve", lambda e, pv4=pv4, half=half: e.tensor_copy(out=x1Tf[:, half * 4:half * 4 + 4, 0:pw], in_=pv4), r=[pttk, xbk + "_%d" % half], w=["x1Tf_%d" % half])
                            P.dma("sp", x1T_d[:, :, t0 + b * 128:t0 + b * 128 + pw], xb_[:, :, 0:pw], r=[xbk + "_0", xbk + "_1"], sem=xbk)
                            prt, prtk = ps()
                            mmg(prt[0:pw, 0:20], [(x1Tf[:, k, 0:pw], wgr[:, k, :]) for k in range(8)], r=["x1Tf_0", "x1Tf_1", "wgr"], w=[prtk])
                            R = slice(0, pw)
                            P.op("dve", lambda e, prt=prt: e.tensor_copy(out=lg[R, :], in_=prt[R, 0:20]), r=[prtk], w=["lg"])
                            P.op("dve", lambda e: e.tensor_copy(out=g8[R, 0:4], in_=lg[R, 0:4]), r=["lg"], w=["g8"])
                            P.op("dve", lambda e: e.max(out=m8[R, :], in_=g8[R, :]), r=["g8"], w=["m8"])
                            P.op("dve", lambda e: e.tensor_scalar(out=ohg[R, :], in0=lg[R, 0:4], scalar1=m8[R, 0:1], scalar2=None, op0=ALU.is_equal), r=["lg", "m8"], w=["ohg"])
                            P.op("dve", lambda e: e.tensor_scalar(out=nm[R, 0:1], in0=m8[R, 0:1], scalar1=-1.0, scalar2=None, op0=ALU.mult), r=["m8"], w=["nm0"])
                            P.op("act", lambda e: e.activation(out=eg[R, :], in_=lg[R, 0:4], func=AF.Exp, bias=nm[R, 0:1], scale=1.0), r=["lg", "nm0"], w=["eg"])
                            P.op("dve", lambda e: e.reduce_sum(out=sgs[R, 0:1], in_=eg[R, :], axis=AX.X), r=["eg"], w=["sgs0"])
                            lg3 = lg[R, 4:20].rearrange("p (g x) -> p g x", x=4)
                            P.op("dve", lambda e, lg3=lg3: e.tensor_tensor(out=sel[R, :].rearrange("p (g x) -> p g x", x=4), in0=lg3,
                                                                           in1=ohg[R, :].unsqueeze(2).to_broadcast([pw, 4, 4]), op=ALU.mult), r=["lg", "ohg"], w=["sel"])
                            P.op("dve", lambda e: e.tensor_reduce(out=e8[R, 0:4], in_=sel[R, :].rearrange("p (g x) -> p x g", x=4), axis=AX.X, op=ALU.add), r=["sel"], w=["e8"])
                            P.op("dve", lambda e: e.max(out=me8[R, :], in_=e8[R, :]), r=["e8"], w=["me8"])
                            P.op("dve", lambda e: e.tensor_scalar(out=nm[R, 1:2], in0=me8[R, 0:1], scalar1=-1.0, scalar2=None, op0=ALU.mult), r=["me8"], w=["nm1"])
                            P.op("act", lambda e: e.activation(out=e1[R, :], in_=e8[R, 0:4], func=AF.Exp, bias=nm[R, 1:2], scale=1.0), r=["e8", "nm1"], w=["e1"])
                            P.op("dve", lambda e: e.tensor_scalar(out=s2m[R, :], in0=e8[R, 0:4], scalar1=me8[R, 1:2], scalar2=None, op0=ALU.is_ge), r=["e8", "me8"], w=["s2m"])
                            P.op("dve", lambda e: e.tensor_tensor(out=e1[R, :], in0=e1[R, :], in1=s2m[R, :], op=ALU.mult), r=["e1", "s2m"], w=["e1"])
                            P.op("dve", lambda e: e.reduce_sum(out=sgs[R, 1:2], in_=e1[R, :], axis=AX.X), r=["e1"], w=["sgs1"])
                            P.op("dve", lambda e: e.tensor_tensor(out=wfac[R, :], in0=sgs[R, 0:1], in1=sgs[R, 1:2], op=ALU.mult), r=["sgs0", "sgs1"], w=["wfac"])
                            P.op("dve", lambda e: e.reciprocal(out=wfac[R, :], in_=wfac[R, :]), r=["wfac"], w=["wfac"])
                            P.op("dve", lambda e: e.tensor_scalar(out=e1[R, :], in0=e1[R, :], scalar1=wfac[R, 0:1], scalar2=None, op0=ALU.mult), r=["e1", "wfac"], w=["e1"])
                            P.op("dve", lambda e, gb=gb: e.tensor_tensor(out=gate[R, gb, :].rearrange("p (g x) -> p g x", x=4),
                                                                         in0=ohg[R, :].unsqueeze(2).to_broadcast([pw, 4, 4]),
                                                                         in1=e1[R, :].unsqueeze(1).to_broadcast([pw, 4, 4]), op=ALU.mult), r=["ohg", "e1", "gate"], w=["gate%d" % gb])
                    dump("gate", gate[:], [128, 17, 16], r=["gate%d" % i for i in range(17)])
                    P.barrier()
            s_oT.close()

            if stage >= 5:
                with ExitStack() as s5:
                    x1T = sb("x1T", [128, 8, NTOK], BF16, s5)
                    yacc = sb("yacc", [128, 17, 1024], F32, s5)
                    g2b = sb("g2b", [128, 1024], F32, s5)
                    b2b = sb("b2b", [128, 1024], F32, s5)
                    w1e = [sb("w1e%d" % i, [128, 8, 512], BF16, s5) for i in range(2)]
                    w3e = [sb("w3e%d" % i, [128, 8, 512], BF16, s5) for i in range(2)]
                    w2e = [sb("w2e%d" % i, [128, 4, 1024], BF16, s5) for i in range(2)]
                    s1t = [sb("s1t%d" % i, [128, 512], F32, s5) for i in range(2)]
                    hT = [sb("hT%d" % i, [128, 4, 512], BF16, s5) for i in range(2)]
                    st6 = sb("st6b", [128, 2, 6], F32, s5)
                    mv = sb("mvb", [128, 2], F32, s5)
                    rstd = sb("rstdb", [128, 1], F32, s5)
                    P.dma("sp", x1T[:], x1T_d, w=["x1T"], sem="x1T")
                    P.dma("sp", yacc[:, 0:16, :], y0_d[0:2048, :].rearrange("(b p) d -> p b d", p=128), w=["yacc"], sem="yacc")
                    P.dma("sp", yacc[0:64, 16, :], y0_d[2048:2112, :], w=["yacc"], sem="yacc")
                    P.dma("sp", g2b[:], ln2_g.partition_broadcast(128), w=["g2b"], sem="g2b")
                    P.dma("sp", b2b[:], ln2_b.partition_broadcast(128), w=["b2b"], sem="b2b")

                    wst[:] = [sb("wstC%d" % i, [128, 2048], F32, s5) for i in range(2)]

                    def load_e(e_):
                        i = e_ % 2
                        return (wchunks(w1e[i], "w1e%d" % i, w1[e_], 8, 512, CH=2048) + wchunks(w3e[i], "w3e%d" % i, w3[e_], 8, 512, CH=2048)
                                + wchunks(w2e[i], "w2e%d" % i, w2[e_], 4, 1024, CH=2048))
                    for f_ in load_e(0):
                        f_()
                    ttiles = [(0, 512), (512, 512), (1024, 512), (1536, 512), (2048, 64)]
                    it = 0
                    for e_ in range(16):
                        pend = load_e(e_ + 1) if e_ + 1 < 16 else []
                        i = e_ % 2
                        a1, a3, a2 = w1e[i], w3e[i], w2e[i]
                        k1, k3, k2 = "w1e%d" % i, "w3e%d" % i, "w2e%d" % i
                        for (t0, W) in ttiles:
                            nb = (W + 127) // 128
                            pw = min(W, 128)
                            h_ = hT[it % 2]
                            hk = "hT%d" % (it % 2)
                            it += 1
                            for f in range(4):
                                fs = slice(f * 128, (f + 1) * 128)
                                p1, p1k = ps()
                                mmg(p1[:, 0:W], [(a1[:, k, fs], x1T[:, k, t0:t0 + W]) for k in range(8)], r=["x1T", k1], w=[p1k])
                                p3, p3k = ps()
                                mmg(p3[:, 0:W], [(a3[:, k, fs], x1T[:, k, t0:t0 + W]) for k in range(8)], r=["x1T", k3], w=[p3k])
                                s_ = s1t[f % 2]
                                sk = "s1t%d" % (f % 2)
                                P.op("act", lambda e, p1=p1, s_=s_: e.activation(out=s_[:, 0:W], in_=p1[:, 0:W], func=AF.Silu), r=[p1k], w=[sk])
                                P.op("dve", lambda e, p3=p3, s_=s_, f=f, h_=h_: e.tensor_tensor(out=h_[:, f, 0:W], in0=s_[:, 0:W], in1=p3[:, 0:W], op=ALU.mult),
                                     r=[p3k, sk], w=[hk + "_%d" % f])
                            for f_ in pend[:2 if t0 == 0 else 1]:
                                f_()
                            pend = pend[2 if t0 == 0 else 1:]
                            hall = [hk + "_%d" % f for f in range(4)]
                            for b in range(nb):
                                gb = t0 // 128 + b
                                for half in range(2):
                                    hs = slice(half * 512, (half + 1) * 512)
                                    py, pyk = ps()
                                    mmg(py[0:pw, :], [(h_[:, f, b * 128:b * 128 + pw], a2[:, f, hs]) for f in range(4)], r=hall + [k2], w=[pyk])
                                    P.op("dve", lambda e, py=py, gb=gb, hs=hs: e.scalar_tensor_tensor(out=yacc[0:pw, gb, hs], in0=py[0:pw, :], scalar=gate[0:pw, gb, e_:e_ + 1],
                                                                                                     in1=yacc[0:pw, gb, hs], op0=ALU.mult, op1=ALU.add),
                                         r=[pyk, "yacc"], w=["yacc%d_%d" % (gb, half)])
                    for gb in range(17):
                        pw = 128 if gb < 16 else 64
                        yk = ["yacc%d_0" % gb, "yacc%d_1" % gb]
                        ya = yacc[0:pw, gb, :]
                        for half in range(2):
                            P.op("dve", lambda e, half=half, gb=gb, pw=pw: e.bn_stats(out=st6[0:pw, half, :], in_=yacc[0:pw, gb, half * 512:(half + 1) * 512]), r=yk, w=["st6_%d" % half])
                        P.op("dve", lambda e, pw=pw: e.bn_aggr(out=mv[0:pw, :], in_=st6[0:pw].rearrange("p a s -> p (a s)")), r=["st6_0", "st6_1"], w=["mv"])
                        P.op("dve", lambda e, pw=pw: e.tensor_scalar(out=rstd[0:pw, :], in0=mv[0:pw, 1:2], scalar1=EPS, scalar2=None, op0=ALU.add), r=["mv"], w=["rstd"])
                        P.op("act", lambda e, pw=pw: e.sqrt(out=rstd[0:pw, :], in_=rstd[0:pw, :]), r=["rstd"], w=["rstd"])
                        P.op("dve", lambda e, pw=pw: e.reciprocal(out=rstd[0:pw, :], in_=rstd[0:pw, :]), r=["rstd"], w=["rstd"])
                        P.op("dve", lambda e, ya=ya, pw=pw: e.tensor_scalar(out=ya, in0=ya, scalar1=mv[0:pw, 0:1], scalar2=rstd[0:pw, 0:1],
                                                                           op0=ALU.subtract, op1=ALU.mult), r=yk + ["mv", "rstd"], w=["yo%d" % gb])
                        P.op("pool", lambda e, ya=ya, pw=pw: e.tensor_tensor(out=ya, in0=ya, in1=g2b[0:pw, :], op=ALU.mult), r=["yo%d" % gb, "g2b"], w=["yo%d" % gb])
                        P.op("pool", lambda e, ya=ya, pw=pw: e.tensor_tensor(out=ya, in0=ya, in1=b2b[0:pw, :], op=ALU.add), r=["yo%d" % gb, "b2b"], w=["yo%d" % gb])
                        if gb < 16:
                            P.dma("sp", y_own[gb * 128:(gb + 1) * 128, :], ya, r=["yo%d" % gb], sem="yo%d" % (gb % 4))
                        else:
                            P.dma("sp", y_s, ya, r=["yo%d" % gb], sem="yo%d" % (gb % 4))
                    P.barrier()
        except StopBuild:
            pass
        P.finish()
    nc._dbg_out = list(dbg_out.keys())
    return nc


def make_in_maps(inputs, cores):
    f32 = np.float32
    xp = np.asarray(inputs["x_prompt"], f32)
    xs = np.asarray(inputs["x_sample"], f32)
    ck = np.asarray(inputs["cache_k"], f32)
    npool = ck.shape[1]
    ck = ck.reshape(npool * 128, 512)
    cv = np.asarray(inputs["cache_v"], f32).reshape(npool * 128, 512)
    cf = np.asarray(inputs["cache_logf"], f32).reshape(npool * 128, 8)
    sc = np.asarray(inputs["state_conv"], f32)[0]
    pt = np.asarray(inputs["page_table"], np.int32)
    g = lambda k: np.asarray(inputs[k], f32)
    common = {
        "cache_k": ck, "cache_v": cv, "cache_f": cf,
        "w_in": g("w_in")[0], "b_f": g("b_f").reshape(1, 8),
        "conv_w": g("conv_w")[0], "w_conv_out": g("w_conv_out")[0],
        "w_attn_out": g("w_attn_out")[0], "w_o": g("w_o")[0],
        "ln1_g": g("ln1_g").reshape(1, D), "ln1_b": g("ln1_b").reshape(1, D),
        "w_gr": np.ascontiguousarray(np.concatenate([g("w_group")[0], g("w_router")[0]], axis=1)),
        "w1": g("w1")[0], "w3": g("w3")[0], "w2": g("w2")[0],
        "ln2_g": g("ln2_g").reshape(1, D), "ln2_b": g("ln2_b").reshape(1, D),
    }
    in_maps = []
    for c in cores:
        s, r = c // 2, c % 2
        own, oth = OWN[r], OWN[1 - r]
        xt = xp[s].reshape(8, 512, D)
        halo = np.zeros((8, D), f32)
        for sl, T in enumerate(own):
            if T > 0:
                halo[2 * sl:2 * sl + 2] = xp[s, 512 * T - 2:512 * T]
        rcv = np.zeros((1, 80), f32)
        order = own + oth
        for p_ in range(8):
            for p2 in range(8):
                rcv[0, p_ * 8 + p2] = 1.0 if order[p_] < order[p2] else 0.0
        for sl in range(4):
            rcv[0, 64 + sl] = 0.0 if oth[sl] < own[sl] else NEG
        m = dict(common)
        m.update({
            "x_own": np.ascontiguousarray(xt[own].reshape(2048, D)),
            "x_oth": np.ascontiguousarray(xt[oth].reshape(2048, D)),
            "x_halo": halo,
            "x_s": np.ascontiguousarray(xs[16 * c:16 * c + 16].reshape(64, D)),
            "state_conv": np.ascontiguousarray(sc[16 * c:16 * c + 16].reshape(32, 512)),
            "page_table": np.ascontiguousarray(pt[16 * c:16 * c + 16].reshape(1, 256)),
            "rolec": rcv,
        })
        in_maps.append(m)
    return in_maps, npool


def assemble(res, cores, nseq=4, nsamp=128):
    f32 = np.float32
    y_p = np.zeros((nseq, 4096, D), f32)
    k_p = np.zeros((1, nseq, 4096, 8, 64), f32)
    v_p = np.zeros((1, nseq, 4096, 8, 64), f32)
    f_p = np.zeros((1, nseq, 4096, 8), f32)
    c_p = np.zeros((1, nseq, 2, 512), f32)
    y_sm = np.zeros((nsamp, 4, D), f32)
    k_sm = np.zeros((1, nsamp, 4, 8, 64), f32)
    v_sm = np.zeros((1, nsamp, 4, 8, 64), f32)
    f_sm = np.zeros((1, nsamp, 4, 8), f32)
    c_sm = np.zeros((1, nsamp, 2, 512), f32)
    for o, c in zip(res, cores):
        s, r = c // 2, c % 2
        for sl, T in enumerate(OWN[r]):
            rows = slice(512 * T, 512 * T + 512)
            y_p[s, rows] = o["y_own"][sl * 512:(sl + 1) * 512]
            k_p[0, s, rows] = o["k_own"][sl * 512:(sl + 1) * 512].reshape(512, 8, 64)
            v_p[0, s, rows] = o["v_own"][sl * 512:(sl + 1) * 512].reshape(512, 8, 64)
            f_p[0, s, rows] = o["f_own"][sl * 512:(sl + 1) * 512]
        if r == 0:
            c_p[0, s] = o["conv_p"]
        sl_ = slice(16 * c, 16 * c + 16)
        y_sm[sl_] = o["y_s"].reshape(16, 4, D)
        k_sm[0, sl_] = o["k_s"].reshape(16, 4, 8, 64)
        v_sm[0, sl_] = o["v_s"].reshape(16, 4, 8, 64)
        f_sm[0, sl_] = o["f_s"].reshape(16, 4, 8)
        c_sm[0, sl_] = o["conv_s"].reshape(16, 2, 512)
    return (y_p, y_sm, k_p, v_p, f_p, c_p, k_sm, v_sm, f_sm, c_sm)


_NC = {}


def kernel(**inputs):
    cores = list(range(8))
    in_maps, npool = make_in_maps(inputs, cores)
    if npool not in _NC:
        _NC[npool] = build_nc(npool)
    res = run_bass_kernel_spmd(_NC[npool], in_maps, core_ids=cores).results
    return assemble(res, cores)
```

```python
import os
import numpy as np
from contextlib import ExitStack
import concourse.bass as bass
import concourse.mybir as mybir
from concourse.bass_utils import run_bass_kernel_spmd

F32 = mybir.dt.float32
BF16 = mybir.dt.bfloat16
I32 = mybir.dt.int32
AF = mybir.ActivationFunctionType
ALU = mybir.AluOpType
AX = mybir.AxisListType

D = 1024
NIN = 5128
OWN = [[0, 3, 4, 7], [1, 2, 5, 6]]
ALPHA = 2.0 ** 0.25
EPS = 1e-5
NEG = -30000.0
C_XC, C_BG, C_CG, C_Q, C_K, C_V, C_F, C_GA, C_GB = 0, 512, 1024, 1536, 2048, 2560, 3072, 3080, 4104
NTOK = 2112


class StopBuild(Exception):
    pass


class Prog:
    def __init__(self, nc, es):
        self.nc = nc
        self.es = es
        self.eng = {"pe": nc.tensor, "act": nc.scalar, "dve": nc.vector, "pool": nc.gpsimd, "sp": nc.sync}
        self.sem = {k: es.enter_context(nc.semaphore("s_" + k)) for k in self.eng}
        self.cnt = {k: 0 for k in self.eng}
        self.waited = {k: {} for k in self.eng}
        self.last_w = {}
        self.readers = {}
        self.dsem = {}
        self.dcnt = {}
        self.dead = False

    def _deps(self, r, w):
        deps = []
        for k in r:
            if k in self.last_w:
                deps.append(self.last_w[k])
        for k in w:
            if k in self.last_w:
                deps.append(self.last_w[k])
            deps.extend(self.readers.get(k, ()))
        return deps

    def _wait(self, eng, deps):
        best = {}
        for (s, v) in deps:
            if eng == "pe" and s == "pe":
                continue
            if v > best.get(s, 0):
                best[s] = v
        for s, v in best.items():
            if self.waited[eng].get(s, 0) < v:
                semh = self.sem[s] if s in self.sem else self.dsem[s]
                self.eng[eng].wait_ge(semh, v)
                self.waited[eng][s] = v

    def _record(self, tok, r, w):
        for k in w:
            self.last_w[k] = tok
            self.readers[k] = []
        for k in r:
            if k not in w:
                self.readers.setdefault(k, []).append(tok)

    def op(self, eng, fn, r=(), w=()):
        if self.dead:
            return
        self._wait(eng, self._deps(r, w))
        ins = fn(self.eng[eng])
        self.cnt[eng] += 1
        ins.then_inc(self.sem[eng], 1)
        self._record((eng, self.cnt[eng]), r, w)

    def _dsem(self, sem):
        if sem not in self.dsem:
            self.dsem[sem] = self.es.enter_context(self.nc.semaphore("d_" + sem))
            self.dcnt[sem] = 0

    def dma(self, q, out, in_, r=(), w=(), sem=None, **kw):
        if self.dead:
            return
        self._dsem(sem)
        self._wait(q, self._deps(r, w))
        ins = self.eng[q].dma_start(out=out, in_=in_, **kw)
        self.dcnt[sem] += 16
        ins.then_inc(self.dsem[sem], 16)
        self._record((sem, self.dcnt[sem]), r, w)

    def idma(self, out, in_, idx, r=(), w=(), sem=None):
        if self.dead:
            return
        self._dsem(sem)
        self._wait("pool", self._deps(r, w))
        ins = self.nc.gpsimd.indirect_dma_start(
            out=out, out_offset=None, in_=in_, in_offset=bass.IndirectOffsetOnAxis(ap=idx, axis=0))
        self.dcnt[sem] += 16
        ins.then_inc(self.dsem[sem], 16)
        self._record((sem, self.dcnt[sem]), r, w)

    def _all(self):
        toks = [(k, self.cnt[k]) for k in self.eng if self.cnt[k] > 0]
        toks += [(k, self.dcnt[k]) for k in self.dsem if self.dcnt[k] > 0]
        return toks

    def barrier(self):
        if self.dead:
            return
        toks = self._all()
        for e in self.eng:
            self._wait(e, toks)
        self.last_w = {}
        self.readers = {}

    def finish(self):
        self._wait("sp", self._all())


def build_nc(npool=2560, stage=99, dbg=False):
    nc = bass.Bass("TRN2", target_bir_lowering=False)

    def din(name, shape, dt=F32):
        return nc.dram_tensor(name, list(shape), dt, kind="ExternalInput").ap()

    def dout(name, shape, dt=F32):
        return nc.dram_tensor(name, list(shape), dt, kind="ExternalOutput").ap()

    x_own = din("x_own", [2048, D])
    x_oth = din("x_oth", [2048, D])
    x_halo = din("x_halo", [8, D])
    x_s = din("x_s", [64, D])
    cache_k = din("cache_k", [npool * 128, 512])
    cache_v = din("cache_v", [npool * 128, 512])
    cache_f = din("cache_f", [npool * 128, 8])
    state_conv = din("state_conv", [32, 512])
    page_table = din("page_table", [1, 256], I32)
    rolec = din("rolec", [1, 80])
    w_in = din("w_in", [D, NIN])
    b_f = din("b_f", [1, 8])
    conv_w = din("conv_w", [3, 512])
    w_conv_out = din("w_conv_out", [512, D])
    w_attn_out = din("w_attn_out", [512, D])
    w_o = din("w_o", [D, D])
    ln1_g = din("ln1_g", [1, D])
    ln1_b = din("ln1_b", [1, D])
    w_gr = din("w_gr", [D, 20])
    w1 = din("w1", [16, D, 512])
    w3 = din("w3", [16, D, 512])
    w2 = din("w2", [16, 512, D])
    ln2_g = din("ln2_g", [1, D])
    ln2_b = din("ln2_b", [1, D])

    y_own = dout("y_own", [2048, D])
    y_s = dout("y_s", [64, D])
    k_own = dout("k_own", [2048, 512])
    v_own = dout("v_own", [2048, 512])
    f_own = dout("f_own", [2048, 8])
    conv_p = dout("conv_p", [2, 512])
    k_s = dout("k_s", [64, 512])
    v_s = dout("v_s", [64, 512])
    f_s = dout("f_s", [64, 8])
    conv_s = dout("conv_s", [32, 512])
    y0_d = nc.dram_tensor("y0_d", [NTOK, D], F32, kind="Internal").ap()
    x1T_d = nc.dram_tensor("x1T_d", [128, 8, NTOK], BF16, kind="Internal").ap()
    dbg_out = {}

    with ExitStack() as es:
        P = Prog(nc, es)

        try:
            def sb(name, shape, dt=F32, stack=es):
                return stack.enter_context(nc.sbuf_tensor(name, list(shape), dt))

            def chk(x):
                if stage < x:
                    P.dead = True

            def dump(name, ap, shape, dt=F32, r=()):
                if dbg:
                    d = dout("dbg_" + name, shape, dt)
                    dbg_out[name] = d
                    P.dma("sp", d, ap, r=list(r), sem="dbg_" + name)

            psf = [es.enter_context(nc.psum_tensor("ps%d" % i, [128, 512], F32)) for i in range(8)]
            ps_rr = [0]
            ps_n = [8]

            def ps():
                i = ps_rr[0] % ps_n[0]
                ps_rr[0] += 1
                return psf[i], "ps%d" % i

            ev_rr = [0]

            def evac(out, in_, r, w):
                ev_rr[0] += 1
                if ev_rr[0] % 2:
                    P.op("act", lambda e: e.copy(out=out, in_=in_), r=r, w=w)
                else:
                    P.op("dve", lambda e: e.tensor_copy(out=out, in_=in_), r=r, w=w)

            def mmg(out, pairs, r, w):
                n = len(pairs)

                def g(e):
                    ins = None
                    for i, (a, b) in enumerate(pairs):
                        ins = e.matmul(out, lhsT=a, rhs=b, start=(i == 0), stop=(i == n - 1))
                    return ins
                P.op("pe", g, r=r, w=w)

            ident = sb("ident", [128, 128])
            ident_b = sb("ident_b", [128, 128], BF16)
            U_f = sb("U_f", [128, 128])
            U_b = sb("U_b", [128, 128], BF16)
            SU4 = sb("SU4", [4, 4])
            ones_f = sb("ones_f", [128, 128])
            ones_b = sb("ones_b", [128, 1], BF16)
            sel127 = sb("sel127", [128, 128])
            hmask = sb("hmask", [32, 8])
            io_i = sb("io_i", [128, 1], I32)
            io_f = sb("io_f", [128, 1])
            bfb = sb("bfb", [128, 8])
            rc = sb("rc", [128, 80])
            LF = sb("LF", [128, 33, 8])
            gate = sb("gate", [128, 17, 16])
            pool_ops = [
                (lambda e: e.memset(ident[:], 1.0), [], ["ident"]),
                (lambda e: e.affine_select(out=ident[:], in_=ident[:], pattern=[[-1, 128]], compare_op=ALU.is_equal,
                                           fill=0.0, base=0, channel_multiplier=1), ["ident"], ["ident"]),
                (lambda e: e.tensor_copy(out=ident_b[:], in_=ident[:]), ["ident"], ["ident_b"]),
                (lambda e: e.memset(U_f[:], 1.0), [], ["U_f"]),
                (lambda e: e.affine_select(out=U_f[:], in_=U_f[:], pattern=[[1, 128]], compare_op=ALU.is_ge,
                                           fill=0.0, base=0, channel_multiplier=-1), ["U_f"], ["U_f"]),
                (lambda e: e.tensor_copy(out=U_b[:], in_=U_f[:]), ["U_f"], ["U_b"]),
                (lambda e: e.memset(SU4[:], 1.0), [], ["SU4"]),
                (lambda e: e.affine_select(out=SU4[:], in_=SU4[:], pattern=[[-1, 4]], compare_op=ALU.is_gt,
                                           fill=0.0, base=0, channel_multiplier=1), ["SU4"], ["SU4"]),
                (lambda e: e.memset(ones_f[:], 1.0), [], ["ones_f"]),
                (lambda e: e.memset(ones_b[:], 1.0), [], ["ones_b"]),
                (lambda e: e.memset(sel127[:], 1.0), [], ["sel127"]),
                (lambda e: e.affine_select(out=sel127[:], in_=sel127[:], pattern=[[0, 128]], compare_op=ALU.is_equal,
                                           fill=0.0, base=-127, channel_multiplier=1), ["sel127"], ["sel127"]),
                (lambda e: e.memset(hmask[:], 1.0), [], ["hmask"]),
                (lambda e: e.affine_select(out=hmask[:], in_=hmask[:], pattern=[[-4, 8]], compare_op=ALU.is_ge,
                                           fill=0.0, base=0, channel_multiplier=1), ["hmask"], ["hmask"]),
                (lambda e: e.affine_select(out=hmask[:], in_=hmask[:], pattern=[[4, 8]], compare_op=ALU.is_ge,
                                           fill=0.0, base=3, channel_multiplier=-1), ["hmask"], ["hmask"]),
                (lambda e: e.iota(out=io_i[:], pattern=[[0, 1]], base=0, channel_multiplier=1), [], ["io_i"]),
                (lambda e: e.tensor_copy(out=io_f[:], in_=io_i[:]), ["io_i"], ["io_f"]),
                (lambda e: e.memset(LF[:], 0.0), [], ["LFall"]),
                (lambda e: e.memset(gate[:], 0.0), [], ["gate"]),
            ]
            for fn, r_, w_ in pool_ops:
                P.op("pool", fn, r=r_, w=w_)
            P.dma("sp", bfb[:], b_f.partition_broadcast(128), w=["bfb"], sem="c0")
            P.dma("sp", rc[:], rolec.partition_broadcast(128), w=["rc"], sem="c1")
            P.barrier()
            chk(0.1)

            s_oT = ExitStack()
            es.enter_context(s_oT)
            oT = sb("oT", [128, 4, NTOK], BF16, s_oT)
            s_A = ExitStack()
            es.enter_context(s_A)
            if os.environ.get("KFIRST"):
                kT = sb("kT", [128, 4, int(os.environ.get("KTN", "4224"))], BF16, s_A)
                qT = sb("qT", [128, 4, NTOK], BF16, s_A)
            else:
                qT = sb("qT", [128, 4, NTOK], BF16, s_A)
                kT = sb("kT", [128, 4, int(os.environ.get("KTN", "4224"))], BF16, s_A)
            Vt = sb("Vt", [128, 33, 8, 66], BF16, s_A)
            Cc = sb("Cc", [128, 32, 8], F32, s_A)
            BIAS = sb("BIAS", [128, 4, 32, 8], F32, s_A)
            P.op("pool", lambda e: e.memset(Vt[:, :, :, 64:65], 1.0), w=["Vones"])

            wst = []
            wst_rr = [0]

            def wchunks(dst, dkey, src, K, N, col0=0, dcol0=0, CH=1024):
                th = []
                if N <= CH:
                    g = max(1, CH // N)
                    for k0 in range(0, K, g):
                        kk = min(g, K - k0)

                        def f(k0=k0, kk=kk):
                            i = wst_rr[0] % len(wst)
                            wst_rr[0] += 1
                            st, sk = wst[i], "wst%d" % i
                            sv = st[:, 0:kk * N].rearrange("p (k n) -> p k n", n=N)
                            P.dma("sp", sv, src[k0 * 128:(k0 + kk) * 128, col0:col0 + N].rearrange("(k p) n -> p k n", p=128), w=[sk], sem=sk)
                            P.op("act", lambda e: e.copy(out=dst[:, k0:k0 + kk, dcol0:dcol0 + N], in_=sv), r=[sk], w=[dkey])
                        th.append(f)
                else:
                    for k in range(K):
                        for c0 in range(0, N, CH):
                            n = min(CH, N - c0)

                            def f(k=k, c0=c0, n=n):
                                i = wst_rr[0] % len(wst)
                                wst_rr[0] += 1
                                st, sk = wst[i], "wst%d" % i
                                P.dma("sp", st[:, 0:n], src[k * 128:(k + 1) * 128, col0 + c0:col0 + c0 + n], w=[sk], sem=sk)
                                P.op("act", lambda e: e.copy(out=dst[:, k, dcol0 + c0:dcol0 + c0 + n], in_=st[:, 0:n]), r=[sk], w=[dkey])
                            th.append(f)
                return th

            def wload(*a, **kw):
                for f in wchunks(*a, **kw):
                    f()

            def load_xT(src, W, xi, xt, xik, xtk):
                nb = (W + 127) // 128
                pw = min(W, 128)
                if W == 512:
                    P.dma("sp", xi[:], src.rearrange("(b p) d -> p b d", p=128), w=[xik], sem=xik)
                else:
                    P.dma("sp", xi[0:pw, 0, :], src, w=[xik], sem=xik)
                for b in range(nb):
                    for half in range(2):
                        pt, ptk = ps()

                        def tr(e, pt=pt, b=b, half=half):
                            ins = None
                            for kk in range(4):
                                k = half * 4 + kk
                                ins = e.transpose(pt[:, kk * 128:kk * 128 + pw], xi[0:pw, b, k * 128:(k + 1) * 128], ident[0:pw, 0:pw])
                            return ins
                        P.op("pe", tr, r=[xik, "ident"], w=[ptk])
                        evac(xt[:, half * 4:half * 4 + 4, b * 128:b * 128 + pw],
                             pt[:].rearrange("p (k t) -> p k t", k=4)[:, :, 0:pw], r=[ptk], w=[xtk + "_%d_%d" % (b, half)])
                return [xtk + "_%d_%d" % (b, h) for b in range(nb) for h in range(2)]

            with ExitStack() as s1:
                wq = sb("wq", [128, 8, 512], BF16, s1)
                wk = sb("wk", [128, 8, 512], BF16, s1)
                wv = sb("wv", [128, 8, 512], BF16, s1)
                wf = sb("wf", [128, 8, 8], BF16, s1)
                wst[:] = [sb("wstA%d" % i, [128, 1024], F32, s1) for i in range(2)]
                for (t, c0, n, nm) in ((wq, C_Q, 512, "wq"), (wk, C_K, 512, "wk"), (wv, C_V, 512, "wv"), (wf, C_F, 8, "wf")):
                    wload(t, nm, w_in, 8, n, col0=c0, CH=1024)
                xin = [sb("xin%d" % i, [128, 4, 1024], F32, s1) for i in range(2)]
                xT = [sb("xT%d" % i, [128, 8, 512], BF16, s1) for i in range(2)]
                stg = [sb("stg%d" % i, [128, 512], F32, s1) for i in range(4)]
                ftmp = [sb("ftmp%d" % i, [128, 8], F32, s1) for i in range(2)]
                stg_rr = [0]
                tiles = []
                for s_ in range(4):
                    tiles.append((x_own[s_ * 512:(s_ + 1) * 512, :], 512, True, s_ * 512, s_ * 512, s_ * 4, s_ * 512, k_own, v_own))
                tiles.append((x_s, 64, True, 2048, 4096, 32, 0, k_s, v_s))
                for s_ in range(4):
                    tiles.append((x_oth[s_ * 512:(s_ + 1) * 512, :], 512, False, None, 2048 + s_ * 512, 16 + s_ * 4, None, None, None))

                for ti, (src, W, own, q0, k0, b0, r0, ko, vo) in enumerate(tiles):
                    if ti == 0:
                        chk(0.2)
                    if ti == 1:
                        chk(0.3)
                    if ti == 5:
                        chk(0.4)
                    xi, xt = xin[ti % 2], xT[ti % 2]
                    xik, xtk = "xin%d" % (ti % 2), "xT%d" % (ti % 2)
                    nb = (W + 127) // 128
                    pw = min(W, 128)
                    xtall = load_xT(src, W, xi, xt, xik, xtk)
                    if ti == 0:
                        chk(0.21)

                    def proj_fm(wt, wnm, dst, d0):
                        for hp in range(4):
                            pt, ptk = ps()
                            mmg(pt[:, 0:W], [(wt[:, k, hp * 128:(hp + 1) * 128], xt[:, k, 0:W]) for k in range(8)], r=xtall + [wnm], w=[ptk])
                            evac(dst[:, hp, d0:d0 + W], pt[:, 0:W], r=[ptk], w=["%s_%d_%d" % (dst.name, hp, d0)])


                    if own and not os.environ.get("SKIPQ"):
                        proj_fm(wq, "wq", qT, q0)
                    if ti == 0:
                        chk(0.22)
                    proj_fm(wq if os.environ.get("USEWQ") else wk, "wq" if os.environ.get("USEWQ") else "wk", kT, k0)
                    if ti == 0:
                        chk(0.23)
                    for b in range(nb):
                        xtb = [xtk + "_%d_%d" % (b, h) for h in range(2)]
                        tsl = slice(b * 128, b * 128 + pw)
                        pt, ptk = ps()
                        mmg(pt[0:pw, :], [(xt[:, k, tsl], wv[:, k, :]) for k in range(8)], r=xtb + ["wv"], w=[ptk])
                        P.op("act", lambda e, pt=pt, b=b: e.copy(out=Vt[0:pw, b0 + b, :, 0:64], in_=pt[0:pw, :].rearrange("p (h d) -> p h d", h=8)),
                             r=[ptk], w=["Vt%d" % (b0 + b)])
                        if ti == 0 and b == 0:
                            chk(0.2311)
                        if own:
                            si = stg_rr[0] % 4
                            stg_rr[0] += 1
                            P.op("dve", lambda e, pt=pt, si=si: e.tensor_copy(out=stg[si][0:pw, :], in_=pt[0:pw, :]), r=[ptk, "Vt%d" % (b0 + b)], w=["stg%d" % si])
                            P.dma("sp", vo[r0 + b * 128:r0 + b * 128 + pw, :], stg[si][0:pw, :], r=["stg%d" % si], sem="stg%d" % si)
                            if ti == 0 and b == 0:
                                chk(0.2312)
                            pt, ptk = ps()
                            mmg(pt[0:pw, :], [(xt[:, k, tsl], wk[:, k, :]) for k in range(8)], r=xtb + ["wk"], w=[ptk])
                            si = stg_rr[0] % 4
                            stg_rr[0] += 1
                            evac(stg[si][0:pw, :], pt[0:pw, :], r=[ptk], w=["stg%d" % si])
                            P.dma("sp", ko[r0 + b * 128:r0 + b * 128 + pw, :], stg[si][0:pw, :], r=["stg%d" % si], sem="stg%d" % si)
                        if ti == 0 and b == 0:
                            chk(0.232)
                        pt, ptk = ps()
                        mmg(pt[0:pw, 0:8], [(xt[:, k, tsl], wf[:, k, :]) for k in range(8)], r=xtb + ["wf"], w=[ptk])
                        ft = ftmp[b % 2]
                        fk = "ftmp%d" % (b % 2)
                        P.op("dve", lambda e, pt=pt, ft=ft: e.tensor_tensor(out=ft[0:pw, :], in0=pt[0:pw, 0:8], in1=bfb[0:pw, :], op=ALU.add),
                             r=[ptk, "bfb"], w=[fk])
                        if ti == 0 and b == 0:
                            chk(0.233)
                        P.op("act", lambda e, ft=ft: e.activation(out=ft[0:pw, :], in_=ft[0:pw, :], func=AF.Exp, scale=-1.0), r=[fk], w=[fk])
                        if ti == 0 and b == 0:
                            chk(0.234)
                        P.op("act", lambda e, ft=ft: e.activation(out=ft[0:pw, :], in_=ft[0:pw, :], func=AF.Ln, bias=1.0, scale=1.0), r=[fk], w=[fk])
                        P.op("dve", lambda e, ft=ft, bb=b0 + b: e.tensor_scalar(out=LF[0:pw, bb, :], in0=ft[0:pw, :], scalar1=-1.0, scalar2=None, op0=ALU.mult),
                             r=[fk, "LFall"], w=["LF%d" % (b0 + b)])
                lfall = ["LF%d" % i for i in range(33)]
                P.dma("sp", f_own.rearrange("(b p) h -> p b h", p=128), LF[:, 0:16, :], r=lfall, sem="fo")
                P.dma("sp", f_s, LF[0:64, 32, :], r=lfall, sem="fs")
                P.barrier()
                chk(0.5)

            with ExitStack() as s2:
                T1 = sb("T1", [128, 8, 8], F32, s2)
                Lprev = sb("Lprev", [128, 8, 8], F32, s2)
                Ltmp = sb("Ltmp", [128, 8, 8], F32, s2)
                Lfull = sb("Lfull", [128, 32, 8], F32, s2)
                Cb = sb("Cb", [128, 4, 8], F32, s2)
                LF4 = LF[:, 0:32, :].rearrange("p (t j) h -> p t j h", j=4)
                Lf4 = Lfull[:].rearrange("p (t j) h -> p t j h", j=4)
                P.op("dve", lambda e: e.tensor_tensor(out=T1[:], in0=LF4[:, :, 0, :], in1=LF4[:, :, 1, :], op=ALU.add), r=[], w=["T1"])
                P.op("dve", lambda e: e.tensor_tensor(out=T1[:], in0=T1[:], in1=LF4[:, :, 2, :], op=ALU.add), r=["T1"], w=["T1"])
                P.op("dve", lambda e: e.tensor_tensor(out=T1[:], in0=T1[:], in1=LF4[:, :, 3, :], op=ALU.add), r=["T1"], w=["T1"])
                rcE = rc[:, 0:64].rearrange("q (a b) -> q a b", b=8)
                for pp in range(8):
                    in0 = T1[:, pp, :].unsqueeze(1).to_broadcast([128, 8, 8])
                    in1 = rcE[:, pp, :].unsqueeze(2).to_broadcast([128, 8, 8])
                    if pp == 0:
                        P.op("dve", lambda e, in0=in0, in1=in1: e.tensor_tensor(out=Lprev[:], in0=in0, in1=in1, op=ALU.mult), r=["T1"], w=["Lprev"])
                    else:
                        P.op("dve", lambda e, in0=in0, in1=in1: e.tensor_tensor(out=Ltmp[:], in0=in0, in1=in1, op=ALU.mult), r=["T1"], w=["Ltmp"])
                        P.op("dve", lambda e: e.tensor_tensor(out=Lprev[:], in0=Lprev[:], in1=Ltmp[:], op=ALU.add), r=["Ltmp", "Lprev"], w=["Lprev"])
                P.op("dve", lambda e: e.tensor_copy(out=Lf4[:, :, 0, :], in_=Lprev[:]), r=["Lprev"], w=["Lfull"])
                for j in range(1, 4):
                    P.op("dve", lambda e, j=j: e.tensor_tensor(out=Lf4[:, :, j, :], in0=Lf4[:, :, j - 1, :], in1=LF4[:, :, j - 1, :], op=ALU.add),
                         r=["Lfull"], w=["Lfull"])
                pc, pck = ps()
                mmg(pc[:, 0:256], [(U_f[:], LF[:, 0:32, :].rearrange("p b h -> p (b h)")), (ones_f[:], Lfull[:].rearrange("p b h -> p (b h)"))],
                    r=["Lfull"], w=[pck])
                P.op("dve", lambda e: e.tensor_copy(out=Cc[:].rearrange("p b h -> p (b h)"), in_=pc[:, 0:256]), r=[pck], w=["Cc"])
                Cc4 = Cc[:].rearrange("p (t j) h -> p t j h", j=4)
                pcb, pcbk = ps()
                mmg(pcb[:, 0:32].rearrange("p (a b) -> p a b", b=8), [(sel127[:], Cc4[:, 0:4, 1, :])], r=["Cc"], w=[pcbk])
                P.op("dve", lambda e: e.tensor_copy(out=Cb[:].rearrange("p a b -> p (a b)"), in_=pcb[:, 0:32]), r=[pcbk], w=["Cb"])
                for sg in range(4):
                    P.op("dve", lambda e, sg=sg: e.scalar_tensor_tensor(out=BIAS[:, sg], in0=Cc[:], scalar=-1.0,
                                                                         in1=Cb[:, sg, :].unsqueeze(1).to_broadcast([128, 32, 8]),
                                                                         op0=ALU.mult, op1=ALU.add), r=["Cc", "Cb"], w=["BIAS"])
                    P.op("dve", lambda e, sg=sg: e.tensor_scalar(out=BIAS[:, sg, 16 + 4 * sg:20 + 4 * sg, :], in0=BIAS[:, sg, 16 + 4 * sg:20 + 4 * sg, :],
                                                                  scalar1=rc[:, 64 + sg:65 + sg], scalar2=None, op0=ALU.add), r=["BIAS"], w=["BIAS"])
                dump("Cc", Cc[:], [128, 32, 8], r=["Cc"])
                P.barrier()
                chk(1.0)

            if stage >= 2:
                with ExitStack() as s2:
                    ptb = sb("ptb", [128, 256], I32, s2)
                    ptf = sb("ptf", [128, 256], F32, s2)
                    idx = sb("idx", [128, 256], I32, s2)
                    lfp = sb("lfp", [128, 16, 16, 8], F32, s2)
                    Linc = sb("Linc", [128, 16, 16, 8], F32, s2)
                    Cbs = sb("Cbs", [128, 16, 8], F32, s2)
                    lfn4 = sb("lfn4", [4, 16, 8], F32, s2)
                    pidx = sb("pidx", [128, 2], I32, s2)
                    bnew = sb("bnew", [4, 16, 8], F32, s2)
                    Vn = sb("Vn", [4, 16, 512], BF16, s2)
                    Qbd = sb("Qbd", [128, 4, 16, 8], BF16, s2)
                    Kp = [sb("Kp%d" % i, [128, 16, 512], BF16, s2) for i in range(1)]
                    Vp = [sb("Vp%d" % i, [128, 16, 512], BF16, s2) for i in range(2)]
                    KTp = [sb("KTp%d" % i, [128, 4, 128], BF16, s2) for i in range(3)]
                    stmp = sb("stmp", [128, 512], F32, s2)
                    Pp = sb("Pp", [128, 16, 32], BF16, s2)
                    tn = sb("tn", [4, 32], F32, s2)
                    Pn = sb("Pn", [4, 32], BF16, s2)
                    t3 = sb("t3", [32, 512], F32, s2)
                    o32 = sb("o32", [32, 64], F32, s2)
                    rd = sb("rd", [32, 1], F32, s2)
                    ps_n[0] = 5
                    P.dma("sp", ptb[:], page_table.partition_broadcast(128), w=["ptb"], sem="ptb")
                    P.op("dve", lambda e: e.tensor_copy(out=ptf[:], in_=ptb[:]), r=["ptb"], w=["ptf"])
                    P.op("dve", lambda e: e.tensor_scalar(out=ptf[:], in0=ptf[:], scalar1=128.0, scalar2=io_f[:, 0:1], op0=ALU.mult, op1=ALU.add),
                         r=["ptf", "io_f"], w=["ptf"])
                    P.op("dve", lambda e: e.tensor_copy(out=idx[:], in_=ptf[:]), r=["ptf"], w=["idx"])
                    P.dma("sp", lfn4[:], f_s.rearrange("(b j) h -> j b h", j=4), w=["lfn4"], sem="lfn4")
                    P.dma("pool", Vn[:], v_s.rearrange("(b j) f -> j b f", j=4), w=["Vn"], sem="Vn")
                    with nc.allow_non_contiguous_dma(reason="tiny page-id transpose"):
                        P.dma("sp", pidx[:], page_table.rearrange("o (g p) -> p (o g)", p=128), w=["pidx"], sem="pidx")
                    cfp = cache_f.rearrange("(n k) h -> n (k h)", k=128)
                    lfT = Linc[:].rearrange("p b g h -> p (b g h)").rearrange("p (a n) -> p a n", a=2)
                    for g_ in range(2):
                        P.idma(lfT[:, g_, :], cfp, pidx[:, g_:g_ + 1], r=["pidx"], w=["lfT%d" % g_], sem="lfT")
                    for g_ in range(2):
                        lv = lfT[:, g_, :].rearrange("p (k h) -> p k h", h=8)
                        for q4 in range(2):
                            ptl, ptlk = ps()
                            P.op("pe", lambda e, ptl=ptl, lv=lv, q4=q4: [e.transpose(ptl[:, hh * 128:(hh + 1) * 128], lv[:, :, 4 * q4 + hh], ident[:]) for hh in range(4)][-1],
                                 r=["lfT0", "lfT1", "ident"], w=[ptlk])
                            evac(lfp[:, 8 * g_:8 * g_ + 8, :, 4 * q4:4 * q4 + 4].rearrange("p b g h -> p h (b g)"),
                                 ptl[:, :].rearrange("p (h s) -> p h s", h=4), r=[ptlk], w=["lfp_%d_%d" % (g_, q4)])
                    lfpall = ["lfp_%d_%d" % (g_, q4) for g_ in range(2) for q4 in range(2)]
                    P.op("dve", lambda e: e.tensor_copy(out=Linc[:, :, 0, :], in_=lfp[:, :, 0, :]), r=lfpall, w=["Linc", "lfp"])
                    for pg in range(1, 16):
                        P.op("dve", lambda e, pg=pg: e.tensor_tensor(out=Linc[:, :, pg, :], in0=Linc[:, :, pg - 1, :], in1=lfp[:, :, pg, :], op=ALU.add),
                             r=["Linc", "lfp"], w=["Linc"])
                    pcs, pcsk = ps()
                    P.op("pe", lambda e: (e.matmul(pcs[:, 0:128].rearrange("p (b h) -> p b h", h=8), lhsT=ones_f[:], rhs=Linc[:, :, 15, :], start=True, stop=False),
                                          e.matmul(pcs[:, 0:128].rearrange("p (b h) -> p b h", h=8), lhsT=ones_f[0:4, :], rhs=lfn4[:], start=False, stop=True))[1],
                         r=["Linc", "lfn4"], w=[pcsk])
                    P.op("dve", lambda e: e.tensor_copy(out=Cbs[:].rearrange("p b h -> p (b h)"), in_=pcs[:, 0:128]), r=[pcsk], w=["Cbs"])
                    P.op("dve", lambda e: e.tensor_tensor(out=Linc[:], in0=Linc[:], in1=lfp[:], op=ALU.subtract), r=["Linc", "lfp", "Cbs"], w=["Linc"])
                    cpast = lfp
                    lfp2 = lfp[:].rearrange("p b g h -> p (b g h)")
                    Lin2 = Linc[:].rearrange("p b g h -> p (b g h)")
                    cp2 = cpast[:].rearrange("p b g h -> p (b g h)")
                    for q4 in range(4):
                        pq, pqk = ps()
                        cs = slice(q4 * 512, (q4 + 1) * 512)
                        mmg(pq[:, :], [(U_f[:], lfp2[:, cs]), (ones_f[:], Lin2[:, cs])], r=["Linc", "lfp"], w=[pqk])
                        for bb in range(4):
                            b_ = 4 * q4 + bb
                            P.op("dve", lambda e, pq=pq, bb=bb, b_=b_: e.scalar_tensor_tensor(
                                out=cpast[:, b_], in0=pq[:, bb * 128:(bb + 1) * 128].rearrange("p (g h) -> p g h", g=16), scalar=-1.0,
                                in1=Cbs[:, b_, :].unsqueeze(1).to_broadcast([128, 16, 8]), op0=ALU.mult, op1=ALU.add),
                                r=[pqk, "Cbs"], w=["cpast", "lfp"])
                    pbn, pbnk = ps()
                    mmg(pbn[0:4, 0:128].rearrange("p (b h) -> p b h", h=8), [(SU4[:], lfn4[:])], r=["lfn4", "SU4"], w=[pbnk])
                    P.op("dve", lambda e: e.tensor_copy(out=bnew[:].rearrange("p b h -> p (b h)"), in_=pbn[0:4, 0:128]), r=[pbnk], w=["bnew"])
                    P.op("pool", lambda e: e.memset(Qbd[:], 0.0), w=["Qbd"])
                    qs = qT[:, :, 2048:2112].rearrange("p c (b j) -> p c b j", j=4)
                    P.op("pool", lambda e: e.tensor_copy(out=Qbd[0:64, :, :, 0:4], in_=qs[0:64]), r=["Qbd"], w=["Qbd"])
                    P.op("pool", lambda e: e.tensor_copy(out=Qbd[64:128, :, :, 4:8], in_=qs[64:128]), r=["Qbd"], w=["Qbd"])
                    kt_rr = 0
                    for b in range(16):
                        kp, vp = Kp[0], Vp[b % 2]
                        kpk, vpk = "Kp0", "Vp%d" % (b % 2)
                        for pg in range(16):
                            P.idma(kp[:, pg, :], cache_k, idx[:, b * 16 + pg:b * 16 + pg + 1], r=["idx"], w=[kpk + "_%d" % pg], sem=kpk)
                        for pg in range(16):
                            P.idma(vp[:, pg, :], cache_v, idx[:, b * 16 + pg:b * 16 + pg + 1], r=["idx"], w=[vpk + "_%d" % pg], sem=vpk)
                        ss, ssk = psf[5], "ps5"
                        sn, snk = psf[6], "ps6"
                        acc, acck = psf[7], "ps7"
                        for pg in range(16):
                            ptb_, ptbk = ps()
                            ptv = ptb_[:].bitcast(BF16)

                            def trk(e, ptv=ptv, kp=kp, pg=pg):
                                ins = None
                                for hp in range(4):
                                    ins = e.transpose(ptv[:, hp * 128:(hp + 1) * 128], kp[:, pg, hp * 128:(hp + 1) * 128], ident_b[:])
                                return ins
                            P.op("pe", trk, r=[kpk + "_%d" % g_ for g_ in range(16)] + ["ident_b"], w=[ptbk])
                            ktp = KTp[kt_rr % 3]
                            ktk = "KTp%d" % (kt_rr % 3)
                            kt_rr += 1
                            evac(ktp[:].rearrange("p c k -> p (c k)"), ptv[:, 0:512], r=[ptbk], w=[ktk])

                            def qk(e, ktp=ktp, pg=pg, b=b):
                                ins = None
                                for hp in range(4):
                                    ins = e.matmul(ss[:, pg * 32 + hp * 8:pg * 32 + hp * 8 + 8], lhsT=ktp[:, hp, :], rhs=Qbd[:, hp, b, :], start=True, stop=True)
                                return ins
                            P.op("pe", qk, r=[ktk, "Qbd"], w=[ssk])

                        def qkn(e, b=b):
                            ins = None
                            for hp in range(4):
                                ins = e.matmul(sn[0:4, hp * 8:hp * 8 + 8], lhsT=kT[:, hp, 4096 + 4 * b:4096 + 4 * b + 4], rhs=Qbd[:, hp, b, :], start=True, stop=True)
                            return ins
                        P.op("pe", qkn, r=["Qbd"], w=[snk])
                        P.op("dve", lambda e, b=b: e.scalar_tensor_tensor(
                            out=stmp[:].rearrange("p (g h q) -> p g h q", g=16, h=8), in0=ss[:, :].rearrange("p (g h q) -> p g h q", g=16, h=8), scalar=0.125,
                            in1=cpast[:, b].unsqueeze(3).to_broadcast([128, 16, 8, 4]), op0=ALU.mult, op1=ALU.add), r=[ssk, "cpast"], w=["stmp"])
                        P.op("act", lambda e: e.activation(out=Pp[:].rearrange("p g c -> p (g c)"), in_=stmp[:], func=AF.Exp), r=["stmp"], w=["Pp"])
                        P.op("dve", lambda e, b=b: e.scalar_tensor_tensor(
                            out=tn[:].rearrange("p (h q) -> p h q", h=8), in0=sn[0:4, 0:32].rearrange("p (h q) -> p h q", h=8), scalar=0.125,
                            in1=bnew[:, b, :].unsqueeze(2).to_broadcast([4, 8, 4]), op0=ALU.mult, op1=ALU.add), r=[snk, "bnew"], w=["tn"])
                        P.op("act", lambda e: e.activation(out=tn[:], in_=tn[:], func=AF.Exp), r=["tn"], w=["tn"])
                        P.op("dve", lambda e: e.tensor_tensor(out=Pn[:].rearrange("p (h q) -> p h q", h=8), in0=tn[:].rearrange("p (h q) -> p h q", h=8),
                                                              in1=U_f[0:4, 0:4].unsqueeze(1).to_broadcast([4, 8, 4]), op=ALU.mult), r=["tn", "U_f"], w=["Pn"])

                        def pv(e, vp=vp, b=b):
                            for pg in range(16):
                                e.matmul(acc[0:32, :], lhsT=Pp[:, pg, :], rhs=vp[:, pg, :], start=(pg == 0), stop=False)
                            return e.matmul(acc[0:32, :], lhsT=Pn[:, :], rhs=Vn[:, b, :], start=False, stop=True)
                        P.op("pe", pv, r=["Pp", "Pn", "Vn"] + [vpk + "_%d" % g_ for g_ in range(16)], w=[acck])

                        def dn(e):
                            for pg in range(16):
                                e.matmul(sn[0:32, 64:65], lhsT=Pp[:, pg, :], rhs=ones_b[:, 0:1], start=(pg == 0), stop=False)
                            return e.matmul(sn[0:32, 64:65], lhsT=Pn[:, :], rhs=ones_b[0:4, 0:1], start=False, stop=True)
                        P.op("pe", dn, r=["Pp", "Pn", "tn"], w=[snk + "d"])
                        P.op("dve", lambda e: e.tensor_tensor(out=t3[:].rearrange("p (h d) -> p h d", h=8), in0=acc[0:32, :].rearrange("p (h d) -> p h d", h=8),
                                                              in1=hmask[:].unsqueeze(2).to_broadcast([32, 8, 64]), op=ALU.mult), r=[acck, "hmask"], w=["t3"])
                        P.op("dve", lambda e: e.tensor_reduce(out=o32[:], in_=t3[:].rearrange("p (h d) -> p d h", h=8), axis=AX.X, op=ALU.add), r=["t3"], w=["o32"])
                        P.op("dve", lambda e: e.reciprocal(out=rd[:], in_=sn[0:32, 64:65]), r=[snk + "d"], w=["rd"])
                        P.op("dve", lambda e: e.tensor_scalar(out=o32[:], in0=o32[:], scalar1=rd[:, 0:1], scalar2=None, op0=ALU.mult), r=["o32", "rd"], w=["o32"])
                        po, pok = ps()
                        P.op("pe", lambda e, po=po: e.transpose(po[0:64, 0:32], o32[:, :], ident[0:32, 0:32]), r=["o32", "ident"], w=[pok])
                        pov = po[0:64, 0:32].rearrange("p (c hh q) -> p c hh q", c=4, hh=2)
                        P.op("dve", lambda e, pov=pov, b=b: e.tensor_copy(out=oT[0:64, :, 2048 + 4 * b:2052 + 4 * b], in_=pov[:, :, 0, :]), r=[pok, snk, snk + "d"], w=["oTs%d" % b])
                        P.op("dve", lambda e, pov=pov, b=b: e.tensor_copy(out=oT[64:128, :, 2048 + 4 * b:2052 + 4 * b], in_=pov[:, :, 1, :]), r=[pok], w=["oTs%d" % b])
                    ps_n[0] = 8
                    P.barrier()

            if stage >= 3:
                with ExitStack() as s3:
                    Pt = [sb("Pt%d" % i, [128, 512], BF16, s3) for i in range(6)]
                    rec = sb("rec", [128, 512], F32, s3)
                    bcs = sb("bcs", [64, 512], F32, s3)
                    ps_n[0] = 5
                    pt_rr = 0
                    n_acc = 0
                    LA = 3
                    pending_norm = [None]
                    for sg in range(4):
                        for h in range(8):
                            hp, hh = h // 2, h % 2
                            pr = slice(hh * 64, hh * 64 + 64)
                            acc, acck = psf[6 + n_acc % 2], "ps%d" % (6 + n_acc % 2)
                            n_acc += 1
                            blocks = list(range(0, 4 * sg)) + list(range(16, 16 + 4 * sg + 4)) + list(range(4 * sg, 4 * sg + 4))
                            nbk = len(blocks)
                            info = {}
                            for i in range(nbk + LA):
                                if i < nbk:
                                    kb = blocks[i]
                                    diag = 4 * sg <= kb < 4 * sg + 4
                                    q0 = 128 * (kb - 4 * sg) if diag else 0
                                    st, stk = ps()
                                    P.op("pe", lambda e, st=st, kb=kb, q0=q0: e.matmul(st[:, q0:512], lhsT=kT[pr, hp, kb * 128:(kb + 1) * 128],
                                                                                         rhs=qT[pr, hp, sg * 512 + q0:(sg + 1) * 512], start=True, stop=True),
                                         r=[], w=[stk])
                                    pti = Pt[pt_rr % 6]
                                    ptk = "Pt%d" % (pt_rr % 6)
                                    pt_rr += 1
                                    P.op("act", lambda e, st=st, pti=pti, kb=kb, q0=q0: e.activation(out=pti[:, q0:512], in_=st[:, q0:512], func=AF.Exp,
                                                                                                      bias=BIAS[:, sg, kb, h:h + 1], scale=0.125),
                                         r=[stk], w=[ptk])
                                    if diag:
                                        P.op("pool", lambda e, pti=pti, q0=q0: e.tensor_tensor(out=pti[:, q0:q0 + 128], in0=pti[:, q0:q0 + 128], in1=U_b[:], op=ALU.mult),
                                             r=[ptk], w=[ptk])
                                    info[i] = (kb, q0, pti, ptk)
                                if i == LA and pending_norm[0] is not None:
                                    pending_norm[0]()
                                    pending_norm[0] = None
                                j = i - LA
                                if j >= 0:
                                    kb, q0, pti, ptk = info[j]
                                    P.op("pe", lambda e, pti=pti, kb=kb, q0=q0, j=j: e.matmul(acc[0:65, q0:512], lhsT=Vt[:, kb, h, 0:65], rhs=pti[:, q0:512],
                                                                                             start=(j == 0), stop=(j == nbk - 1)),
                                         r=[ptk], w=[acck])

                            def norm(acc=acc, acck=acck, pr=pr, hp=hp, sg=sg, h=h):
                                P.op("dve", lambda e: e.reciprocal(out=rec[64:65, :], in_=acc[64:65, :]), r=[acck], w=["rec"])
                                P.op("pe", lambda e: e.matmul(psf[5][0:64, :], lhsT=ones_f[64:65, 0:64], rhs=rec[64:65, :], start=True, stop=True), r=["rec"], w=["ps5"])
                                P.op("act", lambda e: e.copy(out=bcs[:], in_=psf[5][0:64, :]), r=["ps5"], w=["bcs"])
                                P.op("dve", lambda e: e.tensor_tensor(out=oT[pr, hp, sg * 512:(sg + 1) * 512], in0=acc[0:64, :], in1=bcs[:], op=ALU.mult),
                                     r=[acck, "bcs"], w=["oT_%d_%d" % (sg, h)])
                            pending_norm[0] = norm
                    pending_norm[0]()
                    ps_n[0] = 8
                    dump("oT", oT[:], [128, 4, NTOK], BF16)
                    P.barrier()
            s_A.close()

            if stage >= 4:
                with ExitStack() as s4:
                    wcv = sb("wcv", [128, 8, 1536], BF16, s4)
                    wg = sb("wg", [128, 8, 2048], BF16, s4)
                    wco = sb("wco", [128, 4, 1024], BF16, s4)
                    wao = sb("wao", [128, 4, 1024], BF16, s4)
                    wo = sb("wo", [128, 8, 1024], BF16, s4)
                    wgr = sb("wgr", [128, 8, 20], F32, s4)
                    cw = sb("cw", [128, 4, 3], F32, s4)
                    g1b = sb("g1b", [128, 1024], F32, s4)
                    b1b = sb("b1b", [128, 1024], F32, s4)
                    wst[:] = [sb("wstB%d" % i, [128, 1024], F32, s4) for i in range(2)]
                    wload(wcv, "wcv", w_in, 8, 1536, col0=0, CH=1024)
                    wload(wg, "wg", w_in, 8, 1024, col0=C_GA, dcol0=0, CH=1024)
                    wload(wg, "wg", w_in, 8, 1024, col0=C_GB, dcol0=1024, CH=1024)
                    wload(wco, "wco", w_conv_out, 4, 1024, CH=1024)
                    wload(wao, "wao", w_attn_out, 4, 1024, CH=1024)
                    wload(wo, "wo", w_o, 8, 1024, CH=1024)
                    P.dma("sp", wgr[:], w_gr.rearrange("(k p) n -> p k n", p=128), w=["wgr"], sem="wgr")
                    with nc.allow_non_contiguous_dma(reason="tiny conv weight transpose"):
                        for c in range(4):
                            P.dma("sp", cw[:, c, :], conv_w[:, c * 128:(c + 1) * 128].rearrange("j p -> p j"), w=["cw%d" % c], sem="cw")
                    P.dma("sp", g1b[:], ln1_g.partition_broadcast(128), w=["g1b"], sem="g1b")
                    P.dma("sp", b1b[:], ln1_b.partition_broadcast(128), w=["b1b"], sem="b1b")

                    xin = sb("xin", [128, 4, 1024], F32, s4)
                    xT = sb("xT", [128, 8, 512], BF16, s4)
                    xh = sb("xh", [8, 1024], F32, s4)
                    xhT = sb("xhT", [128, 8, 8], BF16, s4)
                    uh = sb("uh", [128, 4, 8], F32, s4)
                    sct = sb("sct", [32, 512], F32, s4)
                    stT = sb("stT", [128, 4, 32], F32, s4)
                    xcs = sb("xcs", [128, 512], F32, s4)
                    ue = [sb("ue%d" % i, [128, 514], F32, s4) for i in range(2)]
                    tcv = sb("tcv", [128, 512], F32, s4)
                    gcv = sb("gcv", [128, 4, 512], BF16, s4)
                    ucp = sb("ucp", [128, 4, 2], F32, s4)
                    ucs = sb("ucs", [128, 4, 32], F32, s4)
                    cps = sb("cps", [2, 512], F32, s4)
                    css = sb("css", [32, 512], F32, s4)
                    sga = sb("sga", [128, 512], F32, s4)
                    sgb = sb("sgb", [128, 512], F32, s4)
                    za = sga
                    zb = sgb
                    zT = sb("zT", [128, 8, 512], BF16, s4)
                    xr = [sb("xr%d" % i, [128, 1024], F32, s4) for i in range(2)]
                    st6 = sb("st6", [128, 2, 6], F32, s4)
                    mv = sb("mv", [128, 2], F32, s4)
                    rstd = sb("rstd", [128, 1], F32, s4)
                    y0t = [sb("y0t%d" % i, [128, 1024], F32, s4) for i in range(1)]
                    x1Tb = [sb("x1Tb%d" % i, [128, 8, 128], BF16, s4) for i in range(2)]
                    x1Tf = sb("x1Tf", [128, 8, 128], F32, s4)
                    lg = sb("lg", [128, 20], F32, s4)
                    g8 = sb("g8", [128, 8], F32, s4)
                    m8 = sb("m8", [128, 8], F32, s4)
                    e8 = sb("e8", [128, 8], F32, s4)
                    me8 = sb("me8", [128, 8], F32, s4)
                    ohg = sb("ohg", [128, 4], F32, s4)
                    nm = sb("nm", [128, 2], F32, s4)
                    eg = sb("eg", [128, 4], F32, s4)
                    sgs = sb("sgs", [128, 2], F32, s4)
                    sel = sb("sel", [128, 16], F32, s4)
                    e1 = sb("e1", [128, 4], F32, s4)
                    s2m = sb("s2m", [128, 4], F32, s4)
                    wfac = sb("wfac", [128, 1], F32, s4)
                    P.op("pool", lambda e: e.memset(g8[:], -1e30), w=["g8"])
                    P.op("pool", lambda e: e.memset(e8[:], -1e30), w=["e8"])

                    P.dma("sp", xh[:], x_halo, w=["xh"], sem="xh")
                    ph, phk = ps()
                    P.op("pe", lambda e: [e.transpose(ph[:, k * 8:(k + 1) * 8], xh[0:8, k * 128:(k + 1) * 128], ident[0:8, 0:8]) for k in range(8)][-1],
                         r=["xh", "ident"], w=[phk])
                    evac(xhT[:].rearrange("p k t -> p (k t)"), ph[:, 0:64], r=[phk], w=["xhT"])
                    for c in range(4):
                        p1, p1k = ps()
                        mmg(p1[:, 0:8], [(wcv[:, k, C_XC + c * 128:C_XC + (c + 1) * 128], xhT[:, k, :]) for k in range(8)], r=["xhT", "wcv"], w=[p1k])
                        p2, p2k = ps()
                        mmg(p2[:, 0:8], [(wcv[:, k, C_CG + c * 128:C_CG + (c + 1) * 128], xhT[:, k, :]) for k in range(8)], r=["xhT", "wcv"], w=[p2k])
                        P.op("act", lambda e, p1=p1: e.copy(out=xcs[:, 0:8], in_=p1[:, 0:8]), r=[p1k], w=["xcs"])
                        P.op("dve", lambda e, p2=p2, c=c: e.tensor_tensor(out=uh[:, c, :], in0=xcs[:, 0:8], in1=p2[:, 0:8], op=ALU.mult), r=[p2k, "xcs"], w=["uh"])
                    P.dma("sp", sct[:], state_conv, w=["sct"], sem="sct")
                    pst, pstk = ps()
                    P.op("pe", lambda e: [e.transpose(pst[:, c * 32:(c + 1) * 32], sct[0:32, c * 128:(c + 1) * 128], ident[0:32, 0:32]) for c in range(4)][-1],
                         r=["sct", "ident"], w=[pstk])
                    evac(stT[:].rearrange("p c t -> p (c t)"), pst[:, 0:128], r=[pstk], w=["stT"])

                    tiles = [(x_own[s_ * 512:(s_ + 1) * 512, :], 512, s_ * 512, s_ * 4, s_) for s_ in range(4)] + [(x_s, 64, 2048, 16, 4)]
                    blk_n = 0
                    for (src, W, t0, gb0, sl) in tiles:
                        nb = (W + 127) // 128
                        pw = min(W, 128)
                        samp = (sl == 4)
                        xtall = load_xT(src, W, xin, xT, "xin", "xT")
                        for c in range(4):
                            u_ = ue[c % 2]
                            uk = "ue%d" % (c % 2)
                            p1, p1k = ps()
                            mmg(p1[:, 0:W], [(wcv[:, k, C_XC + c * 128:C_XC + (c + 1) * 128], xT[:, k, 0:W]) for k in range(8)], r=xtall + ["wcv"], w=[p1k])
                            p2, p2k = ps()
                            mmg(p2[:, 0:W], [(wcv[:, k, C_CG + c * 128:C_CG + (c + 1) * 128], xT[:, k, 0:W]) for k in range(8)], r=xtall + ["wcv"], w=[p2k])
                            p3, p3k = ps()
                            mmg(p3[:, 0:W], [(wcv[:, k, C_BG + c * 128:C_BG + (c + 1) * 128], xT[:, k, 0:W]) for k in range(8)], r=xtall + ["wcv"], w=[p3k])
                            P.op("act", lambda e, p1=p1: e.copy(out=xcs[:, 0:W], in_=p1[:, 0:W]), r=[p1k], w=["xcs"])
                            if not samp:
                                P.op("pool", lambda e, u_=u_, c=c: e.tensor_copy(out=u_[:, 0:2], in_=uh[:, c, 2 * sl:2 * sl + 2]), r=["uh"], w=[uk])
                                P.op("dve", lambda e, u_=u_, p2=p2: e.tensor_tensor(out=u_[:, 2:W + 2], in0=xcs[:, 0:W], in1=p2[:, 0:W], op=ALU.mult),
                                     r=[p2k, "xcs", uk], w=[uk])
                                uv = [u_[:, j:j + W] for j in range(3)]
                                tv = tcv[:, 0:W]
                            else:
                                u3 = u_[:, 0:96].rearrange("p (b i) -> p b i", i=6)
                                P.op("pool", lambda e, u3=u3, c=c: e.tensor_copy(out=u3[:, :, 0:2], in_=stT[:, c, :].rearrange("p (b i) -> p b i", i=2)),
                                     r=["stT"], w=[uk])
                                P.op("dve", lambda e, u3=u3, p2=p2: e.tensor_tensor(out=u3[:, :, 2:6], in0=xcs[:, 0:64].rearrange("p (b j) -> p b j", j=4),
                                                                                     in1=p2[:, 0:64].rearrange("p (b j) -> p b j", j=4), op=ALU.mult),
                                     r=[p2k, "xcs", uk], w=[uk])
                                uv = [u3[:, :, j:j + 4] for j in range(3)]
                                tv = tcv[:, 0:64].rearrange("p (b j) -> p b j", j=4)
                            P.op("pool", lambda e, uv=uv, tv=tv, c=c: e.tensor_scalar(out=tv, in0=uv[0], scalar1=cw[:, c, 0:1], scalar2=None, op0=ALU.mult),
                                 r=[uk] + ["cw%d" % i for i in range(4)], w=["tcv"])
                            for j in (1, 2):
                                P.op("dve", lambda e, uv=uv, tv=tv, c=c, j=j: e.scalar_tensor_tensor(out=tv, in0=uv[j], scalar=cw[:, c, j:j + 1], in1=tv,
                                                                                                     op0=ALU.mult, op1=ALU.add), r=[uk, "tcv"], w=["tcv"])
                            P.op("dve", lambda e, p3=p3, c=c: e.tensor_tensor(out=gcv[:, c, 0:W], in0=tcv[:, 0:W], in1=p3[:, 0:W], op=ALU.mult),
                                 r=[p3k, "tcv"], w=["gcv%d" % c])
                            if sl == 3:
                                P.op("pool", lambda e, u_=u_, c=c: e.tensor_copy(out=ucp[:, c, :], in_=u_[:, W:W + 2]), r=[uk], w=["ucp"])
                            if samp:
                                P.op("pool", lambda e, u3=u3, c=c: e.tensor_copy(out=ucs[:, c, :].rearrange("p (b i) -> p b i", i=2), in_=u3[:, :, 4:6]),
                                     r=[uk], w=["ucs"])
                        if sl == 3:
                            pcp, pcpk = ps()
                            P.op("pe", lambda e, pcp=pcp: [e.transpose(pcp[0:2, c * 128:(c + 1) * 128], ucp[:, c, :], ident[:]) for c in range(4)][-1],
                                 r=["ucp", "ident"], w=[pcpk])
                            evac(cps[:], pcp[0:2, :], r=[pcpk], w=["cps"])
                            P.dma("sp", conv_p, cps[:], r=["cps"], sem="cps")
                        if samp:
                            pcp, pcpk = ps()
                            P.op("pe", lambda e, pcp=pcp: [e.transpose(pcp[0:32, c * 128:(c + 1) * 128], ucs[:, c, :], ident[:]) for c in range(4)][-1],
                                 r=["ucs", "ident"], w=[pcpk])
                            evac(css[:], pcp[0:32, :], r=[pcpk], w=["css"])
                            P.dma("sp", conv_s, css[:], r=["css"], sem="css")
                        gall = ["gcv%d" % c for c in range(4)]
                        for m in range(8):
                            ms = slice(m * 128, (m + 1) * 128)
                            pya, pyak = ps()
                            mmg(pya[:, 0:W], [(wco[:, c, ms], gcv[:, c, 0:W]) for c in range(4)], r=gall + ["wco"], w=[pyak])
                            pga, pgak = ps()
                            mmg(pga[:, 0:W], [(wg[:, k, ms], xT[:, k, 0:W]) for k in range(8)], r=xtall + ["wg"], w=[pgak])
                            pyb, pybk = ps()
                            mmg(pyb[:, 0:W], [(wao[:, c, ms], oT[:, c, t0:t0 + W]) for c in range(4)], r=["wao"], w=[pybk])
                            pgb, pgbk = ps()
                            mmg(pgb[:, 0:W], [(wg[:, k, 1024 + m * 128:1024 + (m + 1) * 128], xT[:, k, 0:W]) for k in range(8)], r=xtall + ["wg"], w=[pgbk])
                            P.op("act", lambda e, pga=pga: e.activation(out=sga[:, 0:W], in_=pga[:, 0:W], func=AF.Sigmoid), r=[pgak], w=["sga"])
                            P.op("act", lambda e, pgb=pgb: e.activation(out=sgb[:, 0:W], in_=pgb[:, 0:W], func=AF.Sigmoid), r=[pgbk], w=["sgb"])
                            P.op("dve", lambda e, pya=pya: e.tensor_tensor(out=za[:, 0:W], in0=sga[:, 0:W], in1=pya[:, 0:W], op=ALU.mult), r=[pyak, "sga"], w=["sga"])
                            P.op("dve", lambda e, pyb=pyb: e.tensor_tensor(out=zb[:, 0:W], in0=sgb[:, 0:W], in1=pyb[:, 0:W], op=ALU.mult), r=[pybk, "sgb"], w=["sgb"])
                            P.op("pool", lambda e, m=m: e.tensor_tensor(out=zT[:, m, 0:W], in0=za[:, 0:W], in1=zb[:, 0:W], op=ALU.add), r=["sga", "sgb"], w=["zT%d" % m])
                        zall = ["zT%d" % m for m in range(8)]
                        for b in range(nb):
                            gb = gb0 + b
                            xr_ = xr[blk_n % 2]
                            xrk = "xr%d" % (blk_n % 2)
                            y0_ = y0t[0]
                            y0k = "y0t0"
                            xb_ = x1Tb[blk_n % 2]
                            xbk = "x1Tb%d" % (blk_n % 2)
                            blk_n += 1
                            tsl = slice(b * 128, b * 128 + pw)
                            for half in range(2):
                                hs = slice(half * 512, (half + 1) * 512)
                                pm, pmk = ps()
                                mmg(pm[0:pw, :], [(zT[:, m, tsl], wo[:, m, hs]) for m in range(8)], r=zall + ["wo"], w=[pmk])
                                P.op("dve", lambda e, pm=pm, hs=hs, xr_=xr_, b=b: e.scalar_tensor_tensor(out=xr_[0:pw, hs], in0=xin[0:pw, b, hs], scalar=ALPHA, in1=pm[0:pw, :],
                                                                                                        op0=ALU.mult, op1=ALU.add), r=[pmk, "xin"], w=[xrk])
                            xrh = [xrk]
                            for half in range(2):
                                P.op("dve", lambda e, half=half, xr_=xr_: e.bn_stats(out=st6[0:pw, half, :], in_=xr_[0:pw, half * 512:(half + 1) * 512]), r=xrh, w=["st6_%d" % half])
                            P.op("dve", lambda e: e.bn_aggr(out=mv[0:pw, :], in_=st6[0:pw].rearrange("p a s -> p (a s)")), r=["st6_0", "st6_1"], w=["mv"])
                            P.op("dve", lambda e: e.tensor_scalar(out=rstd[0:pw, :], in0=mv[0:pw, 1:2], scalar1=EPS, scalar2=None, op0=ALU.add), r=["mv"], w=["rstd"])
                            P.op("act", lambda e: e.sqrt(out=rstd[0:pw, :], in_=rstd[0:pw, :]), r=["rstd"], w=["rstd"])
                            P.op("dve", lambda e: e.reciprocal(out=rstd[0:pw, :], in_=rstd[0:pw, :]), r=["rstd"], w=["rstd"])
                            P.op("dve", lambda e, xr_=xr_: e.tensor_scalar(out=xr_[0:pw, :], in0=xr_[0:pw, :], scalar1=mv[0:pw, 0:1], scalar2=rstd[0:pw, 0:1],
                                                                           op0=ALU.subtract, op1=ALU.mult), r=xrh + ["mv", "rstd"], w=[xrk])
                            P.op("pool", lambda e, xr_=xr_: e.tensor_tensor(out=xr_[0:pw, :], in0=xr_[0:pw, :], in1=g1b[0:pw, :], op=ALU.mult), r=[xrk, "g1b"], w=[xrk])
                            P.op("pool", lambda e, xr_=xr_: e.tensor_tensor(out=xr_[0:pw, :], in0=xr_[0:pw, :], in1=b1b[0:pw, :], op=ALU.add), r=[xrk, "b1b"], w=[xrk])
                            P.op("act", lambda e, xr_=xr_, y0_=y0_: e.mul(out=y0_[0:pw, :], in_=xr_[0:pw, :], mul=ALPHA), r=[xrk], w=[y0k])
                            P.dma("sp", y0_d[t0 + b * 128:t0 + b * 128 + pw, :], y0_[0:pw, :], r=[y0k], sem=y0k)
                            for half in range(2):
                                ptt, pttk = ps()
                                P.op("pe", lambda e, ptt=ptt, half=half, xr_=xr_: [e.transpose(ptt[:, kk * 128:kk * 128 + pw], xr_[0:pw, (half * 4 + kk) * 128:(half * 4 + kk + 1) * 128],
                                                                                               ident[0:pw, 0:pw]) for kk in range(4)][-1], r=[xrk, "ident"], w=[pttk])
                                pv4 = ptt[:].rearrange("p (k t) -> p k t", k=4)[:, :, 0:pw]
                                P.op("act", lambda e, pv4=pv4, half=half, xb_=xb_: e.copy(out=xb_[:, half * 4:half * 4 + 4, 0:pw], in_=pv4), r=[pttk], w=[xbk + "_%d" % half])
                                P.op("dve", lambda e, pv4=pv4, half=half: e.tensor_copy(out=x1Tf[:, half * 4:half * 4 + 4, 0:pw], in_=pv4), r=[pttk, xbk + "_%d" % half], w=["x1Tf_%d" % half])
                            P.dma("sp", x1T_d[:, :, t0 + b * 128:t0 + b * 128 + pw], xb_[:, :, 0:pw], r=[xbk + "_0", xbk + "_1"], sem=xbk)
                            prt, prtk = ps()
                            mmg(prt[0:pw, 0:20], [(x1Tf[:, k, 0:pw], wgr[:, k, :]) for k in range(8)], r=["x1Tf_0", "x1Tf_1", "wgr"], w=[prtk])
                            R = slice(0, pw)
                            P.op("dve", lambda e, prt=prt: e.tensor_copy(out=lg[R, :], in_=prt[R, 0:20]), r=[prtk], w=["lg"])
                            P.op("dve", lambda e: e.tensor_copy(out=g8[R, 0:4], in_=lg[R, 0:4]), r=["lg"], w=["g8"])
                            P.op("dve", lambda e: e.max(out=m8[R, :], in_=g8[R, :]), r=["g8"], w=["m8"])
                            P.op("dve", lambda e: e.tensor_scalar(out=ohg[R, :], in0=lg[R, 0:4], scalar1=m8[R, 0:1], scalar2=None, op0=ALU.is_equal), r=["lg", "m8"], w=["ohg"])
                            P.op("dve", lambda e: e.tensor_scalar(out=nm[R, 0:1], in0=m8[R, 0:1], scalar1=-1.0, scalar2=None, op0=ALU.mult), r=["m8"], w=["nm0"])
                            P.op("act", lambda e: e.activation(out=eg[R, :], in_=lg[R, 0:4], func=AF.Exp, bias=nm[R, 0:1], scale=1.0), r=["lg", "nm0"], w=["eg"])
                            P.op("dve", lambda e: e.reduce_sum(out=sgs[R, 0:1], in_=eg[R, :], axis=AX.X), r=["eg"], w=["sgs0"])
                            lg3 = lg[R, 4:20].rearrange("p (g x) -> p g x", x=4)
                            P.op("dve", lambda e, lg3=lg3: e.tensor_tensor(out=sel[R, :].rearrange("p (g x) -> p g x", x=4), in0=lg3,
                                                                           in1=ohg[R, :].unsqueeze(2).to_broadcast([pw, 4, 4]), op=ALU.mult), r=["lg", "ohg"], w=["sel"])
                            P.op("dve", lambda e: e.tensor_reduce(out=e8[R, 0:4], in_=sel[R, :].rearrange("p (g x) -> p x g", x=4), axis=AX.X, op=ALU.add), r=["sel"], w=["e8"])
                            P.op("dve", lambda e: e.max(out=me8[R, :], in_=e8[R, :]), r=["e8"], w=["me8"])
                            P.op("dve", lambda e: e.tensor_scalar(out=nm[R, 1:2], in0=me8[R, 0:1], scalar1=-1.0, scalar2=None, op0=ALU.mult), r=["me8"], w=["nm1"])
                            P.op("act", lambda e: e.activation(out=e1[R, :], in_=e8[R, 0:4], func=AF.Exp, bias=nm[R, 1:2], scale=1.0), r=["e8", "nm1"], w=["e1"])
                            P.op("dve", lambda e: e.tensor_scalar(out=s2m[R, :], in0=e8[R, 0:4], scalar1=me8[R, 1:2], scalar2=None, op0=ALU.is_ge), r=["e8", "me8"], w=["s2m"])
                            P.op("dve", lambda e: e.tensor_tensor(out=e1[R, :], in0=e1[R, :], in1=s2m[R, :], op=ALU.mult), r=["e1", "s2m"], w=["e1"])
                            P.op("dve", lambda e: e.reduce_sum(out=sgs[R, 1:2], in_=e1[R, :], axis=AX.X), r=["e1"], w=["sgs1"])
                            P.op("dve", lambda e: e.tensor_tensor(out=wfac[R, :], in0=sgs[R, 0:1], in1=sgs[R, 1:2], op=ALU.mult), r=["sgs0", "sgs1"], w=["wfac"])
                            P.op("dve", lambda e: e.reciprocal(out=wfac[R, :], in_=wfac[R, :]), r=["wfac"], w=["wfac"])
                            P.op("dve", lambda e: e.tensor_scalar(out=e1[R, :], in0=e1[R, :], scalar1=wfac[R, 0:1], scalar2=None, op0=ALU.mult), r=["e1", "wfac"], w=["e1"])
                            P.op("dve", lambda e, gb=gb: e.tensor_tensor(out=gate[R, gb, :].rearrange("p (g x) -> p g x", x=4),
                                                                         in0=ohg[R, :].unsqueeze(2).to_broadcast([pw, 4, 4]),
                                                                         in1=e1[R, :].unsqueeze(1).to_broadcast([pw, 4, 4]), op=ALU.mult), r=["ohg", "e1", "gate"], w=["gate%d" % gb])
                    dump("gate", gate[:], [128, 17, 16], r=["gate%d" % i for i in range(17)])
                    P.barrier()
            s_oT.close()

            if stage >= 5:
                with ExitStack() as s5:
                    x1T = sb("x1T", [128, 8, NTOK], BF16, s5)
                    yacc = sb("yacc", [128, 17, 1024], F32, s5)
                    g2b = sb("g2b", [128, 1024], F32, s5)
                    b2b = sb("b2b", [128, 1024], F32, s5)
                    w1e = [sb("w1e%d" % i, [128, 8, 512], BF16, s5) for i in range(2)]
                    w3e = [sb("w3e%d" % i, [128, 8, 512], BF16, s5) for i in range(2)]
                    w2e = [sb("w2e%d" % i, [128, 4, 1024], BF16, s5) for i in range(2)]
                    s1t = [sb("s1t%d" % i, [128, 512], F32, s5) for i in range(2)]
                    hT = [sb("hT%d" % i, [128, 4, 512], BF16, s5) for i in range(2)]
                    st6 = sb("st6b", [128, 2, 6], F32, s5)
                    mv = sb("mvb", [128, 2], F32, s5)
                    rstd = sb("rstdb", [128, 1], F32, s5)
                    P.dma("sp", x1T[:], x1T_d, w=["x1T"], sem="x1T")
                    P.dma("sp", yacc[:, 0:16, :], y0_d[0:2048, :].rearrange("(b p) d -> p b d", p=128), w=["yacc"], sem="yacc")
                    P.dma("sp", yacc[0:64, 16, :], y0_d[2048:2112, :], w=["yacc"], sem="yacc")
                    P.dma("sp", g2b[:], ln2_g.partition_broadcast(128), w=["g2b"], sem="g2b")
                    P.dma("sp", b2b[:], ln2_b.partition_broadcast(128), w=["b2b"], sem="b2b")

                    wst[:] = [sb("wstC%d" % i, [128, 2048], F32, s5) for i in range(2)]

                    def load_e(e_):
                        i = e_ % 2
                        return (wchunks(w1e[i], "w1e%d" % i, w1[e_], 8, 512, CH=2048) + wchunks(w3e[i], "w3e%d" % i, w3[e_], 8, 512, CH=2048)
                                + wchunks(w2e[i], "w2e%d" % i, w2[e_], 4, 1024, CH=2048))
                    for f_ in load_e(0):
                        f_()
                    ttiles = [(0, 512), (512, 512), (1024, 512), (1536, 512), (2048, 64)]
                    it = 0
                    for e_ in range(16):
                        pend = load_e(e_ + 1) if e_ + 1 < 16 else []
                        i = e_ % 2
                        a1, a3, a2 = w1e[i], w3e[i], w2e[i]
                        k1, k3, k2 = "w1e%d" % i, "w3e%d" % i, "w2e%d" % i
                        for (t0, W) in ttiles:
                            nb = (W + 127) // 128
                            pw = min(W, 128)
                            h_ = hT[it % 2]
                            hk = "hT%d" % (it % 2)
                            it += 1
                            for f in range(4):
                                fs = slice(f * 128, (f + 1) * 128)
                                p1, p1k = ps()
                                mmg(p1[:, 0:W], [(a1[:, k, fs], x1T[:, k, t0:t0 + W]) for k in range(8)], r=["x1T", k1], w=[p1k])
                                p3, p3k = ps()
                                mmg(p3[:, 0:W], [(a3[:, k, fs], x1T[:, k, t0:t0 + W]) for k in range(8)], r=["x1T", k3], w=[p3k])
                                s_ = s1t[f % 2]
                                sk = "s1t%d" % (f % 2)
                                P.op("act", lambda e, p1=p1, s_=s_: e.activation(out=s_[:, 0:W], in_=p1[:, 0:W], func=AF.Silu), r=[p1k], w=[sk])
                                P.op("dve", lambda e, p3=p3, s_=s_, f=f, h_=h_: e.tensor_tensor(out=h_[:, f, 0:W], in0=s_[:, 0:W], in1=p3[:, 0:W], op=ALU.mult),
                                     r=[p3k, sk], w=[hk + "_%d" % f])
                            for f_ in pend[:2 if t0 == 0 else 1]:
                                f_()
                            pend = pend[2 if t0 == 0 else 1:]
                            hall = [hk + "_%d" % f for f in range(4)]
                            for b in range(nb):
                                gb = t0 // 128 + b
                                for half in range(2):
                                    hs = slice(half * 512, (half + 1) * 512)
                                    py, pyk = ps()
                                    mmg(py[0:pw, :], [(h_[:, f, b * 128:b * 128 + pw], a2[:, f, hs]) for f in range(4)], r=hall + [k2], w=[pyk])
                                    P.op("dve", lambda e, py=py, gb=gb, hs=hs: e.scalar_tensor_tensor(out=yacc[0:pw, gb, hs], in0=py[0:pw, :], scalar=gate[0:pw, gb, e_:e_ + 1],
                                                                                                     in1=yacc[0:pw, gb, hs], op0=ALU.mult, op1=ALU.add),
                                         r=[pyk, "yacc"], w=["yacc%d_%d" % (gb, half)])
                    for gb in range(17):
                        pw = 128 if gb < 16 else 64
                        yk = ["yacc%d_0" % gb, "yacc%d_1" % gb]
                        ya = yacc[0:pw, gb, :]
                        for half in range(2):
                            P.op("dve", lambda e, half=half, gb=gb, pw=pw: e.bn_stats(out=st6[0:pw, half, :], in_=yacc[0:pw, gb, half * 512:(half + 1) * 512]), r=yk, w=["st6_%d" % half])
                        P.op("dve", lambda e, pw=pw: e.bn_aggr(out=mv[0:pw, :], in_=st6[0:pw].rearrange("p a s -> p (a s)")), r=["st6_0", "st6_1"], w=["mv"])
                        P.op("dve", lambda e, pw=pw: e.tensor_scalar(out=rstd[0:pw, :], in0=mv[0:pw, 1:2], scalar1=EPS, scalar2=None, op0=ALU.add), r=["mv"], w=["rstd"])
                        P.op("act", lambda e, pw=pw: e.sqrt(out=rstd[0:pw, :], in_=rstd[0:pw, :]), r=["rstd"], w=["rstd"])
                        P.op("dve", lambda e, pw=pw: e.reciprocal(out=rstd[0:pw, :], in_=rstd[0:pw, :]), r=["rstd"], w=["rstd"])
                        P.op("dve", lambda e, ya=ya, pw=pw: e.tensor_scalar(out=ya, in0=ya, scalar1=mv[0:pw, 0:1], scalar2=rstd[0:pw, 0:1],
                                                                           op0=ALU.subtract, op1=ALU.mult), r=yk + ["mv", "rstd"], w=["yo%d" % gb])
                        P.op("pool", lambda e, ya=ya, pw=pw: e.tensor_tensor(out=ya, in0=ya, in1=g2b[0:pw, :], op=ALU.mult), r=["yo%d" % gb, "g2b"], w=["yo%d" % gb])
                        P.op("pool", lambda e, ya=ya, pw=pw: e.tensor_tensor(out=ya, in0=ya, in1=b2b[0:pw, :], op=ALU.add), r=["yo%d" % gb, "b2b"], w=["yo%d" % gb])
                        if gb < 16:
                            P.dma("sp", y_own[gb * 128:(gb + 1) * 128, :], ya, r=["yo%d" % gb], sem="yo%d" % (gb % 4))
                        else:
                            P.dma("sp", y_s, ya, r=["yo%d" % gb], sem="yo%d" % (gb % 4))
                    P.barrier()
        except StopBuild:
            pass
        P.finish()
    nc._dbg_out = list(dbg_out.keys())
    return nc


def make_in_maps(inputs, cores):
    f32 = np.float32
    xp = np.asarray(inputs["x_prompt"], f32)
    xs = np.asarray(inputs["x_sample"], f32)
    ck = np.asarray(inputs["cache_k"], f32)
    npool = ck.shape[1]
    ck = ck.reshape(npool * 128, 512)
    cv = np.asarray(inputs["cache_v"], f32).reshape(npool * 128, 512)
    cf = np.asarray(inputs["cache_logf"], f32).reshape(npool * 128, 8)
    sc = np.asarray(inputs["state_conv"], f32)[0]
    pt = np.asarray(inputs["page_table"], np.int32)
    g = lambda k: np.asarray(inputs[k], f32)
    common = {
        "cache_k": ck, "cache_v": cv, "cache_f": cf,
        "w_in": g("w_in")[0], "b_f": g("b_f").reshape(1, 8),
        "conv_w": g("conv_w")[0], "w_conv_out": g("w_conv_out")[0],
        "w_attn_out": g("w_attn_out")[0], "w_o": g("w_o")[0],
        "ln1_g": g("ln1_g").reshape(1, D), "ln1_b": g("ln1_b").reshape(1, D),
        "w_gr": np.ascontiguousarray(np.concatenate([g("w_group")[0], g("w_router")[0]], axis=1)),
        "w1": g("w1")[0], "w3": g("w3")[0], "w2": g("w2")[0],
        "ln2_g": g("ln2_g").reshape(1, D), "ln2_b": g("ln2_b").reshape(1, D),
    }
    in_maps = []
    for c in cores:
        s, r = c // 2, c % 2
        own, oth = OWN[r], OWN[1 - r]
        xt = xp[s].reshape(8, 512, D)
        halo = np.zeros((8, D), f32)
        for sl, T in enumerate(own):
            if T > 0:
                halo[2 * sl:2 * sl + 2] = xp[s, 512 * T - 2:512 * T]
        rcv = np.zeros((1, 80), f32)
        order = own + oth
        for p_ in range(8):
            for p2 in range(8):
                rcv[0, p_ * 8 + p2] = 1.0 if order[p_] < order[p2] else 0.0
        for sl in range(4):
            rcv[0, 64 + sl] = 0.0 if oth[sl] < own[sl] else NEG
        m = dict(common)
        m.update({
            "x_own": np.ascontiguousarray(xt[own].reshape(2048, D)),
            "x_oth": np.ascontiguousarray(xt[oth].reshape(2048, D)),
            "x_halo": halo,
            "x_s": np.ascontiguousarray(xs[16 * c:16 * c + 16].reshape(64, D)),
            "state_conv": np.ascontiguousarray(sc[16 * c:16 * c + 16].reshape(32, 512)),
            "page_table": np.ascontiguousarray(pt[16 * c:16 * c + 16].reshape(1, 256)),
            "rolec": rcv,
        })
        in_maps.append(m)
    return in_maps, npool


def assemble(res, cores, nseq=4, nsamp=128):
    f32 = np.float32
    y_p = np.zeros((nseq, 4096, D), f32)
    k_p = np.zeros((1, nseq, 4096, 8, 64), f32)
    v_p = np.zeros((1, nseq, 4096, 8, 64), f32)
    f_p = np.zeros((1, nseq, 4096, 8), f32)
    c_p = np.zeros((1, nseq, 2, 512), f32)
    y_sm = np.zeros((nsamp, 4, D), f32)
    k_sm = np.zeros((1, nsamp, 4, 8, 64), f32)
    v_sm = np.zeros((1, nsamp, 4, 8, 64), f32)
    f_sm = np.zeros((1, nsamp, 4, 8), f32)
    c_sm = np.zeros((1, nsamp, 2, 512), f32)
    for o, c in zip(res, cores):
        s, r = c // 2, c % 2
        for sl, T in enumerate(OWN[r]):
            rows = slice(512 * T, 512 * T + 512)
            y_p[s, rows] = o["y_own"][sl * 512:(sl + 1) * 512]
            k_p[0, s, rows] = o["k_own"][sl * 512:(sl + 1) * 512].reshape(512, 8, 64)
            v_p[0, s, rows] = o["v_own"][sl * 512:(sl + 1) * 512].reshape(512, 8, 64)
            f_p[0, s, rows] = o["f_own"][sl * 512:(sl + 1) * 512]
        if r == 0:
            c_p[0, s] = o["conv_p"]
        sl_ = slice(16 * c, 16 * c + 16)
        y_sm[sl_] = o["y_s"].reshape(16, 4, D)
        k_sm[0, sl_] = o["k_s"].reshape(16, 4, 8, 64)
        v_sm[0, sl_] = o["v_s"].reshape(16, 4, 8, 64)
        f_sm[0, sl_] = o["f_s"].reshape(16, 4, 8)
        c_sm[0, sl_] = o["conv_s"].reshape(16, 2, 512)
    return (y_p, y_sm, k_p, v_p, f_p, c_p, k_sm, v_sm, f_sm, c_sm)


_NC = {}


def kernel(**inputs):
    cores = list(range(8))
    in_maps, npool = make_in_maps(inputs, cores)
    if npool not in _NC:
        _NC[npool] = build_nc(npool)
    res = run_bass_kernel_spmd(_NC[npool], in_maps, core_ids=cores).results
    return assemble(res, cores)
```

```python
import os
import numpy as np
from contextlib import ExitStack
import concourse.bass as bass
import concourse.mybir as mybir
from concourse.bass_utils import run_bass_kernel_spmd

F32 = mybir.dt.float32
BF16 = mybir.dt.bfloat16
I32 = mybir.dt.int32
AF = mybir.ActivationFunctionType
ALU = mybir.AluOpType
AX = mybir.AxisListType

D = 1024
NIN = 5128
OWN = [[0, 3, 4, 7], [1, 2, 5, 6]]
ALPHA = 2.0 ** 0.25
EPS = 1e-5
NEG = -30000.0
C_XC, C_BG, C_CG, C_Q, C_K, C_V, C_F, C_GA, C_GB = 0, 512, 1024, 1536, 2048, 2560, 3072, 3080, 4104
NTOK = 2112


class StopBuild(Exception):
    pass


class Prog:
    def __init__(self, nc, es):
        self.nc = nc
        self.es = es
        self.eng = {"pe": nc.tensor, "act": nc.scalar, "dve": nc.vector, "pool": nc.gpsimd, "sp": nc.sync}
        self.sem = {k: es.enter_context(nc.semaphore("s_" + k)) for k in self.eng}
        self.cnt = {k: 0 for k in self.eng}
        self.waited = {k: {} for k in self.eng}
        self.last_w = {}
        self.readers = {}
        self.dsem = {}
        self.dcnt = {}
        self.dead = False

    def _deps(self, r, w):
        deps = []
        for k in r:
            if k in self.last_w:
                deps.append(self.last_w[k])
        for k in w:
            if k in self.last_w:
                deps.append(self.last_w[k])
            deps.extend(self.readers.get(k, ()))
        return deps

    def _wait(self, eng, deps):
        best = {}
        for (s, v) in deps:
            if eng == "pe" and s == "pe":
                continue
            if v > best.get(s, 0):
                best[s] = v
        for s, v in best.items():
            if self.waited[eng].get(s, 0) < v:
                semh = self.sem[s] if s in self.sem else self.dsem[s]
                self.eng[eng].wait_ge(semh, v)
                self.waited[eng][s] = v

    def _record(self, tok, r, w):
        for k in w:
            self.last_w[k] = tok
            self.readers[k] = []
        for k in r:
            if k not in w:
                self.readers.setdefault(k, []).append(tok)

    def op(self, eng, fn, r=(), w=()):
        if self.dead:
            return
        self._wait(eng, self._deps(r, w))
        ins = fn(self.eng[eng])
        self.cnt[eng] += 1
        ins.then_inc(self.sem[eng], 1)
        self._record((eng, self.cnt[eng]), r, w)

    def _dsem(self, sem):
        if sem not in self.dsem:
            self.dsem[sem] = self.es.enter_context(self.nc.semaphore("d_" + sem))
            self.dcnt[sem] = 0

    def dma(self, q, out, in_, r=(), w=(), sem=None, **kw):
        if self.dead:
            return
        self._dsem(sem)
        self._wait(q, self._deps(r, w))
        ins = self.eng[q].dma_start(out=out, in_=in_, **kw)
        self.dcnt[sem] += 16
        ins.then_inc(self.dsem[sem], 16)
        self._record((sem, self.dcnt[sem]), r, w)

    def idma(self, out, in_, idx, r=(), w=(), sem=None):
        if self.dead:
            return
        self._dsem(sem)
        self._wait("pool", self._deps(r, w))
        ins = self.nc.gpsimd.indirect_dma_start(
            out=out, out_offset=None, in_=in_, in_offset=bass.IndirectOffsetOnAxis(ap=idx, axis=0))
        self.dcnt[sem] += 16
        ins.then_inc(self.dsem[sem], 16)
        self._record((sem, self.dcnt[sem]), r, w)

    def _all(self):
        toks = [(k, self.cnt[k]) for k in self.eng if self.cnt[k] > 0]
        toks += [(k, self.dcnt[k]) for k in self.dsem if self.dcnt[k] > 0]
        return toks

    def barrier(self):
        if self.dead:
            return
        toks = self._all()
        for e in self.eng:
            self._wait(e, toks)
        self.last_w = {}
        self.readers = {}

    def finish(self):
        self._wait("sp", self._all())


def build_nc(npool=2560, stage=99, dbg=False):
    nc = bass.Bass("TRN2", target_bir_lowering=False)

    def din(name, shape, dt=F32):
        return nc.dram_tensor(name, list(shape), dt, kind="ExternalInput").ap()

    def dout(name, shape, dt=F32):
        return nc.dram_tensor(name, list(shape), dt, kind="ExternalOutput").ap()

    x_own = din("x_own", [2048, D])
    x_oth = din("x_oth", [2048, D])
    x_halo = din("x_halo", [8, D])
    x_s = din("x_s", [64, D])
    cache_k = din("cache_k", [npool * 128, 512])
    cache_v = din("cache_v", [npool * 128, 512])
    cache_f = din("cache_f", [npool * 128, 8])
    state_conv = din("state_conv", [32, 512])
    page_table = din("page_table", [1, 256], I32)
    rolec = din("rolec", [1, 80])
    w_in = din("w_in", [D, NIN])
    b_f = din("b_f", [1, 8])
    conv_w = din("conv_w", [3, 512])
    w_conv_out = din("w_conv_out", [512, D])
    w_attn_out = din("w_attn_out", [512, D])
    w_o = din("w_o", [D, D])
    ln1_g = din("ln1_g", [1, D])
    ln1_b = din("ln1_b", [1, D])
    w_gr = din("w_gr", [D, 20])
    w1 = din("w1", [16, D, 512])
    w3 = din("w3", [16, D, 512])
    w2 = din("w2", [16, 512, D])
    ln2_g = din("ln2_g", [1, D])
    ln2_b = din("ln2_b", [1, D])

    y_own = dout("y_own", [2048, D])
    y_s = dout("y_s", [64, D])
    k_own = dout("k_own", [2048, 512])
    v_own = dout("v_own", [2048, 512])
    f_own = dout("f_own", [2048, 8])
    conv_p = dout("conv_p", [2, 512])
    k_s = dout("k_s", [64, 512])
    v_s = dout("v_s", [64, 512])
    f_s = dout("f_s", [64, 8])
    conv_s = dout("conv_s", [32, 512])
    y0_d = nc.dram_tensor("y0_d", [NTOK, D], F32, kind="Internal").ap()
    x1T_d = nc.dram_tensor("x1T_d", [128, 8, NTOK], BF16, kind="Internal").ap()
    dbg_out = {}

    with ExitStack() as es:
        P = Prog(nc, es)

        try:
            def sb(name, shape, dt=F32, stack=es):
                return stack.enter_context(nc.sbuf_tensor(name, list(shape), dt))

            def chk(x):
                if stage < x:
                    P.dead = True

            def dump(name, ap, shape, dt=F32, r=()):
                if dbg:
                    d = dout("dbg_" + name, shape, dt)
                    dbg_out[name] = d
                    P.dma("sp", d, ap, r=list(r), sem="dbg_" + name)

            psf = [es.enter_context(nc.psum_tensor("ps%d" % i, [128, 512], F32)) for i in range(8)]
            ps_rr = [0]
            ps_n = [8]

            def ps():
                i = ps_rr[0] % ps_n[0]
                ps_rr[0] += 1
                return psf[i], "ps%d" % i

            ev_rr = [0]

            def evac(out, in_, r, w):
                ev_rr[0] += 1
                if ev_rr[0] % 2:
                    P.op("act", lambda e: e.copy(out=out, in_=in_), r=r, w=w)
                else:
                    P.op("dve", lambda e: e.tensor_copy(out=out, in_=in_), r=r, w=w)

            def mmg(out, pairs, r, w):
                n = len(pairs)

                def g(e):
                    ins = None
                    for i, (a, b) in enumerate(pairs):
                        ins = e.matmul(out, lhsT=a, rhs=b, start=(i == 0), stop=(i == n - 1))
                    return ins
                P.op("pe", g, r=r, w=w)

            ident = sb("ident", [128, 128])
            ident_b = sb("ident_b", [128, 128], BF16)
            U_f = sb("U_f", [128, 128])
            U_b = sb("U_b", [128, 128], BF16)
            SU4 = sb("SU4", [4, 4])
            ones_f = sb("ones_f", [128, 128])
            ones_b = sb("ones_b", [128, 1], BF16)
            sel127 = sb("sel127", [128, 128])
            hmask = sb("hmask", [32, 8])
            io_i = sb("io_i", [128, 1], I32)
            io_f = sb("io_f", [128, 1])
            bfb = sb("bfb", [128, 8])
            rc = sb("rc", [128, 80])
            LF = sb("LF", [128, 33, 8])
            gate = sb("gate", [128, 17, 16])
            pool_ops = [
                (lambda e: e.memset(ident[:], 1.0), [], ["ident"]),
                (lambda e: e.affine_select(out=ident[:], in_=ident[:], pattern=[[-1, 128]], compare_op=ALU.is_equal,
                                           fill=0.0, base=0, channel_multiplier=1), ["ident"], ["ident"]),
                (lambda e: e.tensor_copy(out=ident_b[:], in_=ident[:]), ["ident"], ["ident_b"]),
                (lambda e: e.memset(U_f[:], 1.0), [], ["U_f"]),
                (lambda e: e.affine_select(out=U_f[:], in_=U_f[:], pattern=[[1, 128]], compare_op=ALU.is_ge,
                                           fill=0.0, base=0, channel_multiplier=-1), ["U_f"], ["U_f"]),
                (lambda e: e.tensor_copy(out=U_b[:], in_=U_f[:]), ["U_f"], ["U_b"]),
                (lambda e: e.memset(SU4[:], 1.0), [], ["SU4"]),
                (lambda e: e.affine_select(out=SU4[:], in_=SU4[:], pattern=[[-1, 4]], compare_op=ALU.is_gt,
                                           fill=0.0, base=0, channel_multiplier=1), ["SU4"], ["SU4"]),
                (lambda e: e.memset(ones_f[:], 1.0), [], ["ones_f"]),
                (lambda e: e.memset(ones_b[:], 1.0), [], ["ones_b"]),
                (lambda e: e.memset(sel127[:], 1.0), [], ["sel127"]),
                (lambda e: e.affine_select(out=sel127[:], in_=sel127[:], pattern=[[0, 128]], compare_op=ALU.is_equal,
                                           fill=0.0, base=-127, channel_multiplier=1), ["sel127"], ["sel127"]),
                (lambda e: e.memset(hmask[:], 1.0), [], ["hmask"]),
                (lambda e: e.affine_select(out=hmask[:], in_=hmask[:], pattern=[[-4, 8]], compare_op=ALU.is_ge,
                                           fill=0.0, base=0, channel_multiplier=1), ["hmask"], ["hmask"]),
                (lambda e: e.affine_select(out=hmask[:], in_=hmask[:], pattern=[[4, 8]], compare_op=ALU.is_ge,
                                           fill=0.0, base=3, channel_multiplier=-1), ["hmask"], ["hmask"]),
                (lambda e: e.iota(out=io_i[:], pattern=[[0, 1]], base=0, channel_multiplier=1), [], ["io_i"]),
                (lambda e: e.tensor_copy(out=io_f[:], in_=io_i[:]), ["io_i"], ["io_f"]),
                (lambda e: e.memset(LF[:], 0.0), [], ["LFall"]),
                (lambda e: e.memset(gate[:], 0.0), [], ["gate"]),
            ]
            for fn, r_, w_ in pool_ops:
                P.op("pool", fn, r=r_, w=w_)
            P.dma("sp", bfb[:], b_f.partition_broadcast(128), w=["bfb"], sem="c0")
            P.dma("sp", rc[:], rolec.partition_broadcast(128), w=["rc"], sem="c1")
            P.barrier()
            chk(0.1)

            s_oT = ExitStack()
            es.enter_context(s_oT)
            oT = sb("oT", [128, 4, NTOK], BF16, s_oT)
            s_A = ExitStack()
            es.enter_context(s_A)
            if os.environ.get("KFIRST"):
                kT = sb("kT", [128, 4, int(os.environ.get("KTN", "4224"))], BF16, s_A)
                qT = sb("qT", [128, 4, NTOK], BF16, s_A)
            else:
                qT = sb("qT", [128, 4, NTOK], BF16, s_A)
                kT = sb("kT", [128, 4, int(os.environ.get("KTN", "4224"))], BF16, s_A)
            Vt = sb("Vt", [128, 33, 8, 66], BF16, s_A)
            Cc = sb("Cc", [128, 32, 8], F32, s_A)
            BIAS = sb("BIAS", [128, 4, 32, 8], F32, s_A)
            P.op("pool", lambda e: e.memset(Vt[:, :, :, 64:65], 1.0), w=["Vones"])

            wst = []
            wst_rr = [0]

            def wchunks(dst, dkey, src, K, N, col0=0, dcol0=0, CH=1024):
                th = []
                if N <= CH:
                    g = max(1, CH // N)
                    for k0 in range(0, K, g):
                        kk = min(g, K - k0)

                        def f(k0=k0, kk=kk):
                            i = wst_rr[0] % len(wst)
                            wst_rr[0] += 1
                            st, sk = wst[i], "wst%d" % i
                            sv = st[:, 0:kk * N].rearrange("p (k n) -> p k n", n=N)
                            P.dma("sp", sv, src[k0 * 128:(k0 + kk) * 128, col0:col0 + N].rearrange("(k p) n -> p k n", p=128), w=[sk], sem=sk)
                            P.op("act", lambda e: e.copy(out=dst[:, k0:k0 + kk, dcol0:dcol0 + N], in_=sv), r=[sk], w=[dkey])
                        th.append(f)
                else:
                    for k in range(K):
                        for c0 in range(0, N, CH):
                            n = min(CH, N - c0)

                            def f(k=k, c0=c0, n=n):
                                i = wst_rr[0] % len(wst)
                                wst_rr[0] += 1
                                st, sk = wst[i], "wst%d" % i
                                P.dma("sp", st[:, 0:n], src[k * 128:(k + 1) * 128, col0 + c0:col0 + c0 + n], w=[sk], sem=sk)
                                P.op("act", lambda e: e.copy(out=dst[:, k, dcol0 + c0:dcol0 + c0 + n], in_=st[:, 0:n]), r=[sk], w=[dkey])
                            th.append(f)
                return th

            def wload(*a, **kw):
                for f in wchunks(*a, **kw):
                    f()

            def load_xT(src, W, xi, xt, xik, xtk):
                nb = (W + 127) // 128
                pw = min(W, 128)
                if W == 512:
                    P.dma("sp", xi[:], src.rearrange("(b p) d -> p b d", p=128), w=[xik], sem=xik)
                else:
                    P.dma("sp", xi[0:pw, 0, :], src, w=[xik], sem=xik)
                for b in range(nb):
                    for half in range(2):
                        pt, ptk = ps()

                        def tr(e, pt=pt, b=b, half=half):
                            ins = None
                            for kk in range(4):
                                k = half * 4 + kk
                                ins = e.transpose(pt[:, kk * 128:kk * 128 + pw], xi[0:pw, b, k * 128:(k + 1) * 128], ident[0:pw, 0:pw])
                            return ins
                        P.op("pe", tr, r=[xik, "ident"], w=[ptk])
                        evac(xt[:, half * 4:half * 4 + 4, b * 128:b * 128 + pw],
                             pt[:].rearrange("p (k t) -> p k t", k=4)[:, :, 0:pw], r=[ptk], w=[xtk + "_%d_%d" % (b, half)])
                return [xtk + "_%d_%d" % (b, h) for b in range(nb) for h in range(2)]

            with ExitStack() as s1:
                wq = sb("wq", [128, 8, 512], BF16, s1)
                wk = sb("wk", [128, 8, 512], BF16, s1)
                wv = sb("wv", [128, 8, 512], BF16, s1)
                wf = sb("wf", [128, 8, 8], BF16, s1)
                wst[:] = [sb("wstA%d" % i, [128, 1024], F32, s1) for i in range(2)]
                for (t, c0, n, nm) in ((wq, C_Q, 512, "wq"), (wk, C_K, 512, "wk"), (wv, C_V, 512, "wv"), (wf, C_F, 8, "wf")):
                    wload(t, nm, w_in, 8, n, col0=c0, CH=1024)
                xin = [sb("xin%d" % i, [128, 4, 1024], F32, s1) for i in range(2)]
                xT = [sb("xT%d" % i, [128, 8, 512], BF16, s1) for i in range(2)]
                stg = [sb("stg%d" % i, [128, 512], F32, s1) for i in range(4)]
                ftmp = [sb("ftmp%d" % i, [128, 8], F32, s1) for i in range(2)]
                stg_rr = [0]
                tiles = []
                for s_ in range(4):
                    tiles.append((x_own[s_ * 512:(s_ + 1) * 512, :], 512, True, s_ * 512, s_ * 512, s_ * 4, s_ * 512, k_own, v_own))
                tiles.append((x_s, 64, True, 2048, 4096, 32, 0, k_s, v_s))
                for s_ in range(4):
                    tiles.append((x_oth[s_ * 512:(s_ + 1) * 512, :], 512, False, None, 2048 + s_ * 512, 16 + s_ * 4, None, None, None))

                for ti, (src, W, own, q0, k0, b0, r0, ko, vo) in enumerate(tiles):
                    if ti == 0:
                        chk(0.2)
                    if ti == 1:
                        chk(0.3)
                    if ti == 5:
                        chk(0.4)
                    xi, xt = xin[ti % 2], xT[ti % 2]
                    xik, xtk = "xin%d" % (ti % 2), "xT%d" % (ti % 2)
                    nb = (W + 127) // 128
                    pw = min(W, 128)
                    xtall = load_xT(src, W, xi, xt, xik, xtk)
                    if ti == 0:
                        chk(0.21)

                    def proj_fm(wt, wnm, dst, d0):
                        for hp in range(4):
                            pt, ptk = ps()
                            mmg(pt[:, 0:W], [(wt[:, k, hp * 128:(hp + 1) * 128], xt[:, k, 0:W]) for k in range(8)], r=xtall + [wnm], w=[ptk])
                            evac(dst[:, hp, d0:d0 + W], pt[:, 0:W], r=[ptk], w=["%s_%d_%d" % (dst.name, hp, d0)])


                    if own and not os.environ.get("SKIPQ"):
                        proj_fm(wq, "wq", qT, q0)
                    if ti == 0:
                        chk(0.22)
                    proj_fm(wq if os.environ.get("USEWQ") else wk, "wq" if os.environ.get("USEWQ") else "wk", kT, k0)
                    if ti == 0:
                        chk(0.23)
                    for b in range(nb):
                        xtb = [xtk + "_%d_%d" % (b, h) for h in range(2)]
                        tsl = slice(b * 128, b * 128 + pw)
                        pt, ptk = ps()
                        mmg(pt[0:pw, :], [(xt[:, k, tsl], wv[:, k, :]) for k in range(8)], r=xtb + ["wv"], w=[ptk])
                        P.op("act", lambda e, pt=pt, b=b: e.copy(out=Vt[0:pw, b0 + b, :, 0:64], in_=pt[0:pw, :].rearrange("p (h d) -> p h d", h=8)),
                             r=[ptk], w=["Vt%d" % (b0 + b)])
                        if ti == 0 and b == 0:
                            chk(0.2311)
                        if own:
                            si = stg_rr[0] % 4
                            stg_rr[0] += 1
                            P.op("dve", lambda e, pt=pt, si=si: e.tensor_copy(out=stg[si][0:pw, :], in_=pt[0:pw, :]), r=[ptk, "Vt%d" % (b0 + b)], w=["stg%d" % si])
                            P.dma("sp", vo[r0 + b * 128:r0 + b * 128 + pw, :], stg[si][0:pw, :], r=["stg%d" % si], sem="stg%d" % si)
                            if ti == 0 and b == 0:
                                chk(0.2312)
                            pt, ptk = ps()
                            mmg(pt[0:pw, :], [(xt[:, k, tsl], wk[:, k, :]) for k in range(8)], r=xtb + ["wk"], w=[ptk])
                            si = stg_rr[0] % 4
                            stg_rr[0] += 1
                            evac(stg[si][0:pw, :], pt[0:pw, :], r=[ptk], w=["stg%d" % si])
                            P.dma("sp", ko[r0 + b * 128:r0 + b * 128 + pw, :], stg[si][0:pw, :], r=["stg%d" % si], sem="stg%d" % si)
                        if ti == 0 and b == 0:
                            chk(0.232)
                        pt, ptk = ps()
                        mmg(pt[0:pw, 0:8], [(xt[:, k, tsl], wf[:, k, :]) for k in range(8)], r=xtb + ["wf"], w=[ptk])
                        ft = ftmp[b % 2]
                        fk = "ftmp%d" % (b % 2)
                        P.op("dve", lambda e, pt=pt, ft=ft: e.tensor_tensor(out=ft[0:pw, :], in0=pt[0:pw, 0:8], in1=bfb[0:pw, :], op=ALU.add),
                             r=[ptk, "bfb"], w=[fk])
                        if ti == 0 and b == 0:
                            chk(0.233)
                        P.op("act", lambda e, ft=ft: e.activation(out=ft[0:pw, :], in_=ft[0:pw, :], func=AF.Exp, scale=-1.0), r=[fk], w=[fk])
                        if ti == 0 and b == 0:
                            chk(0.234)
                        P.op("act", lambda e, ft=ft: e.activation(out=ft[0:pw, :], in_=ft[0:pw, :], func=AF.Ln, bias=1.0, scale=1.0), r=[fk], w=[fk])
                        P.op("dve", lambda e, ft=ft, bb=b0 + b: e.tensor_scalar(out=LF[0:pw, bb, :], in0=ft[0:pw, :], scalar1=-1.0, scalar2=None, op0=ALU.mult),
                             r=[fk, "LFall"], w=["LF%d" % (b0 + b)])
                lfall = ["LF%d" % i for i in range(33)]
                P.dma("sp", f_own.rearrange("(b p) h -> p b h", p=128), LF[:, 0:16, :], r=lfall, sem="fo")
                P.dma("sp", f_s, LF[0:64, 32, :], r=lfall, sem="fs")
                P.barrier()
                chk(0.5)

            with ExitStack() as s2:
                T1 = sb("T1", [128, 8, 8], F32, s2)
                Lprev = sb("Lprev", [128, 8, 8], F32, s2)
                Ltmp = sb("Ltmp", [128, 8, 8], F32, s2)
                Lfull = sb("Lfull", [128, 32, 8], F32, s2)
                Cb = sb("Cb", [128, 4, 8], F32, s2)
                LF4 = LF[:, 0:32, :].rearrange("p (t j) h -> p t j h", j=4)
                Lf4 = Lfull[:].rearrange("p (t j) h -> p t j h", j=4)
                P.op("dve", lambda e: e.tensor_tensor(out=T1[:], in0=LF4[:, :, 0, :], in1=LF4[:, :, 1, :], op=ALU.add), r=[], w=["T1"])
                P.op("dve", lambda e: e.tensor_tensor(out=T1[:], in0=T1[:], in1=LF4[:, :, 2, :], op=ALU.add), r=["T1"], w=["T1"])
                P.op("dve", lambda e: e.tensor_tensor(out=T1[:], in0=T1[:], in1=LF4[:, :, 3, :], op=ALU.add), r=["T1"], w=["T1"])
                rcE = rc[:, 0:64].rearrange("q (a b) -> q a b", b=8)
                for pp in range(8):
                    in0 = T1[:, pp, :].unsqueeze(1).to_broadcast([128, 8, 8])
                    in1 = rcE[:, pp, :].unsqueeze(2).to_broadcast([128, 8, 8])
                    if pp == 0:
                        P.op("dve", lambda e, in0=in0, in1=in1: e.tensor_tensor(out=Lprev[:], in0=in0, in1=in1, op=ALU.mult), r=["T1"], w=["Lprev"])
                    else:
                        P.op("dve", lambda e, in0=in0, in1=in1: e.tensor_tensor(out=Ltmp[:], in0=in0, in1=in1, op=ALU.mult), r=["T1"], w=["Ltmp"])
                        P.op("dve", lambda e: e.tensor_tensor(out=Lprev[:], in0=Lprev[:], in1=Ltmp[:], op=ALU.add), r=["Ltmp", "Lprev"], w=["Lprev"])
                P.op("dve", lambda e: e.tensor_copy(out=Lf4[:, :, 0, :], in_=Lprev[:]), r=["Lprev"], w=["Lfull"])
                for j in range(1, 4):
                    P.op("dve", lambda e, j=j: e.tensor_tensor(out=Lf4[:, :, j, :], in0=Lf4[:, :, j - 1, :], in1=LF4[:, :, j - 1, :], op=ALU.add),
                         r=["Lfull"], w=["Lfull"])
                pc, pck = ps()
                mmg(pc[:, 0:256], [(U_f[:], LF[:, 0:32, :].rearrange("p b h -> p (b h)")), (ones_f[:], Lfull[:].rearrange("p b h -> p (b h)"))],
                    r=["Lfull"], w=[pck])
                P.op("dve", lambda e: e.tensor_copy(out=Cc[:].rearrange("p b h -> p (b h)"), in_=pc[:, 0:256]), r=[pck], w=["Cc"])
                Cc4 = Cc[:].rearrange("p (t j) h -> p t j h", j=4)
                pcb, pcbk = ps()
                mmg(pcb[:, 0:32].rearrange("p (a b) -> p a b", b=8), [(sel127[:], Cc4[:, 0:4, 1, :])], r=["Cc"], w=[pcbk])
                P.op("dve", lambda e: e.tensor_copy(out=Cb[:].rearrange("p a b -> p (a b)"), in_=pcb[:, 0:32]), r=[pcbk], w=["Cb"])
                for sg in range(4):
                    P.op("dve", lambda e, sg=sg: e.scalar_tensor_tensor(out=BIAS[:, sg], in0=Cc[:], scalar=-1.0,
                                                                         in1=Cb[:, sg, :].unsqueeze(1).to_broadcast([128, 32, 8]),
                                                                         op0=ALU.mult, op1=ALU.add), r=["Cc", "Cb"], w=["BIAS"])
                    P.op("dve", lambda e, sg=sg: e.tensor_scalar(out=BIAS[:, sg, 16 + 4 * sg:20 + 4 * sg, :], in0=BIAS[:, sg, 16 + 4 * sg:20 + 4 * sg, :],
                                                                  scalar1=rc[:, 64 + sg:65 + sg], scalar2=None, op0=ALU.add), r=["BIAS"], w=["BIAS"])
                dump("Cc", Cc[:], [128, 32, 8], r=["Cc"])
                P.barrier()
                chk(1.0)

            if stage >= 2:
                with ExitStack() as s2:
                    ptb = sb("ptb", [128, 256], I32, s2)
                    ptf = sb("ptf", [128, 256], F32, s2)
                    idx = sb("idx", [128, 256], I32, s2)
                    lfp = sb("lfp", [128, 16, 16, 8], F32, s2)
                    Linc = sb("Linc", [128, 16, 16, 8], F32, s2)
                    Cbs = sb("Cbs", [128, 16, 8], F32, s2)
                    lfn4 = sb("lfn4", [4, 16, 8], F32, s2)
                    pidx = sb("pidx", [128, 2], I32, s2)
                    bnew = sb("bnew", [4, 16, 8], F32, s2)
                    Vn = sb("Vn", [4, 16, 512], BF16, s2)
                    Qbd = sb("Qbd", [128, 4, 16, 8], BF16, s2)
                    Kp = [sb("Kp%d" % i, [128, 16, 512], BF16, s2) for i in range(1)]
                    Vp = [sb("Vp%d" % i, [128, 16, 512], BF16, s2) for i in range(2)]
                    KTp = [sb("KTp%d" % i, [128, 4, 128], BF16, s2) for i in range(3)]
                    stmp = sb("stmp", [128, 512], F32, s2)
                    Pp = sb("Pp", [128, 16, 32], BF16, s2)
                    tn = sb("tn", [4, 32], F32, s2)
                    Pn = sb("Pn", [4, 32], BF16, s2)
                    t3 = sb("t3", [32, 512], F32, s2)
                    o32 = sb("o32", [32, 64], F32, s2)
                    rd = sb("rd", [32, 1], F32, s2)
                    ps_n[0] = 5
                    P.dma("sp", ptb[:], page_table.partition_broadcast(128), w=["ptb"], sem="ptb")
                    P.op("dve", lambda e: e.tensor_copy(out=ptf[:], in_=ptb[:]), r=["ptb"], w=["ptf"])
                    P.op("dve", lambda e: e.tensor_scalar(out=ptf[:], in0=ptf[:], scalar1=128.0, scalar2=io_f[:, 0:1], op0=ALU.mult, op1=ALU.add),
                         r=["ptf", "io_f"], w=["ptf"])
                    P.op("dve", lambda e: e.tensor_copy(out=idx[:], in_=ptf[:]), r=["ptf"], w=["idx"])
                    P.dma("sp", lfn4[:], f_s.rearrange("(b j) h -> j b h", j=4), w=["lfn4"], sem="lfn4")
                    P.dma("pool", Vn[:], v_s.rearrange("(b j) f -> j b f", j=4), w=["Vn"], sem="Vn")
                    with nc.allow_non_contiguous_dma(reason="tiny page-id transpose"):
                        P.dma("sp", pidx[:], page_table.rearrange("o (g p) -> p (o g)", p=128), w=["pidx"], sem="pidx")
                    cfp = cache_f.rearrange("(n k) h -> n (k h)", k=128)
                    lfT = Linc[:].rearrange("p b g h -> p (b g h)").rearrange("p (a n) -> p a n", a=2)
                    for g_ in range(2):
                        P.idma(lfT[:, g_, :], cfp, pidx[:, g_:g_ + 1], r=["pidx"], w=["lfT%d" % g_], sem="lfT")
                    for g_ in range(2):
                        lv = lfT[:, g_, :].rearrange("p (k h) -> p k h", h=8)
                        for q4 in range(2):
                            ptl, ptlk = ps()
                            P.op("pe", lambda e, ptl=ptl, lv=lv, q4=q4: [e.transpose(ptl[:, hh * 128:(hh + 1) * 128], lv[:, :, 4 * q4 + hh], ident[:]) for hh in range(4)][-1],
                                 r=["lfT0", "lfT1", "ident"], w=[ptlk])
                            evac(lfp[:, 8 * g_:8 * g_ + 8, :, 4 * q4:4 * q4 + 4].rearrange("p b g h -> p h (b g)"),
                                 ptl[:, :].rearrange("p (h s) -> p h s", h=4), r=[ptlk], w=["lfp_%d_%d" % (g_, q4)])
                    lfpall = ["lfp_%d_%d" % (g_, q4) for g_ in range(2) for q4 in range(2)]
                    P.op("dve", lambda e: e.tensor_copy(out=Linc[:, :, 0, :], in_=lfp[:, :, 0, :]), r=lfpall, w=["Linc", "lfp"])
                    for pg in range(1, 16):
                        P.op("dve", lambda e, pg=pg: e.tensor_tensor(out=Linc[:, :, pg, :], in0=Linc[:, :, pg - 1, :], in1=lfp[:, :, pg, :], op=ALU.add),
                             r=["Linc", "lfp"], w=["Linc"])
                    pcs, pcsk = ps()
                    P.op("pe", lambda e: (e.matmul(pcs[:, 0:128].rearrange("p (b h) -> p b h", h=8), lhsT=ones_f[:], rhs=Linc[:, :, 15, :], start=True, stop=False),
                                          e.matmul(pcs[:, 0:128].rearrange("p (b h) -> p b h", h=8), lhsT=ones_f[0:4, :], rhs=lfn4[:], start=False, stop=True))[1],
                         r=["Linc", "lfn4"], w=[pcsk])
                    P.op("dve", lambda e: e.tensor_copy(out=Cbs[:].rearrange("p b h -> p (b h)"), in_=pcs[:, 0:128]), r=[pcsk], w=["Cbs"])
                    P.op("dve", lambda e: e.tensor_tensor(out=Linc[:], in0=Linc[:], in1=lfp[:], op=ALU.subtract), r=["Linc", "lfp", "Cbs"], w=["Linc"])
                    cpast = lfp
                    lfp2 = lfp[:].rearrange("p b g h -> p (b g h)")
                    Lin2 = Linc[:].rearrange("p b g h -> p (b g h)")
                    cp2 = cpast[:].rearrange("p b g h -> p (b g h)")
                    for q4 in range(4):
                        pq, pqk = ps()
                        cs = slice(q4 * 512, (q4 + 1) * 512)
                        mmg(pq[:, :], [(U_f[:], lfp2[:, cs]), (ones_f[:], Lin2[:, cs])], r=["Linc", "lfp"], w=[pqk])
                        for bb in range(4):
                            b_ = 4 * q4 + bb
                            P.op("dve", lambda e, pq=pq, bb=bb, b_=b_: e.scalar_tensor_tensor(
                                out=cpast[:, b_], in0=pq[:, bb * 128:(bb + 1) * 128].rearrange("p (g h) -> p g h", g=16), scalar=-1.0,
                                in1=Cbs[:, b_, :].unsqueeze(1).to_broadcast([128, 16, 8]), op0=ALU.mult, op1=ALU.add),
                                r=[pqk, "Cbs"], w=["cpast", "lfp"])
                    pbn, pbnk = ps()
                    mmg(pbn[0:4, 0:128].rearrange("p (b h) -> p b h", h=8), [(SU4[:], lfn4[:])], r=["lfn4", "SU4"], w=[pbnk])
                    P.op("dve", lambda e: e.tensor_copy(out=bnew[:].rearrange("p b h -> p (b h)"), in_=pbn[0:4, 0:128]), r=[pbnk], w=["bnew"])
                    P.op("pool", lambda e: e.memset(Qbd[:], 0.0), w=["Qbd"])
                    qs = qT[:, :, 2048:2112].rearrange("p c (b j) -> p c b j", j=4)
                    P.op("pool", lambda e: e.tensor_copy(out=Qbd[0:64, :, :, 0:4], in_=qs[0:64]), r=["Qbd"], w=["Qbd"])
                    P.op("pool", lambda e: e.tensor_copy(out=Qbd[64:128, :, :, 4:8], in_=qs[64:128]), r=["Qbd"], w=["Qbd"])
                    kt_rr = 0
                    for b in range(16):
                        kp, vp = Kp[0], Vp[b % 2]
                        kpk, vpk = "Kp0", "Vp%d" % (b % 2)
                        for pg in range(16):
                            P.idma(kp[:, pg, :], cache_k, idx[:, b * 16 + pg:b * 16 + pg + 1], r=["idx"], w=[kpk + "_%d" % pg], sem=kpk)
                        for pg in range(16):
                            P.idma(vp[:, pg, :], cache_v, idx[:, b * 16 + pg:b * 16 + pg + 1], r=["idx"], w=[vpk + "_%d" % pg], sem=vpk)
                        ss, ssk = psf[5], "ps5"
                        sn, snk = psf[6], "ps6"
                        acc, acck = psf[7], "ps7"
                        for pg in range(16):
                            ptb_, ptbk = ps()
                            ptv = ptb_[:].bitcast(BF16)

                            def trk(e, ptv=ptv, kp=kp, pg=pg):
                                ins = None
                                for hp in range(4):
                                    ins = e.transpose(ptv[:, hp * 128:(hp + 1) * 128], kp[:, pg, hp * 128:(hp + 1) * 128], ident_b[:])
                                return ins
                            P.op("pe", trk, r=[kpk + "_%d" % g_ for g_ in range(16)] + ["ident_b"], w=[ptbk])
                            ktp = KTp[kt_rr % 3]
                            ktk = "KTp%d" % (kt_rr % 3)
                            kt_rr += 1
                            evac(ktp[:].rearrange("p c k -> p (c k)"), ptv[:, 0:512], r=[ptbk], w=[ktk])

                            def qk(e, ktp=ktp, pg=pg, b=b):
                                ins = None
                                for hp in range(4):
                                    ins = e.matmul(ss[:, pg * 32 + hp * 8:pg * 32 + hp * 8 + 8], lhsT=ktp[:, hp, :], rhs=Qbd[:, hp, b, :], start=True, stop=True)
                                return ins
                            P.op("pe", qk, r=[ktk, "Qbd"], w=[ssk])

                        def qkn(e, b=b):
                            ins = None
                            for hp in range(4):
                                ins = e.matmul(sn[0:4, hp * 8:hp * 8 + 8], lhsT=kT[:, hp, 4096 + 4 * b:4096 + 4 * b + 4], rhs=Qbd[:, hp, b, :], start=True, stop=True)
                            return ins
                        P.op("pe", qkn, r=["Qbd"], w=[snk])
                        P.op("dve", lambda e, b=b: e.scalar_tensor_tensor(
                            out=stmp[:].rearrange("p (g h q) -> p g h q", g=16, h=8), in0=ss[:, :].rearrange("p (g h q) -> p g h q", g=16, h=8), scalar=0.125,
                            in1=cpast[:, b].unsqueeze(3).to_broadcast([128, 16, 8, 4]), op0=ALU.mult, op1=ALU.add), r=[ssk, "cpast"], w=["stmp"])
                        P.op("act", lambda e: e.activation(out=Pp[:].rearrange("p g c -> p (g c)"), in_=stmp[:], func=AF.Exp), r=["stmp"], w=["Pp"])
                        P.op("dve", lambda e, b=b: e.scalar_tensor_tensor(
                            out=tn[:].rearrange("p (h q) -> p h q", h=8), in0=sn[0:4, 0:32].rearrange("p (h q) -> p h q", h=8), scalar=0.125,
                            in1=bnew[:, b, :].unsqueeze(2).to_broadcast([4, 8, 4]), op0=ALU.mult, op1=ALU.add), r=[snk, "bnew"], w=["tn"])
                        P.op("act", lambda e: e.activation(out=tn[:], in_=tn[:], func=AF.Exp), r=["tn"], w=["tn"])
                        P.op("dve", lambda e: e.tensor_tensor(out=Pn[:].rearrange("p (h q) -> p h q", h=8), in0=tn[:].rearrange("p (h q) -> p h q", h=8),
                                                              in1=U_f[0:4, 0:4].unsqueeze(1).to_broadcast([4, 8, 4]), op=ALU.mult), r=["tn", "U_f"], w=["Pn"])

                        def pv(e, vp=vp, b=b):
                            for pg in range(16):
                                e.matmul(acc[0:32, :], lhsT=Pp[:, pg, :], rhs=vp[:, pg, :], start=(pg == 0), stop=False)
                            return e.matmul(acc[0:32, :], lhsT=Pn[:, :], rhs=Vn[:, b, :], start=False, stop=True)
                        P.op("pe", pv, r=["Pp", "Pn", "Vn"] + [vpk + "_%d" % g_ for g_ in range(16)], w=[acck])

                        def dn(e):
                            for pg in range(16):
                                e.matmul(sn[0:32, 64:65], lhsT=Pp[:, pg, :], rhs=ones_b[:, 0:1], start=(pg == 0), stop=False)
                            return e.matmul(sn[0:32, 64:65], lhsT=Pn[:, :], rhs=ones_b[0:4, 0:1], start=False, stop=True)
                        P.op("pe", dn, r=["Pp", "Pn", "tn"], w=[snk + "d"])
                        P.op("dve", lambda e: e.tensor_tensor(out=t3[:].rearrange("p (h d) -> p h d", h=8), in0=acc[0:32, :].rearrange("p (h d) -> p h d", h=8),
                                                              in1=hmask[:].unsqueeze(2).to_broadcast([32, 8, 64]), op=ALU.mult), r=[acck, "hmask"], w=["t3"])
                        P.op("dve", lambda e: e.tensor_reduce(out=o32[:], in_=t3[:].rearrange("p (h d) -> p d h", h=8), axis=AX.X, op=ALU.add), r=["t3"], w=["o32"])
                        P.op("dve", lambda e: e.reciprocal(out=rd[:], in_=sn[0:32, 64:65]), r=[snk + "d"], w=["rd"])
                        P.op("dve", lambda e: e.tensor_scalar(out=o32[:], in0=o32[:], scalar1=rd[:, 0:1], scalar2=None, op0=ALU.mult), r=["o32", "rd"], w=["o32"])
                        po, pok = ps()
                        P.op("pe", lambda e, po=po: e.transpose(po[0:64, 0:32], o32[:, :], ident[0:32, 0:32]), r=["o32", "ident"], w=[pok])
                        pov = po[0:64, 0:32].rearrange("p (c hh q) -> p c hh q", c=4, hh=2)
                        P.op("dve", lambda e, pov=pov, b=b: e.tensor_copy(out=oT[0:64, :, 2048 + 4 * b:2052 + 4 * b], in_=pov[:, :, 0, :]), r=[pok, snk, snk + "d"], w=["oTs%d" % b])
                        P.op("dve", lambda e, pov=pov, b=b: e.tensor_copy(out=oT[64:128, :, 2048 + 4 * b:2052 + 4 * b], in_=pov[:, :, 1, :]), r=[pok], w=["oTs%d" % b])
                    ps_n[0] = 8
                    P.barrier()

            if stage >= 3:
                with ExitStack() as s3:
                    Pt = [sb("Pt%d" % i, [128, 512], BF16, s3) for i in range(6)]
                    rec = sb("rec", [128, 512], F32, s3)
                    bcs = sb("bcs", [64, 512], F32, s3)
                    ps_n[0] = 5
                    pt_rr = 0
                    n_acc = 0
                    LA = 3
                    tasks = []
                    for sg in range(4):
                        for h in range(8):
                            acc, acck = psf[6 + n_acc % 2], "ps%d" % (6 + n_acc % 2)
                            n_acc += 1
                            blocks = list(range(0, 4 * sg)) + list(range(16, 16 + 4 * sg + 4)) + list(range(4 * sg, 4 * sg + 4))
                            for i, kb in enumerate(blocks):
                                diag = 4 * sg <= kb < 4 * sg + 4
                                tasks.append(dict(sg=sg, h=h, hp=h // 2, pr=slice((h % 2) * 64, (h % 2) * 64 + 64), acc=acc, acck=acck, kb=kb, diag=diag,
                                                  q0=128 * (kb - 4 * sg) if diag else 0, first=(i == 0), last=(i == len(blocks) - 1)))
                    normq = []

                    def norm(T):
                        acc, acck, pr, hp, sg, h = T["acc"], T["acck"], T["pr"], T["hp"], T["sg"], T["h"]
                        P.op("dve", lambda e: e.reciprocal(out=rec[64:65, :], in_=acc[64:65, :]), r=[acck], w=["rec"])
                        P.op("pe", lambda e: e.matmul(psf[5][0:64, :], lhsT=ones_f[64:65, 0:64], rhs=rec[64:65, :], start=True, stop=True), r=["rec"], w=["ps5"])
                        P.op("act", lambda e: e.copy(out=bcs[:], in_=psf[5][0:64, :]), r=["ps5"], w=["bcs"])
                        P.op("dve", lambda e: e.tensor_tensor(out=oT[pr, hp, sg * 512:(sg + 1) * 512], in0=acc[0:64, :], in1=bcs[:], op=ALU.mult),
                             r=[acck, "bcs"], w=["oT_%d_%d" % (sg, h)])

                    for t in range(len(tasks) + LA + 3):
                        if t < len(tasks):
                            T = tasks[t]
                            sg, h, hp, pr, kb, q0 = T["sg"], T["h"], T["hp"], T["pr"], T["kb"], T["q0"]
                            st, stk = ps()
                            P.op("pe", lambda e, st=st: e.matmul(st[:, q0:512], lhsT=kT[pr, hp, kb * 128:(kb + 1) * 128],
                                                                  rhs=qT[pr, hp, sg * 512 + q0:(sg + 1) * 512], start=True, stop=True), r=[], w=[stk])
                            pti = Pt[pt_rr % 6]
                            ptk = "Pt%d" % (pt_rr % 6)
                            pt_rr += 1
                            P.op("act", lambda e, st=st, pti=pti: e.activation(out=pti[:, q0:512], in_=st[:, q0:512], func=AF.Exp,
                                                                               bias=BIAS[:, sg, kb, h:h + 1], scale=0.125), r=[stk], w=[ptk])
                            if T["diag"]:
                                P.op("pool", lambda e, pti=pti: e.tensor_tensor(out=pti[:, q0:q0 + 128], in0=pti[:, q0:q0 + 128], in1=U_b[:], op=ALU.mult),
                                     r=[ptk], w=[ptk])
                            T["pti"], T["ptk"] = pti, ptk
                        while normq and normq[0][0] <= t:
                            norm(normq.pop(0)[1])
                        j = t - LA
                        if 0 <= j < len(tasks):
                            T = tasks[j]
                            P.op("pe", lambda e, T=T: e.matmul(T["acc"][0:65, T["q0"]:512], lhsT=Vt[:, T["kb"], T["h"], 0:65], rhs=T["pti"][:, T["q0"]:512],
                                                               start=T["first"], stop=T["last"]), r=[T["ptk"]], w=[T["acck"]])
                            if T["last"]:
                                normq.append((t + 2, T))
                    while normq:
                        norm(normq.pop(0)[1])
                    ps_n[0] = 8
                    dump("oT", oT[:], [128, 4, NTOK], BF16)
                    P.barrier()
            s_A.close()

            if stage >= 4:
                with ExitStack() as s4:
                    wcv = sb("wcv", [128, 8, 1536], BF16, s4)
                    wg = sb("wg", [128, 8, 2048], BF16, s4)
                    wco = sb("wco", [128, 4, 1024], BF16, s4)
                    wao = sb("wao", [128, 4, 1024], BF16, s4)
                    wo = sb("wo", [128, 8, 1024], BF16, s4)
                    wgr = sb("wgr", [128, 8, 20], F32, s4)
                    cw = sb("cw", [128, 4, 3], F32, s4)
                    g1b = sb("g1b", [128, 1024], F32, s4)
                    b1b = sb("b1b", [128, 1024], F32, s4)
                    wst[:] = [sb("wstB%d" % i, [128, 1024], F32, s4) for i in range(2)]
                    wload(wcv, "wcv", w_in, 8, 1536, col0=0, CH=1024)
                    wload(wg, "wg", w_in, 8, 1024, col0=C_GA, dcol0=0, CH=1024)
                    wload(wg, "wg", w_in, 8, 1024, col0=C_GB, dcol0=1024, CH=1024)
                    wload(wco, "wco", w_conv_out, 4, 1024, CH=1024)
                    wload(wao, "wao", w_attn_out, 4, 1024, CH=1024)
                    wload(wo, "wo", w_o, 8, 1024, CH=1024)
                    P.dma("sp", wgr[:], w_gr.rearrange("(k p) n -> p k n", p=128), w=["wgr"], sem="wgr")
                    with nc.allow_non_contiguous_dma(reason="tiny conv weight transpose"):
                        for c in range(4):
                            P.dma("sp", cw[:, c, :], conv_w[:, c * 128:(c + 1) * 128].rearrange("j p -> p j"), w=["cw%d" % c], sem="cw")
                    P.dma("sp", g1b[:], ln1_g.partition_broadcast(128), w=["g1b"], sem="g1b")
                    P.dma("sp", b1b[:], ln1_b.partition_broadcast(128), w=["b1b"], sem="b1b")

                    xin = sb("xin", [128, 4, 1024], F32, s4)
                    xT = sb("xT", [128, 8, 512], BF16, s4)
                    xh = sb("xh", [8, 1024], F32, s4)
                    xhT = sb("xhT", [128, 8, 8], BF16, s4)
                    uh = sb("uh", [128, 4, 8], F32, s4)
                    sct = sb("sct", [32, 512], F32, s4)
                    stT = sb("stT", [128, 4, 32], F32, s4)
                    xcs = sb("xcs", [128, 512], F32, s4)
                    ue = [sb("ue%d" % i, [128, 514], F32, s4) for i in range(2)]
                    tcv = sb("tcv", [128, 512], F32, s4)
                    gcv = sb("gcv", [128, 4, 512], BF16, s4)
                    ucp = sb("ucp", [128, 4, 2], F32, s4)
                    ucs = sb("ucs", [128, 4, 32], F32, s4)
                    cps = sb("cps", [2, 512], F32, s4)
                    css = sb("css", [32, 512], F32, s4)
                    sga = sb("sga", [128, 512], F32, s4)
                    sgb = sb("sgb", [128, 512], F32, s4)
                    za = sga
                    zb = sgb
                    zT = sb("zT", [128, 8, 512], BF16, s4)
                    xr = [sb("xr%d" % i, [128, 1024], F32, s4) for i in range(2)]
                    st6 = sb("st6", [128, 2, 6], F32, s4)
                    mv = sb("mv", [128, 2], F32, s4)
                    rstd = sb("rstd", [128, 1], F32, s4)
                    y0t = [sb("y0t%d" % i, [128, 1024], F32, s4) for i in range(1)]
                    x1Tb = [sb("x1Tb%d" % i, [128, 8, 128], BF16, s4) for i in range(2)]
                    x1Tf = sb("x1Tf", [128, 8, 128], F32, s4)
                    lg = sb("lg", [128, 20], F32, s4)
                    g8 = sb("g8", [128, 8], F32, s4)
                    m8 = sb("m8", [128, 8], F32, s4)
                    e8 = sb("e8", [128, 8], F32, s4)
                    me8 = sb("me8", [128, 8], F32, s4)
                    ohg = sb("ohg", [128, 4], F32, s4)
                    nm = sb("nm", [128, 2], F32, s4)
                    eg = sb("eg", [128, 4], F32, s4)
                    sgs = sb("sgs", [128, 2], F32, s4)
                    sel = sb("sel", [128, 16], F32, s4)
                    e1 = sb("e1", [128, 4], F32, s4)
                    s2m = sb("s2m", [128, 4], F32, s4)
                    wfac = sb("wfac", [128, 1], F32, s4)
                    P.op("pool", lambda e: e.memset(g8[:], -1e30), w=["g8"])
                    P.op("pool", lambda e: e.memset(e8[:], -1e30), w=["e8"])

                    P.dma("sp", xh[:], x_halo, w=["xh"], sem="xh")
                    ph, phk = ps()
                    P.op("pe", lambda e: [e.transpose(ph[:, k * 8:(k + 1) * 8], xh[0:8, k * 128:(k + 1) * 128], ident[0:8, 0:8]) for k in range(8)][-1],
                         r=["xh", "ident"], w=[phk])
                    evac(xhT[:].rearrange("p k t -> p (k t)"), ph[:, 0:64], r=[phk], w=["xhT"])
                    for c in range(4):
                        p1, p1k = ps()
                        mmg(p1[:, 0:8], [(wcv[:, k, C_XC + c * 128:C_XC + (c + 1) * 128], xhT[:, k, :]) for k in range(8)], r=["xhT", "wcv"], w=[p1k])
                        p2, p2k = ps()
                        mmg(p2[:, 0:8], [(wcv[:, k, C_CG + c * 128:C_CG + (c + 1) * 128], xhT[:, k, :]) for k in range(8)], r=["xhT", "wcv"], w=[p2k])
                        P.op("act", lambda e, p1=p1: e.copy(out=xcs[:, 0:8], in_=p1[:, 0:8]), r=[p1k], w=["xcs"])
                        P.op("dve", lambda e, p2=p2, c=c: e.tensor_tensor(out=uh[:, c, :], in0=xcs[:, 0:8], in1=p2[:, 0:8], op=ALU.mult), r=[p2k, "xcs"], w=["uh"])
                    P.dma("sp", sct[:], state_conv, w=["sct"], sem="sct")
                    pst, pstk = ps()
                    P.op("pe", lambda e: [e.transpose(pst[:, c * 32:(c + 1) * 32], sct[0:32, c * 128:(c + 1) * 128], ident[0:32, 0:32]) for c in range(4)][-1],
                         r=["sct", "ident"], w=[pstk])
                    evac(stT[:].rearrange("p c t -> p (c t)"), pst[:, 0:128], r=[pstk], w=["stT"])

                    tiles = [(x_own[s_ * 512:(s_ + 1) * 512, :], 512, s_ * 512, s_ * 4, s_) for s_ in range(4)] + [(x_s, 64, 2048, 16, 4)]
                    blk_n = 0
                    for (src, W, t0, gb0, sl) in tiles:
                        nb = (W + 127) // 128
                        pw = min(W, 128)
                        samp = (sl == 4)
                        xtall = load_xT(src, W, xin, xT, "xin", "xT")
                        for c in range(4):
                            u_ = ue[c % 2]
                            uk = "ue%d" % (c % 2)
                            p1, p1k = ps()
                            mmg(p1[:, 0:W], [(wcv[:, k, C_XC + c * 128:C_XC + (c + 1) * 128], xT[:, k, 0:W]) for k in range(8)], r=xtall + ["wcv"], w=[p1k])
                            p2, p2k = ps()
                            mmg(p2[:, 0:W], [(wcv[:, k, C_CG + c * 128:C_CG + (c + 1) * 128], xT[:, k, 0:W]) for k in range(8)], r=xtall + ["wcv"], w=[p2k])
                            p3, p3k = ps()
                            mmg(p3[:, 0:W], [(wcv[:, k, C_BG + c * 128:C_BG + (c + 1) * 128], xT[:, k, 0:W]) for k in range(8)], r=xtall + ["wcv"], w=[p3k])
                            P.op("act", lambda e, p1=p1: e.copy(out=xcs[:, 0:W], in_=p1[:, 0:W]), r=[p1k], w=["xcs"])
                            if not samp:
                                P.op("pool", lambda e, u_=u_, c=c: e.tensor_copy(out=u_[:, 0:2], in_=uh[:, c, 2 * sl:2 * sl + 2]), r=["uh"], w=[uk])
                                P.op("dve", lambda e, u_=u_, p2=p2: e.tensor_tensor(out=u_[:, 2:W + 2], in0=xcs[:, 0:W], in1=p2[:, 0:W], op=ALU.mult),
                                     r=[p2k, "xcs", uk], w=[uk])
                                uv = [u_[:, j:j + W] for j in range(3)]
                                tv = tcv[:, 0:W]
                            else:
                                u3 = u_[:, 0:96].rearrange("p (b i) -> p b i", i=6)
                                P.op("pool", lambda e, u3=u3, c=c: e.tensor_copy(out=u3[:, :, 0:2], in_=stT[:, c, :].rearrange("p (b i) -> p b i", i=2)),
                                     r=["stT"], w=[uk])
                                P.op("dve", lambda e, u3=u3, p2=p2: e.tensor_tensor(out=u3[:, :, 2:6], in0=xcs[:, 0:64].rearrange("p (b j) -> p b j", j=4),
                                                                                     in1=p2[:, 0:64].rearrange("p (b j) -> p b j", j=4), op=ALU.mult),
                                     r=[p2k, "xcs", uk], w=[uk])
                                uv = [u3[:, :, j:j + 4] for j in range(3)]
                                tv = tcv[:, 0:64].rearrange("p (b j) -> p b j", j=4)
                            P.op("pool", lambda e, uv=uv, tv=tv, c=c: e.tensor_scalar(out=tv, in0=uv[0], scalar1=cw[:, c, 0:1], scalar2=None, op0=ALU.mult),
                                 r=[uk] + ["cw%d" % i for i in range(4)], w=["tcv"])
                            for j in (1, 2):
                                P.op("dve", lambda e, uv=uv, tv=tv, c=c, j=j: e.scalar_tensor_tensor(out=tv, in0=uv[j], scalar=cw[:, c, j:j + 1], in1=tv,
                                                                                                     op0=ALU.mult, op1=ALU.add), r=[uk, "tcv"], w=["tcv"])
                            P.op("dve", lambda e, p3=p3, c=c: e.tensor_tensor(out=gcv[:, c, 0:W], in0=tcv[:, 0:W], in1=p3[:, 0:W], op=ALU.mult),
                                 r=[p3k, "tcv"], w=["gcv%d" % c])
                            if sl == 3:
                                P.op("pool", lambda e, u_=u_, c=c: e.tensor_copy(out=ucp[:, c, :], in_=u_[:, W:W + 2]), r=[uk], w=["ucp"])
                            if samp:
                                P.op("pool", lambda e, u3=u3, c=c: e.tensor_copy(out=ucs[:, c, :].rearrange("p (b i) -> p b i", i=2), in_=u3[:, :, 4:6]),
                                     r=[uk], w=["ucs"])
                        if sl == 3:
                            pcp, pcpk = ps()
                            P.op("pe", lambda e, pcp=pcp: [e.transpose(pcp[0:2, c * 128:(c + 1) * 128], ucp[:, c, :], ident[:]) for c in range(4)][-1],
                                 r=["ucp", "ident"], w=[pcpk])
                            evac(cps[:], pcp[0:2, :], r=[pcpk], w=["cps"])
                            P.dma("sp", conv_p, cps[:], r=["cps"], sem="cps")
                        if samp:
                            pcp, pcpk = ps()
                            P.op("pe", lambda e, pcp=pcp: [e.transpose(pcp[0:32, c * 128:(c + 1) * 128], ucs[:, c, :], ident[:]) for c in range(4)][-1],
                                 r=["ucs", "ident"], w=[pcpk])
                            evac(css[:], pcp[0:32, :], r=[pcpk], w=["css"])
                            P.dma("sp", conv_s, css[:], r=["css"], sem="css")
                        gall = ["gcv%d" % c for c in range(4)]
                        for m in range(8):
                            ms = slice(m * 128, (m + 1) * 128)
                            pya, pyak = ps()
                            mmg(pya[:, 0:W], [(wco[:, c, ms], gcv[:, c, 0:W]) for c in range(4)], r=gall + ["wco"], w=[pyak])
                            pga, pgak = ps()
                            mmg(pga[:, 0:W], [(wg[:, k, ms], xT[:, k, 0:W]) for k in range(8)], r=xtall + ["wg"], w=[pgak])
                            pyb, pybk = ps()
                            mmg(pyb[:, 0:W], [(wao[:, c, ms], oT[:, c, t0:t0 + W]) for c in range(4)], r=["wao"], w=[pybk])
                            pgb, pgbk = ps()
                            mmg(pgb[:, 0:W], [(wg[:, k, 1024 + m * 128:1024 + (m + 1) * 128], xT[:, k, 0:W]) for k in range(8)], r=xtall + ["wg"], w=[pgbk])
                            P.op("act", lambda e, pga=pga: e.activation(out=sga[:, 0:W], in_=pga[:, 0:W], func=AF.Sigmoid), r=[pgak], w=["sga"])
                            P.op("act", lambda e, pgb=pgb: e.activation(out=sgb[:, 0:W], in_=pgb[:, 0:W], func=AF.Sigmoid), r=[pgbk], w=["sgb"])
                            P.op("dve", lambda e, pya=pya: e.tensor_tensor(out=za[:, 0:W], in0=sga[:, 0:W], in1=pya[:, 0:W], op=ALU.mult), r=[pyak, "sga"], w=["sga"])
                            P.op("dve", lambda e, pyb=pyb: e.tensor_tensor(out=zb[:, 0:W], in0=sgb[:, 0:W], in1=pyb[:, 0:W], op=ALU.mult), r=[pybk, "sgb"], w=["sgb"])
                            P.op("pool", lambda e, m=m: e.tensor_tensor(out=zT[:, m, 0:W], in0=za[:, 0:W], in1=zb[:, 0:W], op=ALU.add), r=["sga", "sgb"], w=["zT%d" % m])
                        zall = ["zT%d" % m for m in range(8)]
                        for b in range(nb):
                            gb = gb0 + b
                            xr_ = xr[blk_n % 2]
                            xrk = "xr%d" % (blk_n % 2)
                            y0_ = y0t[0]
                            y0k = "y0t0"
                            xb_ = x1Tb[blk_n % 2]
                            xbk = "x1Tb%d" % (blk_n % 2)
                            blk_n += 1
                            tsl = slice(b * 128, b * 128 + pw)
                            for half in range(2):
                                hs = slice(half * 512, (half + 1) * 512)
                                pm, pmk = ps()
                                mmg(pm[0:pw, :], [(zT[:, m, tsl], wo[:, m, hs]) for m in range(8)], r=zall + ["wo"], w=[pmk])
                                P.op("dve", lambda e, pm=pm, hs=hs, xr_=xr_, b=b: e.scalar_tensor_tensor(out=xr_[0:pw, hs], in0=xin[0:pw, b, hs], scalar=ALPHA, in1=pm[0:pw, :],
                                                                                                        op0=ALU.mult, op1=ALU.add), r=[pmk, "xin"], w=[xrk])
                            xrh = [xrk]
                            for half in range(2):
                                P.op("dve", lambda e, half=half, xr_=xr_: e.bn_stats(out=st6[0:pw, half, :], in_=xr_[0:pw, half * 512:(half + 1) * 512]), r=xrh, w=["st6_%d" % half])
                            P.op("dve", lambda e: e.bn_aggr(out=mv[0:pw, :], in_=st6[0:pw].rearrange("p a s -> p (a s)")), r=["st6_0", "st6_1"], w=["mv"])
                            P.op("dve", lambda e: e.tensor_scalar(out=rstd[0:pw, :], in0=mv[0:pw, 1:2], scalar1=EPS, scalar2=None, op0=ALU.add), r=["mv"], w=["rstd"])
                            P.op("act", lambda e: e.sqrt(out=rstd[0:pw, :], in_=rstd[0:pw, :]), r=["rstd"], w=["rstd"])
                            P.op("dve", lambda e: e.reciprocal(out=rstd[0:pw, :], in_=rstd[0:pw, :]), r=["rstd"], w=["rstd"])
                            P.op("dve", lambda e, xr_=xr_: e.tensor_scalar(out=xr_[0:pw, :], in0=xr_[0:pw, :], scalar1=mv[0:pw, 0:1], scalar2=rstd[0:pw, 0:1],
                                                                           op0=ALU.subtract, op1=ALU.mult), r=xrh + ["mv", "rstd"], w=[xrk])
                            P.op("pool", lambda e, xr_=xr_: e.tensor_tensor(out=xr_[0:pw, :], in0=xr_[0:pw, :], in1=g1b[0:pw, :], op=ALU.mult), r=[xrk, "g1b"], w=[xrk])
                            P.op("pool", lambda e, xr_=xr_: e.tensor_tensor(out=xr_[0:pw, :], in0=xr_[0:pw, :], in1=b1b[0:pw, :], op=ALU.add), r=[xrk, "b1b"], w=[xrk])
                            P.op("act", lambda e, xr_=xr_, y0_=y0_: e.mul(out=y0_[0:pw, :], in_=xr_[0:pw, :], mul=ALPHA), r=[xrk], w=[y0k])
                            P.dma("sp", y0_d[t0 + b * 128:t0 + b * 128 + pw, :], y0_[0:pw, :], r=[y0k], sem=y0k)
                            for half in range(2):
                                ptt, pttk = ps()
                                P.op("pe", lambda e, ptt=ptt, half=half, xr_=xr_: [e.transpose(ptt[:, kk * 128:kk * 128 + pw], xr_[0:pw, (half * 4 + kk) * 128:(half * 4 + kk + 1) * 128],
                                                                                               ident[0:pw, 0:pw]) for kk in range(4)][-1], r=[xrk, "ident"], w=[pttk])
                                pv4 = ptt[:].rearrange("p (k t) -> p k t", k=4)[:, :, 0:pw]
                                P.op("act", lambda e, pv4=pv4, half=half, xb_=xb_: e.copy(out=xb_[:, half * 4:half * 4 + 4, 0:pw], in_=pv4), r=[pttk], w=[xbk + "_%d" % half])
                                P.op("dve", lambda e, pv4=pv4, half=half: e.tensor_copy(out=x1Tf[:, half * 4:half * 4 + 4, 0:pw], in_=pv4), r=[pttk, xbk + "_%d" % half], w=["x1Tf_%d" % half])
                            P.dma("sp", x1T_d[:, :, t0 + b * 128:t0 + b * 128 + pw], xb_[:, :, 0:pw], r=[xbk + "_0", xbk + "_1"], sem=xbk)
                            prt, prtk = ps()
                            mmg(prt[0:pw, 0:20], [(x1Tf[:, k, 0:pw], wgr[:, k, :]) for k in range(8)], r=["x1Tf_0", "x1Tf_1", "wgr"], w=[prtk])
                            R = slice(0, pw)
                            P.op("dve", lambda e, prt=prt: e.tensor_copy(out=lg[R, :], in_=prt[R, 0:20]), r=[prtk], w=["lg"])
                            P.op("dve", lambda e: e.tensor_copy(out=g8[R, 0:4], in_=lg[R, 0:4]), r=["lg"], w=["g8"])
                            P.op("dve", lambda e: e.max(out=m8[R, :], in_=g8[R, :]), r=["g8"], w=["m8"])
                            P.op("dve", lambda e: e.tensor_scalar(out=ohg[R, :], in0=lg[R, 0:4], scalar1=m8[R, 0:1], scalar2=None, op0=ALU.is_equal), r=["lg", "m8"], w=["ohg"])
                            P.op("dve", lambda e: e.tensor_scalar(out=nm[R, 0:1], in0=m8[R, 0:1], scalar1=-1.0, scalar2=None, op0=ALU.mult), r=["m8"], w=["nm0"])
                            P.op("act", lambda e: e.activation(out=eg[R, :], in_=lg[R, 0:4], func=AF.Exp, bias=nm[R, 0:1], scale=1.0), r=["lg", "nm0"], w=["eg"])
                            P.op("dve", lambda e: e.reduce_sum(out=sgs[R, 0:1], in_=eg[R, :], axis=AX.X), r=["eg"], w=["sgs0"])
                            lg3 = lg[R, 4:20].rearrange("p (g x) -> p g x", x=4)
                            P.op("dve", lambda e, lg3=lg3: e.tensor_tensor(out=sel[R, :].rearrange("p (g x) -> p g x", x=4), in0=lg3,
                                                                           in1=ohg[R, :].unsqueeze(2).to_broadcast([pw, 4, 4]), op=ALU.mult), r=["lg", "ohg"], w=["sel"])
                            P.op("dve", lambda e: e.tensor_reduce(out=e8[R, 0:4], in_=sel[R, :].rearrange("p (g x) -> p x g", x=4), axis=AX.X, op=ALU.add), r=["sel"], w=["e8"])
                            P.op("dve", lambda e: e.max(out=me8[R, :], in_=e8[R, :]), r=["e8"], w=["me8"])
                            P.op("dve", lambda e: e.tensor_scalar(out=nm[R, 1:2], in0=me8[R, 0:1], scalar1=-1.0, scalar2=None, op0=ALU.mult), r=["me8"], w=["nm1"])
                            P.op("act", lambda e: e.activation(out=e1[R, :], in_=e8[R, 0:4], func=AF.Exp, bias=nm[R, 1:2], scale=1.0), r=["e8", "nm1"], w=["e1"])
                            P.op("dve", lambda e: e.tensor_scalar(out=s2m[R, :], in0=e8[R, 0:4], scalar1=me8[R, 1:2], scalar2=None, op0=ALU.is_ge), r=["e8", "me8"], w=["s2m"])
                            P.op("dve", lambda e: e.tensor_tensor(out=e1[R, :], in0=e1[R, :], in1=s2m[R, :], op=ALU.mult), r=["e1", "s2m"], w=["e1"])
                            P.op("dve", lambda e: e.reduce_sum(out=sgs[R, 1:2], in_=e1[R, :], axis=AX.X), r=["e1"], w=["sgs1"])
                            P.op("dve", lambda e: e.tensor_tensor(out=wfac[R, :], in0=sgs[R, 0:1], in1=sgs[R, 1:2], op=ALU.mult), r=["sgs0", "sgs1"], w=["wfac"])
                            P.op("dve", lambda e: e.reciprocal(out=wfac[R, :], in_=wfac[R, :]), r=["wfac"], w=["wfac"])
                            P.op("dve", lambda e: e.tensor_scalar(out=e1[R, :], in0=e1[R, :], scalar1=wfac[R, 0:1], scalar2=None, op0=ALU.mult), r=["e1", "wfac"], w=["e1"])
                            P.op("dve", lambda e, gb=gb: e.tensor_tensor(out=gate[R, gb, :].rearrange("p (g x) -> p g x", x=4),
                                                                         in0=ohg[R, :].unsqueeze(2).to_broadcast([pw, 4, 4]),
                                                                         in1=e1[R, :].unsqueeze(1).to_broadcast([pw, 4, 4]), op=ALU.mult), r=["ohg", "e1", "gate"], w=["gate%d" % gb])
                    dump("gate", gate[:], [128, 17, 16], r=["gate%d" % i for i in range(17)])
                    P.barrier()
            s_oT.close()

            if stage >= 5:
                with ExitStack() as s5:
                    x1T = sb("x1T", [128, 8, NTOK], BF16, s5)
                    yacc = sb("yacc", [128, 17, 1024], F32, s5)
                    g2b = sb("g2b", [128, 1024], F32, s5)
                    b2b = sb("b2b", [128, 1024], F32, s5)
                    w1e = [sb("w1e%d" % i, [128, 8, 512], BF16, s5) for i in range(2)]
                    w3e = [sb("w3e%d" % i, [128, 8, 512], BF16, s5) for i in range(2)]
                    w2e = [sb("w2e%d" % i, [128, 4, 1024], BF16, s5) for i in range(2)]
                    s1t = [sb("s1t%d" % i, [128, 512], F32, s5) for i in range(2)]
                    hT = [sb("hT%d" % i, [128, 4, 512], BF16, s5) for i in range(2)]
                    st6 = sb("st6b", [128, 2, 6], F32, s5)
                    mv = sb("mvb", [128, 2], F32, s5)
                    rstd = sb("rstdb", [128, 1], F32, s5)
                    P.dma("sp", x1T[:], x1T_d, w=["x1T"], sem="x1T")
                    P.dma("sp", yacc[:, 0:16, :], y0_d[0:2048, :].rearrange("(b p) d -> p b d", p=128), w=["yacc"], sem="yacc")
                    P.dma("sp", yacc[0:64, 16, :], y0_d[2048:2112, :], w=["yacc"], sem="yacc")
                    P.dma("sp", g2b[:], ln2_g.partition_broadcast(128), w=["g2b"], sem="g2b")
                    P.dma("sp", b2b[:], ln2_b.partition_broadcast(128), w=["b2b"], sem="b2b")

                    wst[:] = [sb("wstC%d" % i, [128, 2048], F32, s5) for i in range(2)]

                    def load_e(e_):
                        i = e_ % 2
                        return (wchunks(w1e[i], "w1e%d" % i, w1[e_], 8, 512, CH=2048) + wchunks(w3e[i], "w3e%d" % i, w3[e_], 8, 512, CH=2048)
                                + wchunks(w2e[i], "w2e%d" % i, w2[e_], 4, 1024, CH=2048))
                    for f_ in load_e(0):
                        f_()
                    ttiles = [(0, 512), (512, 512), (1024, 512), (1536, 512), (2048, 64)]
                    it = 0
                    for e_ in range(16):
                        pend = load_e(e_ + 1) if e_ + 1 < 16 else []
                        i = e_ % 2
                        a1, a3, a2 = w1e[i], w3e[i], w2e[i]
                        k1, k3, k2 = "w1e%d" % i, "w3e%d" % i, "w2e%d" % i
                        for (t0, W) in ttiles:
                            nb = (W + 127) // 128
                            pw = min(W, 128)
                            h_ = hT[it % 2]
                            hk = "hT%d" % (it % 2)
                            it += 1
                            for f in range(4):
                                fs = slice(f * 128, (f + 1) * 128)
                                p1, p1k = ps()
                                mmg(p1[:, 0:W], [(a1[:, k, fs], x1T[:, k, t0:t0 + W]) for k in range(8)], r=["x1T", k1], w=[p1k])
                                p3, p3k = ps()
                                mmg(p3[:, 0:W], [(a3[:, k, fs], x1T[:, k, t0:t0 + W]) for k in range(8)], r=["x1T", k3], w=[p3k])
                                s_ = s1t[f % 2]
                                sk = "s1t%d" % (f % 2)
                                P.op("act", lambda e, p1=p1, s_=s_: e.activation(out=s_[:, 0:W], in_=p1[:, 0:W], func=AF.Silu), r=[p1k], w=[sk])
                                P.op("dve", lambda e, p3=p3, s_=s_, f=f, h_=h_: e.tensor_tensor(out=h_[:, f, 0:W], in0=s_[:, 0:W], in1=p3[:, 0:W], op=ALU.mult),
                                     r=[p3k, sk], w=[hk + "_%d" % f])
                            for f_ in pend[:2 if t0 == 0 else 1]:
                                f_()
                            pend = pend[2 if t0 == 0 else 1:]
                            hall = [hk + "_%d" % f for f in range(4)]
                            for b in range(nb):
                                gb = t0 // 128 + b
                                for half in range(2):
                                    hs = slice(half * 512, (half + 1) * 512)
                                    py, pyk = ps()
                                    mmg(py[0:pw, :], [(h_[:, f, b * 128:b * 128 + pw], a2[:, f, hs]) for f in range(4)], r=hall + [k2], w=[pyk])
                                    P.op("dve", lambda e, py=py, gb=gb, hs=hs: e.scalar_tensor_tensor(out=yacc[0:pw, gb, hs], in0=py[0:pw, :], scalar=gate[0:pw, gb, e_:e_ + 1],
                                                                                                     in1=yacc[0:pw, gb, hs], op0=ALU.mult, op1=ALU.add),
                                         r=[pyk, "yacc"], w=["yacc%d_%d" % (gb, half)])
                    for gb in range(17):
                        pw = 128 if gb < 16 else 64
                        yk = ["yacc%d_0" % gb, "yacc%d_1" % gb]
                        ya = yacc[0:pw, gb, :]
                        for half in range(2):
                            P.op("dve", lambda e, half=half, gb=gb, pw=pw: e.bn_stats(out=st6[0:pw, half, :], in_=yacc[0:pw, gb, half * 512:(half + 1) * 512]), r=yk, w=["st6_%d" % half])
                        P.op("dve", lambda e, pw=pw: e.bn_aggr(out=mv[0:pw, :], in_=st6[0:pw].rearrange("p a s -> p (a s)")), r=["st6_0", "st6_1"], w=["mv"])
                        P.op("dve", lambda e, pw=pw: e.tensor_scalar(out=rstd[0:pw, :], in0=mv[0:pw, 1:2], scalar1=EPS, scalar2=None, op0=ALU.add), r=["mv"], w=["rstd"])
                        P.op("act", lambda e, pw=pw: e.sqrt(out=rstd[0:pw, :], in_=rstd[0:pw, :]), r=["rstd"], w=["rstd"])
                        P.op("dve", lambda e, pw=pw: e.reciprocal(out=rstd[0:pw, :], in_=rstd[0:pw, :]), r=["rstd"], w=["rstd"])
                        P.op("dve", lambda e, ya=ya, pw=pw: e.tensor_scalar(out=ya, in0=ya, scalar1=mv[0:pw, 0:1], scalar2=rstd[0:pw, 0:1],
                                                                           op0=ALU.subtract, op1=ALU.mult), r=yk + ["mv", "rstd"], w=["yo%d" % gb])
                        P.op("pool", lambda e, ya=ya, pw=pw: e.tensor_tensor(out=ya, in0=ya, in1=g2b[0:pw, :], op=ALU.mult), r=["yo%d" % gb, "g2b"], w=["yo%d" % gb])
                        P.op("pool", lambda e, ya=ya, pw=pw: e.tensor_tensor(out=ya, in0=ya, in1=b2b[0:pw, :], op=ALU.add), r=["yo%d" % gb, "b2b"], w=["yo%d" % gb])
                        if gb < 16:
                            P.dma("sp", y_own[gb * 128:(gb + 1) * 128, :], ya, r=["yo%d" % gb], sem="yo%d" % (gb % 4))
                        else:
                            P.dma("sp", y_s, ya, r=["yo%d" % gb], sem="yo%d" % (gb % 4))
                    P.barrier()
        except StopBuild:
            pass
        P.finish()
    nc._dbg_out = list(dbg_out.keys())
    return nc


def make_in_maps(inputs, cores):
    f32 = np.float32
    xp = np.asarray(inputs["x_prompt"], f32)
    xs = np.asarray(inputs["x_sample"], f32)
    ck = np.asarray(inputs["cache_k"], f32)
    npool = ck.shape[1]
    ck = ck.reshape(npool * 128, 512)
    cv = np.asarray(inputs["cache_v"], f32).reshape(npool * 128, 512)
    cf = np.asarray(inputs["cache_logf"], f32).reshape(npool * 128, 8)
    sc = np.asarray(inputs["state_conv"], f32)[0]
    pt = np.asarray(inputs["page_table"], np.int32)
    g = lambda k: np.asarray(inputs[k], f32)
    common = {
        "cache_k": ck, "cache_v": cv, "cache_f": cf,
        "w_in": g("w_in")[0], "b_f": g("b_f").reshape(1, 8),
        "conv_w": g("conv_w")[0], "w_conv_out": g("w_conv_out")[0],
        "w_attn_out": g("w_attn_out")[0], "w_o": g("w_o")[0],
        "ln1_g": g("ln1_g").reshape(1, D), "ln1_b": g("ln1_b").reshape(1, D),
        "w_gr": np.ascontiguousarray(np.concatenate([g("w_group")[0], g("w_router")[0]], axis=1)),
        "w1": g("w1")[0], "w3": g("w3")[0], "w2": g("w2")[0],
        "ln2_g": g("ln2_g").reshape(1, D), "ln2_b": g("ln2_b").reshape(1, D),
    }
    in_maps = []
    for c in cores:
        s, r = c // 2, c % 2
        own, oth = OWN[r], OWN[1 - r]
        xt = xp[s].reshape(8, 512, D)
        halo = np.zeros((8, D), f32)
        for sl, T in enumerate(own):
            if T > 0:
                halo[2 * sl:2 * sl + 2] = xp[s, 512 * T - 2:512 * T]
        rcv = np.zeros((1, 80), f32)
        order = own + oth
        for p_ in range(8):
            for p2 in range(8):
                rcv[0, p_ * 8 + p2] = 1.0 if order[p_] < order[p2] else 0.0
        for sl in range(4):
            rcv[0, 64 + sl] = 0.0 if oth[sl] < own[sl] else NEG
        m = dict(common)
        m.update({
            "x_own": np.ascontiguousarray(xt[own].reshape(2048, D)),
            "x_oth": np.ascontiguousarray(xt[oth].reshape(2048, D)),
            "x_halo": halo,
            "x_s": np.ascontiguousarray(xs[16 * c:16 * c + 16].reshape(64, D)),
            "state_conv": np.ascontiguousarray(sc[16 * c:16 * c + 16].reshape(32, 512)),
            "page_table": np.ascontiguousarray(pt[16 * c:16 * c + 16].reshape(1, 256)),
            "rolec": rcv,
        })
        in_maps.append(m)
    return in_maps, npool


def assemble(res, cores, nseq=4, nsamp=128):
    f32 = np.float32
    y_p = np.zeros((nseq, 4096, D), f32)
    k_p = np.zeros((1, nseq, 4096, 8, 64), f32)
    v_p = np.zeros((1, nseq, 4096, 8, 64), f32)
    f_p = np.zeros((1, nseq, 4096, 8), f32)
    c_p = np.zeros((1, nseq, 2, 512), f32)
    y_sm = np.zeros((nsamp, 4, D), f32)
    k_sm = np.zeros((1, nsamp, 4, 8, 64), f32)
    v_sm = np.zeros((1, nsamp, 4, 8, 64), f32)
    f_sm = np.zeros((1, nsamp, 4, 8), f32)
    c_sm = np.zeros((1, nsamp, 2, 512), f32)
    for o, c in zip(res, cores):
        s, r = c // 2, c % 2
        for sl, T in enumerate(OWN[r]):
            rows = slice(512 * T, 512 * T + 512)
            y_p[s, rows] = o["y_own"][sl * 512:(sl + 1) * 512]
            k_p[0, s, rows] = o["k_own"][sl * 512:(sl + 1) * 512].reshape(512, 8, 64)
            v_p[0, s, rows] = o["v_own"][sl * 512:(sl + 1) * 512].reshape(512, 8, 64)
            f_p[0, s, rows] = o["f_own"][sl * 512:(sl + 1) * 512]
        if r == 0:
            c_p[0, s] = o["conv_p"]
        sl_ = slice(16 * c, 16 * c + 16)
        y_sm[sl_] = o["y_s"].reshape(16, 4, D)
        k_sm[0, sl_] = o["k_s"].reshape(16, 4, 8, 64)
        v_sm[0, sl_] = o["v_s"].reshape(16, 4, 8, 64)
        f_sm[0, sl_] = o["f_s"].reshape(16, 4, 8)
        c_sm[0, sl_] = o["conv_s"].reshape(16, 2, 512)
    return (y_p, y_sm, k_p, v_p, f_p, c_p, k_sm, v_sm, f_sm, c_sm)


_NC = {}


def kernel(**inputs):
    cores = list(range(8))
    in_maps, npool = make_in_maps(inputs, cores)
    if npool not in _NC:
        _NC[npool] = build_nc(npool)
    res = run_bass_kernel_spmd(_NC[npool], in_maps, core_ids=cores).results
    return assemble(res, cores)
```

```python
import os
import numpy as np
from contextlib import ExitStack
import concourse.bass as bass
import concourse.mybir as mybir
from concourse.bass_utils import run_bass_kernel_spmd

F32 = mybir.dt.float32
BF16 = mybir.dt.bfloat16
I32 = mybir.dt.int32
AF = mybir.ActivationFunctionType
ALU = mybir.AluOpType
AX = mybir.AxisListType

D = 1024
NIN = 5128
OWN = [[0, 3, 4, 7], [1, 2, 5, 6]]
ALPHA = 2.0 ** 0.25
EPS = 1e-5
NEG = -30000.0
C_XC, C_BG, C_CG, C_Q, C_K, C_V, C_F, C_GA, C_GB = 0, 512, 1024, 1536, 2048, 2560, 3072, 3080, 4104
NTOK = 2112


class StopBuild(Exception):
    pass


class Prog:
    def __init__(self, nc, es):
        self.nc = nc
        self.es = es
        self.eng = {"pe": nc.tensor, "act": nc.scalar, "dve": nc.vector, "pool": nc.gpsimd, "sp": nc.sync}
        self.sem = {k: es.enter_context(nc.semaphore("s_" + k)) for k in self.eng}
        self.cnt = {k: 0 for k in self.eng}
        self.waited = {k: {} for k in self.eng}
        self.last_w = {}
        self.readers = {}
        self.dsem = {}
        self.dcnt = {}
        self.dead = False

    def _deps(self, r, w):
        deps = []
        for k in r:
            if k in self.last_w:
                deps.append(self.last_w[k])
        for k in w:
            if k in self.last_w:
                deps.append(self.last_w[k])
            deps.extend(self.readers.get(k, ()))
        return deps

    def _wait(self, eng, deps):
        best = {}
        for (s, v) in deps:
            if eng == "pe" and s == "pe":
                continue
            if v > best.get(s, 0):
                best[s] = v
        for s, v in best.items():
            if self.waited[eng].get(s, 0) < v:
                semh = self.sem[s] if s in self.sem else self.dsem[s]
                self.eng[eng].wait_ge(semh, v)
                self.waited[eng][s] = v

    def _record(self, tok, r, w):
        for k in w:
            self.last_w[k] = tok
            self.readers[k] = []
        for k in r:
            if k not in w:
                self.readers.setdefault(k, []).append(tok)

    def op(self, eng, fn, r=(), w=()):
        if self.dead:
            return
        self._wait(eng, self._deps(r, w))
        ins = fn(self.eng[eng])
        self.cnt[eng] += 1
        ins.then_inc(self.sem[eng], 1)
        self._record((eng, self.cnt[eng]), r, w)

    def _dsem(self, sem):
        if sem not in self.dsem:
            self.dsem[sem] = self.es.enter_context(self.nc.semaphore("d_" + sem))
            self.dcnt[sem] = 0

    def dma(self, q, out, in_, r=(), w=(), sem=None, **kw):
        if self.dead:
            return
        self._dsem(sem)
        self._wait(q, self._deps(r, w))
        ins = self.eng[q].dma_start(out=out, in_=in_, **kw)
        self.dcnt[sem] += 16
        ins.then_inc(self.dsem[sem], 16)
        self._record((sem, self.dcnt[sem]), r, w)

    def idma(self, out, in_, idx, r=(), w=(), sem=None):
        if self.dead:
            return
        self._dsem(sem)
        self._wait("pool", self._deps(r, w))
        ins = self.nc.gpsimd.indirect_dma_start(
            out=out, out_offset=None, in_=in_, in_offset=bass.IndirectOffsetOnAxis(ap=idx, axis=0))
        self.dcnt[sem] += 16
        ins.then_inc(self.dsem[sem], 16)
        self._record((sem, self.dcnt[sem]), r, w)

    def _all(self):
        toks = [(k, self.cnt[k]) for k in self.eng if self.cnt[k] > 0]
        toks += [(k, self.dcnt[k]) for k in self.dsem if self.dcnt[k] > 0]
        return toks

    def barrier(self):
        if self.dead:
            return
        toks = self._all()
        for e in self.eng:
            self._wait(e, toks)
        self.last_w = {}
        self.readers = {}

    def finish(self):
        self._wait("sp", self._all())


def build_nc(npool=2560, stage=99, dbg=False):
    nc = bass.Bass("TRN2", target_bir_lowering=False)

    def din(name, shape, dt=F32):
        return nc.dram_tensor(name, list(shape), dt, kind="ExternalInput").ap()

    def dout(name, shape, dt=F32):
        return nc.dram_tensor(name, list(shape), dt, kind="ExternalOutput").ap()

    x_own = din("x_own", [2048, D])
    x_oth = din("x_oth", [2048, D])
    x_halo = din("x_halo", [8, D])
    x_s = din("x_s", [64, D])
    cache_k = din("cache_k", [npool * 128, 512])
    cache_v = din("cache_v", [npool * 128, 512])
    cache_f = din("cache_f", [npool * 128, 8])
    state_conv = din("state_conv", [32, 512])
    page_table = din("page_table", [1, 256], I32)
    rolec = din("rolec", [1, 80])
    w_in = din("w_in", [D, NIN])
    b_f = din("b_f", [1, 8])
    conv_w = din("conv_w", [3, 512])
    w_conv_out = din("w_conv_out", [512, D])
    w_attn_out = din("w_attn_out", [512, D])
    w_o = din("w_o", [D, D])
    ln1_g = din("ln1_g", [1, D])
    ln1_b = din("ln1_b", [1, D])
    w_gr = din("w_gr", [D, 20])
    w1 = din("w1", [16, D, 512])
    w3 = din("w3", [16, D, 512])
    w2 = din("w2", [16, 512, D])
    ln2_g = din("ln2_g", [1, D])
    ln2_b = din("ln2_b", [1, D])

    y_own = dout("y_own", [2048, D])
    y_s = dout("y_s", [64, D])
    k_own = dout("k_own", [2048, 512])
    v_own = dout("v_own", [2048, 512])
    f_own = dout("f_own", [2048, 8])
    conv_p = dout("conv_p", [2, 512])
    k_s = dout("k_s", [64, 512])
    v_s = dout("v_s", [64, 512])
    f_s = dout("f_s", [64, 8])
    conv_s = dout("conv_s", [32, 512])
    y0_d = nc.dram_tensor("y0_d", [NTOK, D], F32, kind="Internal").ap()
    x1T_d = nc.dram_tensor("x1T_d", [128, 8, NTOK], BF16, kind="Internal").ap()
    dbg_out = {}

    with ExitStack() as es:
        P = Prog(nc, es)

        try:
            def sb(name, shape, dt=F32, stack=es):
                return stack.enter_context(nc.sbuf_tensor(name, list(shape), dt))

            def chk(x):
                if stage < x:
                    P.dead = True

            def dump(name, ap, shape, dt=F32, r=()):
                if dbg:
                    d = dout("dbg_" + name, shape, dt)
                    dbg_out[name] = d
                    P.dma("sp", d, ap, r=list(r), sem="dbg_" + name)

            psf = [es.enter_context(nc.psum_tensor("ps%d" % i, [128, 512], F32)) for i in range(8)]
            ps_rr = [0]
            ps_n = [8]

            def ps():
                i = ps_rr[0] % ps_n[0]
                ps_rr[0] += 1
                return psf[i], "ps%d" % i

            ev_rr = [0]

            def evac(out, in_, r, w):
                ev_rr[0] += 1
                if ev_rr[0] % 2:
                    P.op("act", lambda e: e.copy(out=out, in_=in_), r=r, w=w)
                else:
                    P.op("dve", lambda e: e.tensor_copy(out=out, in_=in_), r=r, w=w)

            def mmg(out, pairs, r, w):
                n = len(pairs)

                def g(e):
                    ins = None
                    for i, (a, b) in enumerate(pairs):
                        ins = e.matmul(out, lhsT=a, rhs=b, start=(i == 0), stop=(i == n - 1))
                    return ins
                P.op("pe", g, r=r, w=w)

            ident = sb("ident", [128, 128])
            ident_b = sb("ident_b", [128, 128], BF16)
            U_f = sb("U_f", [128, 128])
            U_b = sb("U_b", [128, 128], BF16)
            SU4 = sb("SU4", [4, 4])
            ones_f = sb("ones_f", [128, 128])
            ones_b = sb("ones_b", [128, 1], BF16)
            sel127 = sb("sel127", [128, 128])
            hmask = sb("hmask", [32, 8])
            io_i = sb("io_i", [128, 1], I32)
            io_f = sb("io_f", [128, 1])
            bfb = sb("bfb", [128, 8])
            rc = sb("rc", [128, 80])
            LF = sb("LF", [128, 33, 8])
            gate = sb("gate", [128, 17, 16])
            pool_ops = [
                (lambda e: e.memset(ident[:], 1.0), [], ["ident"]),
                (lambda e: e.affine_select(out=ident[:], in_=ident[:], pattern=[[-1, 128]], compare_op=ALU.is_equal,
                                           fill=0.0, base=0, channel_multiplier=1), ["ident"], ["ident"]),
                (lambda e: e.tensor_copy(out=ident_b[:], in_=ident[:]), ["ident"], ["ident_b"]),
                (lambda e: e.memset(U_f[:], 1.0), [], ["U_f"]),
                (lambda e: e.affine_select(out=U_f[:], in_=U_f[:], pattern=[[1, 128]], compare_op=ALU.is_ge,
                                           fill=0.0, base=0, channel_multiplier=-1), ["U_f"], ["U_f"]),
                (lambda e: e.tensor_copy(out=U_b[:], in_=U_f[:]), ["U_f"], ["U_b"]),
                (lambda e: e.memset(SU4[:], 1.0), [], ["SU4"]),
                (lambda e: e.affine_select(out=SU4[:], in_=SU4[:], pattern=[[-1, 4]], compare_op=ALU.is_gt,
                                           fill=0.0, base=0, channel_multiplier=1), ["SU4"], ["SU4"]),
                (lambda e: e.memset(ones_f[:], 1.0), [], ["ones_f"]),
                (lambda e: e.memset(ones_b[:], 1.0), [], ["ones_b"]),
                (lambda e: e.memset(sel127[:], 1.0), [], ["sel127"]),
                (lambda e: e.affine_select(out=sel127[:], in_=sel127[:], pattern=[[0, 128]], compare_op=ALU.is_equal,
                                           fill=0.0, base=-127, channel_multiplier=1), ["sel127"], ["sel127"]),
                (lambda e: e.memset(hmask[:], 1.0), [], ["hmask"]),
                (lambda e: e.affine_select(out=hmask[:], in_=hmask[:], pattern=[[-4, 8]], compare_op=ALU.is_ge,
                                           fill=0.0, base=0, channel_multiplier=1), ["hmask"], ["hmask"]),
                (lambda e: e.affine_select(out=hmask[:], in_=hmask[:], pattern=[[4, 8]], compare_op=ALU.is_ge,
                                           fill=0.0, base=3, channel_multiplier=-1), ["hmask"], ["hmask"]),
                (lambda e: e.iota(out=io_i[:], pattern=[[0, 1]], base=0, channel_multiplier=1), [], ["io_i"]),
                (lambda e: e.tensor_copy(out=io_f[:], in_=io_i[:]), ["io_i"], ["io_f"]),
                (lambda e: e.memset(LF[:], 0.0), [], ["LFall"]),
                (lambda e: e.memset(gate[:], 0.0), [], ["gate"]),
            ]
            for fn, r_, w_ in pool_ops:
                P.op("pool", fn, r=r_, w=w_)
            P.dma("sp", bfb[:], b_f.partition_broadcast(128), w=["bfb"], sem="c0")
            P.dma("sp", rc[:], rolec.partition_broadcast(128), w=["rc"], sem="c1")
            P.barrier()
            chk(0.1)

            s_oT = ExitStack()
            es.enter_context(s_oT)
            oT = sb("oT", [128, 4, NTOK], BF16, s_oT)
            s_A = ExitStack()
            es.enter_context(s_A)
            if os.environ.get("KFIRST"):
                kT = sb("kT", [128, 4, int(os.environ.get("KTN", "4224"))], BF16, s_A)
                qT = sb("qT", [128, 4, NTOK], BF16, s_A)
            else:
                qT = sb("qT", [128, 4, NTOK], BF16, s_A)
                kT = sb("kT", [128, 4, int(os.environ.get("KTN", "4224"))], BF16, s_A)
            Vt = sb("Vt", [128, 33, 8, 66], BF16, s_A)
            Cc = sb("Cc", [128, 32, 8], F32, s_A)
            BIAS = sb("BIAS", [128, 4, 32, 8], F32, s_A)
            P.op("pool", lambda e: e.memset(Vt[:, :, :, 64:65], 1.0), w=["Vones"])

            wst = []
            wst_rr = [0]

            def wchunks(dst, dkey, src, K, N, col0=0, dcol0=0, CH=1024):
                th = []
                if N <= CH:
                    g = max(1, CH // N)
                    for k0 in range(0, K, g):
                        kk = min(g, K - k0)

                        def f(k0=k0, kk=kk):
                            i = wst_rr[0] % len(wst)
                            wst_rr[0] += 1
                            st, sk = wst[i], "wst%d" % i
                            sv = st[:, 0:kk * N].rearrange("p (k n) -> p k n", n=N)
                            P.dma("sp", sv, src[k0 * 128:(k0 + kk) * 128, col0:col0 + N].rearrange("(k p) n -> p k n", p=128), w=[sk], sem=sk)
                            P.op("act", lambda e: e.copy(out=dst[:, k0:k0 + kk, dcol0:dcol0 + N], in_=sv), r=[sk], w=[dkey])
                        th.append(f)
                else:
                    for k in range(K):
                        for c0 in range(0, N, CH):
                            n = min(CH, N - c0)

                            def f(k=k, c0=c0, n=n):
                                i = wst_rr[0] % len(wst)
                                wst_rr[0] += 1
                                st, sk = wst[i], "wst%d" % i
                                P.dma("sp", st[:, 0:n], src[k * 128:(k + 1) * 128, col0 + c0:col0 + c0 + n], w=[sk], sem=sk)
                                P.op("act", lambda e: e.copy(out=dst[:, k, dcol0 + c0:dcol0 + c0 + n], in_=st[:, 0:n]), r=[sk], w=[dkey])
                            th.append(f)
                return th

            def wload(*a, **kw):
                for f in wchunks(*a, **kw):
                    f()

            def load_xT(src, W, xi, xt, xik, xtk):
                nb = (W + 127) // 128
                pw = min(W, 128)
                if W == 512:
                    P.dma("sp", xi[:], src.rearrange("(b p) d -> p b d", p=128), w=[xik], sem=xik)
                else:
                    P.dma("sp", xi[0:pw, 0, :], src, w=[xik], sem=xik)
                for b in range(nb):
                    for half in range(2):
                        pt, ptk = ps()

                        def tr(e, pt=pt, b=b, half=half):
                            ins = None
                            for kk in range(4):
                                k = half * 4 + kk
                                ins = e.transpose(pt[:, kk * 128:kk * 128 + pw], xi[0:pw, b, k * 128:(k + 1) * 128], ident[0:pw, 0:pw])
                            return ins
                        P.op("pe", tr, r=[xik, "ident"], w=[ptk])
                        evac(xt[:, half * 4:half * 4 + 4, b * 128:b * 128 + pw],
                             pt[:].rearrange("p (k t) -> p k t", k=4)[:, :, 0:pw], r=[ptk], w=[xtk + "_%d_%d" % (b, half)])
                return [xtk + "_%d_%d" % (b, h) for b in range(nb) for h in range(2)]

            with ExitStack() as s1:
                wq = sb("wq", [128, 8, 512], BF16, s1)
                wk = sb("wk", [128, 8, 512], BF16, s1)
                wv = sb("wv", [128, 8, 512], BF16, s1)
                wf = sb("wf", [128, 8, 8], BF16, s1)
                wst[:] = [sb("wstA%d" % i, [128, 1024], F32, s1) for i in range(2)]
                for (t, c0, n, nm) in ((wq, C_Q, 512, "wq"), (wk, C_K, 512, "wk"), (wv, C_V, 512, "wv"), (wf, C_F, 8, "wf")):
                    wload(t, nm, w_in, 8, n, col0=c0, CH=1024)
                xin = [sb("xin%d" % i, [128, 4, 1024], F32, s1) for i in range(2)]
                xT = [sb("xT%d" % i, [128, 8, 512], BF16, s1) for i in range(2)]
                stg = [sb("stg%d" % i, [128, 512], F32, s1) for i in range(4)]
                ftmp = [sb("ftmp%d" % i, [128, 8], F32, s1) for i in range(2)]
                stg_rr = [0]
                tiles = []
                for s_ in range(4):
                    tiles.append((x_own[s_ * 512:(s_ + 1) * 512, :], 512, True, s_ * 512, s_ * 512, s_ * 4, s_ * 512, k_own, v_own))
                tiles.append((x_s, 64, True, 2048, 4096, 32, 0, k_s, v_s))
                for s_ in range(4):
                    tiles.append((x_oth[s_ * 512:(s_ + 1) * 512, :], 512, False, None, 2048 + s_ * 512, 16 + s_ * 4, None, None, None))

                for ti, (src, W, own, q0, k0, b0, r0, ko, vo) in enumerate(tiles):
                    if ti == 0:
                        chk(0.2)
                    if ti == 1:
                        chk(0.3)
                    if ti == 5:
                        chk(0.4)
                    xi, xt = xin[ti % 2], xT[ti % 2]
                    xik, xtk = "xin%d" % (ti % 2), "xT%d" % (ti % 2)
                    nb = (W + 127) // 128
                    pw = min(W, 128)
                    xtall = load_xT(src, W, xi, xt, xik, xtk)
                    if ti == 0:
                        chk(0.21)

                    def proj_fm(wt, wnm, dst, d0):
                        for hp in range(4):
                            pt, ptk = ps()
                            mmg(pt[:, 0:W], [(wt[:, k, hp * 128:(hp + 1) * 128], xt[:, k, 0:W]) for k in range(8)], r=xtall + [wnm], w=[ptk])
                            evac(dst[:, hp, d0:d0 + W], pt[:, 0:W], r=[ptk], w=["%s_%d_%d" % (dst.name, hp, d0)])


                    if own and not os.environ.get("SKIPQ"):
                        proj_fm(wq, "wq", qT, q0)
                    if ti == 0:
                        chk(0.22)
                    proj_fm(wq if os.environ.get("USEWQ") else wk, "wq" if os.environ.get("USEWQ") else "wk", kT, k0)
                    if ti == 0:
                        chk(0.23)
                    for b in range(nb):
                        xtb = [xtk + "_%d_%d" % (b, h) for h in range(2)]
                        tsl = slice(b * 128, b * 128 + pw)
                        pt, ptk = ps()
                        mmg(pt[0:pw, :], [(xt[:, k, tsl], wv[:, k, :]) for k in range(8)], r=xtb + ["wv"], w=[ptk])
                        P.op("act", lambda e, pt=pt, b=b: e.copy(out=Vt[0:pw, b0 + b, :, 0:64], in_=pt[0:pw, :].rearrange("p (h d) -> p h d", h=8)),
                             r=[ptk], w=["Vt%d" % (b0 + b)])
                        if ti == 0 and b == 0:
                            chk(0.2311)
                        if own:
                            si = stg_rr[0] % 4
                            stg_rr[0] += 1
                            P.op("dve", lambda e, pt=pt, si=si: e.tensor_copy(out=stg[si][0:pw, :], in_=pt[0:pw, :]), r=[ptk, "Vt%d" % (b0 + b)], w=["stg%d" % si])
                            P.dma("sp", vo[r0 + b * 128:r0 + b * 128 + pw, :], stg[si][0:pw, :], r=["stg%d" % si], sem="stg%d" % si)
                            if ti == 0 and b == 0:
                                chk(0.2312)
                            pt, ptk = ps()
                            mmg(pt[0:pw, :], [(xt[:, k, tsl], wk[:, k, :]) for k in range(8)], r=xtb + ["wk"], w=[ptk])
                            si = stg_rr[0] % 4
                            stg_rr[0] += 1
                            evac(stg[si][0:pw, :], pt[0:pw, :], r=[ptk], w=["stg%d" % si])
                            P.dma("sp", ko[r0 + b * 128:r0 + b * 128 + pw, :], stg[si][0:pw, :], r=["stg%d" % si], sem="stg%d" % si)
                        if ti == 0 and b == 0:
                            chk(0.232)
                        pt, ptk = ps()
                        mmg(pt[0:pw, 0:8], [(xt[:, k, tsl], wf[:, k, :]) for k in range(8)], r=xtb + ["wf"], w=[ptk])
                        ft = ftmp[b % 2]
                        fk = "ftmp%d" % (b % 2)
                        P.op("dve", lambda e, pt=pt, ft=ft: e.tensor_tensor(out=ft[0:pw, :], in0=pt[0:pw, 0:8], in1=bfb[0:pw, :], op=ALU.add),
                             r=[ptk, "bfb"], w=[fk])
                        if ti == 0 and b == 0:
                            chk(0.233)
                        P.op("act", lambda e, ft=ft: e.activation(out=ft[0:pw, :], in_=ft[0:pw, :], func=AF.Exp, scale=-1.0), r=[fk], w=[fk])
                        if ti == 0 and b == 0:
                            chk(0.234)
                        P.op("act", lambda e, ft=ft: e.activation(out=ft[0:pw, :], in_=ft[0:pw, :], func=AF.Ln, bias=1.0, scale=1.0), r=[fk], w=[fk])
                        P.op("dve", lambda e, ft=ft, bb=b0 + b: e.tensor_scalar(out=LF[0:pw, bb, :], in0=ft[0:pw, :], scalar1=-1.0, scalar2=None, op0=ALU.mult),
                             r=[fk, "LFall"], w=["LF%d" % (b0 + b)])
                lfall = ["LF%d" % i for i in range(33)]
                P.dma("sp", f_own.rearrange("(b p) h -> p b h", p=128), LF[:, 0:16, :], r=lfall, sem="fo")
                P.dma("sp", f_s, LF[0:64, 32, :], r=lfall, sem="fs")
                P.barrier()
                chk(0.5)

            with ExitStack() as s2:
                T1 = sb("T1", [128, 8, 8], F32, s2)
                Lprev = sb("Lprev", [128, 8, 8], F32, s2)
                Ltmp = sb("Ltmp", [128, 8, 8], F32, s2)
                Lfull = sb("Lfull", [128, 32, 8], F32, s2)
                Cb = sb("Cb", [128, 4, 8], F32, s2)
                LF4 = LF[:, 0:32, :].rearrange("p (t j) h -> p t j h", j=4)
                Lf4 = Lfull[:].rearrange("p (t j) h -> p t j h", j=4)
                P.op("dve", lambda e: e.tensor_tensor(out=T1[:], in0=LF4[:, :, 0, :], in1=LF4[:, :, 1, :], op=ALU.add), r=[], w=["T1"])
                P.op("dve", lambda e: e.tensor_tensor(out=T1[:], in0=T1[:], in1=LF4[:, :, 2, :], op=ALU.add), r=["T1"], w=["T1"])
                P.op("dve", lambda e: e.tensor_tensor(out=T1[:], in0=T1[:], in1=LF4[:, :, 3, :], op=ALU.add), r=["T1"], w=["T1"])
                rcE = rc[:, 0:64].rearrange("q (a b) -> q a b", b=8)
                for pp in range(8):
                    in0 = T1[:, pp, :].unsqueeze(1).to_broadcast([128, 8, 8])
                    in1 = rcE[:, pp, :].unsqueeze(2).to_broadcast([128, 8, 8])
                    if pp == 0:
                        P.op("dve", lambda e, in0=in0, in1=in1: e.tensor_tensor(out=Lprev[:], in0=in0, in1=in1, op=ALU.mult), r=["T1"], w=["Lprev"])
                    else:
                        P.op("dve", lambda e, in0=in0, in1=in1: e.tensor_tensor(out=Ltmp[:], in0=in0, in1=in1, op=ALU.mult), r=["T1"], w=["Ltmp"])
                        P.op("dve", lambda e: e.tensor_tensor(out=Lprev[:], in0=Lprev[:], in1=Ltmp[:], op=ALU.add), r=["Ltmp", "Lprev"], w=["Lprev"])
                P.op("dve", lambda e: e.tensor_copy(out=Lf4[:, :, 0, :], in_=Lprev[:]), r=["Lprev"], w=["Lfull"])
                for j in range(1, 4):
                    P.op("dve", lambda e, j=j: e.tensor_tensor(out=Lf4[:, :, j, :], in0=Lf4[:, :, j - 1, :], in1=LF4[:, :, j - 1, :], op=ALU.add),
                         r=["Lfull"], w=["Lfull"])
                pc, pck = ps()
                mmg(pc[:, 0:256], [(U_f[:], LF[:, 0:32, :].rearrange("p b h -> p (b h)")), (ones_f[:], Lfull[:].rearrange("p b h -> p (b h)"))],
                    r=["Lfull"], w=[pck])
                P.op("dve", lambda e: e.tensor_copy(out=Cc[:].rearrange("p b h -> p (b h)"), in_=pc[:, 0:256]), r=[pck], w=["Cc"])
                Cc4 = Cc[:].rearrange("p (t j) h -> p t j h", j=4)
                pcb, pcbk = ps()
                mmg(pcb[:, 0:32].rearrange("p (a b) -> p a b", b=8), [(sel127[:], Cc4[:, 0:4, 1, :])], r=["Cc"], w=[pcbk])
                P.op("dve", lambda e: e.tensor_copy(out=Cb[:].rearrange("p a b -> p (a b)"), in_=pcb[:, 0:32]), r=[pcbk], w=["Cb"])
                for sg in range(4):
                    P.op("dve", lambda e, sg=sg: e.scalar_tensor_tensor(out=BIAS[:, sg], in0=Cc[:], scalar=-1.0,
                                                                         in1=Cb[:, sg, :].unsqueeze(1).to_broadcast([128, 32, 8]),
                                                                         op0=ALU.mult, op1=ALU.add), r=["Cc", "Cb"], w=["BIAS"])
                    P.op("dve", lambda e, sg=sg: e.tensor_scalar(out=BIAS[:, sg, 16 + 4 * sg:20 + 4 * sg, :], in0=BIAS[:, sg, 16 + 4 * sg:20 + 4 * sg, :],
                                                                  scalar1=rc[:, 64 + sg:65 + sg], scalar2=None, op0=ALU.add), r=["BIAS"], w=["BIAS"])
                dump("Cc", Cc[:], [128, 32, 8], r=["Cc"])
                P.barrier()
                chk(1.0)

            if stage >= 2:
                with ExitStack() as s2:
                    ptb = sb("ptb", [128, 256], I32, s2)
                    ptf = sb("ptf", [128, 256], F32, s2)
                    idx = sb("idx", [128, 256], I32, s2)
                    lfp = sb("lfp", [128, 16, 16, 8], F32, s2)
                    Linc = sb("Linc", [128, 16, 16, 8], F32, s2)
                    Cbs = sb("Cbs", [128, 16, 8], F32, s2)
                    lfn4 = sb("lfn4", [4, 16, 8], F32, s2)
                    pidx = sb("pidx", [128, 2], I32, s2)
                    bnew = sb("bnew", [4, 16, 8], F32, s2)
                    Vn = sb("Vn", [4, 16, 512], BF16, s2)
                    Qbd = sb("Qbd", [128, 4, 16, 8], BF16, s2)
                    Kp = [sb("Kp%d" % i, [128, 16, 512], BF16, s2) for i in range(1)]
                    Vp = [sb("Vp%d" % i, [128, 16, 512], BF16, s2) for i in range(2)]
                    KTp = [sb("KTp%d" % i, [128, 4, 128], BF16, s2) for i in range(3)]
                    stmp = sb("stmp", [128, 512], F32, s2)
                    Pp = sb("Pp", [128, 16, 32], BF16, s2)
                    tn = sb("tn", [4, 32], F32, s2)
                    Pn = sb("Pn", [4, 32], BF16, s2)
                    t3 = sb("t3", [32, 512], F32, s2)
                    o32 = sb("o32", [32, 64], F32, s2)
                    rd = sb("rd", [32, 1], F32, s2)
                    ps_n[0] = 5
                    P.dma("sp", ptb[:], page_table.partition_broadcast(128), w=["ptb"], sem="ptb")
                    P.op("dve", lambda e: e.tensor_copy(out=ptf[:], in_=ptb[:]), r=["ptb"], w=["ptf"])
                    P.op("dve", lambda e: e.tensor_scalar(out=ptf[:], in0=ptf[:], scalar1=128.0, scalar2=io_f[:, 0:1], op0=ALU.mult, op1=ALU.add),
                         r=["ptf", "io_f"], w=["ptf"])
                    P.op("dve", lambda e: e.tensor_copy(out=idx[:], in_=ptf[:]), r=["ptf"], w=["idx"])
                    P.dma("sp", lfn4[:], f_s.rearrange("(b j) h -> j b h", j=4), w=["lfn4"], sem="lfn4")
                    P.dma("pool", Vn[:], v_s.rearrange("(b j) f -> j b f", j=4), w=["Vn"], sem="Vn")
                    with nc.allow_non_contiguous_dma(reason="tiny page-id transpose"):
                        P.dma("sp", pidx[:], page_table.rearrange("o (g p) -> p (o g)", p=128), w=["pidx"], sem="pidx")
                    cfp = cache_f.rearrange("(n k) h -> n (k h)", k=128)
                    lfT = Linc[:].rearrange("p b g h -> p (b g h)").rearrange("p (a n) -> p a n", a=2)
                    for g_ in range(2):
                        P.idma(lfT[:, g_, :], cfp, pidx[:, g_:g_ + 1], r=["pidx"], w=["lfT%d" % g_], sem="lfT")
                    for g_ in range(2):
                        lv = lfT[:, g_, :].rearrange("p (k h) -> p k h", h=8)
                        for q4 in range(2):
                            ptl, ptlk = ps()
                            P.op("pe", lambda e, ptl=ptl, lv=lv, q4=q4: [e.transpose(ptl[:, hh * 128:(hh + 1) * 128], lv[:, :, 4 * q4 + hh], ident[:]) for hh in range(4)][-1],
                                 r=["lfT0", "lfT1", "ident"], w=[ptlk])
                            evac(lfp[:, 8 * g_:8 * g_ + 8, :, 4 * q4:4 * q4 + 4].rearrange("p b g h -> p h (b g)"),
                                 ptl[:, :].rearrange("p (h s) -> p h s", h=4), r=[ptlk], w=["lfp_%d_%d" % (g_, q4)])
                    lfpall = ["lfp_%d_%d" % (g_, q4) for g_ in range(2) for q4 in range(2)]
                    P.op("dve", lambda e: e.tensor_copy(out=Linc[:, :, 0, :], in_=lfp[:, :, 0, :]), r=lfpall, w=["Linc", "lfp"])
                    for pg in range(1, 16):
                        P.op("dve", lambda e, pg=pg: e.tensor_tensor(out=Linc[:, :, pg, :], in0=Linc[:, :, pg - 1, :], in1=lfp[:, :, pg, :], op=ALU.add),
                             r=["Linc", "lfp"], w=["Linc"])
                    pcs, pcsk = ps()
                    P.op("pe", lambda e: (e.matmul(pcs[:, 0:128].rearrange("p (b h) -> p b h", h=8), lhsT=ones_f[:], rhs=Linc[:, :, 15, :], start=True, stop=False),
                                          e.matmul(pcs[:, 0:128].rearrange("p (b h) -> p b h", h=8), lhsT=ones_f[0:4, :], rhs=lfn4[:], start=False, stop=True))[1],
                         r=["Linc", "lfn4"], w=[pcsk])
                    P.op("dve", lambda e: e.tensor_copy(out=Cbs[:].rearrange("p b h -> p (b h)"), in_=pcs[:, 0:128]), r=[pcsk], w=["Cbs"])
                    P.op("dve", lambda e: e.tensor_tensor(out=Linc[:], in0=Linc[:], in1=lfp[:], op=ALU.subtract), r=["Linc", "lfp", "Cbs"], w=["Linc"])
                    cpast = lfp
                    lfp2 = lfp[:].rearrange("p b g h -> p (b g h)")
                    Lin2 = Linc[:].rearrange("p b g h -> p (b g h)")
                    cp2 = cpast[:].rearrange("p b g h -> p (b g h)")
                    for q4 in range(4):
                        pq, pqk = ps()
                        cs = slice(q4 * 512, (q4 + 1) * 512)
                        mmg(pq[:, :], [(U_f[:], lfp2[:, cs]), (ones_f[:], Lin2[:, cs])], r=["Linc", "lfp"], w=[pqk])
                        for bb in range(4):
                            b_ = 4 * q4 + bb
                            P.op("dve", lambda e, pq=pq, bb=bb, b_=b_: e.scalar_tensor_tensor(
                                out=cpast[:, b_], in0=pq[:, bb * 128:(bb + 1) * 128].rearrange("p (g h) -> p g h", g=16), scalar=-1.0,
                                in1=Cbs[:, b_, :].unsqueeze(1).to_broadcast([128, 16, 8]), op0=ALU.mult, op1=ALU.add),
                                r=[pqk, "Cbs"], w=["cpast", "lfp"])
                    pbn, pbnk = ps()
                    mmg(pbn[0:4, 0:128].rearrange("p (b h) -> p b h", h=8), [(SU4[:], lfn4[:])], r=["lfn4", "SU4"], w=[pbnk])
                    P.op("dve", lambda e: e.tensor_copy(out=bnew[:].rearrange("p b h -> p (b h)"), in_=pbn[0:4, 0:128]), r=[pbnk], w=["bnew"])
                    P.op("pool", lambda e: e.memset(Qbd[:], 0.0), w=["Qbd"])
                    qs = qT[:, :, 2048:2112].rearrange("p c (b j) -> p c b j", j=4)
                    P.op("pool", lambda e: e.tensor_copy(out=Qbd[0:64, :, :, 0:4], in_=qs[0:64]), r=["Qbd"], w=["Qbd"])
                    P.op("pool", lambda e: e.tensor_copy(out=Qbd[64:128, :, :, 4:8], in_=qs[64:128]), r=["Qbd"], w=["Qbd"])
                    kt_rr = 0
                    for b in range(16):
                        kp, vp = Kp[0], Vp[b % 2]
                        kpk, vpk = "Kp0", "Vp%d" % (b % 2)
                        for pg in range(16):
                            P.idma(kp[:, pg, :], cache_k, idx[:, b * 16 + pg:b * 16 + pg + 1], r=["idx"], w=[kpk + "_%d" % pg], sem=kpk)
                        for pg in range(16):
                            P.idma(vp[:, pg, :], cache_v, idx[:, b * 16 + pg:b * 16 + pg + 1], r=["idx"], w=[vpk + "_%d" % pg], sem=vpk)
                        ss, ssk = psf[5], "ps5"
                        sn, snk = psf[6], "ps6"
                        acc, acck = psf[7], "ps7"
                        for pg in range(16):
                            ptb_, ptbk = ps()
                            ptv = ptb_[:].bitcast(BF16)

                            def trk(e, ptv=ptv, kp=kp, pg=pg):
                                ins = None
                                for hp in range(4):
                                    ins = e.transpose(ptv[:, hp * 128:(hp + 1) * 128], kp[:, pg, hp * 128:(hp + 1) * 128], ident_b[:])
                                return ins
                            P.op("pe", trk, r=[kpk + "_%d" % g_ for g_ in range(16)] + ["ident_b"], w=[ptbk])
                            ktp = KTp[kt_rr % 3]
                            ktk = "KTp%d" % (kt_rr % 3)
                            kt_rr += 1
                            evac(ktp[:].rearrange("p c k -> p (c k)"), ptv[:, 0:512], r=[ptbk], w=[ktk])

                            def qk(e, ktp=ktp, pg=pg, b=b):
                                ins = None
                                for hp in range(4):
                                    ins = e.matmul(ss[:, pg * 32 + hp * 8:pg * 32 + hp * 8 + 8], lhsT=ktp[:, hp, :], rhs=Qbd[:, hp, b, :], start=True, stop=True)
                                return ins
                            P.op("pe", qk, r=[ktk, "Qbd"], w=[ssk])

                        def qkn(e, b=b):
                            ins = None
                            for hp in range(4):
                                ins = e.matmul(sn[0:4, hp * 8:hp * 8 + 8], lhsT=kT[:, hp, 4096 + 4 * b:4096 + 4 * b + 4], rhs=Qbd[:, hp, b, :], start=True, stop=True)
                            return ins
                        P.op("pe", qkn, r=["Qbd"], w=[snk])
                        P.op("dve", lambda e, b=b: e.scalar_tensor_tensor(
                            out=stmp[:].rearrange("p (g h q) -> p g h q", g=16, h=8), in0=ss[:, :].rearrange("p (g h q) -> p g h q", g=16, h=8), scalar=0.125,
                            in1=cpast[:, b].unsqueeze(3).to_broadcast([128, 16, 8, 4]), op0=ALU.mult, op1=ALU.add), r=[ssk, "cpast"], w=["stmp"])
                        P.op("act", lambda e: e.activation(out=Pp[:].rearrange("p g c -> p (g c)"), in_=stmp[:], func=AF.Exp), r=["stmp"], w=["Pp"])
                        P.op("dve", lambda e, b=b: e.scalar_tensor_tensor(
                            out=tn[:].rearrange("p (h q) -> p h q", h=8), in0=sn[0:4, 0:32].rearrange("p (h q) -> p h q", h=8), scalar=0.125,
                            in1=bnew[:, b, :].unsqueeze(2).to_broadcast([4, 8, 4]), op0=ALU.mult, op1=ALU.add), r=[snk, "bnew"], w=["tn"])
                        P.op("act", lambda e: e.activation(out=tn[:], in_=tn[:], func=AF.Exp), r=["tn"], w=["tn"])
                        P.op("dve", lambda e: e.tensor_tensor(out=Pn[:].rearrange("p (h q) -> p h q", h=8), in0=tn[:].rearrange("p (h q) -> p h q", h=8),
                                                              in1=U_f[0:4, 0:4].unsqueeze(1).to_broadcast([4, 8, 4]), op=ALU.mult), r=["tn", "U_f"], w=["Pn"])

                        def pv(e, vp=vp, b=b):
                            for pg in range(16):
                                e.matmul(acc[0:32, :], lhsT=Pp[:, pg, :], rhs=vp[:, pg, :], start=(pg == 0), stop=False)
                            return e.matmul(acc[0:32, :], lhsT=Pn[:, :], rhs=Vn[:, b, :], start=False, stop=True)
                        P.op("pe", pv, r=["Pp", "Pn", "Vn"] + [vpk + "_%d" % g_ for g_ in range(16)], w=[acck])

                        def dn(e):
                            for pg in range(16):
                                e.matmul(sn[0:32, 64:65], lhsT=Pp[:, pg, :], rhs=ones_b[:, 0:1], start=(pg == 0), stop=False)
                            return e.matmul(sn[0:32, 64:65], lhsT=Pn[:, :], rhs=ones_b[0:4, 0:1], start=False, stop=True)
                        P.op("pe", dn, r=["Pp", "Pn", "tn"], w=[snk + "d"])
                        P.op("dve", lambda e: e.tensor_tensor(out=t3[:].rearrange("p (h d) -> p h d", h=8), in0=acc[0:32, :].rearrange("p (h d) -> p h d", h=8),
                                                              in1=hmask[:].unsqueeze(2).to_broadcast([32, 8, 64]), op=ALU.mult), r=[acck, "hmask"], w=["t3"])
                        P.op("dve", lambda e: e.tensor_reduce(out=o32[:], in_=t3[:].rearrange("p (h d) -> p d h", h=8), axis=AX.X, op=ALU.add), r=["t3"], w=["o32"])
                        P.op("dve", lambda e: e.reciprocal(out=rd[:], in_=sn[0:32, 64:65]), r=[snk + "d"], w=["rd"])
                        P.op("dve", lambda e: e.tensor_scalar(out=o32[:], in0=o32[:], scalar1=rd[:, 0:1], scalar2=None, op0=ALU.mult), r=["o32", "rd"], w=["o32"])
                        po, pok = ps()
                        P.op("pe", lambda e, po=po: e.transpose(po[0:64, 0:32], o32[:, :], ident[0:32, 0:32]), r=["o32", "ident"], w=[pok])
                        pov = po[0:64, 0:32].rearrange("p (c hh q) -> p c hh q", c=4, hh=2)
                        P.op("dve", lambda e, pov=pov, b=b: e.tensor_copy(out=oT[0:64, :, 2048 + 4 * b:2052 + 4 * b], in_=pov[:, :, 0, :]), r=[pok, snk, snk + "d"], w=["oTs%d" % b])
                        P.op("dve", lambda e, pov=pov, b=b: e.tensor_copy(out=oT[64:128, :, 2048 + 4 * b:2052 + 4 * b], in_=pov[:, :, 1, :]), r=[pok], w=["oTs%d" % b])
                    ps_n[0] = 8
                    P.barrier()

            if stage >= 3:
                with ExitStack() as s3:
                    Pt = [sb("Pt%d" % i, [128, 512], BF16, s3) for i in range(6)]
                    rec = sb("rec", [128, 512], F32, s3)
                    qTm = [sb("qTm%d" % i, [128, 4, 2048], BF16, s3) for i in range(2)]
                    P.op("pool", lambda e: e.memset(qTm[0][64:128], 0.0), w=["qTm0z"])
                    P.op("pool", lambda e: e.memset(qTm[1][0:64], 0.0), w=["qTm1z"])
                    P.op("act", lambda e: e.copy(out=qTm[0][0:64], in_=qT[0:64, :, 0:2048]), w=["qTm0"])
                    P.op("dve", lambda e: e.tensor_copy(out=qTm[1][64:128], in_=qT[64:128, :, 0:2048]), w=["qTm1"])
                    Vflat = Vt[:].rearrange("p b h d -> p (b h d)")
                    bcs = sb("bcs", [64, 512], F32, s3)
                    ps_n[0] = 5
                    pt_rr = 0
                    n_acc = 0
                    LA = 3
                    tasks = []
                    for sg in range(4):
                        for h in range(8):
                            acc, acck = psf[6 + n_acc % 2], "ps%d" % (6 + n_acc % 2)
                            n_acc += 1
                            blocks = list(range(0, 4 * sg)) + list(range(16, 16 + 4 * sg + 4)) + list(range(4 * sg, 4 * sg + 4))
                            for i, kb in enumerate(blocks):
                                diag = 4 * sg <= kb < 4 * sg + 4
                                tasks.append(dict(sg=sg, h=h, hp=h // 2, pr=slice((h % 2) * 64, (h % 2) * 64 + 64), acc=acc, acck=acck, kb=kb, diag=diag,
                                                  q0=128 * (kb - 4 * sg) if diag else 0, first=(i == 0), last=(i == len(blocks) - 1)))
                    normq = []

                    def norm(T):
                        acc, acck, pr, hp, sg, h = T["acc"], T["acck"], T["pr"], T["hp"], T["sg"], T["h"]
                        P.op("dve", lambda e: e.reciprocal(out=rec[64:65, :], in_=acc[64:65, :]), r=[acck], w=["rec"])
                        P.op("pe", lambda e: e.matmul(psf[5][0:64, :], lhsT=ones_f[64:65, 0:64], rhs=rec[64:65, :], start=True, stop=True), r=["rec"], w=["ps5"])
                        P.op("act", lambda e: e.copy(out=bcs[:], in_=psf[5][0:64, :]), r=["ps5"], w=["bcs"])
                        P.op("dve", lambda e: e.tensor_tensor(out=oT[pr, hp, sg * 512:(sg + 1) * 512], in0=acc[0:64, :], in1=bcs[:], op=ALU.mult),
                             r=[acck, "bcs"], w=["oT_%d_%d" % (sg, h)])

                    for t in range(len(tasks) + LA + 3):
                        if t < len(tasks):
                            T = tasks[t]
                            sg, h, hp, pr, kb, q0 = T["sg"], T["h"], T["hp"], T["pr"], T["kb"], T["q0"]
                            st, stk = ps()
                            P.op("pe", lambda e, st=st: e.matmul(st[:, q0:512], lhsT=kT[:, hp, kb * 128:(kb + 1) * 128],
                                                                  rhs=qTm[h % 2][:, hp, sg * 512 + q0:(sg + 1) * 512], start=True, stop=True),
                                 r=["qTm0z", "qTm1z", "qTm0", "qTm1"], w=[stk])
                            pti = Pt[pt_rr % 6]
                            ptk = "Pt%d" % (pt_rr % 6)
                            pt_rr += 1
                            P.op("act", lambda e, st=st, pti=pti: e.activation(out=pti[:, q0:512], in_=st[:, q0:512], func=AF.Exp,
                                                                               bias=BIAS[:, sg, kb, h:h + 1], scale=0.125), r=[stk], w=[ptk])
                            if T["diag"]:
                                P.op("pool", lambda e, pti=pti: e.tensor_tensor(out=pti[:, q0:q0 + 128], in0=pti[:, q0:q0 + 128], in1=U_b[:], op=ALU.mult),
                                     r=[ptk], w=[ptk])
                            T["pti"], T["ptk"] = pti, ptk
                        while normq and normq[0][0] <= t:
                            norm(normq.pop(0)[1])
                        j = t - LA
                        if 0 <= j < len(tasks):
                            T = tasks[j]
                            P.op("pe", lambda e, T=T: e.matmul(T["acc"][:, T["q0"]:512], lhsT=Vflat[:, (T["kb"] * 8 + T["h"]) * 66:(T["kb"] * 8 + T["h"]) * 66 + 128], rhs=T["pti"][:, T["q0"]:512],
                                                               start=T["first"], stop=T["last"]), r=[T["ptk"]], w=[T["acck"]])
                            if T["last"]:
                                normq.append((t + 2, T))
                    while normq:
                        norm(normq.pop(0)[1])
                    ps_n[0] = 8
                    dump("oT", oT[:], [128, 4, NTOK], BF16)
                    P.barrier()
            s_A.close()

            if stage >= 4:
                with ExitStack() as s4:
                    wcv = sb("wcv", [128, 8, 1536], BF16, s4)
                    wg = sb("wg", [128, 8, 2048], BF16, s4)
                    wco = sb("wco", [128, 4, 1024], BF16, s4)
                    wao = sb("wao", [128, 4, 1024], BF16, s4)
                    wo = sb("wo", [128, 8, 1024], BF16, s4)
                    wgr = sb("wgr", [128, 8, 20], F32, s4)
                    cw = sb("cw", [128, 4, 3], F32, s4)
                    g1b = sb("g1b", [128, 1024], F32, s4)
                    b1b = sb("b1b", [128, 1024], F32, s4)
                    wst[:] = [sb("wstB%d" % i, [128, 1024], F32, s4) for i in range(2)]
                    wload(wcv, "wcv", w_in, 8, 1536, col0=0, CH=1024)
                    wload(wg, "wg", w_in, 8, 1024, col0=C_GA, dcol0=0, CH=1024)
                    wload(wg, "wg", w_in, 8, 1024, col0=C_GB, dcol0=1024, CH=1024)
                    wload(wco, "wco", w_conv_out, 4, 1024, CH=1024)
                    wload(wao, "wao", w_attn_out, 4, 1024, CH=1024)
                    wload(wo, "wo", w_o, 8, 1024, CH=1024)
                    P.dma("sp", wgr[:], w_gr.rearrange("(k p) n -> p k n", p=128), w=["wgr"], sem="wgr")
                    with nc.allow_non_contiguous_dma(reason="tiny conv weight transpose"):
                        for c in range(4):
                            P.dma("sp", cw[:, c, :], conv_w[:, c * 128:(c + 1) * 128].rearrange("j p -> p j"), w=["cw%d" % c], sem="cw")
                    P.dma("sp", g1b[:], ln1_g.partition_broadcast(128), w=["g1b"], sem="g1b")
                    P.dma("sp", b1b[:], ln1_b.partition_broadcast(128), w=["b1b"], sem="b1b")

                    xin = sb("xin", [128, 4, 1024], F32, s4)
                    xT = sb("xT", [128, 8, 512], BF16, s4)
                    xh = sb("xh", [8, 1024], F32, s4)
                    xhT = sb("xhT", [128, 8, 8], BF16, s4)
                    uh = sb("uh", [128, 4, 8], F32, s4)
                    sct = sb("sct", [32, 512], F32, s4)
                    stT = sb("stT", [128, 4, 32], F32, s4)
                    xcs = sb("xcs", [128, 512], F32, s4)
                    ue = [sb("ue%d" % i, [128, 514], F32, s4) for i in range(2)]
                    tcv = sb("tcv", [128, 512], F32, s4)
                    gcv = sb("gcv", [128, 4, 512], BF16, s4)
                    ucp = sb("ucp", [128, 4, 2], F32, s4)
                    ucs = sb("ucs", [128, 4, 32], F32, s4)
                    cps = sb("cps", [2, 512], F32, s4)
                    css = sb("css", [32, 512], F32, s4)
                    sga = sb("sga", [128, 512], F32, s4)
                    sgb = sb("sgb", [128, 512], F32, s4)
                    za = sga
                    zb = sgb
                    zT = sb("zT", [128, 8, 512], BF16, s4)
                    xr = [sb("xr%d" % i, [128, 1024], F32, s4) for i in range(2)]
                    st6 = sb("st6", [128, 2, 6], F32, s4)
                    mv = sb("mv", [128, 2], F32, s4)
                    rstd = sb("rstd", [128, 1], F32, s4)
                    y0t = [sb("y0t%d" % i, [128, 1024], F32, s4) for i in range(1)]
                    x1Tb = [sb("x1Tb%d" % i, [128, 8, 128], BF16, s4) for i in range(2)]
                    x1Tf = sb("x1Tf", [128, 8, 128], F32, s4)
                    lg = sb("lg", [128, 20], F32, s4)
                    g8 = sb("g8", [128, 8], F32, s4)
                    m8 = sb("m8", [128, 8], F32, s4)
                    e8 = sb("e8", [128, 8], F32, s4)
                    me8 = sb("me8", [128, 8], F32, s4)
                    ohg = sb("ohg", [128, 4], F32, s4)
                    nm = sb("nm", [128, 2], F32, s4)
                    eg = sb("eg", [128, 4], F32, s4)
                    sgs = sb("sgs", [128, 2], F32, s4)
                    sel = sb("sel", [128, 16], F32, s4)
                    e1 = sb("e1", [128, 4], F32, s4)
                    s2m = sb("s2m", [128, 4], F32, s4)
                    wfac = sb("wfac", [128, 1], F32, s4)
                    P.op("pool", lambda e: e.memset(g8[:], -1e30), w=["g8"])
                    P.op("pool", lambda e: e.memset(e8[:], -1e30), w=["e8"])

                    P.dma("sp", xh[:], x_halo, w=["xh"], sem="xh")
                    ph, phk = ps()
                    P.op("pe", lambda e: [e.transpose(ph[:, k * 8:(k + 1) * 8], xh[0:8, k * 128:(k + 1) * 128], ident[0:8, 0:8]) for k in range(8)][-1],
                         r=["xh", "ident"], w=[phk])
                    evac(xhT[:].rearrange("p k t -> p (k t)"), ph[:, 0:64], r=[phk], w=["xhT"])
                    for c in range(4):
                        p1, p1k = ps()
                        mmg(p1[:, 0:8], [(wcv[:, k, C_XC + c * 128:C_XC + (c + 1) * 128], xhT[:, k, :]) for k in range(8)], r=["xhT", "wcv"], w=[p1k])
                        p2, p2k = ps()
                        mmg(p2[:, 0:8], [(wcv[:, k, C_CG + c * 128:C_CG + (c + 1) * 128], xhT[:, k, :]) for k in range(8)], r=["xhT", "wcv"], w=[p2k])
                        P.op("act", lambda e, p1=p1: e.copy(out=xcs[:, 0:8], in_=p1[:, 0:8]), r=[p1k], w=["xcs"])
                        P.op("dve", lambda e, p2=p2, c=c: e.tensor_tensor(out=uh[:, c, :], in0=xcs[:, 0:8], in1=p2[:, 0:8], op=ALU.mult), r=[p2k, "xcs"], w=["uh"])
                    P.dma("sp", sct[:], state_conv, w=["sct"], sem="sct")
                    pst, pstk = ps()
                    P.op("pe", lambda e: [e.transpose(pst[:, c * 32:(c + 1) * 32], sct[0:32, c * 128:(c + 1) * 128], ident[0:32, 0:32]) for c in range(4)][-1],
                         r=["sct", "ident"], w=[pstk])
                    evac(stT[:].rearrange("p c t -> p (c t)"), pst[:, 0:128], r=[pstk], w=["stT"])

                    tiles = [(x_own[s_ * 512:(s_ + 1) * 512, :], 512, s_ * 512, s_ * 4, s_) for s_ in range(4)] + [(x_s, 64, 2048, 16, 4)]
                    blk_n = 0
                    for (src, W, t0, gb0, sl) in tiles:
                        nb = (W + 127) // 128
                        pw = min(W, 128)
                        samp = (sl == 4)
                        xtall = load_xT(src, W, xin, xT, "xin", "xT")
                        for c in range(4):
                            u_ = ue[c % 2]
                            uk = "ue%d" % (c % 2)
                            p1, p1k = ps()
                            mmg(p1[:, 0:W], [(wcv[:, k, C_XC + c * 128:C_XC + (c + 1) * 128], xT[:, k, 0:W]) for k in range(8)], r=xtall + ["wcv"], w=[p1k])
                            p2, p2k = ps()
                            mmg(p2[:, 0:W], [(wcv[:, k, C_CG + c * 128:C_CG + (c + 1) * 128], xT[:, k, 0:W]) for k in range(8)], r=xtall + ["wcv"], w=[p2k])
                            p3, p3k = ps()
                            mmg(p3[:, 0:W], [(wcv[:, k, C_BG + c * 128:C_BG + (c + 1) * 128], xT[:, k, 0:W]) for k in range(8)], r=xtall + ["wcv"], w=[p3k])
                            P.op("act", lambda e, p1=p1: e.copy(out=xcs[:, 0:W], in_=p1[:, 0:W]), r=[p1k], w=["xcs"])
                            if not samp:
                                P.op("pool", lambda e, u_=u_, c=c: e.tensor_copy(out=u_[:, 0:2], in_=uh[:, c, 2 * sl:2 * sl + 2]), r=["uh"], w=[uk])
                                P.op("dve", lambda e, u_=u_, p2=p2: e.tensor_tensor(out=u_[:, 2:W + 2], in0=xcs[:, 0:W], in1=p2[:, 0:W], op=ALU.mult),
                                     r=[p2k, "xcs", uk], w=[uk])
                                uv = [u_[:, j:j + W] for j in range(3)]
                                tv = tcv[:, 0:W]
                            else:
                                u3 = u_[:, 0:96].rearrange("p (b i) -> p b i", i=6)
                                P.op("pool", lambda e, u3=u3, c=c: e.tensor_copy(out=u3[:, :, 0:2], in_=stT[:, c, :].rearrange("p (b i) -> p b i", i=2)),
                                     r=["stT"], w=[uk])
                                P.op("dve", lambda e, u3=u3, p2=p2: e.tensor_tensor(out=u3[:, :, 2:6], in0=xcs[:, 0:64].rearrange("p (b j) -> p b j", j=4),
                                                                                     in1=p2[:, 0:64].rearrange("p (b j) -> p b j", j=4), op=ALU.mult),
                                     r=[p2k, "xcs", uk], w=[uk])
                                uv = [u3[:, :, j:j + 4] for j in range(3)]
                                tv = tcv[:, 0:64].rearrange("p (b j) -> p b j", j=4)
                            P.op("pool", lambda e, uv=uv, tv=tv, c=c: e.tensor_scalar(out=tv, in0=uv[0], scalar1=cw[:, c, 0:1], scalar2=None, op0=ALU.mult),
                                 r=[uk] + ["cw%d" % i for i in range(4)], w=["tcv"])
                            for j in (1, 2):
                                P.op("dve", lambda e, uv=uv, tv=tv, c=c, j=j: e.scalar_tensor_tensor(out=tv, in0=uv[j], scalar=cw[:, c, j:j + 1], in1=tv,
                                                                                                     op0=ALU.mult, op1=ALU.add), r=[uk, "tcv"], w=["tcv"])
                            P.op("dve", lambda e, p3=p3, c=c: e.tensor_tensor(out=gcv[:, c, 0:W], in0=tcv[:, 0:W], in1=p3[:, 0:W], op=ALU.mult),
                                 r=[p3k, "tcv"], w=["gcv%d" % c])
                            if sl == 3:
                                P.op("pool", lambda e, u_=u_, c=c: e.tensor_copy(out=ucp[:, c, :], in_=u_[:, W:W + 2]), r=[uk], w=["ucp"])
                            if samp:
                                P.op("pool", lambda e, u3=u3, c=c: e.tensor_copy(out=ucs[:, c, :].rearrange("p (b i) -> p b i", i=2), in_=u3[:, :, 4:6]),
                                     r=[uk], w=["ucs"])
                        if sl == 3:
                            pcp, pcpk = ps()
                            P.op("pe", lambda e, pcp=pcp: [e.transpose(pcp[0:2, c * 128:(c + 1) * 128], ucp[:, c, :], ident[:]) for c in range(4)][-1],
                                 r=["ucp", "ident"], w=[pcpk])
                            evac(cps[:], pcp[0:2, :], r=[pcpk], w=["cps"])
                            P.dma("sp", conv_p, cps[:], r=["cps"], sem="cps")
                        if samp:
                            pcp, pcpk = ps()
                            P.op("pe", lambda e, pcp=pcp: [e.transpose(pcp[0:32, c * 128:(c + 1) * 128], ucs[:, c, :], ident[:]) for c in range(4)][-1],
                                 r=["ucs", "ident"], w=[pcpk])
                            evac(css[:], pcp[0:32, :], r=[pcpk], w=["css"])
                            P.dma("sp", conv_s, css[:], r=["css"], sem="css")
                        gall = ["gcv%d" % c for c in range(4)]
                        for m in range(8):
                            ms = slice(m * 128, (m + 1) * 128)
                            pya, pyak = ps()
                            mmg(pya[:, 0:W], [(wco[:, c, ms], gcv[:, c, 0:W]) for c in range(4)], r=gall + ["wco"], w=[pyak])
                            pga, pgak = ps()
                            mmg(pga[:, 0:W], [(wg[:, k, ms], xT[:, k, 0:W]) for k in range(8)], r=xtall + ["wg"], w=[pgak])
                            pyb, pybk = ps()
                            mmg(pyb[:, 0:W], [(wao[:, c, ms], oT[:, c, t0:t0 + W]) for c in range(4)], r=["wao"], w=[pybk])
                            pgb, pgbk = ps()
                            mmg(pgb[:, 0:W], [(wg[:, k, 1024 + m * 128:1024 + (m + 1) * 128], xT[:, k, 0:W]) for k in range(8)], r=xtall + ["wg"], w=[pgbk])
                            P.op("act", lambda e, pga=pga: e.activation(out=sga[:, 0:W], in_=pga[:, 0:W], func=AF.Sigmoid), r=[pgak], w=["sga"])
                            P.op("act", lambda e, pgb=pgb: e.activation(out=sgb[:, 0:W], in_=pgb[:, 0:W], func=AF.Sigmoid), r=[pgbk], w=["sgb"])
                            P.op("dve", lambda e, pya=pya: e.tensor_tensor(out=za[:, 0:W], in0=sga[:, 0:W], in1=pya[:, 0:W], op=ALU.mult), r=[pyak, "sga"], w=["sga"])
                            P.op("dve", lambda e, pyb=pyb: e.tensor_tensor(out=zb[:, 0:W], in0=sgb[:, 0:W], in1=pyb[:, 0:W], op=ALU.mult), r=[pybk, "sgb"], w=["sgb"])
                            P.op("pool", lambda e, m=m: e.tensor_tensor(out=zT[:, m, 0:W], in0=za[:, 0:W], in1=zb[:, 0:W], op=ALU.add), r=["sga", "sgb"], w=["zT%d" % m])
                        zall = ["zT%d" % m for m in range(8)]
                        for b in range(nb):
                            gb = gb0 + b
                            xr_ = xr[blk_n % 2]
                            xrk = "xr%d" % (blk_n % 2)
                            y0_ = y0t[0]
                            y0k = "y0t0"
                            xb_ = x1Tb[blk_n % 2]
                            xbk = "x1Tb%d" % (blk_n % 2)
                            blk_n += 1
                            tsl = slice(b * 128, b * 128 + pw)
                            for half in range(2):
                                hs = slice(half * 512, (half + 1) * 512)
                                pm, pmk = ps()
                                mmg(pm[0:pw, :], [(zT[:, m, tsl], wo[:, m, hs]) for m in range(8)], r=zall + ["wo"], w=[pmk])
                                P.op("dve", lambda e, pm=pm, hs=hs, xr_=xr_, b=b: e.scalar_tensor_tensor(out=xr_[0:pw, hs], in0=xin[0:pw, b, hs], scalar=ALPHA, in1=pm[0:pw, :],
                                                                                                        op0=ALU.mult, op1=ALU.add), r=[pmk, "xin"], w=[xrk])
                            xrh = [xrk]
                            for half in range(2):
                                P.op("dve", lambda e, half=half, xr_=xr_: e.bn_stats(out=st6[0:pw, half, :], in_=xr_[0:pw, half * 512:(half + 1) * 512]), r=xrh, w=["st6_%d" % half])
                            P.op("dve", lambda e: e.bn_aggr(out=mv[0:pw, :], in_=st6[0:pw].rearrange("p a s -> p (a s)")), r=["st6_0", "st6_1"], w=["mv"])
                            P.op("dve", lambda e: e.tensor_scalar(out=rstd[0:pw, :], in0=mv[0:pw, 1:2], scalar1=EPS, scalar2=None, op0=ALU.add), r=["mv"], w=["rstd"])
                            P.op("act", lambda e: e.sqrt(out=rstd[0:pw, :], in_=rstd[0:pw, :]), r=["rstd"], w=["rstd"])
                            P.op("dve", lambda e: e.reciprocal(out=rstd[0:pw, :], in_=rstd[0:pw, :]), r=["rstd"], w=["rstd"])
                            P.op("dve", lambda e, xr_=xr_: e.tensor_scalar(out=xr_[0:pw, :], in0=xr_[0:pw, :], scalar1=mv[0:pw, 0:1], scalar2=rstd[0:pw, 0:1],
                                                                           op0=ALU.subtract, op1=ALU.mult), r=xrh + ["mv", "rstd"], w=[xrk])
                            P.op("pool", lambda e, xr_=xr_: e.tensor_tensor(out=xr_[0:pw, :], in0=xr_[0:pw, :], in1=g1b[0:pw, :], op=ALU.mult), r=[xrk, "g1b"], w=[xrk])
                            P.op("pool", lambda e, xr_=xr_: e.tensor_tensor(out=xr_[0:pw, :], in0=xr_[0:pw, :], in1=b1b[0:pw, :], op=ALU.add), r=[xrk, "b1b"], w=[xrk])
                            P.op("act", lambda e, xr_=xr_, y0_=y0_: e.mul(out=y0_[0:pw, :], in_=xr_[0:pw, :], mul=ALPHA), r=[xrk], w=[y0k])
                            P.dma("sp", y0_d[t0 + b * 128:t0 + b * 128 + pw, :], y0_[0:pw, :], r=[y0k], sem=y0k)
                            for half in range(2):
                                ptt, pttk = ps()
                                P.op("pe", lambda e, ptt=ptt, half=half, xr_=xr_: [e.transpose(ptt[:, kk * 128:kk * 128 + pw], xr_[0:pw, (half * 4 + kk) * 128:(half * 4 + kk + 1) * 128],
                                                                                               ident[0:pw, 0:pw]) for kk in range(4)][-1], r=[xrk, "ident"], w=[pttk])
                                pv4 = ptt[:].rearrange("p (k t) -> p k t", k=4)[:, :, 0:pw]
                                P.op("act", lambda e, pv4=pv4, half=half, xb_=xb_: e.copy(out=xb_[:, half * 4:half * 4 + 4, 0:pw], in_=pv4), r=[pttk], w=[xbk + "_%d" % half])
                                P.op("dve", lambda e, pv4=pv4, half=half: e.tensor_copy(out=x1Tf[:, half * 4:half * 4 + 4, 0:pw], in_=pv4), r=[pttk, xbk + "_%d" % half], w=["x1Tf_%d" % half])
                            P.dma("sp", x1T_d[:, :, t0 + b * 128:t0 + b * 128 + pw], xb_[:, :, 0:pw], r=[xbk + "_0", xbk + "_1"], sem=xbk)
                            prt, prtk = ps()
                            mmg(prt[0:pw, 0:20], [(x1Tf[:, k, 0:pw], wgr[:, k, :]) for k in range(8)], r=["x1Tf_0", "x1Tf_1", "wgr"], w=[prtk])
                            R = slice(0, pw)
                            P.op("dve", lambda e, prt=prt: e.tensor_copy(out=lg[R, :], in_=prt[R, 0:20]), r=[prtk], w=["lg"])
                            P.op("dve", lambda e: e.tensor_copy(out=g8[R, 0:4], in_=lg[R, 0:4]), r=["lg"], w=["g8"])
                            P.op("dve", lambda e: e.max(out=m8[R, :], in_=g8[R, :]), r=["g8"], w=["m8"])
                            P.op("dve", lambda e: e.tensor_scalar(out=ohg[R, :], in0=lg[R, 0:4], scalar1=m8[R, 0:1], scalar2=None, op0=ALU.is_equal), r=["lg", "m8"], w=["ohg"])
                            P.op("dve", lambda e: e.tensor_scalar(out=nm[R, 0:1], in0=m8[R, 0:1], scalar1=-1.0, scalar2=None, op0=ALU.mult), r=["m8"], w=["nm0"])
                            P.op("act", lambda e: e.activation(out=eg[R, :], in_=lg[R, 0:4], func=AF.Exp, bias=nm[R, 0:1], scale=1.0), r=["lg", "nm0"], w=["eg"])
                            P.op("dve", lambda e: e.reduce_sum(out=sgs[R, 0:1], in_=eg[R, :], axis=AX.X), r=["eg"], w=["sgs0"])
                            lg3 = lg[R, 4:20].rearrange("p (g x) -> p g x", x=4)
                            P.op("dve", lambda e, lg3=lg3: e.tensor_tensor(out=sel[R, :].rearrange("p (g x) -> p g x", x=4), in0=lg3,
                                                                           in1=ohg[R, :].unsqueeze(2).to_broadcast([pw, 4, 4]), op=ALU.mult), r=["lg", "ohg"], w=["sel"])
                            P.op("dve", lambda e: e.tensor_reduce(out=e8[R, 0:4], in_=sel[R, :].rearrange("p (g x) -> p x g", x=4), axis=AX.X, op=ALU.add), r=["sel"], w=["e8"])
                            P.op("dve", lambda e: e.max(out=me8[R, :], in_=e8[R, :]), r=["e8"], w=["me8"])
                            P.op("dve", lambda e: e.tensor_scalar(out=nm[R, 1:2], in0=me8[R, 0:1], scalar1=-1.0, scalar2=None, op0=ALU.mult), r=["me8"], w=["nm1"])
                            P.op("act", lambda e: e.activation(out=e1[R, :], in_=e8[R, 0:4], func=AF.Exp, bias=nm[R, 1:2], scale=1.0), r=["e8", "nm1"], w=["e1"])
                            P.op("dve", lambda e: e.tensor_scalar(out=s2m[R, :], in0=e8[R, 0:4], scalar1=me8[R, 1:2], scalar2=None, op0=ALU.is_ge), r=["e8", "me8"], w=["s2m"])
                            P.op("dve", lambda e: e.tensor_tensor(out=e1[R, :], in0=e1[R, :], in1=s2m[R, :], op=ALU.mult), r=["e1", "s2m"], w=["e1"])
                            P.op("dve", lambda e: e.reduce_sum(out=sgs[R, 1:2], in_=e1[R, :], axis=AX.X), r=["e1"], w=["sgs1"])
                            P.op("dve", lambda e: e.tensor_tensor(out=wfac[R, :], in0=sgs[R, 0:1], in1=sgs[R, 1:2], op=ALU.mult), r=["sgs0", "sgs1"], w=["wfac"])
                            P.op("dve", lambda e: e.reciprocal(out=wfac[R, :], in_=wfac[R, :]), r=["wfac"], w=["wfac"])
                            P.op("dve", lambda e: e.tensor_scalar(out=e1[R, :], in0=e1[R, :], scalar1=wfac[R, 0:1], scalar2=None, op0=ALU.mult), r=["e1", "wfac"], w=["e1"])
                            P.op("dve", lambda e, gb=gb: e.tensor_tensor(out=gate[R, gb, :].rearrange("p (g x) -> p g x", x=4),
                                                                         in0=ohg[R, :].unsqueeze(2).to_broadcast([pw, 4, 4]),
                                                                         in1=e1[R, :].unsqueeze(1).to_broadcast([pw, 4, 4]), op=ALU.mult), r=["ohg", "e1", "gate"], w=["gate%d" % gb])
                    dump("gate", gate[:], [128, 17, 16], r=["gate%d" % i for i in range(17)])
                    P.barrier()
            s_oT.close()

            if stage >= 5:
                with ExitStack() as s5:
                    x1T = sb("x1T", [128, 8, NTOK], BF16, s5)
                    yacc = sb("yacc", [128, 17, 1024], F32, s5)
                    g2b = sb("g2b", [128, 1024], F32, s5)
                    b2b = sb("b2b", [128, 1024], F32, s5)
                    w1e = [sb("w1e%d" % i, [128, 8, 512], BF16, s5) for i in range(2)]
                    w3e = [sb("w3e%d" % i, [128, 8, 512], BF16, s5) for i in range(2)]
                    w2e = [sb("w2e%d" % i, [128, 4, 1024], BF16, s5) for i in range(2)]
                    s1t = [sb("s1t%d" % i, [128, 512], F32, s5) for i in range(2)]
                    hT = [sb("hT%d" % i, [128, 4, 512], BF16, s5) for i in range(2)]
                    st6 = sb("st6b", [128, 2, 6], F32, s5)
                    mv = sb("mvb", [128, 2], F32, s5)
                    rstd = sb("rstdb", [128, 1], F32, s5)
                    P.dma("sp", x1T[:], x1T_d, w=["x1T"], sem="x1T")
                    P.dma("sp", yacc[:, 0:16, :], y0_d[0:2048, :].rearrange("(b p) d -> p b d", p=128), w=["yacc"], sem="yacc")
                    P.dma("sp", yacc[0:64, 16, :], y0_d[2048:2112, :], w=["yacc"], sem="yacc")
                    P.dma("sp", g2b[:], ln2_g.partition_broadcast(128), w=["g2b"], sem="g2b")
                    P.dma("sp", b2b[:], ln2_b.partition_broadcast(128), w=["b2b"], sem="b2b")

                    wst[:] = [sb("wstC%d" % i, [128, 2048], F32, s5) for i in range(2)]

                    def load_e(e_):
                        i = e_ % 2
                        return (wchunks(w1e[i], "w1e%d" % i, w1[e_], 8, 512, CH=2048) + wchunks(w3e[i], "w3e%d" % i, w3[e_], 8, 512, CH=2048)
                                + wchunks(w2e[i], "w2e%d" % i, w2[e_], 4, 1024, CH=2048))
                    for f_ in load_e(0):
                        f_()
                    ttiles = [(0, 512), (512, 512), (1024, 512), (1536, 512), (2048, 64)]
                    it = 0
                    for e_ in range(16):
                        pend = load_e(e_ + 1) if e_ + 1 < 16 else []
                        i = e_ % 2
                        a1, a3, a2 = w1e[i], w3e[i], w2e[i]
                        k1, k3, k2 = "w1e%d" % i, "w3e%d" % i, "w2e%d" % i
                        for (t0, W) in ttiles:
                            nb = (W + 127) // 128
                            pw = min(W, 128)
                            h_ = hT[it % 2]
                            hk = "hT%d" % (it % 2)
                            it += 1
                            for f in range(4):
                                fs = slice(f * 128, (f + 1) * 128)
                                p1, p1k = ps()
                                mmg(p1[:, 0:W], [(a1[:, k, fs], x1T[:, k, t0:t0 + W]) for k in range(8)], r=["x1T", k1], w=[p1k])
                                p3, p3k = ps()
                                mmg(p3[:, 0:W], [(a3[:, k, fs], x1T[:, k, t0:t0 + W]) for k in range(8)], r=["x1T", k3], w=[p3k])
                                s_ = s1t[f % 2]
                                sk = "s1t%d" % (f % 2)
                                P.op("act", lambda e, p1=p1, s_=s_: e.activation(out=s_[:, 0:W], in_=p1[:, 0:W], func=AF.Silu), r=[p1k], w=[sk])
                                P.op("dve", lambda e, p3=p3, s_=s_, f=f, h_=h_: e.tensor_tensor(out=h_[:, f, 0:W], in0=s_[:, 0:W], in1=p3[:, 0:W], op=ALU.mult),
                                     r=[p3k, sk], w=[hk + "_%d" % f])
                            for f_ in pend[:2 if t0 == 0 else 1]:
                                f_()
                            pend = pend[2 if t0 == 0 else 1:]
                            hall = [hk + "_%d" % f for f in range(4)]
                            for b in range(nb):
                                gb = t0 // 128 + b
                                for half in range(2):
                                    hs = slice(half * 512, (half + 1) * 512)
                                    py, pyk = ps()
                                    mmg(py[0:pw, :], [(h_[:, f, b * 128:b * 128 + pw], a2[:, f, hs]) for f in range(4)], r=hall + [k2], w=[pyk])
                                    P.op("dve", lambda e, py=py, gb=gb, hs=hs: e.scalar_tensor_tensor(out=yacc[0:pw, gb, hs], in0=py[0:pw, :], scalar=gate[0:pw, gb, e_:e_ + 1],
                                                                                                     in1=yacc[0:pw, gb, hs], op0=ALU.mult, op1=ALU.add),
                                         r=[pyk, "yacc"], w=["yacc%d_%d" % (gb, half)])
                    for gb in range(17):
                        pw = 128 if gb < 16 else 64
                        yk = ["yacc%d_0" % gb, "yacc%d_1" % gb]
                        ya = yacc[0:pw, gb, :]
                        for half in range(2):
                            P.op("dve", lambda e, half=half, gb=gb, pw=pw: e.bn_stats(out=st6[0:pw, half, :], in_=yacc[0:pw, gb, half * 512:(half + 1) * 512]), r=yk, w=["st6_%d" % half])
                        P.op("dve", lambda e, pw=pw: e.bn_aggr(out=mv[0:pw, :], in_=st6[0:pw].rearrange("p a s -> p (a s)")), r=["st6_0", "st6_1"], w=["mv"])
                        P.op("dve", lambda e, pw=pw: e.tensor_scalar(out=rstd[0:pw, :], in0=mv[0:pw, 1:2], scalar1=EPS, scalar2=None, op0=ALU.add), r=["mv"], w=["rstd"])
                        P.op("act", lambda e, pw=pw: e.sqrt(out=rstd[0:pw, :], in_=rstd[0:pw, :]), r=["rstd"], w=["rstd"])
                        P.op("dve", lambda e, pw=pw: e.reciprocal(out=rstd[0:pw, :], in_=rstd[0:pw, :]), r=["rstd"], w=["rstd"])
                        P.op("dve", lambda e, ya=ya, pw=pw: e.tensor_scalar(out=ya, in0=ya, scalar1=mv[0:pw, 0:1], scalar2=rstd[0:pw, 0:1],
                                                                           op0=ALU.subtract, op1=ALU.mult), r=yk + ["mv", "rstd"], w=["yo%d" % gb])
                        P.op("pool", lambda e, ya=ya, pw=pw: e.tensor_tensor(out=ya, in0=ya, in1=g2b[0:pw, :], op=ALU.mult), r=["yo%d" % gb, "g2b"], w=["yo%d" % gb])
                        P.op("pool", lambda e, ya=ya, pw=pw: e.tensor_tensor(out=ya, in0=ya, in1=b2b[0:pw, :], op=ALU.add), r=["yo%d" % gb, "b2b"], w=["yo%d" % gb])
                        if gb < 16:
                            P.dma("sp", y_own[gb * 128:(gb + 1) * 128, :], ya, r=["yo%d" % gb], sem="yo%d" % (gb % 4))
                        else:
                            P.dma("sp", y_s, ya, r=["yo%d" % gb], sem="yo%d" % (gb % 4))
                    P.barrier()
        except StopBuild:
            pass
        P.finish()
    nc._dbg_out = list(dbg_out.keys())
    return nc


def make_in_maps(inputs, cores):
    f32 = np.float32
    xp = np.asarray(inputs["x_prompt"], f32)
    xs = np.asarray(inputs["x_sample"], f32)
    ck = np.asarray(inputs["cache_k"], f32)
    npool = ck.shape[1]
    ck = ck.reshape(npool * 128, 512)
    cv = np.asarray(inputs["cache_v"], f32).reshape(npool * 128, 512)
    cf = np.asarray(inputs["cache_logf"], f32).reshape(npool * 128, 8)
    sc = np.asarray(inputs["state_conv"], f32)[0]
    pt = np.asarray(inputs["page_table"], np.int32)
    g = lambda k: np.asarray(inputs[k], f32)
    common = {
        "cache_k": ck, "cache_v": cv, "cache_f": cf,
        "w_in": g("w_in")[0], "b_f": g("b_f").reshape(1, 8),
        "conv_w": g("conv_w")[0], "w_conv_out": g("w_conv_out")[0],
        "w_attn_out": g("w_attn_out")[0], "w_o": g("w_o")[0],
        "ln1_g": g("ln1_g").reshape(1, D), "ln1_b": g("ln1_b").reshape(1, D),
        "w_gr": np.ascontiguousarray(np.concatenate([g("w_group")[0], g("w_router")[0]], axis=1)),
        "w1": g("w1")[0], "w3": g("w3")[0], "w2": g("w2")[0],
        "ln2_g": g("ln2_g").reshape(1, D), "ln2_b": g("ln2_b").reshape(1, D),
    }
    in_maps = []
    for c in cores:
        s, r = c // 2, c % 2
        own, oth = OWN[r], OWN[1 - r]
        xt = xp[s].reshape(8, 512, D)
        halo = np.zeros((8, D), f32)
        for sl, T in enumerate(own):
            if T > 0:
                halo[2 * sl:2 * sl + 2] = xp[s, 512 * T - 2:512 * T]
        rcv = np.zeros((1, 80), f32)
        order = own + oth
        for p_ in range(8):
            for p2 in range(8):
                rcv[0, p_ * 8 + p2] = 1.0 if order[p_] < order[p2] else 0.0
        for sl in range(4):
            rcv[0, 64 + sl] = 0.0 if oth[sl] < own[sl] else NEG
        m = dict(common)
        m.update({
            "x_own": np.ascontiguousarray(xt[own].reshape(2048, D)),
            "x_oth": np.ascontiguousarray(xt[oth].reshape(2048, D)),
            "x_halo": halo,
            "x_s": np.ascontiguousarray(xs[16 * c:16 * c + 16].reshape(64, D)),
            "state_conv": np.ascontiguousarray(sc[16 * c:16 * c + 16].reshape(32, 512)),
            "page_table": np.ascontiguousarray(pt[16 * c:16 * c + 16].reshape(1, 256)),
            "rolec": rcv,
        })
        in_maps.append(m)
    return in_maps, npool


def assemble(res, cores, nseq=4, nsamp=128):
    f32 = np.float32
    y_p = np.zeros((nseq, 4096, D), f32)
    k_p = np.zeros((1, nseq, 4096, 8, 64), f32)
    v_p = np.zeros((1, nseq, 4096, 8, 64), f32)
    f_p = np.zeros((1, nseq, 4096, 8), f32)
    c_p = np.zeros((1, nseq, 2, 512), f32)
    y_sm = np.zeros((nsamp, 4, D), f32)
    k_sm = np.zeros((1, nsamp, 4, 8, 64), f32)
    v_sm = np.zeros((1, nsamp, 4, 8, 64), f32)
    f_sm = np.zeros((1, nsamp, 4, 8), f32)
    c_sm = np.zeros((1, nsamp, 2, 512), f32)
    for o, c in zip(res, cores):
        s, r = c // 2, c % 2
        for sl, T in enumerate(OWN[r]):
            rows = slice(512 * T, 512 * T + 512)
            y_p[s, rows] = o["y_own"][sl * 512:(sl + 1) * 512]
            k_p[0, s, rows] = o["k_own"][sl * 512:(sl + 1) * 512].reshape(512, 8, 64)
            v_p[0, s, rows] = o["v_own"][sl * 512:(sl + 1) * 512].reshape(512, 8, 64)
            f_p[0, s, rows] = o["f_own"][sl * 512:(sl + 1) * 512]
        if r == 0:
            c_p[0, s] = o["conv_p"]
        sl_ = slice(16 * c, 16 * c + 16)
        y_sm[sl_] = o["y_s"].reshape(16, 4, D)
        k_sm[0, sl_] = o["k_s"].reshape(16, 4, 8, 64)
        v_sm[0, sl_] = o["v_s"].reshape(16, 4, 8, 64)
        f_sm[0, sl_] = o["f_s"].reshape(16, 4, 8)
        c_sm[0, sl_] = o["conv_s"].reshape(16, 2, 512)
    return (y_p, y_sm, k_p, v_p, f_p, c_p, k_sm, v_sm, f_sm, c_sm)


_NC = {}


def kernel(**inputs):
    cores = list(range(8))
    in_maps, npool = make_in_maps(inputs, cores)
    if npool not in _NC:
        _NC[npool] = build_nc(npool)
    res = run_bass_kernel_spmd(_NC[npool], in_maps, core_ids=cores).results
    return assemble(res, cores)
```
